# Optimizing a Trainium2 kernel written in Bass

```python
import jax, jax.numpy as jnp
from jax import lax
import numpy as np

D_MODEL = 2048
BATCH = 4
SEQ = 8192
DEPTH = 1

CHUNK = 64
LEFT_CHUNKS = 8
ATT_HEADS = 16
ATT_HEAD_DIM = 64
ATT_WIDTH = ATT_HEADS * ATT_HEAD_DIM
MAX_PAST_DIST = 256
REL_TABLE = MAX_PAST_DIST + CHUNK
RWKV_HEADS = 16
RWKV_HEAD_DIM = 64
RWKV_WIDTH = RWKV_HEADS * RWKV_HEAD_DIM
DECAY_LORA = 96
ICLR_LORA = 96
GATE_LORA = 256
SHIFT_WIDTH = 3 * RWKV_WIDTH + DECAY_LORA + ICLR_LORA + GATE_LORA
D_IN = 3 * ATT_WIDTH + SHIFT_WIDTH + 2 * D_MODEL
N_EXPERTS = 32
TOP_K = 4
D_EXPERT = D_MODEL
SWIGLU_LIMIT = 7.0
SWIGLU_ALPHA = 1.702
MOE_BLOCK = 256
LN_EPS = 1e-5
GN_EPS = 64e-5
DEEPNORM_ALPHA = (2 * DEPTH) ** 0.25
DEEPNORM_BETA = (8 * DEPTH) ** -0.25

kernel_name = 'hybrid_chunkattn_rwkv7_moe_deepnorm'


def _layer_norm(x, w, b):
    xf = x.astype(jnp.float32)
    mu = jnp.mean(xf, axis=-1, keepdims=True)
    var = jnp.mean(jnp.square(xf - mu), axis=-1, keepdims=True)
    return ((xf - mu) * lax.rsqrt(var + LN_EPS) * w + b).astype(x.dtype)


def _chunk_attention(q, k, v, rel_bias):
    B, S, H, Dh = q.shape
    n_chunks = S // CHUNK
    pad = LEFT_CHUNKS * CHUNK
    band = pad + CHUNK
    kp = jnp.pad(k, ((0, 0), (pad, 0), (0, 0), (0, 0)))
    vp = jnp.pad(v, ((0, 0), (pad, 0), (0, 0), (0, 0)))
    qi = jnp.arange(CHUNK)[:, None]
    kj = jnp.arange(band)[None, :]
    dist = qi + pad - kj
    idx = jnp.minimum(dist, MAX_PAST_DIST) + (CHUNK - 1)
    bias = rel_bias.astype(jnp.float32)[:, idx]
    scale = Dh ** -0.5
    key_slot = jnp.arange(band)

    def one_chunk(c):
        start = c * CHUNK
        qc = lax.dynamic_slice_in_dim(q, start, CHUNK, axis=1)
        kc = lax.dynamic_slice_in_dim(kp, start, band, axis=1)
        vc = lax.dynamic_slice_in_dim(vp, start, band, axis=1)
        s = jnp.einsum('bqhd,bkhd->bhqk', qc, kc).astype(jnp.float32) * scale + bias
        valid = key_slot + start >= pad
        s = jnp.where(valid, s, jnp.finfo(jnp.float32).min)
        p = jax.nn.softmax(s, axis=-1)
        return jnp.einsum('bhqk,bkhd->bqhd', p.astype(vc.dtype), vc)

    out = lax.map(one_chunk, jnp.arange(n_chunks))
    return jnp.moveaxis(out, 0, 1).reshape(B, S, H * Dh)


def _rwkv7_scan(r, decay, k, v, a, b):
    B, S, H, N = r.shape
    xs = tuple(jnp.moveaxis(t, 1, 0) for t in (r, decay, k, v, a, b))

    def step(state, inp):
        r_t, w_t, k_t, v_t, a_t, b_t = inp
        sa = jnp.einsum('bhvk,bhk->bhv', state, a_t)
        state = (state * w_t[:, :, None, :] + sa[..., None] * b_t[:, :, None, :]
                 + v_t[..., None] * k_t[:, :, None, :])
        y = jnp.einsum('bhvk,bhk->bhv', state, r_t)
        return state, y

    state0 = jnp.zeros((B, H, N, N), jnp.float32)
    _, ys = lax.scan(step, state0, xs)
    return jnp.moveaxis(ys, 0, 1)


def _rwkv7_time_mix(r, k, v, wd, ad, gd, w0, w_up, a0, a_up, g_up, k_k, k_a, r_k, lnx_w, lnx_b):
    B, S, _ = r.shape
    H, N = RWKV_HEADS, RWKV_HEAD_DIM
    out_dtype = r.dtype
    f32 = jnp.float32
    r, k, v = r.astype(f32), k.astype(f32), v.astype(f32)
    w = -jax.nn.softplus(-(w0 + jnp.tanh(wd) @ w_up).astype(f32)) - 0.5
    decay = jnp.exp(-jnp.exp(w))
    a = jax.nn.sigmoid((a0 + ad @ a_up).astype(f32))
    g = (jax.nn.sigmoid(gd) @ g_up).astype(f32)
    heads = lambda t: t.reshape(B, S, H, N)
    kk = heads(k * k_k)
    kk = kk / jnp.maximum(jnp.sqrt(jnp.sum(jnp.square(kk), axis=-1, keepdims=True)), 1e-12)
    k = k * (1.0 + (a - 1.0) * k_a)
    rh, kh, vh, ah = heads(r), heads(k), heads(v), heads(a)
    y = _rwkv7_scan(rh, heads(decay), kh, vh, -kk, kk * ah)
    mu = jnp.mean(y, axis=-1, keepdims=True)
    var = jnp.mean(jnp.square(y - mu), axis=-1, keepdims=True)
    y = ((y - mu) * lax.rsqrt(var + GN_EPS)).reshape(B, S, H * N) * lnx_w + lnx_b
    bonus = (jnp.sum(rh * kh * r_k, axis=-1, keepdims=True) * vh).reshape(B, S, H * N)
    return ((y + bonus) * g).astype(out_dtype)


def _token_mixing(x, w_in, rel_bias, shift_mu, w0, w_up, a0, a_up, g_up, k_k, k_a, r_k,
                  lnx_w, lnx_b, proj_a, proj_b, w_out):
    B, S, D = x.shape
    proj = jnp.einsum('bsd,de->bse', x, w_in)
    o1 = 3 * ATT_WIDTH
    o2 = o1 + SHIFT_WIDTH
    q, k, v = jnp.split(proj[..., :o1], 3, axis=-1)
    seg = proj[..., o1:o2]
    prev = jnp.pad(seg, ((0, 0), (1, 0), (0, 0)))[:, :-1]
    seg = seg + (prev - seg) * shift_mu
    W = RWKV_WIDTH
    r_b, k_b, v_b, wd, ad, gd = jnp.split(
        seg, [W, 2 * W, 3 * W, 3 * W + DECAY_LORA, 3 * W + DECAY_LORA + ICLR_LORA], axis=-1)
    gate_a, gate_b = jnp.split(jax.nn.sigmoid(proj[..., o2:]), 2, axis=-1)
    heads = lambda t: t.reshape(B, S, ATT_HEADS, ATT_HEAD_DIM)
    y_a = _chunk_attention(heads(q), heads(k), heads(v), rel_bias)
    y_b = _rwkv7_time_mix(r_b, k_b, v_b, wd, ad, gd, w0, w_up, a0, a_up, g_up,
                          k_k, k_a, r_k, lnx_w, lnx_b)
    merged = (gate_a * jnp.einsum('bsc,cd->bsd', y_a, proj_a)
              + gate_b * jnp.einsum('bsc,cd->bsd', y_b, proj_b))
    return jnp.einsum('bsd,de->bse', merged, w_out)


def _moe(h, w_router, b_router, w_gu, b_gu, w_down, b_down):
    T, D = h.shape
    logits = jnp.einsum('td,de->te', h, w_router).astype(jnp.float32) + b_router.astype(jnp.float32)
    top_v, top_i = lax.top_k(logits, TOP_K)
    gate = jax.nn.softmax(top_v, axis=-1)
    M = T * TOP_K
    slot_e = top_i.reshape(M).astype(jnp.int32)
    slot_t = jnp.arange(M, dtype=jnp.int32) // TOP_K
    slot_g = gate.reshape(M)
    order = jnp.argsort(slot_e)
    e_sorted = slot_e[order]
    counts = jnp.zeros((N_EXPERTS,), jnp.int32).at[slot_e].add(1)
    starts = jnp.cumsum(counts) - counts
    padded = (counts + MOE_BLOCK - 1) // MOE_BLOCK * MOE_BLOCK
    pad_ends = jnp.cumsum(padded)
    pad_starts = pad_ends - padded
    dest = pad_starts[e_sorted] + (jnp.arange(M, dtype=jnp.int32) - starts[e_sorted])
    n_blocks = (M + N_EXPERTS * (MOE_BLOCK - 1) + MOE_BLOCK - 1) // MOE_BLOCK
    R = n_blocks * MOE_BLOCK
    row_tok = jnp.zeros((R,), jnp.int32).at[dest].set(slot_t[order])
    row_w = jnp.zeros((R,), jnp.float32).at[dest].set(slot_g[order])
    blk_e = jnp.minimum(jnp.searchsorted(pad_ends, jnp.arange(n_blocks) * MOE_BLOCK, side='right'),
                        N_EXPERTS - 1)

    def body(acc, inp):
        e, toks, wts = inp
        xb = h[toks]
        gu = xb @ w_gu[e] + b_gu[e]
        g, u = jnp.split(gu, 2, axis=-1)
        g = jnp.minimum(g, SWIGLU_LIMIT)
        u = jnp.clip(u, -SWIGLU_LIMIT, SWIGLU_LIMIT)
        act = (u + 1.0) * (g * jax.nn.sigmoid(SWIGLU_ALPHA * g))
        y = act @ w_down[e] + b_down[e]
        acc = acc.at[toks].add((y * wts[:, None]).astype(acc.dtype))
        return acc, None

    out, _ = lax.scan(body, jnp.zeros_like(h),
                      (blk_e, row_tok.reshape(n_blocks, MOE_BLOCK), row_w.reshape(n_blocks, MOE_BLOCK)))
    return out


def setup_inputs(seed: int = 0) -> dict:
    key = jax.random.key(seed)
    ks = jax.random.split(key, 27)
    f32 = jnp.float32
    L = DEPTH
    nrm = lambda k, shape, scale: jax.random.normal(k, shape, f32) * scale
    F = D_EXPERT
    return {
        'x': nrm(ks[0], (BATCH, SEQ, D_MODEL), 1.0),
        'w_in': nrm(ks[1], (L, D_MODEL, D_IN), D_MODEL ** -0.5),
        'rel_bias': nrm(ks[2], (L, ATT_HEADS, REL_TABLE), 0.2),
        'shift_mu': jax.random.uniform(ks[3], (L, SHIFT_WIDTH), f32, 0.1, 0.9),
        'w0': jax.random.uniform(ks[4], (L, RWKV_WIDTH), f32, -4.0, 0.0),
        'w_up': nrm(ks[5], (L, DECAY_LORA, RWKV_WIDTH), 0.5 * DECAY_LORA ** -0.5),
        'a0': nrm(ks[6], (L, RWKV_WIDTH), 0.3),
        'a_up': nrm(ks[7], (L, ICLR_LORA, RWKV_WIDTH), 0.5 * ICLR_LORA ** -0.5),
        'g_up': nrm(ks[8], (L, GATE_LORA, RWKV_WIDTH), GATE_LORA ** -0.5),
        'k_k': 0.85 + nrm(ks[9], (L, RWKV_WIDTH), 0.05),
        'k_a': 1.0 + nrm(ks[10], (L, RWKV_WIDTH), 0.05),
        'r_k': nrm(ks[11], (L, RWKV_HEADS, RWKV_HEAD_DIM), 0.1),
        'lnx_w': 1.0 + nrm(ks[12], (L, RWKV_WIDTH), 0.05),
        'lnx_b': nrm(ks[13], (L, RWKV_WIDTH), 0.01),
        'proj_a': nrm(ks[14], (L, ATT_WIDTH, D_MODEL), DEEPNORM_BETA * ATT_WIDTH ** -0.5),
        'proj_b': nrm(ks[15], (L, RWKV_WIDTH, D_MODEL), DEEPNORM_BETA * RWKV_WIDTH ** -0.5),
        'w_out': nrm(ks[16], (L, D_MODEL, D_MODEL), DEEPNORM_BETA * D_MODEL ** -0.5),
        'ln1_w': 1.0 + nrm(ks[17], (L, D_MODEL), 0.05),
        'ln1_b': nrm(ks[18], (L, D_MODEL), 0.01),
        'w_router': nrm(ks[19], (L, D_MODEL, N_EXPERTS), D_MODEL ** -0.5),
        'b_router': nrm(ks[20], (L, N_EXPERTS), 0.01),
        'w_gu': nrm(ks[21], (L, N_EXPERTS, D_MODEL, 2 * F), D_MODEL ** -0.5),
        'b_gu': nrm(ks[22], (L, N_EXPERTS, 2 * F), 0.01),
        'w_down': nrm(ks[23], (L, N_EXPERTS, F, D_MODEL), DEEPNORM_BETA * F ** -0.5),
        'b_down': nrm(ks[24], (L, N_EXPERTS, D_MODEL), 0.01),
        'ln2_w': 1.0 + nrm(ks[25], (L, D_MODEL), 0.05),
        'ln2_b': nrm(ks[26], (L, D_MODEL), 0.01),
    }


def reference(x, w_in, rel_bias, shift_mu, w0, w_up, a0, a_up, g_up, k_k, k_a, r_k, lnx_w, lnx_b,
              proj_a, proj_b, w_out, ln1_w, ln1_b, w_router, b_router, w_gu, b_gu, w_down, b_down,
              ln2_w, ln2_b):
    h = x
    B, S, D = x.shape
    for l in range(DEPTH):
        mix = _token_mixing(h, w_in[l], rel_bias[l], shift_mu[l], w0[l], w_up[l], a0[l], a_up[l],
                            g_up[l], k_k[l], k_a[l], r_k[l], lnx_w[l], lnx_b[l],
                            proj_a[l], proj_b[l], w_out[l])
        h = _layer_norm(DEEPNORM_ALPHA * h + mix, ln1_w[l], ln1_b[l])
        ffn = _moe(h.reshape(B * S, D), w_router[l], b_router[l], w_gu[l], b_gu[l],
                   w_down[l], b_down[l]).reshape(B, S, D)
        h = _layer_norm(DEEPNORM_ALPHA * h + ffn, ln2_w[l], ln2_b[l])
    return h
```

```python
import contextlib
import os
import numpy as np
import concourse.bass as bass
import concourse.mybir as mybir
from concourse.bass_utils import run_bass_kernel_spmd


class Buf:
    __slots__ = ("name", "w", "r")

    def __init__(self, name=""):
        self.name = name
        self.w = None
        self.r = []


class Sched:
    ENGS = ("pe", "act", "dve", "pool", "sp")

    def __init__(self, nc, ndma_sems=10):
        self.nc = nc
        self.prog = {e: [] for e in self.ENGS}
        self.sems = {}
        self.cnt = {}
        self.known = {e: {} for e in self.ENGS}
        self._stack = []
        for e in ("pe", "act", "dve", "pool"):
            self._mksem("E_" + e)
        self.dpool = {}
        for q in ("sp", "pool", "act"):
            names = [f"D_{q}{i}" for i in range(ndma_sems)]
            for n in names:
                self._mksem(n)
            self.dpool[q] = [names, 0]
        self.ninstr = 0

    def _mksem(self, name):
        cm = self.nc.semaphore(name)
        s = cm.__enter__()
        self._stack.append(cm)
        self.sems[name] = s
        self.cnt[name] = 0

    def _wait(self, eng, ev):
        key, val = ev
        if eng == "pe" and key == "E_pe":
            return
        if self.known[eng].get(key, 0) >= val:
            return
        self.known[eng][key] = val
        sem = self.sems[key]
        self.prog[eng].append(lambda e, sem=sem, val=val: e.wait_ge(sem, val))

    def _deps(self, eng, reads, writes):
        for b in reads:
            if b.w is not None:
                self._wait(eng, b.w)
        for b in writes:
            if b.w is not None:
                self._wait(eng, b.w)
            for ev in b.r:
                self._wait(eng, ev)

    def _commit(self, ev, reads, writes):
        for b in writes:
            b.w = ev
            b.r = []
        for b in reads:
            if b.w is ev:
                continue
            b.r.append(ev)
            if len(b.r) > 24:
                last = {}
                for k, v in b.r:
                    if last.get(k, 0) < v:
                        last[k] = v
                b.r = list(last.items())

    def op(self, eng, fn, reads=(), writes=()):
        self._deps(eng, reads, writes)
        key = "E_" + eng
        self.cnt[key] += 1
        val = self.cnt[key]
        sem = self.sems[key]
        self.prog[eng].append(lambda e, fn=fn, sem=sem: fn(e).then_inc(sem, 1))
        self._commit((key, val), reads, writes)
        self.ninstr += 1

    def dma(self, q, fn, reads=(), writes=()):
        names, idx = self.dpool[q]
        name = names[idx % len(names)]
        self.dpool[q][1] = idx + 1
        if self.cnt[name] > 0:
            self._wait(q, (name, self.cnt[name]))
        self._deps(q, reads, writes)
        self.cnt[name] += 16
        val = self.cnt[name]
        sem = self.sems[name]
        self.prog[q].append(lambda e, fn=fn, sem=sem: fn(e).then_inc(sem, 16))
        self._commit((name, val), reads, writes)
        self.ninstr += 1

    def barrier(self):
        for eng in self.ENGS:
            for key, val in self.cnt.items():
                if val > 0:
                    if eng == "pe" and key == "E_pe":
                        continue
                    self._wait(eng, (key, val))

    def finish(self):
        self.barrier()
        nc = self.nc
        with nc.Block() as block:
            def mk(name):
                def f(e):
                    for t in self.prog[name]:
                        t(e)
                return f
            block.tensor(mk("pe"))
            block.scalar(mk("act"))
            block.vector(mk("dve"))
            block.gpsimd(mk("pool"))
            block.sync(mk("sp"))
        for cm in reversed(self._stack):
            cm.__exit__(None, None, None)


F32 = mybir.dt.float32
BF16 = mybir.dt.bfloat16
I32 = mybir.dt.int32
ALU = mybir.AluOpType
AF = mybir.ActivationFunctionType
AX = mybir.AxisListType

AW = 1024
NH = 16
HD = 64
SHIFTW = 3 * AW + 96 + 96 + 256
C0 = float(np.exp(-0.5))


class Cfg:
    def __init__(self, DM=2048, NPRE=4096, NOWN=4096, ST=2048, NE=32, CAP=640, depth_alpha=2 ** 0.25):
        self.DM = DM; self.NPRE = NPRE; self.NOWN = NOWN; self.ST = ST
        self.NE = NE; self.CAP = CAP; self.FF = DM
        self.NTOK = NPRE + NOWN
        self.KC = DM // 128
        self.DIN = 3 * AW + SHIFTW + 2 * DM
        self.alpha = depth_alpha


class K:
    def __init__(self, cfg, dbg=()):
        self.cfg = cfg
        self.nc = bass.Bass("TRN2", target_bir_lowering=False)
        self.S = Sched(self.nc)
        self.dbg = set(dbg)
        self.stack = contextlib.ExitStack()
        self.pstack = None
        self.ins = {}

    def inp(self, name, shape, dt=F32):
        t = self.nc.dram_tensor(name, list(shape), dt, kind="ExternalInput").ap()
        self.ins[name] = t
        return t

    def scratch(self, name, shape, dt):
        kind = "ExternalOutput" if name in self.dbg else "Internal"
        return self.nc.dram_tensor(name, list(shape), dt, kind=kind).ap()

    def phase(self):
        if self.pstack is not None:
            self.S.barrier()
            self.pstack.close()
        self.pstack = contextlib.ExitStack()

    def sb(self, name, shape, dt):
        return self.pstack.enter_context(self.nc.sbuf_tensor(name, list(shape), dt))

    def ps(self, name, shape, dt=F32):
        return self.pstack.enter_context(self.nc.psum_tensor(name, list(shape), dt))

    def gsb(self, name, shape, dt):
        return self.stack.enter_context(self.nc.sbuf_tensor(name, list(shape), dt))


_REGS = {}


def _bound_reg(e, val):
    key = (id(e), val)
    if key not in _REGS:
        _REGS[key] = e.to_reg(val)
    return _REGS[key]


class Ring:
    def __init__(self, tiles):
        self.t = tiles
        self.b = [Buf() for _ in tiles]
        self.i = 0

    def next(self):
        j = self.i % len(self.t)
        self.i += 1
        return self.t[j], self.b[j]


def load_consts(k):
    S = k.S
    c = {}
    ident = k.inp("c_ident", [128, 128])
    c["ident_f"] = k.gsb("ident_f", [128, 128], F32)
    c["ident_b"] = k.gsb("ident_b", [128, 128], BF16)
    c["b_ident"] = Buf()
    S.dma("sp", lambda e: e.dma_start(out=c["ident_f"][:], in_=ident[:, :]), writes=[c["b_ident"]])
    S.op("act", lambda e: e.activation(out=c["ident_b"][:], in_=c["ident_f"][:], func=AF.Copy),
         reads=[c["b_ident"]], writes=[c["b_ident"]])
    k.c = c
    NT = k.cfg.NOWN // 128
    k.dest_all = k.gsb("dest_all", [128, NT, 4], I32); k.b_dest = Buf()
    k.gate_all = k.gsb("gate_all", [128, NT, 4], F32); k.b_gate = Buf()
    k.bgT = k.gsb("bgT", [128, 2 * (k.cfg.FF // 128), k.cfg.NE], F32); k.b_bgT = Buf()


def phase1(k, xe, w_in, mu_cols):
    cfg, S, nc, c = k.cfg, k.S, k.nc, k.c
    DM, KC, ST, NTOK, NPRE, NOWN = cfg.DM, cfg.KC, cfg.ST, cfg.NTOK, cfg.NPRE, cfg.NOWN
    o1, o2 = 3 * AW, 3 * AW + SHIFTW
    HALO = 512
    sc = {}
    sc["QT"] = k.scratch("QT", [AW, NOWN], BF16)
    sc["KT"] = k.scratch("KT", [AW, HALO + NOWN], BF16)
    sc["V"] = k.scratch("V", [HALO + NOWN, NH, 65], BF16)
    sc["RB"] = k.scratch("RB", [AW, NTOK], BF16)
    sc["KB"] = k.scratch("KB", [AW, NTOK], BF16)
    sc["VB"] = k.scratch("VB", [AW, NTOK], BF16)
    sc["WD"] = k.scratch("WD", [96, NTOK], BF16)
    sc["AD"] = k.scratch("AD", [96, NTOK], BF16)
    sc["GD"] = k.scratch("GD", [256, NTOK], BF16)
    sc["GA"] = k.scratch("GA", [DM, NOWN], BF16)
    sc["GB"] = k.scratch("GB", [DM, NOWN], BF16)
    k.sc = sc

    k.phase()
    xT = k.sb("xT", [128, KC, ST], BF16); b_xT = Buf()
    xin = Ring([k.sb(f"xin{i}", [128, DM], F32) for i in range(2)])
    xbf = Ring([k.sb(f"xbf{i}", [128, DM], BF16) for i in range(2)])
    wst = Ring([k.sb(f"wst{i}", [128, KC, 256], F32) for i in range(2)])
    wbf = Ring([k.sb(f"wbf{i}", [128, KC, 256], BF16) for i in range(2)])
    lbuf = [k.sb(f"lbuf{i}", [128, 516], F32) for i in range(2)]
    b_lbuf = [Buf(), Buf()]
    ltmp = Ring([k.sb(f"ltmp{i}", [128, 512], F32) for i in range(2)])
    lseg = Ring([k.sb(f"lseg{i}", [128, 512], F32) for i in range(2)])
    ost = Ring([k.sb(f"ost{i}", [128, 512], BF16) for i in range(4)])
    vst = Ring([k.sb(f"vst{i}", [128, 4, 65], BF16) for i in range(3)])
    carry = k.sb("carry", [128, 32], F32); b_carry = Buf()
    mu = k.sb("mu", [128, 32], F32); b_mu = Buf()
    pst = Ring([k.ps(f"pT{i}", [128, 8, 128], BF16) for i in range(2)])
    psg = Ring([k.ps(f"pg{i}", [128, 512], F32) for i in range(4)])

    S.dma("sp", lambda e: e.dma_start(out=mu[:, 0:28], in_=mu_cols[:, :]), writes=[b_mu])
    S.op("pool", lambda e: e.memset(carry[:], 0.0), writes=[b_carry])
    for t, b in zip(vst.t, vst.b):
        S.op("pool", lambda e, t=t: e.memset(t[:], 1.0), writes=[b])

    blocks = []
    for c0 in range(0, AW, 256):
        blocks.append((c0, 256, "q", sc["QT"], c0, "own", None, None))
    for c0 in range(0, AW, 256):
        blocks.append((AW + c0, 256, "k", sc["KT"], c0, "halo", None, None))
    for c0 in range(0, AW, 256):
        blocks.append((2 * AW + c0, 256, "v", sc["V"], c0 // 64, "halo", None, None))
    for j, nm in enumerate(("RB", "KB", "VB")):
        for c0 in range(0, AW, 256):
            blocks.append((o1 + j * AW + c0, 256, "rw", sc[nm], c0, "all", None, (j * AW + c0) // 128))
    blocks.append((o1 + 3 * AW, 96, "rw", sc["WD"], 0, "all", AF.Tanh, 24))
    blocks.append((o1 + 3 * AW + 96, 96, "rw", sc["AD"], 0, "all", AF.Copy, 25))
    blocks.append((o1 + 3 * AW + 192, 256, "rw", sc["GD"], 0, "all", AF.Sigmoid, 26))
    for c0 in range(0, DM, 256):
        blocks.append((o2 + c0, 256, "gate", sc["GA"], c0, "own", None, None))
    for c0 in range(0, DM, 256):
        blocks.append((o2 + DM + c0, 256, "gate", sc["GB"], c0, "own", None, None))

    w_v = w_in.rearrange("(kc p) c -> p kc c", p=128)
    nST = NTOK // ST
    for s in range(nST):
        tok0 = s * ST
        for i in range(ST // 128):
            xi, bxi = xin.next()
            xb, bxb = xbf.next()
            r0 = tok0 + i * 128
            S.dma("sp", lambda e, xi=xi, r0=r0: e.dma_start(out=xi[:], in_=xe[r0:r0 + 128, :]), writes=[bxi])
            S.op("act", lambda e, xi=xi, xb=xb: e.activation(out=xb[:], in_=xi[:], func=AF.Copy),
                 reads=[bxi], writes=[bxb])
            for g0 in range(0, KC, 8):
                ng = min(8, KC - g0)
                pt, bpt = pst.next()
                for j in range(ng):
                    S.op("pe", lambda e, pt=pt, xb=xb, j=j, g0=g0: e.transpose(
                        out=pt[:, j, :], in_=xb[:, (g0 + j) * 128:(g0 + j + 1) * 128], identity=c["ident_b"][:]),
                        reads=[bxb, c["b_ident"]], writes=[bpt])
                S.op("dve", lambda e, pt=pt, g0=g0, ng=ng, i=i: e.tensor_copy(
                    out=xT[:, g0:g0 + ng, i * 128:(i + 1) * 128], in_=pt[:, 0:ng, :]),
                    reads=[bpt], writes=[b_xT])
        own_s = tok0 >= NPRE
        for bi, (c0, ncols, kind, dst, drow0, tokmode, post, rwc) in enumerate(blocks):
            tts = []
            for tt in range(ST // 512):
                t_ext = tok0 + tt * 512
                if tokmode == "all" or (tokmode == "own" and t_ext >= NPRE) or \
                        (tokmode == "halo" and t_ext >= NPRE - HALO):
                    tts.append(tt)
            if not tts:
                continue
            ws, bws = wst.next()
            wb, bwb = wbf.next()
            S.dma("sp" if bi % 2 == 0 else "act", lambda e, ws=ws, c0=c0, ncols=ncols: e.dma_start(
                out=ws[:, :, 0:ncols], in_=w_v[:, :, c0:c0 + ncols]), writes=[bws])
            ceng = "pool" if bi % 2 == 0 else "act"
            if ceng == "pool":
                S.op("pool", lambda e, ws=ws, wb=wb, ncols=ncols: e.tensor_copy(
                    out=wb[:, :, 0:ncols], in_=ws[:, :, 0:ncols]), reads=[bws], writes=[bwb])
            else:
                S.op("act", lambda e, ws=ws, wb=wb, ncols=ncols: e.activation(
                    out=wb[:, :, 0:ncols], in_=ws[:, :, 0:ncols], func=AF.Copy), reads=[bws], writes=[bwb])
            if kind == "v":
                for tt in tts:
                    for sub in range(4):
                        tk = tt * 512 + sub * 128
                        pg, bpg = psg.next()
                        for kc in range(KC):
                            S.op("pe", lambda e, pg=pg, kc=kc, tk=tk, wb=wb: e.matmul(
                                pg[:, 0:256], lhsT=xT[:, kc, tk:tk + 128], rhs=wb[:, kc, 0:256],
                                start=(kc == 0), stop=(kc == KC - 1)), reads=[b_xT, bwb], writes=[bpg])
                        vs, bvs = vst.next()
                        S.op("act", lambda e, vs=vs, pg=pg: e.activation(
                            out=vs[:, :, 0:64], in_=pg[:, 0:256].rearrange("p (h d) -> p h d", d=64), func=AF.Copy),
                            reads=[bpg], writes=[bvs])
                        vrow = tok0 + tk - (NPRE - HALO)
                        S.dma("sp", lambda e, vs=vs, vrow=vrow, drow0=drow0, dst=dst: e.dma_start(
                            out=dst[vrow:vrow + 128, drow0:drow0 + 4, :], in_=vs[:]), reads=[bvs])
                continue
            nch = (ncols + 127) // 128
            for ch in range(nch):
                m = min(128, ncols - ch * 128)
                j0 = ch * 128
                if kind == "rw":
                    rci = rwc + ch
                    p = 0
                    S.op("act", lambda e, p=p, rci=rci, m=m: e.activation(
                        out=lbuf[p][0:m, 0:1], in_=carry[0:m, rci:rci + 1], func=AF.Copy),
                        reads=[b_carry], writes=[b_lbuf[p]])
                for tt in tts:
                    tk = tt * 512
                    pg, bpg = psg.next()
                    for kc in range(KC):
                        S.op("pe", lambda e, pg=pg, kc=kc, tk=tk, wb=wb, j0=j0, m=m: e.matmul(
                            pg[0:m, :], lhsT=wb[:, kc, j0:j0 + m], rhs=xT[:, kc, tk:tk + 512],
                            start=(kc == 0), stop=(kc == KC - 1)), reads=[b_xT, bwb], writes=[bpg])
                    o, bo = ost.next()
                    if kind == "q":
                        S.op("act", lambda e, o=o, pg=pg, m=m: e.activation(
                            out=o[0:m, :], in_=pg[0:m, :], func=AF.Copy, scale=0.125), reads=[bpg], writes=[bo])
                        dcol = tok0 + tk - NPRE
                    elif kind == "k":
                        S.op("act", lambda e, o=o, pg=pg, m=m: e.activation(
                            out=o[0:m, :], in_=pg[0:m, :], func=AF.Copy), reads=[bpg], writes=[bo])
                        dcol = tok0 + tk - (NPRE - HALO)
                    elif kind == "gate":
                        S.op("act", lambda e, o=o, pg=pg, m=m: e.activation(
                            out=o[0:m, :], in_=pg[0:m, :], func=AF.Sigmoid), reads=[bpg], writes=[bo])
                        dcol = tok0 + tk - NPRE
                    else:
                        lb, blb = lbuf[p], b_lbuf[p]
                        S.op("act", lambda e, lb=lb, pg=pg, m=m: e.activation(
                            out=lb[0:m, 1:513], in_=pg[0:m, :], func=AF.Copy), reads=[bpg], writes=[blb])
                        S.op("act", lambda e, lb=lb, p=p, m=m: e.activation(
                            out=lbuf[1 - p][0:m, 0:1], in_=lb[0:m, 512:513], func=AF.Copy),
                            reads=[blb], writes=[b_lbuf[1 - p]])
                        lt, blt = ltmp.next()
                        S.op("dve", lambda e, lt=lt, lb=lb, m=m: e.tensor_tensor(
                            out=lt[0:m, :], in0=lb[0:m, 0:512], in1=lb[0:m, 1:513], op=ALU.subtract),
                            reads=[blb], writes=[blt])
                        if post is None:
                            S.op("dve", lambda e, o=o, lt=lt, lb=lb, m=m, rci=rci: e.scalar_tensor_tensor(
                                out=o[0:m, :], in0=lt[0:m, :], scalar=mu[0:m, rci:rci + 1], in1=lb[0:m, 1:513],
                                op0=ALU.mult, op1=ALU.add), reads=[blt, blb, b_mu], writes=[bo])
                        else:
                            ls, bls = lseg.next()
                            S.op("dve", lambda e, ls=ls, lt=lt, lb=lb, m=m, rci=rci: e.scalar_tensor_tensor(
                                out=ls[0:m, :], in0=lt[0:m, :], scalar=mu[0:m, rci:rci + 1], in1=lb[0:m, 1:513],
                                op0=ALU.mult, op1=ALU.add), reads=[blt, blb, b_mu], writes=[bls])
                            S.op("act", lambda e, o=o, ls=ls, m=m, post=post: e.activation(
                                out=o[0:m, :], in_=ls[0:m, :], func=post), reads=[bls], writes=[bo])
                        p = 1 - p
                        dcol = tok0 + tk
                    r0 = drow0 + j0
                    S.dma("sp" if (tt % 2 == 0) else "pool", lambda e, o=o, r0=r0, m=m, dcol=dcol, dst=dst: e.dma_start(
                        out=dst[r0:r0 + m, dcol:dcol + 512], in_=o[0:m, :]), reads=[bo])
                if kind == "rw":
                    S.op("act", lambda e, p=p, rci=rci, m=m: e.activation(
                        out=carry[0:m, rci:rci + 1], in_=lbuf[p][0:m, 0:1], func=AF.Copy),
                        reads=[b_lbuf[p]], writes=[b_carry])


def phase2(k, att_bias, halo_mask):
    cfg, S, nc, c, sc = k.cfg, k.S, k.nc, k.c, k.sc
    NOWN = cfg.NOWN
    sc["YAT"] = k.scratch("YAT", [AW, NOWN], BF16)
    k.phase()
    QTv = sc["QT"].rearrange("(c p) t -> p c t", p=128)
    KTv = sc["KT"].rearrange("(c p) t -> p c t", p=128)
    YATv = sc["YAT"].rearrange("(c p) t -> p c t", p=128)
    qt = Ring([k.sb(f"qt{i}", [128, 8, 512], BF16) for i in range(2)])
    kt = Ring([k.sb(f"kt{i}", [128, 8, 1024], BF16) for i in range(2)])
    vv = Ring([k.sb(f"vv{i}", [128, 8, NH, 65], BF16) for i in range(2)])
    bias = k.sb("abias", [128, 5, NH, 128], F32); b_bias = Buf()
    halo = k.sb("halo", [128, 1], F32); b_halo = Buf()
    scs = Ring([k.sb(f"scs{i}", [128, 512], F32) for i in range(2)])
    pTs = Ring([k.sb(f"pTs{i}", [128, 512], BF16) for i in range(3)])
    ya = Ring([k.sb(f"ya{i}", [128, AW], BF16) for i in range(2)])
    yst = Ring([k.sb(f"yst{i}", [128, 8, 512], BF16) for i in range(2)])
    rec = Ring([k.sb(f"rec{i}", [128, 4], F32) for i in range(4)])
    ps_s = Ring([k.ps(f"ps_s{i}", [128, 512], F32) for i in range(2)])
    po = [k.ps(f"po{i}", [128, 512], F32) for i in range(4)]
    b_po = [Buf() for _ in range(4)]
    psT = Ring([k.ps("psT2", [128, 8, 128], BF16)])

    for kb in range(5):
        S.dma("sp", lambda e, kb=kb: e.dma_start(out=bias[:, kb, :, :], in_=att_bias[:, kb, :, :]), writes=[b_bias])
    S.dma("sp", lambda e: e.dma_start(out=halo[:], in_=halo_mask[:, :]), writes=[b_halo])

    HG = [[0, 2, 4, 6], [1, 3, 5, 7], [8, 10, 12, 14], [9, 11, 13, 15]]
    for g in range(NOWN // 512):
        q_, bq = qt.next(); k_, bk = kt.next(); v_, bv = vv.next()
        S.dma("sp", lambda e, q_=q_, g=g: e.dma_start(out=q_[:], in_=QTv[:, :, 512 * g:512 * g + 512]), writes=[bq])
        S.dma("act", lambda e, k_=k_, g=g: e.dma_start(out=k_[:], in_=KTv[:, :, 512 * g:512 * g + 1024]), writes=[bk])
        S.dma("pool", lambda e, v_=v_, g=g: e.dma_start(
            out=v_[:], in_=sc["V"][512 * g:512 * g + 1024, :, :].rearrange("(i p) h d -> p i h d", p=128)), writes=[bv])
        ys, bys = yst.next()
        for p in range(4):
            y_, by = ya.next()
            for hg in range(4):
                for kb in range(5):
                    ps, bps = ps_s.next()
                    for hh in range(4):
                        h = HG[hg][hh]
                        hp, h2 = h % 2, h // 2
                        S.op("pe", lambda e, ps=ps, k_=k_, q_=q_, hp=hp, h2=h2, p=p, kb=kb, hh=hh: e.matmul(
                            ps[:, hh * 128:(hh + 1) * 128],
                            lhsT=k_[hp * 64:(hp + 1) * 64, h2, 128 * (p + kb):128 * (p + kb) + 128],
                            rhs=q_[hp * 64:(hp + 1) * 64, h2, 128 * p:128 * p + 128],
                            start=True, stop=True), reads=[bk, bq], writes=[bps])
                    s_, bs = scs.next()
                    S.op("dve", lambda e, s_=s_, ps=ps, kb=kb, hg=hg: e.tensor_tensor(
                        out=s_[:].rearrange("p (h q) -> p h q", q=128), in0=ps[:].rearrange("p (h q) -> p h q", q=128),
                        in1=bias[:, kb, 4 * hg:4 * hg + 4, :], op=ALU.add), reads=[bps, b_bias], writes=[bs])
                    pt, bpt = pTs.next()
                    masked = (g == 0 and p + kb < 4)
                    if masked:
                        S.op("act", lambda e, pt=pt, s_=s_: e.activation(
                            out=pt[:], in_=s_[:], func=AF.Exp, bias=halo[:, 0:1]), reads=[bs, b_halo], writes=[bpt])
                    else:
                        S.op("act", lambda e, pt=pt, s_=s_: e.activation(
                            out=pt[:], in_=s_[:], func=AF.Exp), reads=[bs], writes=[bpt])
                    for hh in range(4):
                        h = HG[hg][hh]
                        S.op("pe", lambda e, pt=pt, v_=v_, hg=hg, hh=hh, h=h, p=p, kb=kb: e.matmul(
                            po[hg][:, hh * 65:(hh + 1) * 65], lhsT=pt[:, hh * 128:(hh + 1) * 128],
                            rhs=v_[:, p + kb, h, :], start=(kb == 0 and hh == 0), stop=(kb == 4 and hh == 3),
                            skip_group_check=True), reads=[bpt, bv], writes=[b_po[hg]])
                r_, br = rec.next()
                pov = po[hg][:, 0:260].rearrange("p (h d) -> p h d", d=65)
                S.op("dve", lambda e, r_=r_, pov=pov: e.reciprocal(out=r_[:, 0:4], in_=pov[:, :, 64]),
                     reads=[b_po[hg]], writes=[br])
                for hh in range(4):
                    h = HG[hg][hh]
                    eng = "act" if hh % 2 == 0 else "dve"
                    if eng == "act":
                        S.op("act", lambda e, y_=y_, pov=pov, r_=r_, hh=hh, h=h: e.activation(
                            out=y_[:, h * 64:(h + 1) * 64], in_=pov[:, hh, 0:64], func=AF.Copy, scale=r_[:, hh:hh + 1]),
                            reads=[b_po[hg], br], writes=[by])
                    else:
                        S.op("dve", lambda e, y_=y_, pov=pov, r_=r_, hh=hh, h=h: e.tensor_scalar(
                            out=y_[:, h * 64:(h + 1) * 64], in0=pov[:, hh, 0:64], scalar1=r_[:, hh:hh + 1], scalar2=None,
                            op0=ALU.mult), reads=[b_po[hg], br], writes=[by])
            pt_, bpt_ = psT.next()
            for j in range(8):
                S.op("pe", lambda e, pt_=pt_, y_=y_, j=j: e.transpose(
                    out=pt_[:, j, :], in_=y_[:, j * 128:(j + 1) * 128], identity=c["ident_b"][:]),
                    reads=[by, c["b_ident"]], writes=[bpt_])
            S.op("act", lambda e, ys=ys, pt_=pt_, p=p: e.activation(
                out=ys[:, :, p * 128:(p + 1) * 128], in_=pt_[:], func=AF.Copy), reads=[bpt_], writes=[bys])
        S.dma("sp", lambda e, ys=ys, g=g: e.dma_start(out=YATv[:, :, 512 * g:512 * g + 512], in_=ys[:]), reads=[bys])


def phase3(k, prm):
    cfg, S, nc, c, sc = k.cfg, k.S, k.nc, k.c, k.sc
    NTOK, NPRE, NOWN = cfg.NTOK, cfg.NPRE, cfg.NOWN
    sc["YB"] = k.scratch("YB", [NOWN, AW], F32)
    sc["BON"] = k.scratch("BON", [NH, NOWN], F32)
    k.phase()
    HB = 8
    def cload(name, shape, dt=F32, src=None, cast=None):
        t = k.sb("s3_" + name, shape, dt); b = Buf()
        S.dma("sp", lambda e: e.dma_start(out=t[:], in_=src), writes=[b])
        return t, b
    wupf, b_wupf = cload("wupf", [96, AW], F32, prm["w_up"][:, :])
    aupf, b_aupf = cload("aupf", [96, AW], F32, prm["a_up"][:, :])
    wup = k.sb("wup", [96, AW], BF16); aup = k.sb("aup", [96, AW], BF16)
    S.op("act", lambda e: e.activation(out=wup[:], in_=wupf[:], func=AF.Copy), reads=[b_wupf], writes=[b_wupf])
    S.op("act", lambda e: e.activation(out=aup[:], in_=aupf[:], func=AF.Copy), reads=[b_aupf], writes=[b_aupf])
    hc, b_hc = cload("hc", [64, 6, NH], F32, prm["hcols"][:, :, :])
    S.op("dve", lambda e: e.tensor_scalar(out=hc[:, 4, :], in0=hc[:, 3, :], scalar1=-1.0, scalar2=1.0, op0=ALU.mult, op1=ALU.add),
         reads=[b_hc], writes=[b_hc])
    rmask, b_rm = cload("rmask", [64, 512], F32, prm["rmask"][:, :])
    m_su, b_su = cload("m_su", [64, 8, 64], F32, prm["m_su"][:, :, :])
    m_ui, b_ui = cload("m_ui", [64, 8, 64], F32, prm["m_ui"][:, :, :])
    m_sl, b_sl = cload("m_sl", [64, 8, 64], F32, prm["m_sl"][:, :, :])
    i8, b_i8 = cload("i8", [64, 8, 64], F32, prm["i8"][:, :, :])
    ones64 = k.sb("ones64", [64, 64], F32); b_ones = Buf()
    S.op("pool", lambda e: e.memset(ones64[:], 1.0), writes=[b_ones])
    cb = [b_hc, b_rm]

    S32 = k.sb("S32", [64, NH, 64], F32); b_S32 = [Buf() for _ in range(NH)]
    Sb = k.sb("Sb", [64, NH, 64], BF16); b_Sb = [Buf() for _ in range(NH)]
    S.op("pool", lambda e: e.memset(S32[:], 0.0), writes=b_S32)
    S.op("pool", lambda e: e.memset(Sb[:], 0.0), writes=b_Sb)

    wdt = Ring([k.sb(f"wdt{i}", [96, 512], BF16) for i in range(2)])
    adt = Ring([k.sb(f"adt{i}", [96, 512], BF16) for i in range(2)])
    def htiles(name, shape, dt):
        return [k.sb(f"{name}{i}", shape, dt) for i in range(HB)], [Buf() for _ in range(HB)]
    AR, b_AR = htiles("AR", [64, 8, 2, 64], BF16)
    Tm, b_Tm = htiles("Tm", [64, 8, 64], BF16)
    Mka, b_Mka = htiles("Mka", [64, 8, 64], BF16)
    Mbr, b_Mbr = htiles("Mbr", [64, 8, 64], BF16)
    Mkr, b_Mkr = htiles("Mkr", [64, 8, 64], BF16)
    BhT, b_BhT = htiles("BhT", [64, 8, 64], BF16)
    KhT, b_KhT = htiles("KhT", [64, 8, 64], BF16)
    VT, b_VT = htiles("VT", [64, 8, 64], BF16)
    pC, b_pC = htiles("pC", [64, 8], F32)
    def tring(name, n, shape=[64, 512], dt=F32):
        return Ring([k.sb(f"{name}{i}", shape, dt) for i in range(n)])
    t_Ep = tring("t_Ep", 2, [64, 8, 64], F32)
    rin = tring("rin", 2, dt=BF16); kin = tring("kin", 2, dt=BF16); vin = tring("vin", 2, dt=BF16)
    t_s = tring("t_s", 2); t_cum = tring("t_cum", 2); t_a = tring("t_a", 2); t_kkr = tring("t_kkr", 2)
    t_sq = tring("t_sq", 2); t_nr = tring("t_nr", 2); t_kk = tring("t_kk", 2); t_t1 = tring("t_t1", 2)
    t_kp = tring("t_kp", 2); t_bv = tring("t_bv", 2); t_d1 = tring("t_d1", 2); t_Epv = tring("t_Epv", 2)
    t_Em = tring("t_Em", 2); t_EC = tring("t_EC", 2)
    t_Bt = tring("t_Bt", 2, dt=BF16); t_Kt = tring("t_Kt", 2, dt=BF16)
    t_Bh = tring("t_Bh", 2, dt=BF16); t_Kh = tring("t_Kh", 2, dt=BF16)
    t_rkp = tring("t_rkp", 2); t_brow = tring("t_brow", 2, [1, 512], F32)
    t_N = tring("t_N", 4, [64, 8, 64], BF16); t_NT = tring("t_NT", 4, [64, 8, 64], BF16)
    WTs = tring("WTs", 2, [64, HB, 64], BF16); UTs = tring("UTs", 2, [64, HB, 64], BF16)
    ysb = tring("ysb", 1, [64, HB, 64], F32)
    pA = Ring([k.ps(f"p3a{i}", [64, 512], F32) for i in range(2)])
    pG = Ring([k.ps(f"p3g{i}", [64, 4, 128], F32) for i in range(2)])
    pTb = Ring([k.ps(f"p3t{i}", [64, 8, 128], BF16) for i in range(2)])
    pW = k.ps("p3W", [64, HB, 64], F32); b_pW = Buf()
    pU = k.ps("p3U", [64, HB, 64], F32); b_pU = Buf()
    pS = pW; b_pS = b_pW

    RBv, KBv, VBv = sc["RB"], sc["KB"], sc["VB"]
    for tt in range(NTOK // 512):
        t0 = tt * 512
        own = t0 >= NPRE
        wd_, bwd = wdt.next(); ad_, bad = adt.next()
        S.dma("sp", lambda e, wd_=wd_, t0=t0: e.dma_start(out=wd_[:], in_=sc["WD"][:, t0:t0 + 512]), writes=[bwd])
        S.dma("sp", lambda e, ad_=ad_, t0=t0: e.dma_start(out=ad_[:], in_=sc["AD"][:, t0:t0 + 512]), writes=[bad])
        for hb0 in range(0, NH, HB):
            def prep(hi, hb0=hb0, own=own, t0=t0, wd_=wd_, ad_=ad_, bwd=bwd, bad=bad):
                h = hb0 + hi
                hs = slice(h * 64, (h + 1) * 64)
                col = lambda j: hc[:, j, h:h + 1]
                r_, br = rin.next(); k_, bk = kin.next(); v_, bv = vin.next()
                yield S.dma("sp", lambda e, r_=r_, hs=hs, t0=t0: e.dma_start(out=r_[:], in_=RBv[hs, t0:t0 + 512]), writes=[br])
                yield S.dma("act", lambda e, k_=k_, hs=hs, t0=t0: e.dma_start(out=k_[:], in_=KBv[hs, t0:t0 + 512]), writes=[bk])
                yield S.dma("sp", lambda e, v_=v_, hs=hs, t0=t0: e.dma_start(out=v_[:], in_=VBv[hs, t0:t0 + 512]), writes=[bv])
                p1, bp1 = pA.next()
                yield S.op("pe", lambda e, p1=p1, hs=hs, wd_=wd_: e.matmul(p1[:, :], lhsT=wup[:, hs], rhs=wd_[:, :], start=True, stop=True),
                     reads=[b_wupf, bwd], writes=[bp1])
                s_, bs = t_s.next()
                yield S.op("act", lambda e, s_=s_, p1=p1, h=h: e.activation(out=s_[:], in_=p1[:, :], func=AF.Sigmoid, bias=hc[:, 0, h:h + 1]),
                     reads=[bp1, b_hc], writes=[bs])
                cum, bcum = t_cum.next()
                yield S.op("dve", lambda e, cum=cum, s_=s_: e.tensor_tensor_scan(out=cum[:], data0=rmask[:], data1=s_[:], initial=0.0,
                                                                          op0=ALU.mult, op1=ALU.add), reads=[bs, b_rm], writes=[bcum])
                p2, bp2 = pA.next()
                yield S.op("pe", lambda e, p2=p2, hs=hs, ad_=ad_: e.matmul(p2[:, :], lhsT=aup[:, hs], rhs=ad_[:, :], start=True, stop=True),
                     reads=[b_aupf, bad], writes=[bp2])
                a_, ba = t_a.next()
                yield S.op("act", lambda e, a_=a_, p2=p2, h=h: e.activation(out=a_[:], in_=p2[:, :], func=AF.Sigmoid, bias=hc[:, 1, h:h + 1]),
                     reads=[bp2, b_hc], writes=[ba])
                kkr, bkkr = t_kkr.next()
                yield S.op("dve", lambda e, kkr=kkr, k_=k_, h=h: e.tensor_scalar(out=kkr[:], in0=k_[:], scalar1=hc[:, 2, h:h + 1], scalar2=None,
                                                                          op0=ALU.mult), reads=[bk, b_hc], writes=[bkkr])
                sq, bsq = t_sq.next()
                yield S.op("pool", lambda e, sq=sq, kkr=kkr: e.tensor_tensor(out=sq[:], in0=kkr[:], in1=kkr[:], op=ALU.mult), reads=[bkkr], writes=[bsq])
                p3, bp3 = pA.next()
                yield S.op("pe", lambda e, p3=p3, sq=sq: e.matmul(p3[:, :], lhsT=ones64[:, :], rhs=sq[:, :], start=True, stop=True),
                     reads=[b_ones, bsq], writes=[bp3])
                nr, bnr = t_nr.next()
                yield S.op("act", lambda e, nr=nr, p3=p3: e.activation(out=nr[:], in_=p3[:, :], func=AF.Sqrt), reads=[bp3], writes=[bnr])
                yield S.op("dve", lambda e, nr=nr: e.tensor_scalar(out=nr[:], in0=nr[:], scalar1=1e-12, scalar2=None, op0=ALU.max), reads=[bnr], writes=[bnr])
                yield S.op("dve", lambda e, nr=nr: e.reciprocal(out=nr[:], in_=nr[:]), reads=[bnr], writes=[bnr])
                kk, bkk = t_kk.next()
                yield S.op("dve", lambda e, kk=kk, kkr=kkr, nr=nr: e.tensor_tensor(out=kk[:], in0=kkr[:], in1=nr[:], op=ALU.mult), reads=[bkkr, bnr], writes=[bkk])
                t1, bt1 = t_t1.next()
                yield S.op("dve", lambda e, t1=t1, a_=a_, h=h: e.tensor_scalar(out=t1[:], in0=a_[:], scalar1=hc[:, 3, h:h + 1], scalar2=hc[:, 4, h:h + 1],
                                                                        op0=ALU.mult, op1=ALU.add), reads=[ba, b_hc], writes=[bt1])
                kp, bkp = t_kp.next()
                yield S.op("pool", lambda e, kp=kp, k_=k_, t1=t1: e.tensor_tensor(out=kp[:], in0=k_[:], in1=t1[:], op=ALU.mult), reads=[bk, bt1], writes=[bkp])
                bv_, bbv = t_bv.next()
                yield S.op("pool", lambda e, bv_=bv_, kk=kk, a_=a_: e.tensor_tensor(out=bv_[:], in0=kk[:], in1=a_[:], op=ALU.mult), reads=[bkk, ba], writes=[bbv])
                if own:
                    rkp, brkp = t_rkp.next()
                    yield S.op("dve", lambda e, rkp=rkp, r_=r_, kp=kp, h=h: e.scalar_tensor_tensor(out=rkp[:], in0=r_[:], scalar=hc[:, 5, h:h + 1], in1=kp[:],
                                                                                           op0=ALU.mult, op1=ALU.mult), reads=[br, bkp, b_hc], writes=[brkp])
                    pb, bpb = pA.next()
                    yield S.op("pe", lambda e, pb=pb, rkp=rkp: e.matmul(pb[0:1, :], lhsT=ones64[:, 0:1], rhs=rkp[:, :], start=True, stop=True),
                         reads=[b_ones, brkp], writes=[bpb])
                    brow, bbrow = t_brow.next()
                    yield S.op("act", lambda e, brow=brow, pb=pb: e.activation(out=brow[0:1, :], in_=pb[0:1, :], func=AF.Copy), reads=[bpb], writes=[bbrow])
                    yield S.dma("sp", lambda e, brow=brow, h=h, t0=t0: e.dma_start(out=sc["BON"][h:h + 1, t0 - NPRE:t0 - NPRE + 512], in_=brow[0:1, :]), reads=[bbrow])
                ep, bep = t_Ep.next()
                epf = ep[:].rearrange("p c t -> p (c t)")
                yield S.op("act", lambda e, epf=epf, cum=cum: e.activation(out=epf, in_=cum[:], func=AF.Exp, scale=-C0), reads=[bcum], writes=[bep])
                d1, bd1 = t_d1.next()
                yield S.op("pool", lambda e, d1=d1, cum=cum, s_=s_: e.tensor_tensor(out=d1[:], in0=cum[:], in1=s_[:], op=ALU.subtract), reads=[bcum, bs], writes=[bd1])
                epv, bepv = t_Epv.next()
                yield S.op("act", lambda e, epv=epv, d1=d1: e.activation(out=epv[:], in_=d1[:], func=AF.Exp, scale=-C0), reads=[bd1], writes=[bepv])
                em, bem = t_Em.next()
                yield S.op("act", lambda e, em=em, cum=cum: e.activation(out=em[:], in_=cum[:], func=AF.Exp, scale=C0), reads=[bcum], writes=[bem])
                yield S.op("pool", lambda e, ep=ep, hi=hi: e.tensor_copy(out=pC[hi][:, :], in_=ep[:, :, 63]), reads=[bep], writes=[b_pC[hi]])
                ec, bec = t_EC.next()
                for cc in range(8):
                    yield S.op("dve" if cc % 2 else "pool", lambda e, ec=ec, em=em, ep=ep, cc=cc: e.tensor_scalar(
                        out=ec[:, cc * 64:(cc + 1) * 64], in0=em[:, cc * 64:(cc + 1) * 64], scalar1=ep[:, cc, 63:64], scalar2=None, op0=ALU.mult),
                        reads=[bem, bep], writes=[bec])
                ar = AR[hi]; bar = b_AR[hi]
                yield S.op("dve", lambda e, ar=ar, r_=r_, epf=epf: e.tensor_tensor(out=ar[:, :, 1, :], in0=r_[:].rearrange("p (c t) -> p c t", t=64),
                                                                           in1=epf.rearrange("p (c t) -> p c t", t=64), op=ALU.mult), reads=[br, bep], writes=[bar])
                yield S.op("dve", lambda e, ar=ar, kk=kk, epv=epv: e.scalar_tensor_tensor(out=ar[:, :, 0, :], in0=kk[:].rearrange("p (c t) -> p c t", t=64), scalar=-1.0,
                                                                                  in1=epv[:].rearrange("p (c t) -> p c t", t=64), op0=ALU.mult, op1=ALU.mult), reads=[bkk, bepv], writes=[bar])
                Bt, bBt = t_Bt.next(); Kt, bKt = t_Kt.next(); Bh, bBh = t_Bh.next(); Kh, bKh = t_Kh.next()
                yield S.op("pool", lambda e, Bt=Bt, bv_=bv_, em=em: e.tensor_tensor(out=Bt[:], in0=bv_[:], in1=em[:], op=ALU.mult), reads=[bbv, bem], writes=[bBt])
                yield S.op("dve", lambda e, Kt=Kt, kp=kp, em=em: e.tensor_tensor(out=Kt[:], in0=kp[:], in1=em[:], op=ALU.mult), reads=[bkp, bem], writes=[bKt])
                yield S.op("pool", lambda e, Bh=Bh, bv_=bv_, ec=ec: e.tensor_tensor(out=Bh[:], in0=bv_[:], in1=ec[:], op=ALU.mult), reads=[bbv, bec], writes=[bBh])
                yield S.op("dve", lambda e, Kh=Kh, kp=kp, ec=ec: e.tensor_tensor(out=Kh[:], in0=kp[:], in1=ec[:], op=ALU.mult), reads=[bkp, bec], writes=[bKh])
                for src, bsrc, dstl, bdstl in ((Bh, bBh, BhT, b_BhT), (Kh, bKh, KhT, b_KhT), (v_, bv, VT, b_VT)):
                    pt, bpt = pTb.next()
                    for cc in range(8):
                        yield S.op("pe", lambda e, pt=pt, src=src, cc=cc: e.transpose(out=pt[:, cc, 0:64], in_=src[:, cc * 64:(cc + 1) * 64],
                                                                               identity=c["ident_b"][0:64, 0:64]), reads=[bsrc, c["b_ident"]], writes=[bpt])
                    yield S.op("act", lambda e, pt=pt, d=dstl[hi]: e.activation(out=d[:], in_=pt[:, :, 0:64], func=AF.Copy), reads=[bpt], writes=[bdstl[hi]])
                N0, bN0 = t_N.next()
                for hv in range(2):
                    pg, bpg = pG.next()
                    for c4 in range(4):
                        cc = 4 * hv + c4
                        yield S.op("pe", lambda e, pg=pg, Bt=Bt, ar=ar, cc=cc, c4=c4: e.matmul(pg[:, c4, :], lhsT=Bt[:, cc * 64:(cc + 1) * 64],
                                                                                        rhs=ar[:, cc, :, :].rearrange("p a t -> p (a t)"), start=True, stop=True),
                             reads=[bBt, bar], writes=[bpg])
                    yield S.op("dve", lambda e, N0=N0, pg=pg, hv=hv: e.tensor_tensor(out=N0[:, 4 * hv:4 * hv + 4, :], in0=pg[:, :, 0:64], in1=m_su[:, 0:4, :], op=ALU.mult),
                         reads=[bpg, b_su], writes=[bN0])
                    yield S.op("dve", lambda e, pg=pg, d=Mbr[hi], hv=hv: e.tensor_tensor(out=d[:, 4 * hv:4 * hv + 4, :], in0=pg[:, :, 64:128], in1=m_ui[:, 0:4, :], op=ALU.mult),
                         reads=[bpg, b_ui], writes=[b_Mbr[hi]])
                for hv in range(2):
                    pg, bpg = pG.next()
                    for c4 in range(4):
                        cc = 4 * hv + c4
                        yield S.op("pe", lambda e, pg=pg, Kt=Kt, ar=ar, cc=cc, c4=c4: e.matmul(pg[:, c4, :], lhsT=Kt[:, cc * 64:(cc + 1) * 64],
                                                                                        rhs=ar[:, cc, :, :].rearrange("p a t -> p (a t)"), start=True, stop=True),
                             reads=[bKt, bar], writes=[bpg])
                    yield S.op("dve", lambda e, pg=pg, d=Mka[hi], hv=hv: e.tensor_tensor(out=d[:, 4 * hv:4 * hv + 4, :], in0=pg[:, :, 0:64], in1=m_su[:, 0:4, :], op=ALU.mult),
                         reads=[bpg, b_su], writes=[b_Mka[hi]])
                    yield S.op("dve", lambda e, pg=pg, d=Mkr[hi], hv=hv: e.tensor_tensor(out=d[:, 4 * hv:4 * hv + 4, :], in0=pg[:, :, 64:128], in1=m_ui[:, 0:4, :], op=ALU.mult),
                         reads=[bpg, b_ui], writes=[b_Mkr[hi]])
                p4, bp4 = pA.next()
                for cc in range(8):
                    yield S.op("pe", lambda e, p4=p4, ar=ar, Bt=Bt, cc=cc: e.matmul(p4[:, cc * 64:(cc + 1) * 64], lhsT=ar[:, cc, 0, :], rhs=Bt[:, cc * 64:(cc + 1) * 64],
                                                                             start=True, stop=True), reads=[bar, bBt], writes=[bp4])
                NT0, bNT0 = t_NT.next()
                yield S.op("dve", lambda e, NT0=NT0, p4=p4: e.tensor_tensor(out=NT0[:], in0=p4[:, :].rearrange("p (c t) -> p c t", t=64), in1=m_sl[:], op=ALU.mult),
                     reads=[bp4, b_sl], writes=[bNT0])
                T_ = Tm[hi]; bT = b_Tm[hi]
                yield S.op("pool", lambda e, T_=T_, N0=N0: e.tensor_tensor(out=T_[:], in0=N0[:], in1=i8[:], op=ALU.add), reads=[bN0, b_i8], writes=[bT])
                Nc, bNc, NTc, bNTc = N0, bN0, NT0, bNT0
                for lvl in range(1, 6):
                    pnt, bpnt = pA.next()
                    for cc in range(8):
                        yield S.op("pe", lambda e, pnt=pnt, Nc=Nc, NTc=NTc, cc=cc: e.matmul(pnt[:, cc * 64:(cc + 1) * 64], lhsT=Nc[:, cc, :], rhs=NTc[:, cc, :],
                                                                                     start=True, stop=True), reads=[bNc, bNTc], writes=[bpnt])
                    NTn, bNTn = t_NT.next()
                    yield S.op("act", lambda e, NTn=NTn, pnt=pnt: e.activation(out=NTn[:].rearrange("p c t -> p (c t)"), in_=pnt[:, :], func=AF.Copy),
                         reads=[bpnt], writes=[bNTn])
                    if lvl < 5:
                        pn, bpn = pA.next()
                        for cc in range(8):
                            yield S.op("pe", lambda e, pn=pn, Nc=Nc, NTc=NTc, cc=cc: e.matmul(pn[:, cc * 64:(cc + 1) * 64], lhsT=NTc[:, cc, :], rhs=Nc[:, cc, :],
                                                                                       start=True, stop=True), reads=[bNc, bNTc], writes=[bpn])
                        Nn, bNn = t_N.next()
                        yield S.op("act", lambda e, Nn=Nn, pn=pn: e.activation(out=Nn[:].rearrange("p c t -> p (c t)"), in_=pn[:, :], func=AF.Copy),
                             reads=[bpn], writes=[bNn])
                    ptt, bptt = pA.next()
                    for cc in range(8):
                        yield S.op("pe", lambda e, ptt=ptt, NTn=NTn, T_=T_, cc=cc: e.matmul(ptt[:, cc * 64:(cc + 1) * 64], lhsT=NTn[:, cc, :], rhs=T_[:, cc, :],
                                                                                     start=True, stop=True), reads=[bNTn, bT], writes=[bptt])
                    yield S.op("dve", lambda e, T_=T_, ptt=ptt: e.tensor_tensor(out=T_[:].rearrange("p c t -> p (c t)"), in0=ptt[:, :],
                                                                         in1=T_[:].rearrange("p c t -> p (c t)"), op=ALU.add), reads=[bptt, bT], writes=[bT])
                    NTc, bNTc = NTn, bNTn
                    if lvl < 5:
                        Nc, bNc = Nn, bNn
            GRP = 2
            for gq in range(0, HB, GRP):
                gens = [prep(hi) for hi in range(gq, gq + GRP)]
                while gens:
                    for g_ in list(gens):
                        try:
                            next(g_)
                        except StopIteration:
                            gens.remove(g_)
            for cc in range(8):
                hbufs = lambda lst: [lst[i] for i in range(HB)]
                for hi in range(HB):
                    h = hb0 + hi
                    S.op("pe", lambda e, hi=hi, h=h, cc=cc: e.matmul(pW[:, hi, :], lhsT=AR[hi][:, cc, 0, :], rhs=Sb[:, h, :], start=(hi == 0), stop=False,
                                                                    skip_group_check=True), reads=[b_AR[hi], b_Sb[h]], writes=[b_pW])
                    S.op("pe", lambda e, hi=hi, cc=cc: e.matmul(pW[:, hi, :], lhsT=Mka[hi][:, cc, :], rhs=VT[hi][:, cc, :], start=False, stop=True,
                                                               skip_group_check=True), reads=[b_Mka[hi], b_VT[hi]], writes=[b_pW])
                wt, bwt = WTs.next()
                S.op("act", lambda e, wt=wt: e.activation(out=wt[:], in_=pW[:], func=AF.Copy), reads=[b_pW], writes=[bwt])
                for hi in range(HB):
                    S.op("pe", lambda e, hi=hi, cc=cc, wt=wt: e.matmul(pU[:, hi, :], lhsT=Tm[hi][:, cc, :], rhs=wt[:, hi, :], start=(hi == 0), stop=True,
                                                                      skip_group_check=True), reads=[b_Tm[hi], bwt], writes=[b_pU])
                ut, but = UTs.next()
                S.op("dve", lambda e, ut=ut: e.tensor_copy(out=ut[:], in_=pU[:]), reads=[b_pU], writes=[but])
                if own:
                    py, bpy = pA.next()
                    for hi in range(HB):
                        h = hb0 + hi
                        S.op("pe", lambda e, py=py, hi=hi, h=h, cc=cc: e.matmul(py[:, hi * 64:(hi + 1) * 64], lhsT=AR[hi][:, cc, 1, :], rhs=Sb[:, h, :],
                                                                               start=(hi == 0), stop=False, skip_group_check=True),
                             reads=[b_AR[hi], b_Sb[h]], writes=[bpy])
                    for hi in range(HB):
                        S.op("pe", lambda e, py=py, hi=hi, cc=cc, ut=ut: e.matmul(py[:, hi * 64:(hi + 1) * 64], lhsT=Mbr[hi][:, cc, :], rhs=ut[:, hi, :],
                                                                                 start=False, stop=False, skip_group_check=True),
                             reads=[b_Mbr[hi], but], writes=[bpy])
                        S.op("pe", lambda e, py=py, hi=hi, cc=cc: e.matmul(py[:, hi * 64:(hi + 1) * 64], lhsT=Mkr[hi][:, cc, :], rhs=VT[hi][:, cc, :],
                                                                          start=False, stop=True, skip_group_check=True),
                             reads=[b_Mkr[hi], b_VT[hi]], writes=[bpy])
                    ys, bys = ysb.next()
                    S.op("act", lambda e, ys=ys, py=py: e.activation(out=ys[:].rearrange("p h v -> p (h v)"), in_=py[:, :], func=AF.Copy), reads=[bpy], writes=[bys])
                    trow = t0 - NPRE + cc * 64
                    S.dma("sp", lambda e, ys=ys, trow=trow, hb0=hb0: e.dma_start(
                        out=sc["YB"][trow:trow + 64, hb0 * 64:(hb0 + HB) * 64], in_=ys[:].rearrange("p h v -> p (h v)")), reads=[bys])
                for hi in range(HB):
                    S.op("pe", lambda e, hi=hi, cc=cc, ut=ut: e.matmul(pS[:, hi, :], lhsT=BhT[hi][:, cc, :], rhs=ut[:, hi, :], start=(hi == 0), stop=False,
                                                                      skip_group_check=True), reads=[b_BhT[hi], but], writes=[b_pS])
                    S.op("pe", lambda e, hi=hi, cc=cc: e.matmul(pS[:, hi, :], lhsT=KhT[hi][:, cc, :], rhs=VT[hi][:, cc, :], start=False, stop=True,
                                                               skip_group_check=True), reads=[b_KhT[hi], b_VT[hi]], writes=[b_pS])
                for hi in range(HB):
                    h = hb0 + hi
                    S.op("dve", lambda e, hi=hi, h=h, cc=cc: e.scalar_tensor_tensor(out=S32[:, h, :], in0=S32[:, h, :], scalar=pC[hi][:, cc:cc + 1],
                                                                                  in1=pS[:, hi, :], op0=ALU.mult, op1=ALU.add),
                         reads=[b_S32[h], b_pC[hi], b_pS], writes=[b_S32[h]])
                    S.op("act", lambda e, h=h: e.activation(out=Sb[:, h, :], in_=S32[:, h, :], func=AF.Copy), reads=[b_S32[h]], writes=[b_Sb[h]])


def phase3b(k, prm):
    cfg, S, nc, c, sc = k.cfg, k.S, k.nc, k.c, k.sc
    NTOK, NPRE, NOWN = cfg.NTOK, cfg.NPRE, cfg.NOWN
    sc["YBT"] = k.scratch("YBT", [AW, NOWN], BF16)
    k.phase()
    YBTv = sc["YBT"].rearrange("(c p) t -> p c t", p=128)
    VBv = sc["VB"].rearrange("(c p) t -> p c t", p=128)
    GDv = sc["GD"].rearrange("(c p) t -> p c t", p=128)
    gupf = k.sb("gupf", [128, 2, AW], F32); gup = k.sb("gup", [128, 2, AW], BF16); b_gup = Buf()
    S.dma("sp", lambda e: e.dma_start(out=gupf[:], in_=prm["g_up"].rearrange("(c p) n -> p c n", p=128)), writes=[b_gup])
    S.op("act", lambda e: e.activation(out=gup[:], in_=gupf[:], func=AF.Copy), reads=[b_gup], writes=[b_gup])
    lw = k.sb("lnxw", [128, AW], F32); lb = k.sb("lnxb", [128, AW], F32); b_l = Buf()
    S.dma("sp", lambda e: e.dma_start(out=lw[:], in_=prm["lnx_w"][0:1, :].partition_broadcast(128)), writes=[b_l])
    S.dma("sp", lambda e: e.dma_start(out=lb[:], in_=prm["lnx_b"][0:1, :].partition_broadcast(128)), writes=[b_l])
    yin = Ring([k.sb(f"yin{i}", [128, AW], F32) for i in range(2)])
    ysq = Ring([k.sb(f"ysq{i}", [128, AW], F32) for i in range(2)])
    st = Ring([k.sb(f"gst{i}", [128, 6, NH], F32) for i in range(2)])
    yn = Ring([k.sb(f"yn{i}", [128, AW], F32) for i in range(2)])
    vfm = Ring([k.sb(f"vfm{i}", [128, 8, 128], BF16) for i in range(2)])
    vtm = Ring([k.sb(f"vtm{i}", [128, AW], BF16) for i in range(2)])
    gdl = Ring([k.sb(f"gdl{i}", [128, 2, 128], BF16) for i in range(2)])
    bonf = Ring([k.sb(f"bonf{i}", [NH, 128], F32) for i in range(2)])
    bont = Ring([k.sb(f"bont{i}", [128, NH], F32) for i in range(2)])
    yo = Ring([k.sb(f"yo{i}", [128, AW], BF16) for i in range(2)])
    ost = Ring([k.sb(f"ybst{i}", [128, 8, 512], BF16) for i in range(2)])
    pT = Ring([k.ps(f"p3bT{i}", [128, 8, 128], BF16) for i in range(2)])
    pg = Ring([k.ps(f"p3bg{i}", [128, 512], F32) for i in range(2)])
    pbn = Ring([k.ps("p3bbn", [128, NH], F32)])
    bc = lambda ap: ap.unsqueeze(2).to_broadcast([128, NH, 64])
    v3 = lambda t: t[:].rearrange("p (h d) -> p h d", d=64)
    for ti in range(NOWN // 128):
        tok0 = ti * 128
        if ti % 4 == 0:
            os_, bos = ost.next()
        y_, by = yin.next()
        S.dma("sp", lambda e, y_=y_, tok0=tok0: e.dma_start(out=y_[:], in_=sc["YB"][tok0:tok0 + 128, :]), writes=[by])
        vf, bvf = vfm.next()
        S.dma("act", lambda e, vf=vf, tok0=tok0: e.dma_start(out=vf[:], in_=VBv[:, :, NPRE + tok0:NPRE + tok0 + 128]), writes=[bvf])
        gd, bgd = gdl.next()
        S.dma("act", lambda e, gd=gd, tok0=tok0: e.dma_start(out=gd[:], in_=GDv[:, :, NPRE + tok0:NPRE + tok0 + 128]), writes=[bgd])
        bf_, bbf = bonf.next()
        S.dma("sp", lambda e, bf_=bf_, tok0=tok0: e.dma_start(out=bf_[:], in_=sc["BON"][:, tok0:tok0 + 128]), writes=[bbf])
        s_, bs = st.next()
        S.op("dve", lambda e, s_=s_, y_=y_: e.tensor_reduce(out=s_[:, 0, :], in_=v3(y_), axis=AX.X, op=ALU.add), reads=[by], writes=[bs])
        q_, bq = ysq.next()
        S.op("act", lambda e, q_=q_, y_=y_: e.activation(out=q_[:], in_=y_[:], func=AF.Square), reads=[by], writes=[bq])
        S.op("dve", lambda e, s_=s_, q_=q_: e.tensor_reduce(out=s_[:, 1, :], in_=v3(q_), axis=AX.X, op=ALU.add), reads=[bq], writes=[bs])
        S.op("dve", lambda e, s_=s_: e.tensor_scalar(out=s_[:, 2, :], in0=s_[:, 0, :], scalar1=1.0 / 64, scalar2=None, op0=ALU.mult), reads=[bs], writes=[bs])
        S.op("dve", lambda e, s_=s_: e.tensor_tensor(out=s_[:, 3, :], in0=s_[:, 2, :], in1=s_[:, 2, :], op=ALU.mult), reads=[bs], writes=[bs])
        S.op("dve", lambda e, s_=s_: e.scalar_tensor_tensor(out=s_[:, 4, :], in0=s_[:, 1, :], scalar=1.0 / 64, in1=s_[:, 3, :], op0=ALU.mult, op1=ALU.subtract),
             reads=[bs], writes=[bs])
        S.op("dve", lambda e, s_=s_: e.tensor_scalar(out=s_[:, 4, :], in0=s_[:, 4, :], scalar1=64e-5, scalar2=None, op0=ALU.add), reads=[bs], writes=[bs])
        S.op("act", lambda e, s_=s_: e.activation(out=s_[:, 5, :], in_=s_[:, 4, :], func=AF.Sqrt), reads=[bs], writes=[bs])
        S.op("dve", lambda e, s_=s_: e.reciprocal(out=s_[:, 5, :], in_=s_[:, 5, :]), reads=[bs], writes=[bs])
        n_, bn = yn.next()
        S.op("dve", lambda e, n_=n_, y_=y_, s_=s_: e.tensor_tensor(out=v3(n_), in0=v3(y_), in1=bc(s_[:, 2, :]), op=ALU.subtract), reads=[by, bs], writes=[bn])
        S.op("pool", lambda e, n_=n_, s_=s_: e.tensor_tensor(out=v3(n_), in0=v3(n_), in1=bc(s_[:, 5, :]), op=ALU.mult), reads=[bn, bs], writes=[bn])
        S.op("dve", lambda e, n_=n_: e.tensor_tensor(out=n_[:], in0=n_[:], in1=lw[:], op=ALU.mult), reads=[bn, b_l], writes=[bn])
        S.op("pool", lambda e, n_=n_: e.tensor_tensor(out=n_[:], in0=n_[:], in1=lb[:], op=ALU.add), reads=[bn, b_l], writes=[bn])
        pb, bpb = pbn.next()
        S.op("pe", lambda e, pb=pb, bf_=bf_: e.transpose(out=pb[:, :], in_=bf_[:, :], identity=c["ident_f"][0:NH, 0:NH]), reads=[bbf, c["b_ident"]], writes=[bpb])
        bt, bbt = bont.next()
        S.op("act", lambda e, bt=bt, pb=pb: e.activation(out=bt[:], in_=pb[:, :], func=AF.Copy), reads=[bpb], writes=[bbt])
        pt, bpt = pT.next()
        for j in range(8):
            S.op("pe", lambda e, pt=pt, vf=vf, j=j: e.transpose(out=pt[:, j, :], in_=vf[:, j, :], identity=c["ident_b"][:]), reads=[bvf, c["b_ident"]], writes=[bpt])
        vt, bvt = vtm.next()
        S.op("act", lambda e, vt=vt, pt=pt: e.activation(out=vt[:].rearrange("p (j c) -> p j c", c=128), in_=pt[:], func=AF.Copy), reads=[bpt], writes=[bvt])
        q2, bq2 = ysq.next()
        S.op("pool", lambda e, q2=q2, vt=vt, bt=bt: e.tensor_tensor(out=v3(q2), in0=v3(vt), in1=bc(bt[:, :]), op=ALU.mult), reads=[bvt, bbt], writes=[bq2])
        S.op("dve", lambda e, n_=n_, q2=q2: e.tensor_tensor(out=n_[:], in0=n_[:], in1=q2[:], op=ALU.add), reads=[bn, bq2], writes=[bn])
        o_, bo = yo.next()
        for half in range(2):
            pg_, bpg = pg.next()
            for kc in range(2):
                S.op("pe", lambda e, pg_=pg_, gd=gd, kc=kc, half=half: e.matmul(pg_[:, :], lhsT=gd[:, kc, :], rhs=gup[:, kc, half * 512:(half + 1) * 512],
                                                                               start=(kc == 0), stop=(kc == 1)), reads=[bgd, b_gup], writes=[bpg])
            S.op("dve", lambda e, o_=o_, n_=n_, pg_=pg_, half=half: e.tensor_tensor(out=o_[:, half * 512:(half + 1) * 512], in0=pg_[:, :],
                                                                                   in1=n_[:, half * 512:(half + 1) * 512], op=ALU.mult), reads=[bpg, bn], writes=[bo])
        pt2, bpt2 = pT.next()
        for j in range(8):
            S.op("pe", lambda e, pt2=pt2, o_=o_, j=j: e.transpose(out=pt2[:, j, :], in_=o_[:, j * 128:(j + 1) * 128], identity=c["ident_b"][:]),
                 reads=[bo, c["b_ident"]], writes=[bpt2])
        S.op("act", lambda e, os_=os_, pt2=pt2, ti=ti: e.activation(out=os_[:, :, (ti % 4) * 128:(ti % 4 + 1) * 128], in_=pt2[:], func=AF.Copy), reads=[bpt2], writes=[bos])
        if ti % 4 == 3:
            g0 = (ti // 4) * 512
            S.dma("sp", lambda e, os_=os_, g0=g0: e.dma_start(out=YBTv[:, :, g0:g0 + 512], in_=os_[:]), reads=[bos])


def cast_load(k, S, dst_bf, bdst, src_ap_fn, nk, ncols, stg, step=256):
    for i, c0 in enumerate(range(0, ncols, step)):
        n = min(step, ncols - c0)
        st, bst = stg.next()
        S.dma("sp" if i % 2 == 0 else "act", lambda e, st=st, c0=c0, n=n: e.dma_start(out=st[:, 0:nk, 0:n], in_=src_ap_fn(c0, n)), writes=[bst])
        if i % 2 == 0:
            S.op("act", lambda e, st=st, c0=c0, n=n: e.activation(out=dst_bf[:, 0:nk, c0:c0 + n], in_=st[:, 0:nk, 0:n], func=AF.Copy), reads=[bst], writes=[bdst])
        else:
            S.op("pool", lambda e, st=st, c0=c0, n=n: e.tensor_copy(out=dst_bf[:, 0:nk, c0:c0 + n], in_=st[:, 0:nk, 0:n]), reads=[bst], writes=[bdst])


def phase4a(k, proj_a, proj_b):
    cfg, S, nc, c, sc = k.cfg, k.S, k.nc, k.c, k.sc
    DM, KC, NOWN = cfg.DM, cfg.KC, cfg.NOWN
    sc["MT"] = k.scratch("MT", [DM, NOWN], BF16)
    k.phase()
    PA = k.sb("PAb", [128, 8, DM], BF16); PB = k.sb("PBb", [128, 8, DM], BF16); bPA = Buf(); bPB = Buf()
    stg = Ring([k.sb(f"p4stg{i}", [128, 8, 256], F32) for i in range(2)])
    pav = proj_a.rearrange("(c p) n -> p c n", p=128); pbv = proj_b.rearrange("(c p) n -> p c n", p=128)
    cast_load(k, S, PA, bPA, lambda c0, n: pav[:, :, c0:c0 + n], 8, DM, stg)
    cast_load(k, S, PB, bPB, lambda c0, n: pbv[:, :, c0:c0 + n], 8, DM, stg)
    YATv = sc["YAT"].rearrange("(c p) t -> p c t", p=128); YBTv = sc["YBT"].rearrange("(c p) t -> p c t", p=128)
    ya = Ring([k.sb(f"p4ya{i}", [128, 8, 512], BF16) for i in range(2)])
    yb = Ring([k.sb(f"p4yb{i}", [128, 8, 512], BF16) for i in range(2)])
    ga = Ring([k.sb(f"p4ga{i}", [128, 512], BF16) for i in range(3)])
    gb = Ring([k.sb(f"p4gb{i}", [128, 512], BF16) for i in range(3)])
    t1 = Ring([k.sb(f"p4t1{i}", [128, 512], F32) for i in range(2)])
    t2 = Ring([k.sb(f"p4t2{i}", [128, 512], F32) for i in range(2)])
    mo = Ring([k.sb(f"p4mo{i}", [128, 512], BF16) for i in range(3)])
    psa = Ring([k.ps(f"p4pa{i}", [128, 512], F32) for i in range(2)])
    psb = Ring([k.ps(f"p4pb{i}", [128, 512], F32) for i in range(2)])
    for tt in range(NOWN // 512):
        t0 = tt * 512
        a_, ba = ya.next(); b_, bb = yb.next()
        S.dma("sp", lambda e, a_=a_, t0=t0: e.dma_start(out=a_[:], in_=YATv[:, :, t0:t0 + 512]), writes=[ba])
        S.dma("act", lambda e, b_=b_, t0=t0: e.dma_start(out=b_[:], in_=YBTv[:, :, t0:t0 + 512]), writes=[bb])
        for i in range(KC):
            g1, bg1 = ga.next(); g2, bg2 = gb.next()
            S.dma("sp", lambda e, g1=g1, i=i, t0=t0: e.dma_start(out=g1[:], in_=sc["GA"][i * 128:(i + 1) * 128, t0:t0 + 512]), writes=[bg1])
            S.dma("act", lambda e, g2=g2, i=i, t0=t0: e.dma_start(out=g2[:], in_=sc["GB"][i * 128:(i + 1) * 128, t0:t0 + 512]), writes=[bg2])
            p1, bp1 = psa.next(); p2, bp2 = psb.next()
            for kc in range(8):
                S.op("pe", lambda e, p1=p1, a_=a_, kc=kc, i=i: e.matmul(p1[:, :], lhsT=PA[:, kc, i * 128:(i + 1) * 128], rhs=a_[:, kc, :],
                                                                       start=(kc == 0), stop=(kc == 7)), reads=[bPA, ba], writes=[bp1])
            for kc in range(8):
                S.op("pe", lambda e, p2=p2, b_=b_, kc=kc, i=i: e.matmul(p2[:, :], lhsT=PB[:, kc, i * 128:(i + 1) * 128], rhs=b_[:, kc, :],
                                                                       start=(kc == 0), stop=(kc == 7)), reads=[bPB, bb], writes=[bp2])
            x1, bx1 = t1.next(); x2, bx2 = t2.next(); m_, bm = mo.next()
            S.op("dve", lambda e, x1=x1, p1=p1, g1=g1: e.tensor_tensor(out=x1[:], in0=p1[:, :], in1=g1[:], op=ALU.mult), reads=[bp1, bg1], writes=[bx1])
            S.op("dve", lambda e, x2=x2, p2=p2, g2=g2: e.tensor_tensor(out=x2[:], in0=p2[:, :], in1=g2[:], op=ALU.mult), reads=[bp2, bg2], writes=[bx2])
            S.op("pool", lambda e, m_=m_, x1=x1, x2=x2: e.tensor_tensor(out=m_[:], in0=x1[:], in1=x2[:], op=ALU.add), reads=[bx1, bx2], writes=[bm])
            S.dma("pool", lambda e, m_=m_, i=i, t0=t0: e.dma_start(out=sc["MT"][i * 128:(i + 1) * 128, t0:t0 + 512], in_=m_[:]), reads=[bm])


def phase4b(k, xe, w_out, prm):
    cfg, S, nc, c, sc = k.cfg, k.S, k.nc, k.c, k.sc
    DM, KC, NOWN, NPRE, NE, CAP = cfg.DM, cfg.KC, cfg.NOWN, cfg.NPRE, cfg.NE, cfg.CAP
    NT = NOWN // 128
    sc["H"] = k.scratch("H", [NOWN, DM], F32)
    sc["XE"] = k.scratch("XE", [NE * CAP, DM], BF16)
    k.phase()
    WO = k.sb("WOb", [128, KC, DM], BF16); bWO = Buf()
    stg = Ring([k.sb(f"p4bstg{i}", [128, KC, 128], F32) for i in range(2)])
    wov = w_out.rearrange("(c p) n -> p c n", p=128)
    cast_load(k, S, WO, bWO, lambda c0, n: wov[:, :, c0:c0 + n], KC, DM, stg, step=128)
    lw = k.sb("ln1w", [128, DM], F32); lb = k.sb("ln1b", [128, DM], F32); b_l = Buf()
    S.dma("sp", lambda e: e.dma_start(out=lw[:], in_=prm["ln1_w"][0:1, :].partition_broadcast(128)), writes=[b_l])
    S.dma("sp", lambda e: e.dma_start(out=lb[:], in_=prm["ln1_b"][0:1, :].partition_broadcast(128)), writes=[b_l])
    wr = k.sb("wr", [128, KC, NE], F32); b_wr = Buf()
    S.dma("sp", lambda e: e.dma_start(out=wr[:], in_=prm["w_router"].rearrange("(c p) n -> p c n", p=128)), writes=[b_wr])
    brt = k.sb("brt", [128, NE], F32)
    S.dma("sp", lambda e: e.dma_start(out=brt[:], in_=prm["b_router"][0:1, :].partition_broadcast(128)), writes=[b_wr])
    iot = k.sb("iot", [128, NE], F32); usf = k.sb("usf", [128, 128], F32); usb = k.sb("usb", [128, 128], BF16)
    onb = k.sb("onb", [128, 128], BF16); b_cst = Buf()
    S.dma("sp", lambda e: e.dma_start(out=iot[:], in_=prm["iota32"][:, :]), writes=[b_cst])
    S.dma("sp", lambda e: e.dma_start(out=usf[:], in_=prm["ustrict"][:, :]), writes=[b_cst])
    S.op("act", lambda e: e.activation(out=usb[:], in_=usf[:], func=AF.Copy), reads=[b_cst], writes=[b_cst])
    S.op("pool", lambda e: e.memset(onb[:], 1.0), writes=[b_cst])
    base = k.sb("rbase", [128, NE], F32); b_base = Buf()
    S.op("pool", lambda e: e.memset(base[:], 0.0), writes=[b_base])
    zt = k.sb("zt", [128, DM], BF16); b_zt = Buf()
    S.op("pool", lambda e: e.memset(zt[:], 0.0), writes=[b_zt])
    b_XE = Buf()
    for r0 in range(0, NE * CAP, 128):
        S.dma("sp" if (r0 // 128) % 2 == 0 else "act", lambda e, r0=r0: e.dma_start(out=sc["XE"][r0:r0 + 128, :], in_=zt[:]), reads=[b_zt], writes=[b_XE])
    MTv = sc["MT"].rearrange("(c p) t -> p c t", p=128)
    mt = Ring([k.sb(f"p4mt{i}", [128, KC, 128], BF16) for i in range(2)])
    xt = Ring([k.sb(f"p4xt{i}", [128, DM], F32) for i in range(1)])
    zz = Ring([k.sb(f"p4z{i}", [128, DM], F32) for i in range(2)])
    hb = Ring([k.sb(f"p4hb{i}", [128, DM], BF16) for i in range(2)])
    hT = Ring([k.sb(f"p4hT{i}", [128, KC, 128], F32) for i in range(1)])
    sm = Ring([k.sb(f"p4sm{i}", [128, 256], F32) for i in range(2)])
    oh = Ring([k.sb(f"p4oh{i}", [128, 4, NE], F32) for i in range(2)])
    pr = Ring([k.sb(f"p4pr{i}", [128, 4, NE], F32) for i in range(2)])
    selb = Ring([k.sb(f"p4selb{i}", [128, NE], BF16) for i in range(2)])
    bst = Ring([k.sb(f"p4bst{i}", [128, 4, 6], F32) for i in range(2)])
    pz = Ring([k.ps(f"p4pz{i}", [128, 512], F32) for i in range(3)])
    pT = Ring([k.ps(f"p4T{i}", [128, 4, 128], F32) for i in range(2)])
    pl = Ring([k.ps("p4l", [128, 128], F32)])
    for ti in range(NT):
        tok0 = ti * 128
        m_, bm = mt.next(); x_, bx = xt.next(); z_, bz = zz.next()
        S.dma("sp", lambda e, m_=m_, tok0=tok0: e.dma_start(out=m_[:], in_=MTv[:, :, tok0:tok0 + 128]), writes=[bm])
        S.dma("act", lambda e, x_=x_, tok0=tok0: e.dma_start(out=x_[:], in_=xe[NPRE + tok0:NPRE + tok0 + 128, :]), writes=[bx])
        s_, bs = bst.next()
        ncg = DM // 512 if DM >= 512 else 1
        cw = DM // ncg
        for cg in range(ncg):
            p_, bp = pz.next()
            for kc in range(KC):
                S.op("pe", lambda e, p_=p_, m_=m_, kc=kc, cg=cg: e.matmul(p_[:, 0:cw], lhsT=m_[:, kc, :], rhs=WO[:, kc, cg * cw:(cg + 1) * cw],
                                                                         start=(kc == 0), stop=(kc == KC - 1)), reads=[bm, bWO], writes=[bp])
            S.op("dve", lambda e, z_=z_, x_=x_, p_=p_, cg=cg: e.scalar_tensor_tensor(out=z_[:, cg * cw:(cg + 1) * cw], in0=x_[:, cg * cw:(cg + 1) * cw], scalar=float(cfg.alpha),
                                                                                   in1=p_[:, 0:cw], op0=ALU.mult, op1=ALU.add), reads=[bx, bp], writes=[bz])
            S.op("dve", lambda e, s_=s_, z_=z_, cg=cg: e.bn_stats(out=s_[:, cg, :], in_=z_[:, cg * cw:(cg + 1) * cw]), reads=[bz], writes=[bs])
        q_, bq = sm.next()
        S.op("dve", lambda e, q_=q_, s_=s_: e.bn_aggr(out=q_[:, 0:2], in_=s_[:, 0:ncg, :].rearrange("p a b -> p (a b)")), reads=[bs], writes=[bq])
        S.op("dve", lambda e, q_=q_: e.tensor_scalar(out=q_[:, 2:3], in0=q_[:, 1:2], scalar1=1e-5, scalar2=None, op0=ALU.add), reads=[bq], writes=[bq])
        S.op("act", lambda e, q_=q_: e.activation(out=q_[:, 3:4], in_=q_[:, 2:3], func=AF.Sqrt), reads=[bq], writes=[bq])
        S.op("dve", lambda e, q_=q_: e.reciprocal(out=q_[:, 4:5], in_=q_[:, 3:4]), reads=[bq], writes=[bq])
        S.op("dve", lambda e, z_=z_, q_=q_: e.tensor_scalar(out=z_[:], in0=z_[:], scalar1=q_[:, 0:1], scalar2=q_[:, 4:5], op0=ALU.subtract, op1=ALU.mult),
             reads=[bz, bq], writes=[bz])
        S.op("pool", lambda e, z_=z_: e.tensor_tensor(out=z_[:], in0=z_[:], in1=lw[:], op=ALU.mult), reads=[bz, b_l], writes=[bz])
        S.op("dve", lambda e, z_=z_: e.tensor_tensor(out=z_[:], in0=z_[:], in1=lb[:], op=ALU.add), reads=[bz, b_l], writes=[bz])
        S.dma("sp", lambda e, z_=z_, tok0=tok0: e.dma_start(out=sc["H"][tok0:tok0 + 128, :], in_=z_[:]), reads=[bz])
        h_, bh = hb.next()
        S.op("act", lambda e, h_=h_, z_=z_: e.activation(out=h_[:], in_=z_[:], func=AF.Copy), reads=[bz], writes=[bh])
        t_, bt = hT.next()
        for g0 in range(0, KC, 4):
            ng = min(4, KC - g0)
            pt, bpt = pT.next()
            for j in range(ng):
                S.op("pe", lambda e, pt=pt, z_=z_, j=j, g0=g0: e.transpose(out=pt[:, j, :], in_=z_[:, (g0 + j) * 128:(g0 + j + 1) * 128], identity=c["ident_f"][:]),
                     reads=[bz, c["b_ident"]], writes=[bpt])
            S.op("act", lambda e, t_=t_, pt=pt, g0=g0, ng=ng: e.activation(out=t_[:, g0:g0 + ng, :], in_=pt[:, 0:ng, :], func=AF.Copy), reads=[bpt], writes=[bt])
        pl_, bpl = pl.next()
        for kc in range(KC):
            S.op("pe", lambda e, pl_=pl_, t_=t_, kc=kc: e.matmul(pl_[:, 0:NE], lhsT=t_[:, kc, :], rhs=wr[:, kc, :], start=(kc == 0), stop=(kc == KC - 1)),
                 reads=[bt, b_wr], writes=[bpl])
        lg = q_[:, 8:8 + NE]
        S.op("dve", lambda e, lg=lg, pl_=pl_: e.tensor_tensor(out=lg, in0=pl_[:, 0:NE], in1=brt[:], op=ALU.add), reads=[bpl, b_wr], writes=[bq])
        top = q_[:, 48:56]
        S.op("dve", lambda e, top=top, lg=lg: e.max(out=top, in_=lg), reads=[bq], writes=[bq])
        S.op("dve", lambda e, q_=q_: e.tensor_scalar(out=q_[:, 56:57], in0=q_[:, 48:49], scalar1=-1.0, scalar2=None, op0=ALU.mult), reads=[bq], writes=[bq])
        S.op("act", lambda e, q_=q_: e.activation(out=q_[:, 60:64], in_=q_[:, 48:52], func=AF.Exp, bias=q_[:, 56:57]), reads=[bq], writes=[bq])
        S.op("dve", lambda e, q_=q_: e.tensor_reduce(out=q_[:, 57:58], in_=q_[:, 60:64], axis=AX.X, op=ALU.add), reads=[bq], writes=[bq])
        S.op("dve", lambda e, q_=q_: e.reciprocal(out=q_[:, 58:59], in_=q_[:, 57:58]), reads=[bq], writes=[bq])
        S.op("dve", lambda e, q_=q_, ti=ti: e.tensor_scalar(out=k.gate_all[:, ti, :], in0=q_[:, 60:64], scalar1=q_[:, 58:59], scalar2=None, op0=ALU.mult),
             reads=[bq], writes=[k.b_gate])
        o_, bo = oh.next()
        for kk_ in range(4):
            S.op("dve" if kk_ % 2 == 0 else "pool", lambda e, o_=o_, lg=lg, q_=q_, kk_=kk_: e.tensor_scalar(
                out=o_[:, kk_, :], in0=lg, scalar1=q_[:, 48 + kk_:49 + kk_], scalar2=None, op0=ALU.is_equal), reads=[bq], writes=[bo])
        sel = q_[:, 64:64 + NE]
        S.op("dve", lambda e, sel=sel, o_=o_: e.tensor_reduce(out=sel, in_=o_[:].rearrange("p k e -> p e k"), axis=AX.X, op=ALU.add), reads=[bo], writes=[bq])
        sb_, bsb = selb.next()
        S.op("act", lambda e, sb_=sb_, sel=sel: e.activation(out=sb_[:], in_=sel, func=AF.Copy), reads=[bq], writes=[bsb])
        pl2, bpl2 = pl.next()
        S.op("pe", lambda e, pl2=pl2, sb_=sb_: e.matmul(pl2[:, 0:NE], lhsT=usb[:, :], rhs=sb_[:, :], start=True, stop=True), reads=[b_cst, bsb], writes=[bpl2])
        S.op("pe", lambda e, pl2=pl2, sb_=sb_: e.matmul(pl2[:, 64:64 + NE], lhsT=onb[:, :], rhs=sb_[:, :], start=True, stop=True), reads=[b_cst, bsb], writes=[bpl2])
        pos = q_[:, 96:96 + NE]
        S.op("dve", lambda e, pos=pos, pl2=pl2: e.tensor_tensor(out=pos, in0=pl2[:, 0:NE], in1=base[:], op=ALU.add), reads=[bpl2, b_base], writes=[bq])
        S.op("dve", lambda e, pl2=pl2: e.tensor_tensor(out=base[:], in0=pl2[:, 64:64 + NE], in1=base[:], op=ALU.add), reads=[bpl2, b_base, bq], writes=[b_base])
        p_r, bpr = pr.next()
        bck = lambda ap: ap.unsqueeze(1).to_broadcast([128, 4, NE])
        S.op("pool", lambda e, p_r=p_r, o_=o_: e.tensor_tensor(out=p_r[:], in0=o_[:], in1=bck(iot[:, :]), op=ALU.mult), reads=[bo, b_cst], writes=[bpr])
        S.op("dve", lambda e, q_=q_, p_r=p_r: e.tensor_reduce(out=q_[:, 128:132], in_=p_r[:], axis=AX.X, op=ALU.add), reads=[bpr], writes=[bq])
        p_r2, bpr2 = pr.next()
        S.op("pool", lambda e, p_r2=p_r2, o_=o_, pos=pos: e.tensor_tensor(out=p_r2[:], in0=o_[:], in1=bck(pos), op=ALU.mult), reads=[bo, bq], writes=[bpr2])
        S.op("dve", lambda e, q_=q_, p_r2=p_r2: e.tensor_reduce(out=q_[:, 132:136], in_=p_r2[:], axis=AX.X, op=ALU.add), reads=[bpr2], writes=[bq])
        S.op("dve", lambda e, q_=q_: e.scalar_tensor_tensor(out=q_[:, 136:140], in0=q_[:, 128:132], scalar=float(CAP), in1=q_[:, 132:136], op0=ALU.mult, op1=ALU.add),
             reads=[bq], writes=[bq])
        S.op("dve", lambda e, q_=q_: e.tensor_scalar(out=q_[:, 140:144], in0=q_[:, 132:136], scalar1=float(CAP), scalar2=1.0e7, op0=ALU.is_ge, op1=ALU.mult),
             reads=[bq], writes=[bq])
        S.op("dve", lambda e, q_=q_: e.tensor_tensor(out=q_[:, 144:148], in0=q_[:, 136:140], in1=q_[:, 140:144], op=ALU.add), reads=[bq], writes=[bq])
        S.op("dve", lambda e, q_=q_, ti=ti: e.tensor_copy(out=k.dest_all[:, ti, :], in_=q_[:, 144:148]), reads=[bq], writes=[k.b_dest])
        for kk_ in range(4):
            S.dma("pool", lambda e, h_=h_, ti=ti, kk_=kk_: e.indirect_dma_start(
                out=sc["XE"][:, :], out_offset=bass.IndirectOffsetOnAxis(ap=k.dest_all[:, ti, kk_:kk_ + 1], axis=0),
                in_=h_[:, :], in_offset=None, bounds_check=_bound_reg(e, NE * CAP - 1), oob_is_err=False), reads=[bh, k.b_dest, b_XE])


def phase5(k, w_gu, b_gu, w_down, b_down):
    cfg, S, nc, c, sc = k.cfg, k.S, k.nc, k.c, k.sc
    DM, KC, NE, CAP, FF = cfg.DM, cfg.KC, cfg.NE, cfg.CAP, cfg.FF
    FC = FF // 128
    NS = CAP // 128
    nhalf = 2 if CAP > 512 else 1
    HALF = CAP // nhalf
    GW = min(512, FF)
    DW = min(512, DM)
    KH = max(1, KC // 2)
    sc["YE"] = k.scratch("YE", [NE * CAP, DM], F32)
    k.phase()
    bgT = k.bgT; b_bgT = k.b_bgT
    bgf = k.sb("bgf", [NE, 2 * FF], F32); b_bgf = Buf()
    S.dma("sp", lambda e: e.dma_start(out=bgf[:], in_=b_gu[:, :]), writes=[b_bgf])
    pTf = Ring([k.ps("p5Tf", [128, 4, NE], F32)])
    for g0 in range(0, 2 * FC, 4):
        pt, bpt = pTf.next()
        for j in range(4):
            S.op("pe", lambda e, pt=pt, j=j, g0=g0: e.transpose(out=pt[:, j, :], in_=bgf[:, (g0 + j) * 128:(g0 + j + 1) * 128], identity=c["ident_f"][0:NE, 0:NE]),
                 reads=[b_bgf, c["b_ident"]], writes=[bpt])
        S.op("act", lambda e, pt=pt, g0=g0: e.activation(out=bgT[:, g0:g0 + 4, :], in_=pt[:], func=AF.Copy), reads=[bpt], writes=[b_bgT])
    k.phase()
    xs = Ring([k.sb(f"p5xs{i}", [128, NS, DM], BF16) for i in range(1)])
    XT = Ring([k.sb(f"p5XT{i}", [128, KC, CAP], BF16) for i in range(1)])
    HT = Ring([k.sb(f"p5HT{i}", [128, FC, CAP], BF16) for i in range(1)])
    ws = Ring([k.sb(f"p5ws{i}", [128, KH, GW], F32) for i in range(2)])
    wb = Ring([k.sb(f"p5wb{i}", [128, KC, 2 * GW], BF16) for i in range(2)])
    bd = Ring([k.sb(f"p5bd{i}", [128, DM], F32) for i in range(2)])
    tg = Ring([k.sb(f"p5tg{i}", [128, HALF], F32) for i in range(2)])
    tsg = Ring([k.sb(f"p5ts{i}", [128, HALF], F32) for i in range(2)])
    tu = Ring([k.sb(f"p5tu{i}", [128, HALF], F32) for i in range(2)])
    tgs = Ring([k.sb(f"p5tgs{i}", [128, HALF], F32) for i in range(2)])
    yst = Ring([k.sb(f"p5yst{i}", [128, DW], F32) for i in range(3)])
    pT = Ring([k.ps(f"p5T{i}", [128, 8, 128], BF16) for i in range(1)])
    pg = Ring([k.ps(f"p5g{i}", [128, 512], F32) for i in range(2)])
    pu = Ring([k.ps(f"p5u{i}", [128, 512], F32) for i in range(2)])
    py = Ring([k.ps(f"p5y{i}", [128, 512], F32) for i in range(2)])
    wgv = w_gu.rearrange("e (c p) n -> e p c n", p=128)
    wdv = w_down.rearrange("e (c p) n -> e p c n", p=128)
    cnt = [0]

    def load_piece(wb_, bwb, src_fn, kc0, nk, dcol, ncol):
        w_, bw = ws.next()
        cnt[0] += 1
        S.dma("sp" if cnt[0] % 2 == 0 else "act", lambda e, w_=w_: e.dma_start(out=w_[:, 0:nk, 0:ncol], in_=src_fn()), writes=[bw])
        if cnt[0] % 2 == 0:
            S.op("act", lambda e, w_=w_, wb_=wb_: e.activation(out=wb_[:, kc0:kc0 + nk, dcol:dcol + ncol], in_=w_[:, 0:nk, 0:ncol], func=AF.Copy),
                 reads=[bw], writes=[bwb])
        else:
            S.op("pool", lambda e, w_=w_, wb_=wb_: e.tensor_copy(out=wb_[:, kc0:kc0 + nk, dcol:dcol + ncol], in_=w_[:, 0:nk, 0:ncol]),
                 reads=[bw], writes=[bwb])

    for ex in range(NE):
        x_, bx = xs.next()
        S.dma("sp", lambda e, x_=x_, ex=ex: e.dma_start(out=x_[:], in_=sc["XE"][ex * CAP:(ex + 1) * CAP, :].rearrange("(s p) d -> p s d", p=128)), writes=[bx])
        xt, bxt = XT.next()
        for s in range(NS):
            for g0 in range(0, KC, 8):
                ng = min(8, KC - g0)
                pt, bpt = pT.next()
                for j in range(ng):
                    S.op("pe", lambda e, pt=pt, x_=x_, s=s, j=j, g0=g0: e.transpose(out=pt[:, j, :], in_=x_[:, s, (g0 + j) * 128:(g0 + j + 1) * 128], identity=c["ident_b"][:]),
                         reads=[bx, c["b_ident"]], writes=[bpt])
                S.op("act", lambda e, xt=xt, pt=pt, s=s, g0=g0, ng=ng: e.activation(out=xt[:, g0:g0 + ng, s * 128:(s + 1) * 128], in_=pt[:, 0:ng, :], func=AF.Copy),
                     reads=[bpt], writes=[bxt])
        b_, bb = bd.next()
        S.dma("act", lambda e, b_=b_, ex=ex: e.dma_start(out=b_[:], in_=b_down[ex:ex + 1, :].partition_broadcast(128)), writes=[bb])
        ht, bht = HT.next()
        for gw in range(FF // GW):
            wb_, bwb = wb.next()
            for part in range(2):
                for kc0 in range(0, KC, KH):
                    c0 = part * FF + gw * GW
                    load_piece(wb_, bwb, lambda ex=ex, kc0=kc0, c0=c0: wgv[ex, :, kc0:kc0 + KH, c0:c0 + GW], kc0, KH, part * GW, GW)
            for fl in range(GW // 128):
                fb = gw * (GW // 128) + fl
                for hf in range(nhalf):
                    sl = slice(hf * HALF, (hf + 1) * HALF)
                    pg_, bpg = pg.next(); pu_, bpu = pu.next()
                    for kc in range(KC):
                        S.op("pe", lambda e, pg_=pg_, wb_=wb_, xt=xt, kc=kc, sl=sl, fl=fl: e.matmul(pg_[:, 0:HALF], lhsT=wb_[:, kc, fl * 128:(fl + 1) * 128], rhs=xt[:, kc, sl],
                                                                                                   start=(kc == 0), stop=(kc == KC - 1)), reads=[bwb, bxt], writes=[bpg])
                    for kc in range(KC):
                        S.op("pe", lambda e, pu_=pu_, wb_=wb_, xt=xt, kc=kc, sl=sl, fl=fl: e.matmul(pu_[:, 0:HALF], lhsT=wb_[:, kc, GW + fl * 128:GW + (fl + 1) * 128], rhs=xt[:, kc, sl],
                                                                                                   start=(kc == 0), stop=(kc == KC - 1)), reads=[bwb, bxt], writes=[bpu])
                    g_, bg = tg.next(); s_, bs = tsg.next(); u_, bu = tu.next(); gs_, bgs = tgs.next()
                    S.op("dve", lambda e, g_=g_, pg_=pg_, fb=fb, ex=ex: e.tensor_scalar(out=g_[:], in0=pg_[:, 0:HALF], scalar1=bgT[:, fb, ex:ex + 1], scalar2=7.0,
                                                                                       op0=ALU.add, op1=ALU.min), reads=[bpg, b_bgT], writes=[bg])
                    S.op("act", lambda e, s_=s_, g_=g_: e.activation(out=s_[:], in_=g_[:], func=AF.Sigmoid, scale=1.702), reads=[bg], writes=[bs])
                    S.op("dve", lambda e, u_=u_, pu_=pu_, fb=fb, ex=ex: e.tensor_scalar(out=u_[:], in0=pu_[:, 0:HALF], scalar1=bgT[:, FC + fb, ex:ex + 1], scalar2=7.0,
                                                                                       op0=ALU.add, op1=ALU.min), reads=[bpu, b_bgT], writes=[bu])
                    S.op("dve", lambda e, u_=u_: e.tensor_scalar(out=u_[:], in0=u_[:], scalar1=-7.0, scalar2=1.0, op0=ALU.max, op1=ALU.add), reads=[bu], writes=[bu])
                    S.op("pool", lambda e, gs_=gs_, g_=g_, s_=s_: e.tensor_tensor(out=gs_[:], in0=g_[:], in1=s_[:], op=ALU.mult), reads=[bg, bs], writes=[bgs])
                    S.op("pool", lambda e, ht=ht, gs_=gs_, u_=u_, fb=fb, sl=sl: e.tensor_tensor(out=ht[:, fb, sl], in0=gs_[:], in1=u_[:], op=ALU.mult),
                         reads=[bgs, bu], writes=[bht])
        nblk = 2 * GW // DW
        for cb0 in range(0, DM // DW, nblk):
            wb_, bwb = wb.next()
            nb = min(nblk, DM // DW - cb0)
            for bi in range(nb):
                for kc0 in range(0, FC, KH):
                    c0 = (cb0 + bi) * DW
                    load_piece(wb_, bwb, lambda ex=ex, kc0=kc0, c0=c0: wdv[ex, :, kc0:kc0 + KH, c0:c0 + DW], kc0, KH, bi * DW, DW)
            for bi in range(nb):
                cb = cb0 + bi
                for s in range(NS):
                    py_, bpy = py.next()
                    for fc in range(FC):
                        S.op("pe", lambda e, py_=py_, ht=ht, wb_=wb_, fc=fc, s=s, bi=bi: e.matmul(py_[:, 0:DW], lhsT=ht[:, fc, s * 128:(s + 1) * 128], rhs=wb_[:, fc, bi * DW:(bi + 1) * DW],
                                                                                                 start=(fc == 0), stop=(fc == FC - 1)), reads=[bht, bwb], writes=[bpy])
                    y_, by = yst.next()
                    S.op("dve", lambda e, y_=y_, py_=py_, b_=b_, cb=cb: e.tensor_tensor(out=y_[:], in0=py_[:, 0:DW], in1=b_[:, cb * DW:(cb + 1) * DW], op=ALU.add),
                         reads=[bpy, bb], writes=[by])
                    r0 = ex * CAP + s * 128
                    S.dma("pool" if s % 2 == 0 else "sp", lambda e, y_=y_, r0=r0, cb=cb: e.dma_start(out=sc["YE"][r0:r0 + 128, cb * DW:(cb + 1) * DW], in_=y_[:]), reads=[by])


def phase6(k, out, prm):
    cfg, S, nc, c, sc = k.cfg, k.S, k.nc, k.c, k.sc
    DM, NOWN, NE, CAP = cfg.DM, cfg.NOWN, cfg.NE, cfg.CAP
    NT = NOWN // 128
    k.phase()
    lw = k.sb("ln2w", [128, DM], F32); lb = k.sb("ln2b", [128, DM], F32); b_l = Buf()
    S.dma("sp", lambda e: e.dma_start(out=lw[:], in_=prm["ln2_w"][0:1, :].partition_broadcast(128)), writes=[b_l])
    S.dma("sp", lambda e: e.dma_start(out=lb[:], in_=prm["ln2_b"][0:1, :].partition_broadcast(128)), writes=[b_l])
    hh = Ring([k.sb(f"p6h{i}", [128, DM], F32) for i in range(2)])
    yk = Ring([k.sb(f"p6y{i}", [128, DM], F32) for i in range(4)])
    sm = Ring([k.sb(f"p6sm{i}", [128, 16], F32) for i in range(2)])
    bst = Ring([k.sb(f"p6bst{i}", [128, 4, 6], F32) for i in range(2)])
    ncg = DM // 512 if DM >= 512 else 1
    cw = DM // ncg
    for ti in range(NT):
        tok0 = ti * 128
        h_, bh = hh.next()
        S.dma("sp", lambda e, h_=h_, tok0=tok0: e.dma_start(out=h_[:], in_=sc["H"][tok0:tok0 + 128, :]), writes=[bh])
        S.op("dve", lambda e, h_=h_: e.tensor_scalar(out=h_[:], in0=h_[:], scalar1=float(cfg.alpha), scalar2=None, op0=ALU.mult), reads=[bh], writes=[bh])
        for kk_ in range(4):
            y_, by = yk.next()
            S.op("pool", lambda e, y_=y_: e.memset(y_[:], 0.0), writes=[by])
            S.dma("pool", lambda e, y_=y_, ti=ti, kk_=kk_: e.indirect_dma_start(
                out=y_[:, :], out_offset=None, in_=sc["YE"][:, :], in_offset=bass.IndirectOffsetOnAxis(ap=k.dest_all[:, ti, kk_:kk_ + 1], axis=0),
                bounds_check=_bound_reg(e, NE * CAP - 1), oob_is_err=False), reads=[k.b_dest], writes=[by])
            S.op("dve", lambda e, h_=h_, y_=y_, ti=ti, kk_=kk_: e.scalar_tensor_tensor(out=h_[:], in0=y_[:], scalar=k.gate_all[:, ti, kk_:kk_ + 1], in1=h_[:],
                                                                                     op0=ALU.mult, op1=ALU.add), reads=[by, bh, k.b_gate], writes=[bh])
        s_, bs = bst.next(); q_, bq = sm.next()
        for cg in range(ncg):
            S.op("dve", lambda e, s_=s_, h_=h_, cg=cg: e.bn_stats(out=s_[:, cg, :], in_=h_[:, cg * cw:(cg + 1) * cw]), reads=[bh], writes=[bs])
        S.op("dve", lambda e, q_=q_, s_=s_: e.bn_aggr(out=q_[:, 0:2], in_=s_[:, 0:ncg, :].rearrange("p a b -> p (a b)")), reads=[bs], writes=[bq])
        S.op("dve", lambda e, q_=q_: e.tensor_scalar(out=q_[:, 2:3], in0=q_[:, 1:2], scalar1=1e-5, scalar2=None, op0=ALU.add), reads=[bq], writes=[bq])
        S.op("act", lambda e, q_=q_: e.activation(out=q_[:, 3:4], in_=q_[:, 2:3], func=AF.Sqrt), reads=[bq], writes=[bq])
        S.op("dve", lambda e, q_=q_: e.reciprocal(out=q_[:, 4:5], in_=q_[:, 3:4]), reads=[bq], writes=[bq])
        S.op("dve", lambda e, h_=h_, q_=q_: e.tensor_scalar(out=h_[:], in0=h_[:], scalar1=q_[:, 0:1], scalar2=q_[:, 4:5], op0=ALU.subtract, op1=ALU.mult),
             reads=[bh, bq], writes=[bh])
        S.op("pool", lambda e, h_=h_: e.tensor_tensor(out=h_[:], in0=h_[:], in1=lw[:], op=ALU.mult), reads=[bh, b_l], writes=[bh])
        S.op("dve", lambda e, h_=h_: e.tensor_tensor(out=h_[:], in0=h_[:], in1=lb[:], op=ALU.add), reads=[bh, b_l], writes=[bh])
        S.dma("sp", lambda e, h_=h_, tok0=tok0: e.dma_start(out=out[tok0:tok0 + 128, :], in_=h_[:]), reads=[bh])


HORD = [0, 2, 4, 6, 1, 3, 5, 7, 8, 10, 12, 14, 9, 11, 13, 15]


def build_full(cfg, dbg=()):
    k = K(cfg, dbg=dbg)
    DM, NE = cfg.DM, cfg.NE
    xe = k.inp("xe", [cfg.NTOK, DM])
    w_in = k.inp("w_in", [DM, cfg.DIN])
    mu_cols = k.inp("mu_cols", [128, 28])
    att_bias = k.inp("att_bias", [128, 5, 16, 128]); halo_mask = k.inp("halo_mask", [128, 1])
    prm = {n: k.inp(n, s) for n, s in [
        ("w_up", [96, 1024]), ("a_up", [96, 1024]), ("g_up", [256, 1024]), ("hcols", [64, 6, 16]), ("rmask", [64, 512]),
        ("m_su", [64, 8, 64]), ("m_ui", [64, 8, 64]), ("m_sl", [64, 8, 64]), ("i8", [64, 8, 64]),
        ("lnx_w", [1, 1024]), ("lnx_b", [1, 1024]), ("ln1_w", [1, DM]), ("ln1_b", [1, DM]), ("ln2_w", [1, DM]), ("ln2_b", [1, DM]),
        ("w_router", [DM, NE]), ("b_router", [1, NE]), ("iota32", [128, NE]), ("ustrict", [128, 128])]}
    proj_a = k.inp("proj_a", [1024, DM]); proj_b = k.inp("proj_b", [1024, DM]); w_out = k.inp("w_out", [DM, DM])
    w_gu = k.inp("w_gu", [NE, DM, 2 * cfg.FF]); b_gu = k.inp("b_gu", [NE, 2 * cfg.FF])
    w_down = k.inp("w_down", [NE, cfg.FF, DM]); b_down = k.inp("b_down", [NE, DM])
    out = k.nc.dram_tensor("out", [cfg.NOWN, DM], F32, kind="ExternalOutput").ap()
    load_consts(k)
    phase1(k, xe, w_in, mu_cols)
    phase2(k, att_bias, halo_mask)
    phase3(k, prm)
    phase3b(k, prm)
    phase4a(k, proj_a, proj_b)
    phase4b(k, xe, w_out, prm)
    phase5(k, w_gu, b_gu, w_down, b_down)
    phase6(k, out, prm)
    k.S.finish()
    return k


def host_common(inp, cfg):
    f = lambda a: np.ascontiguousarray(np.asarray(a, dtype=np.float32))
    smu = f(inp["shift_mu"][0])
    m = np.zeros((128, 28), np.float32)
    for ci in range(24):
        m[:, ci] = smu[ci * 128:(ci + 1) * 128]
    m[:96, 24] = smu[3072:3168]; m[:96, 25] = smu[3168:3264]
    m[:, 26] = smu[3264:3392]; m[:, 27] = smu[3392:3520]
    relb = f(inp["rel_bias"][0])
    kr = np.arange(640)[:, None]; q = np.arange(128)[None, :]
    dist = 512 + q - kr
    qc = (512 + q) // 64; kc = kr // 64
    valid = (qc - kc >= 0) & (qc - kc <= 8)
    idx = np.clip(np.minimum(dist, 256) + 63, 0, 319)
    b = relb[:, idx]
    b = np.where(valid[None], b, np.float32(-30000.0)).astype(np.float32)[HORD]
    att_bias = np.ascontiguousarray(b.reshape(16, 5, 128, 128).transpose(2, 1, 0, 3))
    hv = lambda v: f(v).reshape(16, 64).T
    k_a = f(inp["k_a"][0])
    one_minus_ka = np.zeros_like(k_a)
    hcols = np.ascontiguousarray(np.stack([hv(inp["w0"][0]), hv(inp["a0"][0]), hv(inp["k_k"][0]), hv(k_a), hv(one_minus_ka), hv(inp["r_k"][0])], 1))
    rmask = np.ones((64, 512), np.float32); rmask[:, ::64] = 0
    j = np.arange(64)[:, None]; t = np.arange(64)[None, :]
    rep = lambda mm: np.ascontiguousarray(np.repeat(mm.astype(np.float32)[:, None, :], 8, 1))
    r2 = lambda v: f(v).reshape(1, -1)
    com = {
        "w_in": f(inp["w_in"][0]), "mu_cols": m, "att_bias": att_bias, "c_ident": np.eye(128, dtype=np.float32),
        "w_up": f(inp["w_up"][0]), "a_up": f(inp["a_up"][0]), "g_up": f(inp["g_up"][0]), "hcols": hcols, "rmask": rmask,
        "m_su": rep(j < t), "m_ui": rep(j <= t), "m_sl": rep(j > t), "i8": rep(j == t),
        "lnx_w": r2(inp["lnx_w"][0]), "lnx_b": r2(inp["lnx_b"][0]), "ln1_w": r2(inp["ln1_w"][0]), "ln1_b": r2(inp["ln1_b"][0]),
        "ln2_w": r2(inp["ln2_w"][0]), "ln2_b": r2(inp["ln2_b"][0]),
        "w_router": f(inp["w_router"][0]), "b_router": r2(inp["b_router"][0]),
        "iota32": np.ascontiguousarray(np.broadcast_to(np.arange(cfg.NE, dtype=np.float32), (128, cfg.NE))),
        "ustrict": (np.arange(128)[:, None] < np.arange(128)[None, :]).astype(np.float32),
        "proj_a": f(inp["proj_a"][0]), "proj_b": f(inp["proj_b"][0]), "w_out": f(inp["w_out"][0]),
        "w_gu": f(inp["w_gu"][0]), "b_gu": f(inp["b_gu"][0]), "w_down": f(inp["w_down"][0]), "b_down": f(inp["b_down"][0]),
    }
    return com


def host_core(x, cfg, b, half):
    NOWN = cfg.NOWN
    xe = np.zeros((cfg.NTOK, cfg.DM), np.float32)
    if half == 0:
        xe[cfg.NPRE:] = x[b, 0:NOWN]
        hm = np.full((128, 1), -30000.0, np.float32)
    else:
        xe[:] = x[b, 0:2 * NOWN]
        hm = np.zeros((128, 1), np.float32)
    return {"xe": xe, "halo_mask": hm}


_CACHE = {}


def kernel(**inputs):
    cfg = Cfg()
    if "k" not in _CACHE:
        _CACHE["k"] = build_full(cfg)
    k = _CACHE["k"]
    com = host_common(inputs, cfg)
    x = np.asarray(inputs["x"], dtype=np.float32)
    in_maps = []
    for core in range(8):
        m = dict(com)
        m.update(host_core(x, cfg, core // 2, core % 2))
        in_maps.append(m)
    res = run_bass_kernel_spmd(k.nc, in_maps, core_ids=list(range(8)))
    out = np.empty((4, 2 * cfg.NOWN, cfg.DM), np.float32)
    for core in range(8):
        h = core % 2
        out[core // 2, h * cfg.NOWN:(h + 1) * cfg.NOWN] = np.asarray(res.results[core]["out"])
    return out
```

```python
import contextlib
import os
import numpy as np
import concourse.bass as bass
import concourse.mybir as mybir
from concourse.bass_utils import run_bass_kernel_spmd


class Buf:
    __slots__ = ("name", "w", "r")

    def __init__(self, name=""):
        self.name = name
        self.w = None
        self.r = []


class Sched:
    ENGS = ("pe", "act", "dve", "pool", "sp")

    def __init__(self, nc, ndma_sems=10):
        self.nc = nc
        self.prog = {e: [] for e in self.ENGS}
        self.sems = {}
        self.cnt = {}
        self.known = {e: {} for e in self.ENGS}
        self._stack = []
        for e in ("pe", "act", "dve", "pool"):
            self._mksem("E_" + e)
        self.dpool = {}
        for q in ("sp", "pool", "act"):
            names = [f"D_{q}{i}" for i in range(ndma_sems)]
            for n in names:
                self._mksem(n)
            self.dpool[q] = [names, 0]
        self.ninstr = 0

    def _mksem(self, name):
        cm = self.nc.semaphore(name)
        s = cm.__enter__()
        self._stack.append(cm)
        self.sems[name] = s
        self.cnt[name] = 0

    def _wait(self, eng, ev):
        key, val = ev
        if eng == "pe" and key == "E_pe":
            return
        if self.known[eng].get(key, 0) >= val:
            return
        self.known[eng][key] = val
        sem = self.sems[key]
        self.prog[eng].append(lambda e, sem=sem, val=val: e.wait_ge(sem, val))

    def _deps(self, eng, reads, writes):
        for b in reads:
            if b.w is not None:
                self._wait(eng, b.w)
        for b in writes:
            if b.w is not None:
                self._wait(eng, b.w)
            for ev in b.r:
                self._wait(eng, ev)

    def _commit(self, ev, reads, writes):
        for b in writes:
            b.w = ev
            b.r = []
        for b in reads:
            if b.w is ev:
                continue
            b.r.append(ev)
            if len(b.r) > 24:
                last = {}
                for k, v in b.r:
                    if last.get(k, 0) < v:
                        last[k] = v
                b.r = list(last.items())

    def op(self, eng, fn, reads=(), writes=()):
        self._deps(eng, reads, writes)
        key = "E_" + eng
        self.cnt[key] += 1
        val = self.cnt[key]
        sem = self.sems[key]
        self.prog[eng].append(lambda e, fn=fn, sem=sem: fn(e).then_inc(sem, 1))
        self._commit((key, val), reads, writes)
        self.ninstr += 1

    def dma(self, q, fn, reads=(), writes=()):
        names, idx = self.dpool[q]
        name = names[idx % len(names)]
        self.dpool[q][1] = idx + 1
        if self.cnt[name] > 0:
            self._wait(q, (name, self.cnt[name]))
        self._deps(q, reads, writes)
        self.cnt[name] += 16
        val = self.cnt[name]
        sem = self.sems[name]
        self.prog[q].append(lambda e, fn=fn, sem=sem: fn(e).then_inc(sem, 16))
        self._commit((name, val), reads, writes)
        self.ninstr += 1

    def barrier(self):
        for eng in self.ENGS:
            for key, val in self.cnt.items():
                if val > 0:
                    if eng == "pe" and key == "E_pe":
                        continue
                    self._wait(eng, (key, val))

    def finish(self):
        self.barrier()
        nc = self.nc
        with nc.Block() as block:
            def mk(name):
                def f(e):
                    for t in self.prog[name]:
                        t(e)
                return f
            block.tensor(mk("pe"))
            block.scalar(mk("act"))
            block.vector(mk("dve"))
            block.gpsimd(mk("pool"))
            block.sync(mk("sp"))
        for cm in reversed(self._stack):
            cm.__exit__(None, None, None)


F32 = mybir.dt.float32
BF16 = mybir.dt.bfloat16
I32 = mybir.dt.int32
ALU = mybir.AluOpType
AF = mybir.ActivationFunctionType
AX = mybir.AxisListType

AW = 1024
NH = 16
HD = 64
SHIFTW = 3 * AW + 96 + 96 + 256
C0 = float(np.exp(-0.5))


class Cfg:
    def __init__(self, DM=2048, NPRE=4096, NOWN=4096, ST=2048, NE=32, CAP=640, depth_alpha=2 ** 0.25):
        self.DM = DM; self.NPRE = NPRE; self.NOWN = NOWN; self.ST = ST
        self.NE = NE; self.CAP = CAP; self.FF = DM
        self.NTOK = NPRE + NOWN
        self.KC = DM // 128
        self.DIN = 3 * AW + SHIFTW + 2 * DM
        self.alpha = depth_alpha


class K:
    def __init__(self, cfg, dbg=()):
        self.cfg = cfg
        self.nc = bass.Bass("TRN2", target_bir_lowering=False)
        self.S = Sched(self.nc)
        self.dbg = set(dbg)
        self.stack = contextlib.ExitStack()
        self.pstack = None
        self.ins = {}

    def inp(self, name, shape, dt=F32):
        t = self.nc.dram_tensor(name, list(shape), dt, kind="ExternalInput").ap()
        self.ins[name] = t
        return t

    def scratch(self, name, shape, dt):
        kind = "ExternalOutput" if name in self.dbg else "Internal"
        return self.nc.dram_tensor(name, list(shape), dt, kind=kind).ap()

    def phase(self):
        if self.pstack is not None:
            self.S.barrier()
            self.pstack.close()
        self.pstack = contextlib.ExitStack()

    def sb(self, name, shape, dt):
        return self.pstack.enter_context(self.nc.sbuf_tensor(name, list(shape), dt))

    def ps(self, name, shape, dt=F32):
        return self.pstack.enter_context(self.nc.psum_tensor(name, list(shape), dt))

    def gsb(self, name, shape, dt):
        return self.stack.enter_context(self.nc.sbuf_tensor(name, list(shape), dt))


_REGS = {}


def _bound_reg(e, val):
    key = (id(e), val)
    if key not in _REGS:
        _REGS[key] = e.to_reg(val)
    return _REGS[key]


class Ring:
    def __init__(self, tiles):
        self.t = tiles
        self.b = [Buf() for _ in tiles]
        self.i = 0

    def next(self):
        j = self.i % len(self.t)
        self.i += 1
        return self.t[j], self.b[j]


def load_consts(k):
    S = k.S
    c = {}
    ident = k.inp("c_ident", [128, 128])
    c["ident_f"] = k.gsb("ident_f", [128, 128], F32)
    c["ident_b"] = k.gsb("ident_b", [128, 128], BF16)
    c["b_ident"] = Buf()
    S.dma("sp", lambda e: e.dma_start(out=c["ident_f"][:], in_=ident[:, :]), writes=[c["b_ident"]])
    S.op("act", lambda e: e.activation(out=c["ident_b"][:], in_=c["ident_f"][:], func=AF.Copy),
         reads=[c["b_ident"]], writes=[c["b_ident"]])
    k.c = c
    NT = k.cfg.NOWN // 128
    k.dest_all = k.gsb("dest_all", [128, NT, 4], I32); k.b_dest = Buf()
    k.gate_all = k.gsb("gate_all", [128, NT, 4], F32); k.b_gate = Buf()
    k.bgT = k.gsb("bgT", [128, 2 * (k.cfg.FF // 128), k.cfg.NE], F32); k.b_bgT = Buf()


def phase1(k, xe, w_in, mu_cols):
    cfg, S, nc, c = k.cfg, k.S, k.nc, k.c
    DM, KC, ST, NTOK, NPRE, NOWN = cfg.DM, cfg.KC, cfg.ST, cfg.NTOK, cfg.NPRE, cfg.NOWN
    o1, o2 = 3 * AW, 3 * AW + SHIFTW
    HALO = 512
    sc = {}
    sc["QT"] = k.scratch("QT", [AW, NOWN], BF16)
    sc["KT"] = k.scratch("KT", [AW, HALO + NOWN], BF16)
    sc["V"] = k.scratch("V", [HALO + NOWN, NH, 65], BF16)
    sc["RB"] = k.scratch("RB", [AW, NTOK], BF16)
    sc["KB"] = k.scratch("KB", [AW, NTOK], BF16)
    sc["VB"] = k.scratch("VB", [AW, NTOK], BF16)
    sc["WD"] = k.scratch("WD", [96, NTOK], BF16)
    sc["AD"] = k.scratch("AD", [96, NTOK], BF16)
    sc["GD"] = k.scratch("GD", [256, NTOK], BF16)
    sc["GA"] = k.scratch("GA", [DM, NOWN], BF16)
    sc["GB"] = k.scratch("GB", [DM, NOWN], BF16)
    k.sc = sc

    k.phase()
    xT = k.sb("xT", [128, KC, ST], BF16); b_xT = Buf()
    xin = Ring([k.sb(f"xin{i}", [128, DM], F32) for i in range(2)])
    xbf = Ring([k.sb(f"xbf{i}", [128, DM], BF16) for i in range(2)])
    wst = Ring([k.sb(f"wst{i}", [128, KC, 256], F32) for i in range(2)])
    wbf = Ring([k.sb(f"wbf{i}", [128, KC, 256], BF16) for i in range(2)])
    lbuf = [k.sb(f"lbuf{i}", [128, 516], F32) for i in range(2)]
    b_lbuf = [Buf(), Buf()]
    ltmp = Ring([k.sb(f"ltmp{i}", [128, 512], F32) for i in range(2)])
    lseg = Ring([k.sb(f"lseg{i}", [128, 512], F32) for i in range(2)])
    ost = Ring([k.sb(f"ost{i}", [128, 512], BF16) for i in range(4)])
    vst = Ring([k.sb(f"vst{i}", [128, 4, 65], BF16) for i in range(3)])
    carry = k.sb("carry", [128, 32], F32); b_carry = Buf()
    mu = k.sb("mu", [128, 32], F32); b_mu = Buf()
    pst = Ring([k.ps(f"pT{i}", [128, 8, 128], BF16) for i in range(2)])
    psg = Ring([k.ps(f"pg{i}", [128, 512], F32) for i in range(4)])

    S.dma("sp", lambda e: e.dma_start(out=mu[:, 0:28], in_=mu_cols[:, :]), writes=[b_mu])
    S.op("pool", lambda e: e.memset(carry[:], 0.0), writes=[b_carry])
    for t, b in zip(vst.t, vst.b):
        S.op("pool", lambda e, t=t: e.memset(t[:], 1.0), writes=[b])

    blocks = []
    for c0 in range(0, AW, 256):
        blocks.append((c0, 256, "q", sc["QT"], c0, "own", None, None))
    for c0 in range(0, AW, 256):
        blocks.append((AW + c0, 256, "k", sc["KT"], c0, "halo", None, None))
    for c0 in range(0, AW, 256):
        blocks.append((2 * AW + c0, 256, "v", sc["V"], c0 // 64, "halo", None, None))
    for j, nm in enumerate(("RB", "KB", "VB")):
        for c0 in range(0, AW, 256):
            blocks.append((o1 + j * AW + c0, 256, "rw", sc[nm], c0, "all", None, (j * AW + c0) // 128))
    blocks.append((o1 + 3 * AW, 96, "rw", sc["WD"], 0, "all", AF.Tanh, 24))
    blocks.append((o1 + 3 * AW + 96, 96, "rw", sc["AD"], 0, "all", AF.Copy, 25))
    blocks.append((o1 + 3 * AW + 192, 256, "rw", sc["GD"], 0, "all", AF.Sigmoid, 26))
    for c0 in range(0, DM, 256):
        blocks.append((o2 + c0, 256, "gate", sc["GA"], c0, "own", None, None))
    for c0 in range(0, DM, 256):
        blocks.append((o2 + DM + c0, 256, "gate", sc["GB"], c0, "own", None, None))

    w_v = w_in.rearrange("(kc p) c -> p kc c", p=128)
    nST = NTOK // ST
    active = []
    for s in range(nST):
        for bi, blk in enumerate(blocks):
            tts = []
            for tt in range(ST // 512):
                t_ext = s * ST + tt * 512
                if blk[5] == "all" or (blk[5] == "own" and t_ext >= NPRE) or (blk[5] == "halo" and t_ext >= NPRE - HALO):
                    tts.append(tt)
            if tts:
                active.append((s, bi, tts))
    wloaded = {}

    def wload(ai):
        c0, ncols = blocks[active[ai][1]][0:2]
        ws, bws = wst.next()
        wb, bwb = wbf.next()
        S.dma("sp", lambda e: e.dma_start(out=ws[:, :, 0:ncols], in_=w_v[:, :, c0:c0 + ncols]), writes=[bws])
        S.op("pool", lambda e: e.tensor_copy(out=wb[:, :, 0:ncols], in_=ws[:, :, 0:ncols]), reads=[bws], writes=[bwb])
        wloaded[ai] = (wb, bwb)

    wload(0)
    for s in range(nST):
        tok0 = s * ST
        for i in range(ST // 128):
            xi, bxi = xin.next()
            xb, bxb = xbf.next()
            r0 = tok0 + i * 128
            S.dma("sp", lambda e, xi=xi, r0=r0: e.dma_start(out=xi[:], in_=xe[r0:r0 + 128, :]), writes=[bxi])
            S.op("act", lambda e, xi=xi, xb=xb: e.activation(out=xb[:], in_=xi[:], func=AF.Copy),
                 reads=[bxi], writes=[bxb])
            for g0 in range(0, KC, 8):
                ng = min(8, KC - g0)
                pt, bpt = pst.next()
                for j in range(ng):
                    S.op("pe", lambda e, pt=pt, xb=xb, j=j, g0=g0: e.transpose(
                        out=pt[:, j, :], in_=xb[:, (g0 + j) * 128:(g0 + j + 1) * 128], identity=c["ident_b"][:]),
                        reads=[bxb, c["b_ident"]], writes=[bpt])
                S.op("dve", lambda e, pt=pt, g0=g0, ng=ng, i=i: e.tensor_copy(
                    out=xT[:, g0:g0 + ng, i * 128:(i + 1) * 128], in_=pt[:, 0:ng, :]),
                    reads=[bpt], writes=[b_xT])
        own_s = tok0 >= NPRE
        for ai in [a for a in range(len(active)) if active[a][0] == s]:
            _, bi, tts = active[ai]
            c0, ncols, kind, dst, drow0, tokmode, post, rwc = blocks[bi]
            wb, bwb = wloaded.pop(ai)
            if ai + 1 < len(active):
                wload(ai + 1)
            if kind == "v":
                for tt in tts:
                    for sub in range(4):
                        tk = tt * 512 + sub * 128
                        pg, bpg = psg.next()
                        for kc in range(KC):
                            S.op("pe", lambda e, pg=pg, kc=kc, tk=tk, wb=wb: e.matmul(
                                pg[:, 0:256], lhsT=xT[:, kc, tk:tk + 128], rhs=wb[:, kc, 0:256],
                                start=(kc == 0), stop=(kc == KC - 1)), reads=[b_xT, bwb], writes=[bpg])
                        vs, bvs = vst.next()
                        S.op("act", lambda e, vs=vs, pg=pg: e.activation(
                            out=vs[:, :, 0:64], in_=pg[:, 0:256].rearrange("p (h d) -> p h d", d=64), func=AF.Copy),
                            reads=[bpg], writes=[bvs])
                        vrow = tok0 + tk - (NPRE - HALO)
                        S.dma("sp", lambda e, vs=vs, vrow=vrow, drow0=drow0, dst=dst: e.dma_start(
                            out=dst[vrow:vrow + 128, drow0:drow0 + 4, :], in_=vs[:]), reads=[bvs])
                continue
            nch = (ncols + 127) // 128
            for ch in range(nch):
                m = min(128, ncols - ch * 128)
                j0 = ch * 128
                if kind == "rw":
                    rci = rwc + ch
                    p = 0
                    S.op("act", lambda e, p=p, rci=rci, m=m: e.activation(
                        out=lbuf[p][0:m, 0:1], in_=carry[0:m, rci:rci + 1], func=AF.Copy),
                        reads=[b_carry], writes=[b_lbuf[p]])
                for tt in tts:
                    tk = tt * 512
                    pg, bpg = psg.next()
                    for kc in range(KC):
                        S.op("pe", lambda e, pg=pg, kc=kc, tk=tk, wb=wb, j0=j0, m=m: e.matmul(
                            pg[0:m, :], lhsT=wb[:, kc, j0:j0 + m], rhs=xT[:, kc, tk:tk + 512],
                            start=(kc == 0), stop=(kc == KC - 1)), reads=[b_xT, bwb], writes=[bpg])
                    o, bo = ost.next()
                    if kind == "q":
                        S.op("act", lambda e, o=o, pg=pg, m=m: e.activation(
                            out=o[0:m, :], in_=pg[0:m, :], func=AF.Copy, scale=0.125), reads=[bpg], writes=[bo])
                        dcol = tok0 + tk - NPRE
                    elif kind == "k":
                        S.op("act", lambda e, o=o, pg=pg, m=m: e.activation(
                            out=o[0:m, :], in_=pg[0:m, :], func=AF.Copy), reads=[bpg], writes=[bo])
                        dcol = tok0 + tk - (NPRE - HALO)
                    elif kind == "gate":
                        S.op("act", lambda e, o=o, pg=pg, m=m: e.activation(
                            out=o[0:m, :], in_=pg[0:m, :], func=AF.Sigmoid), reads=[bpg], writes=[bo])
                        dcol = tok0 + tk - NPRE
                    else:
                        lb, blb = lbuf[p], b_lbuf[p]
                        S.op("act", lambda e, lb=lb, pg=pg, m=m: e.activation(
                            out=lb[0:m, 1:513], in_=pg[0:m, :], func=AF.Copy), reads=[bpg], writes=[blb])
                        S.op("act", lambda e, lb=lb, p=p, m=m: e.activation(
                            out=lbuf[1 - p][0:m, 0:1], in_=lb[0:m, 512:513], func=AF.Copy),
                            reads=[blb], writes=[b_lbuf[1 - p]])
                        lt, blt = ltmp.next()
                        S.op("dve", lambda e, lt=lt, lb=lb, m=m: e.tensor_tensor(
                            out=lt[0:m, :], in0=lb[0:m, 0:512], in1=lb[0:m, 1:513], op=ALU.subtract),
                            reads=[blb], writes=[blt])
                        if post is None:
                            S.op("dve", lambda e, o=o, lt=lt, lb=lb, m=m, rci=rci: e.scalar_tensor_tensor(
                                out=o[0:m, :], in0=lt[0:m, :], scalar=mu[0:m, rci:rci + 1], in1=lb[0:m, 1:513],
                                op0=ALU.mult, op1=ALU.add), reads=[blt, blb, b_mu], writes=[bo])
                        else:
                            ls, bls = lseg.next()
                            S.op("dve", lambda e, ls=ls, lt=lt, lb=lb, m=m, rci=rci: e.scalar_tensor_tensor(
                                out=ls[0:m, :], in0=lt[0:m, :], scalar=mu[0:m, rci:rci + 1], in1=lb[0:m, 1:513],
                                op0=ALU.mult, op1=ALU.add), reads=[blt, blb, b_mu], writes=[bls])
                            S.op("act", lambda e, o=o, ls=ls, m=m, post=post: e.activation(
                                out=o[0:m, :], in_=ls[0:m, :], func=post), reads=[bls], writes=[bo])
                        p = 1 - p
                        dcol = tok0 + tk
                    r0 = drow0 + j0
                    S.dma("sp" if (tt % 2 == 0) else "pool", lambda e, o=o, r0=r0, m=m, dcol=dcol, dst=dst: e.dma_start(
                        out=dst[r0:r0 + m, dcol:dcol + 512], in_=o[0:m, :]), reads=[bo])
                if kind == "rw":
                    S.op("act", lambda e, p=p, rci=rci, m=m: e.activation(
                        out=carry[0:m, rci:rci + 1], in_=lbuf[p][0:m, 0:1], func=AF.Copy),
                        reads=[b_lbuf[p]], writes=[b_carry])


def phase2(k, att_bias, halo_mask):
    cfg, S, nc, c, sc = k.cfg, k.S, k.nc, k.c, k.sc
    NOWN = cfg.NOWN
    sc["YAT"] = k.scratch("YAT", [AW, NOWN], BF16)
    k.phase()
    QTv = sc["QT"].rearrange("(c p) t -> p c t", p=128)
    KTv = sc["KT"].rearrange("(c p) t -> p c t", p=128)
    YATv = sc["YAT"].rearrange("(c p) t -> p c t", p=128)
    qt = Ring([k.sb(f"qt{i}", [128, 8, 512], BF16) for i in range(2)])
    kt = Ring([k.sb(f"kt{i}", [128, 8, 1024], BF16) for i in range(2)])
    vv = Ring([k.sb(f"vv{i}", [128, 8, NH, 65], BF16) for i in range(2)])
    bias = k.sb("abias", [128, 5, NH, 128], F32); b_bias = Buf()
    halo = k.sb("halo", [128, 1], F32); b_halo = Buf()
    scs = Ring([k.sb(f"scs{i}", [128, 512], F32) for i in range(2)])
    pTs = Ring([k.sb(f"pTs{i}", [128, 512], BF16) for i in range(3)])
    ya = Ring([k.sb(f"ya{i}", [128, AW], BF16) for i in range(2)])
    yst = Ring([k.sb(f"yst{i}", [128, 8, 512], BF16) for i in range(2)])
    rec = Ring([k.sb(f"rec{i}", [128, 4], F32) for i in range(4)])
    ps_s = Ring([k.ps(f"ps_s{i}", [128, 512], F32) for i in range(2)])
    po = [k.ps(f"po{i}", [128, 512], F32) for i in range(4)]
    b_po = [Buf() for _ in range(4)]
    psT = Ring([k.ps("psT2", [128, 8, 128], BF16)])

    for kb in range(5):
        S.dma("sp", lambda e, kb=kb: e.dma_start(out=bias[:, kb, :, :], in_=att_bias[:, kb, :, :]), writes=[b_bias])
    S.dma("sp", lambda e: e.dma_start(out=halo[:], in_=halo_mask[:, :]), writes=[b_halo])

    HG = [[0, 2, 4, 6], [1, 3, 5, 7], [8, 10, 12, 14], [9, 11, 13, 15]]
    for g in range(NOWN // 512):
        q_, bq = qt.next(); k_, bk = kt.next(); v_, bv = vv.next()
        S.dma("sp", lambda e, q_=q_, g=g: e.dma_start(out=q_[:], in_=QTv[:, :, 512 * g:512 * g + 512]), writes=[bq])
        S.dma("act", lambda e, k_=k_, g=g: e.dma_start(out=k_[:], in_=KTv[:, :, 512 * g:512 * g + 1024]), writes=[bk])
        S.dma("pool", lambda e, v_=v_, g=g: e.dma_start(
            out=v_[:], in_=sc["V"][512 * g:512 * g + 1024, :, :].rearrange("(i p) h d -> p i h d", p=128)), writes=[bv])
        ys, bys = yst.next()
        for p in range(4):
            y_, by = ya.next()
            for hg in range(4):
                for kb in range(5):
                    ps, bps = ps_s.next()
                    for hh in range(4):
                        h = HG[hg][hh]
                        hp, h2 = h % 2, h // 2
                        S.op("pe", lambda e, ps=ps, k_=k_, q_=q_, hp=hp, h2=h2, p=p, kb=kb, hh=hh: e.matmul(
                            ps[:, hh * 128:(hh + 1) * 128],
                            lhsT=k_[hp * 64:(hp + 1) * 64, h2, 128 * (p + kb):128 * (p + kb) + 128],
                            rhs=q_[hp * 64:(hp + 1) * 64, h2, 128 * p:128 * p + 128],
                            start=True, stop=True), reads=[bk, bq], writes=[bps])
                    s_, bs = scs.next()
                    S.op("dve", lambda e, s_=s_, ps=ps, kb=kb, hg=hg: e.tensor_tensor(
                        out=s_[:].rearrange("p (h q) -> p h q", q=128), in0=ps[:].rearrange("p (h q) -> p h q", q=128),
                        in1=bias[:, kb, 4 * hg:4 * hg + 4, :], op=ALU.add), reads=[bps, b_bias], writes=[bs])
                    pt, bpt = pTs.next()
                    masked = (g == 0 and p + kb < 4)
                    if masked:
                        S.op("act", lambda e, pt=pt, s_=s_: e.activation(
                            out=pt[:], in_=s_[:], func=AF.Exp, bias=halo[:, 0:1]), reads=[bs, b_halo], writes=[bpt])
                    else:
                        S.op("act", lambda e, pt=pt, s_=s_: e.activation(
                            out=pt[:], in_=s_[:], func=AF.Exp), reads=[bs], writes=[bpt])
                    for hh in range(4):
                        h = HG[hg][hh]
                        S.op("pe", lambda e, pt=pt, v_=v_, hg=hg, hh=hh, h=h, p=p, kb=kb: e.matmul(
                            po[hg][:, hh * 65:(hh + 1) * 65], lhsT=pt[:, hh * 128:(hh + 1) * 128],
                            rhs=v_[:, p + kb, h, :], start=(kb == 0 and hh == 0), stop=(kb == 4 and hh == 3),
                            skip_group_check=True), reads=[bpt, bv], writes=[b_po[hg]])
                r_, br = rec.next()
                pov = po[hg][:, 0:260].rearrange("p (h d) -> p h d", d=65)
                S.op("dve", lambda e, r_=r_, pov=pov: e.reciprocal(out=r_[:, 0:4], in_=pov[:, :, 64]),
                     reads=[b_po[hg]], writes=[br])
                for hh in range(4):
                    h = HG[hg][hh]
                    eng = "act" if hh % 2 == 0 else "dve"
                    if eng == "act":
                        S.op("act", lambda e, y_=y_, pov=pov, r_=r_, hh=hh, h=h: e.activation(
                            out=y_[:, h * 64:(h + 1) * 64], in_=pov[:, hh, 0:64], func=AF.Copy, scale=r_[:, hh:hh + 1]),
                            reads=[b_po[hg], br], writes=[by])
                    else:
                        S.op("dve", lambda e, y_=y_, pov=pov, r_=r_, hh=hh, h=h: e.tensor_scalar(
                            out=y_[:, h * 64:(h + 1) * 64], in0=pov[:, hh, 0:64], scalar1=r_[:, hh:hh + 1], scalar2=None,
                            op0=ALU.mult), reads=[b_po[hg], br], writes=[by])
            pt_, bpt_ = psT.next()
            for j in range(8):
                S.op("pe", lambda e, pt_=pt_, y_=y_, j=j: e.transpose(
                    out=pt_[:, j, :], in_=y_[:, j * 128:(j + 1) * 128], identity=c["ident_b"][:]),
                    reads=[by, c["b_ident"]], writes=[bpt_])
            S.op("act", lambda e, ys=ys, pt_=pt_, p=p: e.activation(
                out=ys[:, :, p * 128:(p + 1) * 128], in_=pt_[:], func=AF.Copy), reads=[bpt_], writes=[bys])
        S.dma("sp", lambda e, ys=ys, g=g: e.dma_start(out=YATv[:, :, 512 * g:512 * g + 512], in_=ys[:]), reads=[bys])


def phase3(k, prm):
    cfg, S, nc, c, sc = k.cfg, k.S, k.nc, k.c, k.sc
    NTOK, NPRE, NOWN = cfg.NTOK, cfg.NPRE, cfg.NOWN
    sc["YB"] = k.scratch("YB", [NOWN, AW], F32)
    sc["BON"] = k.scratch("BON", [NH, NOWN], F32)
    k.phase()
    HB = 8
    def cload(name, shape, dt=F32, src=None, cast=None):
        t = k.sb("s3_" + name, shape, dt); b = Buf()
        S.dma("sp", lambda e: e.dma_start(out=t[:], in_=src), writes=[b])
        return t, b
    wupf, b_wupf = cload("wupf", [96, AW], F32, prm["w_up"][:, :])
    aupf, b_aupf = cload("aupf", [96, AW], F32, prm["a_up"][:, :])
    wup = k.sb("wup", [96, AW], BF16); aup = k.sb("aup", [96, AW], BF16)
    S.op("act", lambda e: e.activation(out=wup[:], in_=wupf[:], func=AF.Copy), reads=[b_wupf], writes=[b_wupf])
    S.op("act", lambda e: e.activation(out=aup[:], in_=aupf[:], func=AF.Copy), reads=[b_aupf], writes=[b_aupf])
    hc, b_hc = cload("hc", [64, 6, NH], F32, prm["hcols"][:, :, :])
    S.op("dve", lambda e: e.tensor_scalar(out=hc[:, 4, :], in0=hc[:, 3, :], scalar1=-1.0, scalar2=1.0, op0=ALU.mult, op1=ALU.add),
         reads=[b_hc], writes=[b_hc])
    rmask, b_rm = cload("rmask", [64, 512], F32, prm["rmask"][:, :])
    m_su, b_su = cload("m_su", [64, 8, 64], F32, prm["m_su"][:, :, :])
    m_ui, b_ui = cload("m_ui", [64, 8, 64], F32, prm["m_ui"][:, :, :])
    m_sl, b_sl = cload("m_sl", [64, 8, 64], F32, prm["m_sl"][:, :, :])
    i8, b_i8 = cload("i8", [64, 8, 64], F32, prm["i8"][:, :, :])
    ones64 = k.sb("ones64", [64, 64], F32); b_ones = Buf()
    S.op("pool", lambda e: e.memset(ones64[:], 1.0), writes=[b_ones])
    cb = [b_hc, b_rm]

    S32 = k.sb("S32", [64, NH, 64], F32); b_S32 = [Buf() for _ in range(NH)]
    Sb = k.sb("Sb", [64, NH, 64], BF16); b_Sb = [Buf() for _ in range(NH)]
    S.op("pool", lambda e: e.memset(S32[:], 0.0), writes=b_S32)
    S.op("pool", lambda e: e.memset(Sb[:], 0.0), writes=b_Sb)

    wdt = Ring([k.sb(f"wdt{i}", [96, 512], BF16) for i in range(2)])
    adt = Ring([k.sb(f"adt{i}", [96, 512], BF16) for i in range(2)])
    def htiles(name, shape, dt):
        return [k.sb(f"{name}{i}", shape, dt) for i in range(HB)], [Buf() for _ in range(HB)]
    AR, b_AR = htiles("AR", [64, 8, 2, 64], BF16)
    Tm, b_Tm = htiles("Tm", [64, 8, 64], BF16)
    Mka, b_Mka = htiles("Mka", [64, 8, 64], BF16)
    Mbr, b_Mbr = htiles("Mbr", [64, 8, 64], BF16)
    Mkr, b_Mkr = htiles("Mkr", [64, 8, 64], BF16)
    BhT, b_BhT = htiles("BhT", [64, 8, 64], BF16)
    KhT, b_KhT = htiles("KhT", [64, 8, 64], BF16)
    VT, b_VT = htiles("VT", [64, 8, 64], BF16)
    pC, b_pC = htiles("pC", [64, 8], F32)
    def tring(name, n, shape=[64, 512], dt=F32):
        return Ring([k.sb(f"{name}{i}", shape, dt) for i in range(n)])
    t_Ep = tring("t_Ep", 2, [64, 8, 64], F32)
    rin = tring("rin", 2, dt=BF16); kin = tring("kin", 2, dt=BF16); vin = tring("vin", 2, dt=BF16)
    t_s = tring("t_s", 2); t_cum = tring("t_cum", 2); t_a = tring("t_a", 2); t_kkr = tring("t_kkr", 2)
    t_sq = tring("t_sq", 2); t_nr = tring("t_nr", 2); t_kk = tring("t_kk", 2); t_t1 = tring("t_t1", 2)
    t_kp = tring("t_kp", 2); t_bv = tring("t_bv", 2); t_d1 = tring("t_d1", 2); t_Epv = tring("t_Epv", 2)
    t_Em = tring("t_Em", 2); t_EC = tring("t_EC", 2)
    t_Bt = tring("t_Bt", 2, dt=BF16); t_Kt = tring("t_Kt", 2, dt=BF16)
    t_Bh = tring("t_Bh", 2, dt=BF16); t_Kh = tring("t_Kh", 2, dt=BF16)
    t_rkp = tring("t_rkp", 2); t_brow = tring("t_brow", 2, [1, 512], F32)
    t_N = tring("t_N", 4, [64, 8, 64], BF16); t_NT = tring("t_NT", 4, [64, 8, 64], BF16)
    WTs = tring("WTs", 2, [64, HB, 64], BF16); UTs = tring("UTs", 2, [64, HB, 64], BF16)
    ysb = tring("ysb", 1, [64, HB, 64], F32)
    pA = Ring([k.ps(f"p3a{i}", [64, 512], F32) for i in range(2)])
    pG = Ring([k.ps(f"p3g{i}", [64, 4, 128], F32) for i in range(2)])
    pTb = Ring([k.ps(f"p3t{i}", [64, 8, 128], BF16) for i in range(2)])
    pW = k.ps("p3W", [64, HB, 64], F32); b_pW = Buf()
    pU = k.ps("p3U", [64, HB, 64], F32); b_pU = Buf()
    pS = pW; b_pS = b_pW

    RBv, KBv, VBv = sc["RB"], sc["KB"], sc["VB"]
    for tt in range(NTOK // 512):
        t0 = tt * 512
        own = t0 >= NPRE
        wd_, bwd = wdt.next(); ad_, bad = adt.next()
        S.dma("sp", lambda e, wd_=wd_, t0=t0: e.dma_start(out=wd_[:], in_=sc["WD"][:, t0:t0 + 512]), writes=[bwd])
        S.dma("sp", lambda e, ad_=ad_, t0=t0: e.dma_start(out=ad_[:], in_=sc["AD"][:, t0:t0 + 512]), writes=[bad])
        for hb0 in range(0, NH, HB):
            def prep(hi, hb0=hb0, own=own, t0=t0, wd_=wd_, ad_=ad_, bwd=bwd, bad=bad):
                h = hb0 + hi
                hs = slice(h * 64, (h + 1) * 64)
                col = lambda j: hc[:, j, h:h + 1]
                r_, br = rin.next(); k_, bk = kin.next(); v_, bv = vin.next()
                yield S.dma("sp", lambda e, r_=r_, hs=hs, t0=t0: e.dma_start(out=r_[:], in_=RBv[hs, t0:t0 + 512]), writes=[br])
                yield S.dma("act", lambda e, k_=k_, hs=hs, t0=t0: e.dma_start(out=k_[:], in_=KBv[hs, t0:t0 + 512]), writes=[bk])
                yield S.dma("sp", lambda e, v_=v_, hs=hs, t0=t0: e.dma_start(out=v_[:], in_=VBv[hs, t0:t0 + 512]), writes=[bv])
                p1, bp1 = pA.next()
                yield S.op("pe", lambda e, p1=p1, hs=hs, wd_=wd_: e.matmul(p1[:, :], lhsT=wup[:, hs], rhs=wd_[:, :], start=True, stop=True),
                     reads=[b_wupf, bwd], writes=[bp1])
                s_, bs = t_s.next()
                yield S.op("act", lambda e, s_=s_, p1=p1, h=h: e.activation(out=s_[:], in_=p1[:, :], func=AF.Sigmoid, bias=hc[:, 0, h:h + 1]),
                     reads=[bp1, b_hc], writes=[bs])
                cum, bcum = t_cum.next()
                yield S.op("dve", lambda e, cum=cum, s_=s_: e.tensor_tensor_scan(out=cum[:], data0=rmask[:], data1=s_[:], initial=0.0,
                                                                          op0=ALU.mult, op1=ALU.add), reads=[bs, b_rm], writes=[bcum])
                p2, bp2 = pA.next()
                yield S.op("pe", lambda e, p2=p2, hs=hs, ad_=ad_: e.matmul(p2[:, :], lhsT=aup[:, hs], rhs=ad_[:, :], start=True, stop=True),
                     reads=[b_aupf, bad], writes=[bp2])
                a_, ba = t_a.next()
                yield S.op("act", lambda e, a_=a_, p2=p2, h=h: e.activation(out=a_[:], in_=p2[:, :], func=AF.Sigmoid, bias=hc[:, 1, h:h + 1]),
                     reads=[bp2, b_hc], writes=[ba])
                kkr, bkkr = t_kkr.next()
                yield S.op("dve", lambda e, kkr=kkr, k_=k_, h=h: e.tensor_scalar(out=kkr[:], in0=k_[:], scalar1=hc[:, 2, h:h + 1], scalar2=None,
                                                                          op0=ALU.mult), reads=[bk, b_hc], writes=[bkkr])
                sq, bsq = t_sq.next()
                yield S.op("pool", lambda e, sq=sq, kkr=kkr: e.tensor_tensor(out=sq[:], in0=kkr[:], in1=kkr[:], op=ALU.mult), reads=[bkkr], writes=[bsq])
                p3, bp3 = pA.next()
                yield S.op("pe", lambda e, p3=p3, sq=sq: e.matmul(p3[:, :], lhsT=ones64[:, :], rhs=sq[:, :], start=True, stop=True),
                     reads=[b_ones, bsq], writes=[bp3])
                nr, bnr = t_nr.next()
                yield S.op("act", lambda e, nr=nr, p3=p3: e.activation(out=nr[:], in_=p3[:, :], func=AF.Sqrt), reads=[bp3], writes=[bnr])
                yield S.op("dve", lambda e, nr=nr: e.tensor_scalar(out=nr[:], in0=nr[:], scalar1=1e-12, scalar2=None, op0=ALU.max), reads=[bnr], writes=[bnr])
                yield S.op("dve", lambda e, nr=nr: e.reciprocal(out=nr[:], in_=nr[:]), reads=[bnr], writes=[bnr])
                kk, bkk = t_kk.next()
                yield S.op("dve", lambda e, kk=kk, kkr=kkr, nr=nr: e.tensor_tensor(out=kk[:], in0=kkr[:], in1=nr[:], op=ALU.mult), reads=[bkkr, bnr], writes=[bkk])
                t1, bt1 = t_t1.next()
                yield S.op("dve", lambda e, t1=t1, a_=a_, h=h: e.tensor_scalar(out=t1[:], in0=a_[:], scalar1=hc[:, 3, h:h + 1], scalar2=hc[:, 4, h:h + 1],
                                                                        op0=ALU.mult, op1=ALU.add), reads=[ba, b_hc], writes=[bt1])
                kp, bkp = t_kp.next()
                yield S.op("pool", lambda e, kp=kp, k_=k_, t1=t1: e.tensor_tensor(out=kp[:], in0=k_[:], in1=t1[:], op=ALU.mult), reads=[bk, bt1], writes=[bkp])
                bv_, bbv = t_bv.next()
                yield S.op("pool", lambda e, bv_=bv_, kk=kk, a_=a_: e.tensor_tensor(out=bv_[:], in0=kk[:], in1=a_[:], op=ALU.mult), reads=[bkk, ba], writes=[bbv])
                if own:
                    rkp, brkp = t_rkp.next()
                    yield S.op("dve", lambda e, rkp=rkp, r_=r_, kp=kp, h=h: e.scalar_tensor_tensor(out=rkp[:], in0=r_[:], scalar=hc[:, 5, h:h + 1], in1=kp[:],
                                                                                           op0=ALU.mult, op1=ALU.mult), reads=[br, bkp, b_hc], writes=[brkp])
                    pb, bpb = pA.next()
                    yield S.op("pe", lambda e, pb=pb, rkp=rkp: e.matmul(pb[0:1, :], lhsT=ones64[:, 0:1], rhs=rkp[:, :], start=True, stop=True),
                         reads=[b_ones, brkp], writes=[bpb])
                    brow, bbrow = t_brow.next()
                    yield S.op("act", lambda e, brow=brow, pb=pb: e.activation(out=brow[0:1, :], in_=pb[0:1, :], func=AF.Copy), reads=[bpb], writes=[bbrow])
                    yield S.dma("sp", lambda e, brow=brow, h=h, t0=t0: e.dma_start(out=sc["BON"][h:h + 1, t0 - NPRE:t0 - NPRE + 512], in_=brow[0:1, :]), reads=[bbrow])
                ep, bep = t_Ep.next()
                epf = ep[:].rearrange("p c t -> p (c t)")
                yield S.op("act", lambda e, epf=epf, cum=cum: e.activation(out=epf, in_=cum[:], func=AF.Exp, scale=-C0), reads=[bcum], writes=[bep])
                d1, bd1 = t_d1.next()
                yield S.op("pool", lambda e, d1=d1, cum=cum, s_=s_: e.tensor_tensor(out=d1[:], in0=cum[:], in1=s_[:], op=ALU.subtract), reads=[bcum, bs], writes=[bd1])
                epv, bepv = t_Epv.next()
                yield S.op("act", lambda e, epv=epv, d1=d1: e.activation(out=epv[:], in_=d1[:], func=AF.Exp, scale=-C0), reads=[bd1], writes=[bepv])
                em, bem = t_Em.next()
                yield S.op("act", lambda e, em=em, cum=cum: e.activation(out=em[:], in_=cum[:], func=AF.Exp, scale=C0), reads=[bcum], writes=[bem])
                yield S.op("pool", lambda e, ep=ep, hi=hi: e.tensor_copy(out=pC[hi][:, :], in_=ep[:, :, 63]), reads=[bep], writes=[b_pC[hi]])
                ec, bec = t_EC.next()
                for cc in range(8):
                    yield S.op("dve" if cc % 2 else "pool", lambda e, ec=ec, em=em, ep=ep, cc=cc: e.tensor_scalar(
                        out=ec[:, cc * 64:(cc + 1) * 64], in0=em[:, cc * 64:(cc + 1) * 64], scalar1=ep[:, cc, 63:64], scalar2=None, op0=ALU.mult),
                        reads=[bem, bep], writes=[bec])
                ar = AR[hi]; bar = b_AR[hi]
                yield S.op("dve", lambda e, ar=ar, r_=r_, epf=epf: e.tensor_tensor(out=ar[:, :, 1, :], in0=r_[:].rearrange("p (c t) -> p c t", t=64),
                                                                           in1=epf.rearrange("p (c t) -> p c t", t=64), op=ALU.mult), reads=[br, bep], writes=[bar])
                yield S.op("dve", lambda e, ar=ar, kk=kk, epv=epv: e.scalar_tensor_tensor(out=ar[:, :, 0, :], in0=kk[:].rearrange("p (c t) -> p c t", t=64), scalar=-1.0,
                                                                                  in1=epv[:].rearrange("p (c t) -> p c t", t=64), op0=ALU.mult, op1=ALU.mult), reads=[bkk, bepv], writes=[bar])
                Bt, bBt = t_Bt.next(); Kt, bKt = t_Kt.next(); Bh, bBh = t_Bh.next(); Kh, bKh = t_Kh.next()
                yield S.op("pool", lambda e, Bt=Bt, bv_=bv_, em=em: e.tensor_tensor(out=Bt[:], in0=bv_[:], in1=em[:], op=ALU.mult), reads=[bbv, bem], writes=[bBt])
                yield S.op("dve", lambda e, Kt=Kt, kp=kp, em=em: e.tensor_tensor(out=Kt[:], in0=kp[:], in1=em[:], op=ALU.mult), reads=[bkp, bem], writes=[bKt])
                yield S.op("pool", lambda e, Bh=Bh, bv_=bv_, ec=ec: e.tensor_tensor(out=Bh[:], in0=bv_[:], in1=ec[:], op=ALU.mult), reads=[bbv, bec], writes=[bBh])
                yield S.op("dve", lambda e, Kh=Kh, kp=kp, ec=ec: e.tensor_tensor(out=Kh[:], in0=kp[:], in1=ec[:], op=ALU.mult), reads=[bkp, bec], writes=[bKh])
                for src, bsrc, dstl, bdstl in ((Bh, bBh, BhT, b_BhT), (Kh, bKh, KhT, b_KhT), (v_, bv, VT, b_VT)):
                    pt, bpt = pTb.next()
                    for cc in range(8):
                        yield S.op("pe", lambda e, pt=pt, src=src, cc=cc: e.transpose(out=pt[:, cc, 0:64], in_=src[:, cc * 64:(cc + 1) * 64],
                                                                               identity=c["ident_b"][0:64, 0:64]), reads=[bsrc, c["b_ident"]], writes=[bpt])
                    yield S.op("act", lambda e, pt=pt, d=dstl[hi]: e.activation(out=d[:], in_=pt[:, :, 0:64], func=AF.Copy), reads=[bpt], writes=[bdstl[hi]])
                N0, bN0 = t_N.next()
                for hv in range(2):
                    pg, bpg = pG.next()
                    for c4 in range(4):
                        cc = 4 * hv + c4
                        yield S.op("pe", lambda e, pg=pg, Bt=Bt, ar=ar, cc=cc, c4=c4: e.matmul(pg[:, c4, :], lhsT=Bt[:, cc * 64:(cc + 1) * 64],
                                                                                        rhs=ar[:, cc, :, :].rearrange("p a t -> p (a t)"), start=True, stop=True),
                             reads=[bBt, bar], writes=[bpg])
                    yield S.op("dve", lambda e, N0=N0, pg=pg, hv=hv: e.tensor_tensor(out=N0[:, 4 * hv:4 * hv + 4, :], in0=pg[:, :, 0:64], in1=m_su[:, 0:4, :], op=ALU.mult),
                         reads=[bpg, b_su], writes=[bN0])
                    yield S.op("dve", lambda e, pg=pg, d=Mbr[hi], hv=hv: e.tensor_tensor(out=d[:, 4 * hv:4 * hv + 4, :], in0=pg[:, :, 64:128], in1=m_ui[:, 0:4, :], op=ALU.mult),
                         reads=[bpg, b_ui], writes=[b_Mbr[hi]])
                for hv in range(2):
                    pg, bpg = pG.next()
                    for c4 in range(4):
                        cc = 4 * hv + c4
                        yield S.op("pe", lambda e, pg=pg, Kt=Kt, ar=ar, cc=cc, c4=c4: e.matmul(pg[:, c4, :], lhsT=Kt[:, cc * 64:(cc + 1) * 64],
                                                                                        rhs=ar[:, cc, :, :].rearrange("p a t -> p (a t)"), start=True, stop=True),
                             reads=[bKt, bar], writes=[bpg])
                    yield S.op("dve", lambda e, pg=pg, d=Mka[hi], hv=hv: e.tensor_tensor(out=d[:, 4 * hv:4 * hv + 4, :], in0=pg[:, :, 0:64], in1=m_su[:, 0:4, :], op=ALU.mult),
                         reads=[bpg, b_su], writes=[b_Mka[hi]])
                    yield S.op("dve", lambda e, pg=pg, d=Mkr[hi], hv=hv: e.tensor_tensor(out=d[:, 4 * hv:4 * hv + 4, :], in0=pg[:, :, 64:128], in1=m_ui[:, 0:4, :], op=ALU.mult),
                         reads=[bpg, b_ui], writes=[b_Mkr[hi]])
                p4, bp4 = pA.next()
                for cc in range(8):
                    yield S.op("pe", lambda e, p4=p4, ar=ar, Bt=Bt, cc=cc: e.matmul(p4[:, cc * 64:(cc + 1) * 64], lhsT=ar[:, cc, 0, :], rhs=Bt[:, cc * 64:(cc + 1) * 64],
                                                                             start=True, stop=True), reads=[bar, bBt], writes=[bp4])
                NT0, bNT0 = t_NT.next()
                yield S.op("dve", lambda e, NT0=NT0, p4=p4: e.tensor_tensor(out=NT0[:], in0=p4[:, :].rearrange("p (c t) -> p c t", t=64), in1=m_sl[:], op=ALU.mult),
                     reads=[bp4, b_sl], writes=[bNT0])
                T_ = Tm[hi]; bT = b_Tm[hi]
                yield S.op("pool", lambda e, T_=T_, N0=N0: e.tensor_tensor(out=T_[:], in0=N0[:], in1=i8[:], op=ALU.add), reads=[bN0, b_i8], writes=[bT])
                Nc, bNc, NTc, bNTc = N0, bN0, NT0, bNT0
                for lvl in range(1, 6):
                    pnt, bpnt = pA.next()
                    for cc in range(8):
                        yield S.op("pe", lambda e, pnt=pnt, Nc=Nc, NTc=NTc, cc=cc: e.matmul(pnt[:, cc * 64:(cc + 1) * 64], lhsT=Nc[:, cc, :], rhs=NTc[:, cc, :],
                                                                                     start=True, stop=True), reads=[bNc, bNTc], writes=[bpnt])
                    NTn, bNTn = t_NT.next()
                    yield S.op("act", lambda e, NTn=NTn, pnt=pnt: e.activation(out=NTn[:].rearrange("p c t -> p (c t)"), in_=pnt[:, :], func=AF.Copy),
                         reads=[bpnt], writes=[bNTn])
                    if lvl < 5:
                        pn, bpn = pA.next()
                        for cc in range(8):
                            yield S.op("pe", lambda e, pn=pn, Nc=Nc, NTc=NTc, cc=cc: e.matmul(pn[:, cc * 64:(cc + 1) * 64], lhsT=NTc[:, cc, :], rhs=Nc[:, cc, :],
                                                                                       start=True, stop=True), reads=[bNc, bNTc], writes=[bpn])
                        Nn, bNn = t_N.next()
                        yield S.op("act", lambda e, Nn=Nn, pn=pn: e.activation(out=Nn[:].rearrange("p c t -> p (c t)"), in_=pn[:, :], func=AF.Copy),
                             reads=[bpn], writes=[bNn])
                    ptt, bptt = pA.next()
                    for cc in range(8):
                        yield S.op("pe", lambda e, ptt=ptt, NTn=NTn, T_=T_, cc=cc: e.matmul(ptt[:, cc * 64:(cc + 1) * 64], lhsT=NTn[:, cc, :], rhs=T_[:, cc, :],
                                                                                     start=True, stop=True), reads=[bNTn, bT], writes=[bptt])
                    yield S.op("dve", lambda e, T_=T_, ptt=ptt: e.tensor_tensor(out=T_[:].rearrange("p c t -> p (c t)"), in0=ptt[:, :],
                                                                         in1=T_[:].rearrange("p c t -> p (c t)"), op=ALU.add), reads=[bptt, bT], writes=[bT])
                    NTc, bNTc = NTn, bNTn
                    if lvl < 5:
                        Nc, bNc = Nn, bNn
            GRP = 2
            for gq in range(0, HB, GRP):
                gens = [prep(hi) for hi in range(gq, gq + GRP)]
                while gens:
                    for g_ in list(gens):
                        try:
                            next(g_)
                        except StopIteration:
                            gens.remove(g_)
            for cc in range(8):
                hbufs = lambda lst: [lst[i] for i in range(HB)]
                for hi in range(HB):
                    h = hb0 + hi
                    S.op("pe", lambda e, hi=hi, h=h, cc=cc: e.matmul(pW[:, hi, :], lhsT=AR[hi][:, cc, 0, :], rhs=Sb[:, h, :], start=(hi == 0), stop=False,
                                                                    skip_group_check=True), reads=[b_AR[hi], b_Sb[h]], writes=[b_pW])
                    S.op("pe", lambda e, hi=hi, cc=cc: e.matmul(pW[:, hi, :], lhsT=Mka[hi][:, cc, :], rhs=VT[hi][:, cc, :], start=False, stop=True,
                                                               skip_group_check=True), reads=[b_Mka[hi], b_VT[hi]], writes=[b_pW])
                wt, bwt = WTs.next()
                S.op("act", lambda e, wt=wt: e.activation(out=wt[:], in_=pW[:], func=AF.Copy), reads=[b_pW], writes=[bwt])
                for hi in range(HB):
                    S.op("pe", lambda e, hi=hi, cc=cc, wt=wt: e.matmul(pU[:, hi, :], lhsT=Tm[hi][:, cc, :], rhs=wt[:, hi, :], start=(hi == 0), stop=True,
                                                                      skip_group_check=True), reads=[b_Tm[hi], bwt], writes=[b_pU])
                ut, but = UTs.next()
                S.op("dve", lambda e, ut=ut: e.tensor_copy(out=ut[:], in_=pU[:]), reads=[b_pU], writes=[but])
                if own:
                    py, bpy = pA.next()
                    for hi in range(HB):
                        h = hb0 + hi
                        S.op("pe", lambda e, py=py, hi=hi, h=h, cc=cc: e.matmul(py[:, hi * 64:(hi + 1) * 64], lhsT=AR[hi][:, cc, 1, :], rhs=Sb[:, h, :],
                                                                               start=(hi == 0), stop=False, skip_group_check=True),
                             reads=[b_AR[hi], b_Sb[h]], writes=[bpy])
                    for hi in range(HB):
                        S.op("pe", lambda e, py=py, hi=hi, cc=cc, ut=ut: e.matmul(py[:, hi * 64:(hi + 1) * 64], lhsT=Mbr[hi][:, cc, :], rhs=ut[:, hi, :],
                                                                                 start=False, stop=False, skip_group_check=True),
                             reads=[b_Mbr[hi], but], writes=[bpy])
                        S.op("pe", lambda e, py=py, hi=hi, cc=cc: e.matmul(py[:, hi * 64:(hi + 1) * 64], lhsT=Mkr[hi][:, cc, :], rhs=VT[hi][:, cc, :],
                                                                          start=False, stop=True, skip_group_check=True),
                             reads=[b_Mkr[hi], b_VT[hi]], writes=[bpy])
                    ys, bys = ysb.next()
                    S.op("act", lambda e, ys=ys, py=py: e.activation(out=ys[:].rearrange("p h v -> p (h v)"), in_=py[:, :], func=AF.Copy), reads=[bpy], writes=[bys])
                    trow = t0 - NPRE + cc * 64
                    S.dma("sp", lambda e, ys=ys, trow=trow, hb0=hb0: e.dma_start(
                        out=sc["YB"][trow:trow + 64, hb0 * 64:(hb0 + HB) * 64], in_=ys[:].rearrange("p h v -> p (h v)")), reads=[bys])
                for hi in range(HB):
                    S.op("pe", lambda e, hi=hi, cc=cc, ut=ut: e.matmul(pS[:, hi, :], lhsT=BhT[hi][:, cc, :], rhs=ut[:, hi, :], start=(hi == 0), stop=False,
                                                                      skip_group_check=True), reads=[b_BhT[hi], but], writes=[b_pS])
                    S.op("pe", lambda e, hi=hi, cc=cc: e.matmul(pS[:, hi, :], lhsT=KhT[hi][:, cc, :], rhs=VT[hi][:, cc, :], start=False, stop=True,
                                                               skip_group_check=True), reads=[b_KhT[hi], b_VT[hi]], writes=[b_pS])
                for hi in range(HB):
                    h = hb0 + hi
                    S.op("dve", lambda e, hi=hi, h=h, cc=cc: e.scalar_tensor_tensor(out=S32[:, h, :], in0=S32[:, h, :], scalar=pC[hi][:, cc:cc + 1],
                                                                                  in1=pS[:, hi, :], op0=ALU.mult, op1=ALU.add),
                         reads=[b_S32[h], b_pC[hi], b_pS], writes=[b_S32[h]])
                    S.op("act", lambda e, h=h: e.activation(out=Sb[:, h, :], in_=S32[:, h, :], func=AF.Copy), reads=[b_S32[h]], writes=[b_Sb[h]])


def phase3b(k, prm):
    cfg, S, nc, c, sc = k.cfg, k.S, k.nc, k.c, k.sc
    NTOK, NPRE, NOWN = cfg.NTOK, cfg.NPRE, cfg.NOWN
    sc["YBT"] = k.scratch("YBT", [AW, NOWN], BF16)
    k.phase()
    YBTv = sc["YBT"].rearrange("(c p) t -> p c t", p=128)
    VBv = sc["VB"].rearrange("(c p) t -> p c t", p=128)
    GDv = sc["GD"].rearrange("(c p) t -> p c t", p=128)
    gupf = k.sb("gupf", [128, 2, AW], F32); gup = k.sb("gup", [128, 2, AW], BF16); b_gup = Buf()
    S.dma("sp", lambda e: e.dma_start(out=gupf[:], in_=prm["g_up"].rearrange("(c p) n -> p c n", p=128)), writes=[b_gup])
    S.op("act", lambda e: e.activation(out=gup[:], in_=gupf[:], func=AF.Copy), reads=[b_gup], writes=[b_gup])
    lw = k.sb("lnxw", [128, AW], F32); lb = k.sb("lnxb", [128, AW], F32); b_l = Buf()
    S.dma("sp", lambda e: e.dma_start(out=lw[:], in_=prm["lnx_w"][0:1, :].partition_broadcast(128)), writes=[b_l])
    S.dma("sp", lambda e: e.dma_start(out=lb[:], in_=prm["lnx_b"][0:1, :].partition_broadcast(128)), writes=[b_l])
    yin = Ring([k.sb(f"yin{i}", [128, AW], F32) for i in range(2)])
    ysq = Ring([k.sb(f"ysq{i}", [128, AW], F32) for i in range(2)])
    st = Ring([k.sb(f"gst{i}", [128, 6, NH], F32) for i in range(2)])
    yn = Ring([k.sb(f"yn{i}", [128, AW], F32) for i in range(2)])
    vfm = Ring([k.sb(f"vfm{i}", [128, 8, 128], BF16) for i in range(2)])
    vtm = Ring([k.sb(f"vtm{i}", [128, AW], BF16) for i in range(2)])
    gdl = Ring([k.sb(f"gdl{i}", [128, 2, 128], BF16) for i in range(2)])
    bonf = Ring([k.sb(f"bonf{i}", [NH, 128], F32) for i in range(2)])
    bont = Ring([k.sb(f"bont{i}", [128, NH], F32) for i in range(2)])
    yo = Ring([k.sb(f"yo{i}", [128, AW], BF16) for i in range(2)])
    ost = Ring([k.sb(f"ybst{i}", [128, 8, 512], BF16) for i in range(2)])
    pT = Ring([k.ps(f"p3bT{i}", [128, 8, 128], BF16) for i in range(2)])
    pg = Ring([k.ps(f"p3bg{i}", [128, 512], F32) for i in range(2)])
    pbn = Ring([k.ps("p3bbn", [128, NH], F32)])
    bc = lambda ap: ap.unsqueeze(2).to_broadcast([128, NH, 64])
    v3 = lambda t: t[:].rearrange("p (h d) -> p h d", d=64)
    for ti in range(NOWN // 128):
        tok0 = ti * 128
        if ti % 4 == 0:
            os_, bos = ost.next()
        y_, by = yin.next()
        S.dma("sp", lambda e, y_=y_, tok0=tok0: e.dma_start(out=y_[:], in_=sc["YB"][tok0:tok0 + 128, :]), writes=[by])
        vf, bvf = vfm.next()
        S.dma("act", lambda e, vf=vf, tok0=tok0: e.dma_start(out=vf[:], in_=VBv[:, :, NPRE + tok0:NPRE + tok0 + 128]), writes=[bvf])
        gd, bgd = gdl.next()
        S.dma("act", lambda e, gd=gd, tok0=tok0: e.dma_start(out=gd[:], in_=GDv[:, :, NPRE + tok0:NPRE + tok0 + 128]), writes=[bgd])
        bf_, bbf = bonf.next()
        S.dma("sp", lambda e, bf_=bf_, tok0=tok0: e.dma_start(out=bf_[:], in_=sc["BON"][:, tok0:tok0 + 128]), writes=[bbf])
        s_, bs = st.next()
        S.op("dve", lambda e, s_=s_, y_=y_: e.tensor_reduce(out=s_[:, 0, :], in_=v3(y_), axis=AX.X, op=ALU.add), reads=[by], writes=[bs])
        q_, bq = ysq.next()
        S.op("act", lambda e, q_=q_, y_=y_: e.activation(out=q_[:], in_=y_[:], func=AF.Square), reads=[by], writes=[bq])
        S.op("dve", lambda e, s_=s_, q_=q_: e.tensor_reduce(out=s_[:, 1, :], in_=v3(q_), axis=AX.X, op=ALU.add), reads=[bq], writes=[bs])
        S.op("dve", lambda e, s_=s_: e.tensor_scalar(out=s_[:, 2, :], in0=s_[:, 0, :], scalar1=1.0 / 64, scalar2=None, op0=ALU.mult), reads=[bs], writes=[bs])
        S.op("dve", lambda e, s_=s_: e.tensor_tensor(out=s_[:, 3, :], in0=s_[:, 2, :], in1=s_[:, 2, :], op=ALU.mult), reads=[bs], writes=[bs])
        S.op("dve", lambda e, s_=s_: e.scalar_tensor_tensor(out=s_[:, 4, :], in0=s_[:, 1, :], scalar=1.0 / 64, in1=s_[:, 3, :], op0=ALU.mult, op1=ALU.subtract),
             reads=[bs], writes=[bs])
        S.op("dve", lambda e, s_=s_: e.tensor_scalar(out=s_[:, 4, :], in0=s_[:, 4, :], scalar1=64e-5, scalar2=None, op0=ALU.add), reads=[bs], writes=[bs])
        S.op("act", lambda e, s_=s_: e.activation(out=s_[:, 5, :], in_=s_[:, 4, :], func=AF.Sqrt), reads=[bs], writes=[bs])
        S.op("dve", lambda e, s_=s_: e.reciprocal(out=s_[:, 5, :], in_=s_[:, 5, :]), reads=[bs], writes=[bs])
        n_, bn = yn.next()
        S.op("dve", lambda e, n_=n_, y_=y_, s_=s_: e.tensor_tensor(out=v3(n_), in0=v3(y_), in1=bc(s_[:, 2, :]), op=ALU.subtract), reads=[by, bs], writes=[bn])
        S.op("pool", lambda e, n_=n_, s_=s_: e.tensor_tensor(out=v3(n_), in0=v3(n_), in1=bc(s_[:, 5, :]), op=ALU.mult), reads=[bn, bs], writes=[bn])
        S.op("dve", lambda e, n_=n_: e.tensor_tensor(out=n_[:], in0=n_[:], in1=lw[:], op=ALU.mult), reads=[bn, b_l], writes=[bn])
        S.op("pool", lambda e, n_=n_: e.tensor_tensor(out=n_[:], in0=n_[:], in1=lb[:], op=ALU.add), reads=[bn, b_l], writes=[bn])
        pb, bpb = pbn.next()
        S.op("pe", lambda e, pb=pb, bf_=bf_: e.transpose(out=pb[:, :], in_=bf_[:, :], identity=c["ident_f"][0:NH, 0:NH]), reads=[bbf, c["b_ident"]], writes=[bpb])
        bt, bbt = bont.next()
        S.op("act", lambda e, bt=bt, pb=pb: e.activation(out=bt[:], in_=pb[:, :], func=AF.Copy), reads=[bpb], writes=[bbt])
        pt, bpt = pT.next()
        for j in range(8):
            S.op("pe", lambda e, pt=pt, vf=vf, j=j: e.transpose(out=pt[:, j, :], in_=vf[:, j, :], identity=c["ident_b"][:]), reads=[bvf, c["b_ident"]], writes=[bpt])
        vt, bvt = vtm.next()
        S.op("act", lambda e, vt=vt, pt=pt: e.activation(out=vt[:].rearrange("p (j c) -> p j c", c=128), in_=pt[:], func=AF.Copy), reads=[bpt], writes=[bvt])
        q2, bq2 = ysq.next()
        S.op("pool", lambda e, q2=q2, vt=vt, bt=bt: e.tensor_tensor(out=v3(q2), in0=v3(vt), in1=bc(bt[:, :]), op=ALU.mult), reads=[bvt, bbt], writes=[bq2])
        S.op("dve", lambda e, n_=n_, q2=q2: e.tensor_tensor(out=n_[:], in0=n_[:], in1=q2[:], op=ALU.add), reads=[bn, bq2], writes=[bn])
        o_, bo = yo.next()
        for half in range(2):
            pg_, bpg = pg.next()
            for kc in range(2):
                S.op("pe", lambda e, pg_=pg_, gd=gd, kc=kc, half=half: e.matmul(pg_[:, :], lhsT=gd[:, kc, :], rhs=gup[:, kc, half * 512:(half + 1) * 512],
                                                                               start=(kc == 0), stop=(kc == 1)), reads=[bgd, b_gup], writes=[bpg])
            S.op("dve", lambda e, o_=o_, n_=n_, pg_=pg_, half=half: e.tensor_tensor(out=o_[:, half * 512:(half + 1) * 512], in0=pg_[:, :],
                                                                                   in1=n_[:, half * 512:(half + 1) * 512], op=ALU.mult), reads=[bpg, bn], writes=[bo])
        pt2, bpt2 = pT.next()
        for j in range(8):
            S.op("pe", lambda e, pt2=pt2, o_=o_, j=j: e.transpose(out=pt2[:, j, :], in_=o_[:, j * 128:(j + 1) * 128], identity=c["ident_b"][:]),
                 reads=[bo, c["b_ident"]], writes=[bpt2])
        S.op("act", lambda e, os_=os_, pt2=pt2, ti=ti: e.activation(out=os_[:, :, (ti % 4) * 128:(ti % 4 + 1) * 128], in_=pt2[:], func=AF.Copy), reads=[bpt2], writes=[bos])
        if ti % 4 == 3:
            g0 = (ti // 4) * 512
            S.dma("sp", lambda e, os_=os_, g0=g0: e.dma_start(out=YBTv[:, :, g0:g0 + 512], in_=os_[:]), reads=[bos])


def cast_load(k, S, dst_bf, bdst, src_ap_fn, nk, ncols, stg, step=256):
    for i, c0 in enumerate(range(0, ncols, step)):
        n = min(step, ncols - c0)
        st, bst = stg.next()
        S.dma("sp" if i % 2 == 0 else "act", lambda e, st=st, c0=c0, n=n: e.dma_start(out=st[:, 0:nk, 0:n], in_=src_ap_fn(c0, n)), writes=[bst])
        if i % 2 == 0:
            S.op("act", lambda e, st=st, c0=c0, n=n: e.activation(out=dst_bf[:, 0:nk, c0:c0 + n], in_=st[:, 0:nk, 0:n], func=AF.Copy), reads=[bst], writes=[bdst])
        else:
            S.op("pool", lambda e, st=st, c0=c0, n=n: e.tensor_copy(out=dst_bf[:, 0:nk, c0:c0 + n], in_=st[:, 0:nk, 0:n]), reads=[bst], writes=[bdst])


def phase4a(k, proj_a, proj_b):
    cfg, S, nc, c, sc = k.cfg, k.S, k.nc, k.c, k.sc
    DM, KC, NOWN = cfg.DM, cfg.KC, cfg.NOWN
    sc["MT"] = k.scratch("MT", [DM, NOWN], BF16)
    k.phase()
    PA = k.sb("PAb", [128, 8, DM], BF16); PB = k.sb("PBb", [128, 8, DM], BF16); bPA = Buf(); bPB = Buf()
    stg = Ring([k.sb(f"p4stg{i}", [128, 8, 256], F32) for i in range(2)])
    pav = proj_a.rearrange("(c p) n -> p c n", p=128); pbv = proj_b.rearrange("(c p) n -> p c n", p=128)
    cast_load(k, S, PA, bPA, lambda c0, n: pav[:, :, c0:c0 + n], 8, DM, stg)
    cast_load(k, S, PB, bPB, lambda c0, n: pbv[:, :, c0:c0 + n], 8, DM, stg)
    YATv = sc["YAT"].rearrange("(c p) t -> p c t", p=128); YBTv = sc["YBT"].rearrange("(c p) t -> p c t", p=128)
    ya = Ring([k.sb(f"p4ya{i}", [128, 8, 512], BF16) for i in range(2)])
    yb = Ring([k.sb(f"p4yb{i}", [128, 8, 512], BF16) for i in range(2)])
    ga = Ring([k.sb(f"p4ga{i}", [128, 512], BF16) for i in range(3)])
    gb = Ring([k.sb(f"p4gb{i}", [128, 512], BF16) for i in range(3)])
    t1 = Ring([k.sb(f"p4t1{i}", [128, 512], F32) for i in range(2)])
    t2 = Ring([k.sb(f"p4t2{i}", [128, 512], F32) for i in range(2)])
    mo = Ring([k.sb(f"p4mo{i}", [128, 512], BF16) for i in range(3)])
    psa = Ring([k.ps(f"p4pa{i}", [128, 512], F32) for i in range(2)])
    psb = Ring([k.ps(f"p4pb{i}", [128, 512], F32) for i in range(2)])
    for tt in range(NOWN // 512):
        t0 = tt * 512
        a_, ba = ya.next(); b_, bb = yb.next()
        S.dma("sp", lambda e, a_=a_, t0=t0: e.dma_start(out=a_[:], in_=YATv[:, :, t0:t0 + 512]), writes=[ba])
        S.dma("act", lambda e, b_=b_, t0=t0: e.dma_start(out=b_[:], in_=YBTv[:, :, t0:t0 + 512]), writes=[bb])
        for i in range(KC):
            g1, bg1 = ga.next(); g2, bg2 = gb.next()
            S.dma("sp", lambda e, g1=g1, i=i, t0=t0: e.dma_start(out=g1[:], in_=sc["GA"][i * 128:(i + 1) * 128, t0:t0 + 512]), writes=[bg1])
            S.dma("act", lambda e, g2=g2, i=i, t0=t0: e.dma_start(out=g2[:], in_=sc["GB"][i * 128:(i + 1) * 128, t0:t0 + 512]), writes=[bg2])
            p1, bp1 = psa.next(); p2, bp2 = psb.next()
            for kc in range(8):
                S.op("pe", lambda e, p1=p1, a_=a_, kc=kc, i=i: e.matmul(p1[:, :], lhsT=PA[:, kc, i * 128:(i + 1) * 128], rhs=a_[:, kc, :],
                                                                       start=(kc == 0), stop=(kc == 7)), reads=[bPA, ba], writes=[bp1])
            for kc in range(8):
                S.op("pe", lambda e, p2=p2, b_=b_, kc=kc, i=i: e.matmul(p2[:, :], lhsT=PB[:, kc, i * 128:(i + 1) * 128], rhs=b_[:, kc, :],
                                                                       start=(kc == 0), stop=(kc == 7)), reads=[bPB, bb], writes=[bp2])
            x1, bx1 = t1.next(); x2, bx2 = t2.next(); m_, bm = mo.next()
            S.op("dve", lambda e, x1=x1, p1=p1, g1=g1: e.tensor_tensor(out=x1[:], in0=p1[:, :], in1=g1[:], op=ALU.mult), reads=[bp1, bg1], writes=[bx1])
            S.op("dve", lambda e, x2=x2, p2=p2, g2=g2: e.tensor_tensor(out=x2[:], in0=p2[:, :], in1=g2[:], op=ALU.mult), reads=[bp2, bg2], writes=[bx2])
            S.op("pool", lambda e, m_=m_, x1=x1, x2=x2: e.tensor_tensor(out=m_[:], in0=x1[:], in1=x2[:], op=ALU.add), reads=[bx1, bx2], writes=[bm])
            S.dma("pool", lambda e, m_=m_, i=i, t0=t0: e.dma_start(out=sc["MT"][i * 128:(i + 1) * 128, t0:t0 + 512], in_=m_[:]), reads=[bm])


def phase4b(k, xe, w_out, prm):
    cfg, S, nc, c, sc = k.cfg, k.S, k.nc, k.c, k.sc
    DM, KC, NOWN, NPRE, NE, CAP = cfg.DM, cfg.KC, cfg.NOWN, cfg.NPRE, cfg.NE, cfg.CAP
    NT = NOWN // 128
    sc["H"] = k.scratch("H", [NOWN, DM], F32)
    sc["XE"] = k.scratch("XE", [NE * CAP, DM], BF16)
    k.phase()
    WO = k.sb("WOb", [128, KC, DM], BF16); bWO = Buf()
    stg = Ring([k.sb(f"p4bstg{i}", [128, KC, 128], F32) for i in range(2)])
    wov = w_out.rearrange("(c p) n -> p c n", p=128)
    cast_load(k, S, WO, bWO, lambda c0, n: wov[:, :, c0:c0 + n], KC, DM, stg, step=128)
    lw = k.sb("ln1w", [128, DM], F32); lb = k.sb("ln1b", [128, DM], F32); b_l = Buf()
    S.dma("sp", lambda e: e.dma_start(out=lw[:], in_=prm["ln1_w"][0:1, :].partition_broadcast(128)), writes=[b_l])
    S.dma("sp", lambda e: e.dma_start(out=lb[:], in_=prm["ln1_b"][0:1, :].partition_broadcast(128)), writes=[b_l])
    wr = k.sb("wr", [128, KC, NE], F32); b_wr = Buf()
    S.dma("sp", lambda e: e.dma_start(out=wr[:], in_=prm["w_router"].rearrange("(c p) n -> p c n", p=128)), writes=[b_wr])
    brt = k.sb("brt", [128, NE], F32)
    S.dma("sp", lambda e: e.dma_start(out=brt[:], in_=prm["b_router"][0:1, :].partition_broadcast(128)), writes=[b_wr])
    iot = k.sb("iot", [128, NE], F32); usf = k.sb("usf", [128, 128], F32); usb = k.sb("usb", [128, 128], BF16)
    onb = k.sb("onb", [128, 128], BF16); b_cst = Buf()
    S.dma("sp", lambda e: e.dma_start(out=iot[:], in_=prm["iota32"][:, :]), writes=[b_cst])
    S.dma("sp", lambda e: e.dma_start(out=usf[:], in_=prm["ustrict"][:, :]), writes=[b_cst])
    S.op("act", lambda e: e.activation(out=usb[:], in_=usf[:], func=AF.Copy), reads=[b_cst], writes=[b_cst])
    S.op("pool", lambda e: e.memset(onb[:], 1.0), writes=[b_cst])
    base = k.sb("rbase", [128, NE], F32); b_base = Buf()
    S.op("pool", lambda e: e.memset(base[:], 0.0), writes=[b_base])
    zt = k.sb("zt", [128, DM], BF16); b_zt = Buf()
    S.op("pool", lambda e: e.memset(zt[:], 0.0), writes=[b_zt])
    b_XE = Buf()
    for r0 in range(0, NE * CAP, 128):
        S.dma("sp" if (r0 // 128) % 2 == 0 else "act", lambda e, r0=r0: e.dma_start(out=sc["XE"][r0:r0 + 128, :], in_=zt[:]), reads=[b_zt], writes=[b_XE])
    MTv = sc["MT"].rearrange("(c p) t -> p c t", p=128)
    mt = Ring([k.sb(f"p4mt{i}", [128, KC, 128], BF16) for i in range(2)])
    xt = Ring([k.sb(f"p4xt{i}", [128, DM], F32) for i in range(1)])
    zz = Ring([k.sb(f"p4z{i}", [128, DM], F32) for i in range(2)])
    hb = Ring([k.sb(f"p4hb{i}", [128, DM], BF16) for i in range(2)])
    hT = Ring([k.sb(f"p4hT{i}", [128, KC, 128], F32) for i in range(1)])
    sm = Ring([k.sb(f"p4sm{i}", [128, 256], F32) for i in range(2)])
    oh = Ring([k.sb(f"p4oh{i}", [128, 4, NE], F32) for i in range(2)])
    pr = Ring([k.sb(f"p4pr{i}", [128, 4, NE], F32) for i in range(2)])
    selb = Ring([k.sb(f"p4selb{i}", [128, NE], BF16) for i in range(2)])
    bst = Ring([k.sb(f"p4bst{i}", [128, 4, 6], F32) for i in range(2)])
    pz = Ring([k.ps(f"p4pz{i}", [128, 512], F32) for i in range(3)])
    pT = Ring([k.ps(f"p4T{i}", [128, 4, 128], F32) for i in range(2)])
    pl = Ring([k.ps("p4l", [128, 128], F32)])
    for ti in range(NT):
        tok0 = ti * 128
        m_, bm = mt.next(); x_, bx = xt.next(); z_, bz = zz.next()
        S.dma("sp", lambda e, m_=m_, tok0=tok0: e.dma_start(out=m_[:], in_=MTv[:, :, tok0:tok0 + 128]), writes=[bm])
        S.dma("act", lambda e, x_=x_, tok0=tok0: e.dma_start(out=x_[:], in_=xe[NPRE + tok0:NPRE + tok0 + 128, :]), writes=[bx])
        s_, bs = bst.next()
        ncg = DM // 512 if DM >= 512 else 1
        cw = DM // ncg
        for cg in range(ncg):
            p_, bp = pz.next()
            for kc in range(KC):
                S.op("pe", lambda e, p_=p_, m_=m_, kc=kc, cg=cg: e.matmul(p_[:, 0:cw], lhsT=m_[:, kc, :], rhs=WO[:, kc, cg * cw:(cg + 1) * cw],
                                                                         start=(kc == 0), stop=(kc == KC - 1)), reads=[bm, bWO], writes=[bp])
            S.op("dve", lambda e, z_=z_, x_=x_, p_=p_, cg=cg: e.scalar_tensor_tensor(out=z_[:, cg * cw:(cg + 1) * cw], in0=x_[:, cg * cw:(cg + 1) * cw], scalar=float(cfg.alpha),
                                                                                   in1=p_[:, 0:cw], op0=ALU.mult, op1=ALU.add), reads=[bx, bp], writes=[bz])
            S.op("dve", lambda e, s_=s_, z_=z_, cg=cg: e.bn_stats(out=s_[:, cg, :], in_=z_[:, cg * cw:(cg + 1) * cw]), reads=[bz], writes=[bs])
        q_, bq = sm.next()
        S.op("dve", lambda e, q_=q_, s_=s_: e.bn_aggr(out=q_[:, 0:2], in_=s_[:, 0:ncg, :].rearrange("p a b -> p (a b)")), reads=[bs], writes=[bq])
        S.op("dve", lambda e, q_=q_: e.tensor_scalar(out=q_[:, 2:3], in0=q_[:, 1:2], scalar1=1e-5, scalar2=None, op0=ALU.add), reads=[bq], writes=[bq])
        S.op("act", lambda e, q_=q_: e.activation(out=q_[:, 3:4], in_=q_[:, 2:3], func=AF.Sqrt), reads=[bq], writes=[bq])
        S.op("dve", lambda e, q_=q_: e.reciprocal(out=q_[:, 4:5], in_=q_[:, 3:4]), reads=[bq], writes=[bq])
        S.op("dve", lambda e, z_=z_, q_=q_: e.tensor_scalar(out=z_[:], in0=z_[:], scalar1=q_[:, 0:1], scalar2=q_[:, 4:5], op0=ALU.subtract, op1=ALU.mult),
             reads=[bz, bq], writes=[bz])
        S.op("pool", lambda e, z_=z_: e.tensor_tensor(out=z_[:], in0=z_[:], in1=lw[:], op=ALU.mult), reads=[bz, b_l], writes=[bz])
        S.op("dve", lambda e, z_=z_: e.tensor_tensor(out=z_[:], in0=z_[:], in1=lb[:], op=ALU.add), reads=[bz, b_l], writes=[bz])
        S.dma("sp", lambda e, z_=z_, tok0=tok0: e.dma_start(out=sc["H"][tok0:tok0 + 128, :], in_=z_[:]), reads=[bz])
        h_, bh = hb.next()
        S.op("act", lambda e, h_=h_, z_=z_: e.activation(out=h_[:], in_=z_[:], func=AF.Copy), reads=[bz], writes=[bh])
        t_, bt = hT.next()
        for g0 in range(0, KC, 4):
            ng = min(4, KC - g0)
            pt, bpt = pT.next()
            for j in range(ng):
                S.op("pe", lambda e, pt=pt, z_=z_, j=j, g0=g0: e.transpose(out=pt[:, j, :], in_=z_[:, (g0 + j) * 128:(g0 + j + 1) * 128], identity=c["ident_f"][:]),
                     reads=[bz, c["b_ident"]], writes=[bpt])
            S.op("act", lambda e, t_=t_, pt=pt, g0=g0, ng=ng: e.activation(out=t_[:, g0:g0 + ng, :], in_=pt[:, 0:ng, :], func=AF.Copy), reads=[bpt], writes=[bt])
        pl_, bpl = pl.next()
        for kc in range(KC):
            S.op("pe", lambda e, pl_=pl_, t_=t_, kc=kc: e.matmul(pl_[:, 0:NE], lhsT=t_[:, kc, :], rhs=wr[:, kc, :], start=(kc == 0), stop=(kc == KC - 1)),
                 reads=[bt, b_wr], writes=[bpl])
        lg = q_[:, 8:8 + NE]
        S.op("dve", lambda e, lg=lg, pl_=pl_: e.tensor_tensor(out=lg, in0=pl_[:, 0:NE], in1=brt[:], op=ALU.add), reads=[bpl, b_wr], writes=[bq])
        top = q_[:, 48:56]
        S.op("dve", lambda e, top=top, lg=lg: e.max(out=top, in_=lg), reads=[bq], writes=[bq])
        S.op("dve", lambda e, q_=q_: e.tensor_scalar(out=q_[:, 56:57], in0=q_[:, 48:49], scalar1=-1.0, scalar2=None, op0=ALU.mult), reads=[bq], writes=[bq])
        S.op("act", lambda e, q_=q_: e.activation(out=q_[:, 60:64], in_=q_[:, 48:52], func=AF.Exp, bias=q_[:, 56:57]), reads=[bq], writes=[bq])
        S.op("dve", lambda e, q_=q_: e.tensor_reduce(out=q_[:, 57:58], in_=q_[:, 60:64], axis=AX.X, op=ALU.add), reads=[bq], writes=[bq])
        S.op("dve", lambda e, q_=q_: e.reciprocal(out=q_[:, 58:59], in_=q_[:, 57:58]), reads=[bq], writes=[bq])
        S.op("dve", lambda e, q_=q_, ti=ti: e.tensor_scalar(out=k.gate_all[:, ti, :], in0=q_[:, 60:64], scalar1=q_[:, 58:59], scalar2=None, op0=ALU.mult),
             reads=[bq], writes=[k.b_gate])
        o_, bo = oh.next()
        for kk_ in range(4):
            S.op("dve" if kk_ % 2 == 0 else "pool", lambda e, o_=o_, lg=lg, q_=q_, kk_=kk_: e.tensor_scalar(
                out=o_[:, kk_, :], in0=lg, scalar1=q_[:, 48 + kk_:49 + kk_], scalar2=None, op0=ALU.is_equal), reads=[bq], writes=[bo])
        sel = q_[:, 64:64 + NE]
        S.op("dve", lambda e, sel=sel, o_=o_: e.tensor_reduce(out=sel, in_=o_[:].rearrange("p k e -> p e k"), axis=AX.X, op=ALU.add), reads=[bo], writes=[bq])
        sb_, bsb = selb.next()
        S.op("act", lambda e, sb_=sb_, sel=sel: e.activation(out=sb_[:], in_=sel, func=AF.Copy), reads=[bq], writes=[bsb])
        pl2, bpl2 = pl.next()
        S.op("pe", lambda e, pl2=pl2, sb_=sb_: e.matmul(pl2[:, 0:NE], lhsT=usb[:, :], rhs=sb_[:, :], start=True, stop=True), reads=[b_cst, bsb], writes=[bpl2])
        S.op("pe", lambda e, pl2=pl2, sb_=sb_: e.matmul(pl2[:, 64:64 + NE], lhsT=onb[:, :], rhs=sb_[:, :], start=True, stop=True), reads=[b_cst, bsb], writes=[bpl2])
        pos = q_[:, 96:96 + NE]
        S.op("dve", lambda e, pos=pos, pl2=pl2: e.tensor_tensor(out=pos, in0=pl2[:, 0:NE], in1=base[:], op=ALU.add), reads=[bpl2, b_base], writes=[bq])
        S.op("dve", lambda e, pl2=pl2: e.tensor_tensor(out=base[:], in0=pl2[:, 64:64 + NE], in1=base[:], op=ALU.add), reads=[bpl2, b_base, bq], writes=[b_base])
        p_r, bpr = pr.next()
        bck = lambda ap: ap.unsqueeze(1).to_broadcast([128, 4, NE])
        S.op("pool", lambda e, p_r=p_r, o_=o_: e.tensor_tensor(out=p_r[:], in0=o_[:], in1=bck(iot[:, :]), op=ALU.mult), reads=[bo, b_cst], writes=[bpr])
        S.op("dve", lambda e, q_=q_, p_r=p_r: e.tensor_reduce(out=q_[:, 128:132], in_=p_r[:], axis=AX.X, op=ALU.add), reads=[bpr], writes=[bq])
        p_r2, bpr2 = pr.next()
        S.op("pool", lambda e, p_r2=p_r2, o_=o_, pos=pos: e.tensor_tensor(out=p_r2[:], in0=o_[:], in1=bck(pos), op=ALU.mult), reads=[bo, bq], writes=[bpr2])
        S.op("dve", lambda e, q_=q_, p_r2=p_r2: e.tensor_reduce(out=q_[:, 132:136], in_=p_r2[:], axis=AX.X, op=ALU.add), reads=[bpr2], writes=[bq])
        S.op("dve", lambda e, q_=q_: e.scalar_tensor_tensor(out=q_[:, 136:140], in0=q_[:, 128:132], scalar=float(CAP), in1=q_[:, 132:136], op0=ALU.mult, op1=ALU.add),
             reads=[bq], writes=[bq])
        S.op("dve", lambda e, q_=q_: e.tensor_scalar(out=q_[:, 140:144], in0=q_[:, 132:136], scalar1=float(CAP), scalar2=1.0e7, op0=ALU.is_ge, op1=ALU.mult),
             reads=[bq], writes=[bq])
        S.op("dve", lambda e, q_=q_: e.tensor_tensor(out=q_[:, 144:148], in0=q_[:, 136:140], in1=q_[:, 140:144], op=ALU.add), reads=[bq], writes=[bq])
        S.op("dve", lambda e, q_=q_, ti=ti: e.tensor_copy(out=k.dest_all[:, ti, :], in_=q_[:, 144:148]), reads=[bq], writes=[k.b_dest])
        for kk_ in range(4):
            S.dma("pool", lambda e, h_=h_, ti=ti, kk_=kk_: e.indirect_dma_start(
                out=sc["XE"][:, :], out_offset=bass.IndirectOffsetOnAxis(ap=k.dest_all[:, ti, kk_:kk_ + 1], axis=0),
                in_=h_[:, :], in_offset=None, bounds_check=_bound_reg(e, NE * CAP - 1), oob_is_err=False), reads=[bh, k.b_dest, b_XE])


def phase5(k, w_gu, b_gu, w_down, b_down):
    cfg, S, nc, c, sc = k.cfg, k.S, k.nc, k.c, k.sc
    DM, KC, NE, CAP, FF = cfg.DM, cfg.KC, cfg.NE, cfg.CAP, cfg.FF
    FC = FF // 128
    NS = CAP // 128
    nhalf = 2 if CAP > 512 else 1
    HALF = CAP // nhalf
    GW = min(512, FF)
    DW = min(512, DM)
    KH = max(1, KC // 2)
    sc["YE"] = k.scratch("YE", [NE * CAP, DM], F32)
    k.phase()
    bgT = k.bgT; b_bgT = k.b_bgT
    bgf = k.sb("bgf", [NE, 2 * FF], F32); b_bgf = Buf()
    S.dma("sp", lambda e: e.dma_start(out=bgf[:], in_=b_gu[:, :]), writes=[b_bgf])
    pTf = Ring([k.ps("p5Tf", [128, 4, NE], F32)])
    for g0 in range(0, 2 * FC, 4):
        pt, bpt = pTf.next()
        for j in range(4):
            S.op("pe", lambda e, pt=pt, j=j, g0=g0: e.transpose(out=pt[:, j, :], in_=bgf[:, (g0 + j) * 128:(g0 + j + 1) * 128], identity=c["ident_f"][0:NE, 0:NE]),
                 reads=[b_bgf, c["b_ident"]], writes=[bpt])
        S.op("act", lambda e, pt=pt, g0=g0: e.activation(out=bgT[:, g0:g0 + 4, :], in_=pt[:], func=AF.Copy), reads=[bpt], writes=[b_bgT])
    k.phase()
    xs = Ring([k.sb(f"p5xs{i}", [128, NS, DM], BF16) for i in range(1)])
    XT = Ring([k.sb(f"p5XT{i}", [128, KC, CAP], BF16) for i in range(1)])
    HT = Ring([k.sb(f"p5HT{i}", [128, FC, CAP], BF16) for i in range(1)])
    KP = max(1, KC // 4)
    NPK = KC // KP
    ws = Ring([k.sb(f"p5ws{i}", [128, KP, GW], F32) for i in range(5)])
    wb = [k.sb(f"p5wb{i}", [128, KC, 2 * GW], BF16) for i in range(2)]
    b_wb = [[Buf() for _ in range(2 * NPK)] for _ in range(2)]
    bd = Ring([k.sb(f"p5bd{i}", [128, DM], F32) for i in range(2)])
    tg = Ring([k.sb(f"p5tg{i}", [128, HALF], F32) for i in range(2)])
    tsg = Ring([k.sb(f"p5ts{i}", [128, HALF], F32) for i in range(2)])
    tu = Ring([k.sb(f"p5tu{i}", [128, HALF], F32) for i in range(2)])
    tgs = Ring([k.sb(f"p5tgs{i}", [128, HALF], F32) for i in range(2)])
    yst = Ring([k.sb(f"p5yst{i}", [128, DW], F32) for i in range(4)])
    pT = Ring([k.ps(f"p5T{i}", [128, 8, 128], BF16) for i in range(2)])
    pg = Ring([k.ps(f"p5g{i}", [128, 512], F32) for i in range(2)])
    pu = Ring([k.ps(f"p5u{i}", [128, 512], F32) for i in range(2)])
    py = Ring([k.ps(f"p5y{i}", [128, 512], F32) for i in range(2)])
    wgv = w_gu.rearrange("e (c p) n -> e p c n", p=128)
    wdv = w_down.rearrange("e (c p) n -> e p c n", p=128)
    cnt = [0]
    nblk = 2 * GW // DW
    groups = []
    for ex in range(NE):
        for gw in range(FF // GW):
            groups.append(("gu", ex, gw))
        for cb0 in range(0, DM // DW, nblk):
            groups.append(("dn", ex, cb0))

    def load_piece(wb_, bw_piece, src_fn, kc0, dcol, ncol):
        w_, bw = ws.next()
        cnt[0] += 1
        S.dma("sp", lambda e, w_=w_: e.dma_start(out=w_[:, 0:KP, 0:ncol], in_=src_fn()), writes=[bw])
        if cnt[0] % 2 == 0:
            S.op("act", lambda e, w_=w_, wb_=wb_: e.activation(out=wb_[:, kc0:kc0 + KP, dcol:dcol + ncol], in_=w_[:, 0:KP, 0:ncol], func=AF.Copy),
                 reads=[bw], writes=[bw_piece])
        else:
            S.op("dve", lambda e, w_=w_, wb_=wb_: e.tensor_copy(out=wb_[:, kc0:kc0 + KP, dcol:dcol + ncol], in_=w_[:, 0:KP, 0:ncol]),
                 reads=[bw], writes=[bw_piece])

    def piece_loads(gi):
        kind, ex, idx = groups[gi]
        wb_, bl = wb[gi % 2], b_wb[gi % 2]
        out = []
        if kind == "gu":
            for part in range(2):
                for kc0 in range(0, KC, KP):
                    c0 = part * FF + idx * GW
                    out.append((wb_, bl[part * NPK + kc0 // KP], (lambda ex=ex, kc0=kc0, c0=c0: wgv[ex, :, kc0:kc0 + KP, c0:c0 + GW]), kc0, part * GW, GW))
        else:
            nb = min(nblk, DM // DW - idx)
            for bi in range(nb):
                for kc0 in range(0, FC, KP):
                    c0 = (idx + bi) * DW
                    out.append((wb_, bl[bi * NPK + kc0 // KP], (lambda ex=ex, kc0=kc0, c0=c0: wdv[ex, :, kc0:kc0 + KP, c0:c0 + DW]), kc0, bi * DW, DW))
        return out

    x_, bx = xs.next(); xt, bxt = XT.next(); ht, bht = HT.next()
    cur = {}

    def xload(ex):
        S.dma("act", lambda e, ex=ex: e.dma_start(out=x_[:], in_=sc["XE"][ex * CAP:(ex + 1) * CAP, :].rearrange("(s p) d -> p s d", p=128)), writes=[bx])

    def preamble(ex):
        for s in range(NS):
            for g0 in range(0, KC, 8):
                ng = min(8, KC - g0)
                pt, bpt = pT.next()
                for j in range(ng):
                    S.op("pe", lambda e, pt=pt, s=s, j=j, g0=g0: e.transpose(out=pt[:, j, :], in_=x_[:, s, (g0 + j) * 128:(g0 + j + 1) * 128], identity=c["ident_b"][:]),
                         reads=[bx, c["b_ident"]], writes=[bpt])
                S.op("act", lambda e, pt=pt, s=s, g0=g0, ng=ng: e.activation(out=xt[:, g0:g0 + ng, s * 128:(s + 1) * 128], in_=pt[:, 0:ng, :], func=AF.Copy),
                     reads=[bpt], writes=[bxt])
        cur["b"] = bd.next()
        b_, bb = cur["b"]
        S.dma("act", lambda e, b_=b_, ex=ex: e.dma_start(out=b_[:], in_=b_down[ex:ex + 1, :].partition_broadcast(128)), writes=[bb])
        if ex + 1 < NE:
            xload(ex + 1)

    def gu_unit(ex, gw, fl, hf, wb_, bl):
        fb = gw * (GW // 128) + fl
        sl = slice(hf * HALF, (hf + 1) * HALF)
        pg_, bpg = pg.next(); pu_, bpu = pu.next()
        for kc in range(KC):
            S.op("pe", lambda e, pg_=pg_, kc=kc: e.matmul(pg_[:, 0:HALF], lhsT=wb_[:, kc, fl * 128:(fl + 1) * 128], rhs=xt[:, kc, sl],
                                                        start=(kc == 0), stop=(kc == KC - 1)), reads=[bl[kc // KP], bxt], writes=[bpg])
        for kc in range(KC):
            S.op("pe", lambda e, pu_=pu_, kc=kc: e.matmul(pu_[:, 0:HALF], lhsT=wb_[:, kc, GW + fl * 128:GW + (fl + 1) * 128], rhs=xt[:, kc, sl],
                                                        start=(kc == 0), stop=(kc == KC - 1)), reads=[bl[NPK + kc // KP], bxt], writes=[bpu])
        g_, bg = tg.next(); s_, bs = tsg.next(); u_, bu = tu.next(); gs_, bgs = tgs.next()
        S.op("dve", lambda e: e.tensor_scalar(out=g_[:], in0=pg_[:, 0:HALF], scalar1=bgT[:, fb, ex:ex + 1], scalar2=7.0,
                                              op0=ALU.add, op1=ALU.min), reads=[bpg, b_bgT], writes=[bg])
        S.op("act", lambda e: e.activation(out=s_[:], in_=g_[:], func=AF.Sigmoid, scale=1.702), reads=[bg], writes=[bs])
        S.op("dve", lambda e: e.tensor_scalar(out=u_[:], in0=pu_[:, 0:HALF], scalar1=bgT[:, FC + fb, ex:ex + 1], scalar2=7.0,
                                              op0=ALU.add, op1=ALU.min), reads=[bpu, b_bgT], writes=[bu])
        S.op("dve", lambda e: e.tensor_scalar(out=u_[:], in0=u_[:], scalar1=-7.0, scalar2=1.0, op0=ALU.max, op1=ALU.add), reads=[bu], writes=[bu])
        S.op("pool", lambda e: e.tensor_tensor(out=gs_[:], in0=g_[:], in1=s_[:], op=ALU.mult), reads=[bg, bs], writes=[bgs])
        S.op("pool", lambda e: e.tensor_tensor(out=ht[:, fb, sl], in0=gs_[:], in1=u_[:], op=ALU.mult), reads=[bgs, bu], writes=[bht])

    def dn_unit(ex, cb, bi, s, wb_, bl):
        b_, bb = cur["b"]
        py_, bpy = py.next()
        for fc in range(FC):
            S.op("pe", lambda e, fc=fc: e.matmul(py_[:, 0:DW], lhsT=ht[:, fc, s * 128:(s + 1) * 128], rhs=wb_[:, fc, bi * DW:(bi + 1) * DW],
                                               start=(fc == 0), stop=(fc == FC - 1)), reads=[bht, bl[bi * NPK + fc // KP]], writes=[bpy])
        y_, by = yst.next()
        S.op("dve", lambda e: e.tensor_tensor(out=y_[:], in0=py_[:, 0:DW], in1=b_[:, cb * DW:(cb + 1) * DW], op=ALU.add), reads=[bpy, bb], writes=[by])
        r0 = ex * CAP + s * 128
        S.dma("pool", lambda e: e.dma_start(out=sc["YE"][r0:r0 + 128, cb * DW:(cb + 1) * DW], in_=y_[:]), reads=[by])

    def make_units(gi):
        kind, ex, idx = groups[gi]
        wb_, bl = wb[gi % 2], b_wb[gi % 2]
        if kind == "gu":
            return [(lambda fl=fl, hf=hf: gu_unit(ex, idx, fl, hf, wb_, bl)) for fl in range(GW // 128) for hf in range(nhalf)]
        nb = min(nblk, DM // DW - idx)
        return [(lambda bi=bi, s=s: dn_unit(ex, idx + bi, bi, s, wb_, bl)) for bi in range(nb) for s in range(NS)]

    xload(0)
    for ld in piece_loads(0):
        load_piece(*ld)
    for gi in range(len(groups)):
        kind, ex, idx = groups[gi]
        nxt = piece_loads(gi + 1) if gi + 1 < len(groups) else []
        units = make_units(gi)
        if kind == "gu" and idx == 0:
            preamble(ex)
        nu = len(units)
        for ui, u in enumerate(units):
            for ld in nxt[ui * len(nxt) // nu:(ui + 1) * len(nxt) // nu]:
                load_piece(*ld)
            u()


def phase6(k, out, prm):
    cfg, S, nc, c, sc = k.cfg, k.S, k.nc, k.c, k.sc
    DM, NOWN, NE, CAP = cfg.DM, cfg.NOWN, cfg.NE, cfg.CAP
    NT = NOWN // 128
    k.phase()
    lw = k.sb("ln2w", [128, DM], F32); lb = k.sb("ln2b", [128, DM], F32); b_l = Buf()
    S.dma("sp", lambda e: e.dma_start(out=lw[:], in_=prm["ln2_w"][0:1, :].partition_broadcast(128)), writes=[b_l])
    S.dma("sp", lambda e: e.dma_start(out=lb[:], in_=prm["ln2_b"][0:1, :].partition_broadcast(128)), writes=[b_l])
    hh = Ring([k.sb(f"p6h{i}", [128, DM], F32) for i in range(2)])
    yk = Ring([k.sb(f"p6y{i}", [128, DM], F32) for i in range(4)])
    sm = Ring([k.sb(f"p6sm{i}", [128, 16], F32) for i in range(2)])
    bst = Ring([k.sb(f"p6bst{i}", [128, 4, 6], F32) for i in range(2)])
    ncg = DM // 512 if DM >= 512 else 1
    cw = DM // ncg
    for ti in range(NT):
        tok0 = ti * 128
        h_, bh = hh.next()
        S.dma("sp", lambda e, h_=h_, tok0=tok0: e.dma_start(out=h_[:], in_=sc["H"][tok0:tok0 + 128, :]), writes=[bh])
        S.op("dve", lambda e, h_=h_: e.tensor_scalar(out=h_[:], in0=h_[:], scalar1=float(cfg.alpha), scalar2=None, op0=ALU.mult), reads=[bh], writes=[bh])
        for kk_ in range(4):
            y_, by = yk.next()
            S.op("pool", lambda e, y_=y_: e.memset(y_[:], 0.0), writes=[by])
            S.dma("pool", lambda e, y_=y_, ti=ti, kk_=kk_: e.indirect_dma_start(
                out=y_[:, :], out_offset=None, in_=sc["YE"][:, :], in_offset=bass.IndirectOffsetOnAxis(ap=k.dest_all[:, ti, kk_:kk_ + 1], axis=0),
                bounds_check=_bound_reg(e, NE * CAP - 1), oob_is_err=False), reads=[k.b_dest], writes=[by])
            S.op("dve", lambda e, h_=h_, y_=y_, ti=ti, kk_=kk_: e.scalar_tensor_tensor(out=h_[:], in0=y_[:], scalar=k.gate_all[:, ti, kk_:kk_ + 1], in1=h_[:],
                                                                                     op0=ALU.mult, op1=ALU.add), reads=[by, bh, k.b_gate], writes=[bh])
        s_, bs = bst.next(); q_, bq = sm.next()
        for cg in range(ncg):
            S.op("dve", lambda e, s_=s_, h_=h_, cg=cg: e.bn_stats(out=s_[:, cg, :], in_=h_[:, cg * cw:(cg + 1) * cw]), reads=[bh], writes=[bs])
        S.op("dve", lambda e, q_=q_, s_=s_: e.bn_aggr(out=q_[:, 0:2], in_=s_[:, 0:ncg, :].rearrange("p a b -> p (a b)")), reads=[bs], writes=[bq])
        S.op("dve", lambda e, q_=q_: e.tensor_scalar(out=q_[:, 2:3], in0=q_[:, 1:2], scalar1=1e-5, scalar2=None, op0=ALU.add), reads=[bq], writes=[bq])
        S.op("act", lambda e, q_=q_: e.activation(out=q_[:, 3:4], in_=q_[:, 2:3], func=AF.Sqrt), reads=[bq], writes=[bq])
        S.op("dve", lambda e, q_=q_: e.reciprocal(out=q_[:, 4:5], in_=q_[:, 3:4]), reads=[bq], writes=[bq])
        S.op("dve", lambda e, h_=h_, q_=q_: e.tensor_scalar(out=h_[:], in0=h_[:], scalar1=q_[:, 0:1], scalar2=q_[:, 4:5], op0=ALU.subtract, op1=ALU.mult),
             reads=[bh, bq], writes=[bh])
        S.op("pool", lambda e, h_=h_: e.tensor_tensor(out=h_[:], in0=h_[:], in1=lw[:], op=ALU.mult), reads=[bh, b_l], writes=[bh])
        S.op("dve", lambda e, h_=h_: e.tensor_tensor(out=h_[:], in0=h_[:], in1=lb[:], op=ALU.add), reads=[bh, b_l], writes=[bh])
        S.dma("sp", lambda e, h_=h_, tok0=tok0: e.dma_start(out=out[tok0:tok0 + 128, :], in_=h_[:]), reads=[bh])


HORD = [0, 2, 4, 6, 1, 3, 5, 7, 8, 10, 12, 14, 9, 11, 13, 15]


def build_full(cfg, dbg=()):
    k = K(cfg, dbg=dbg)
    DM, NE = cfg.DM, cfg.NE
    xe = k.inp("xe", [cfg.NTOK, DM])
    w_in = k.inp("w_in", [DM, cfg.DIN])
    mu_cols = k.inp("mu_cols", [128, 28])
    att_bias = k.inp("att_bias", [128, 5, 16, 128]); halo_mask = k.inp("halo_mask", [128, 1])
    prm = {n: k.inp(n, s) for n, s in [
        ("w_up", [96, 1024]), ("a_up", [96, 1024]), ("g_up", [256, 1024]), ("hcols", [64, 6, 16]), ("rmask", [64, 512]),
        ("m_su", [64, 8, 64]), ("m_ui", [64, 8, 64]), ("m_sl", [64, 8, 64]), ("i8", [64, 8, 64]),
        ("lnx_w", [1, 1024]), ("lnx_b", [1, 1024]), ("ln1_w", [1, DM]), ("ln1_b", [1, DM]), ("ln2_w", [1, DM]), ("ln2_b", [1, DM]),
        ("w_router", [DM, NE]), ("b_router", [1, NE]), ("iota32", [128, NE]), ("ustrict", [128, 128])]}
    proj_a = k.inp("proj_a", [1024, DM]); proj_b = k.inp("proj_b", [1024, DM]); w_out = k.inp("w_out", [DM, DM])
    w_gu = k.inp("w_gu", [NE, DM, 2 * cfg.FF]); b_gu = k.inp("b_gu", [NE, 2 * cfg.FF])
    w_down = k.inp("w_down", [NE, cfg.FF, DM]); b_down = k.inp("b_down", [NE, DM])
    out = k.nc.dram_tensor("out", [cfg.NOWN, DM], F32, kind="ExternalOutput").ap()
    load_consts(k)
    phase1(k, xe, w_in, mu_cols)
    phase2(k, att_bias, halo_mask)
    phase3(k, prm)
    phase3b(k, prm)
    phase4a(k, proj_a, proj_b)
    phase4b(k, xe, w_out, prm)
    phase5(k, w_gu, b_gu, w_down, b_down)
    phase6(k, out, prm)
    k.S.finish()
    return k


def host_common(inp, cfg):
    f = lambda a: np.ascontiguousarray(np.asarray(a, dtype=np.float32))
    smu = f(inp["shift_mu"][0])
    m = np.zeros((128, 28), np.float32)
    for ci in range(24):
        m[:, ci] = smu[ci * 128:(ci + 1) * 128]
    m[:96, 24] = smu[3072:3168]; m[:96, 25] = smu[3168:3264]
    m[:, 26] = smu[3264:3392]; m[:, 27] = smu[3392:3520]
    relb = f(inp["rel_bias"][0])
    kr = np.arange(640)[:, None]; q = np.arange(128)[None, :]
    dist = 512 + q - kr
    qc = (512 + q) // 64; kc = kr // 64
    valid = (qc - kc >= 0) & (qc - kc <= 8)
    idx = np.clip(np.minimum(dist, 256) + 63, 0, 319)
    b = relb[:, idx]
    b = np.where(valid[None], b, np.float32(-30000.0)).astype(np.float32)[HORD]
    att_bias = np.ascontiguousarray(b.reshape(16, 5, 128, 128).transpose(2, 1, 0, 3))
    hv = lambda v: f(v).reshape(16, 64).T
    k_a = f(inp["k_a"][0])
    one_minus_ka = np.zeros_like(k_a)
    hcols = np.ascontiguousarray(np.stack([hv(inp["w0"][0]), hv(inp["a0"][0]), hv(inp["k_k"][0]), hv(k_a), hv(one_minus_ka), hv(inp["r_k"][0])], 1))
    rmask = np.ones((64, 512), np.float32); rmask[:, ::64] = 0
    j = np.arange(64)[:, None]; t = np.arange(64)[None, :]
    rep = lambda mm: np.ascontiguousarray(np.repeat(mm.astype(np.float32)[:, None, :], 8, 1))
    r2 = lambda v: f(v).reshape(1, -1)
    com = {
        "w_in": f(inp["w_in"][0]), "mu_cols": m, "att_bias": att_bias, "c_ident": np.eye(128, dtype=np.float32),
        "w_up": f(inp["w_up"][0]), "a_up": f(inp["a_up"][0]), "g_up": f(inp["g_up"][0]), "hcols": hcols, "rmask": rmask,
        "m_su": rep(j < t), "m_ui": rep(j <= t), "m_sl": rep(j > t), "i8": rep(j == t),
        "lnx_w": r2(inp["lnx_w"][0]), "lnx_b": r2(inp["lnx_b"][0]), "ln1_w": r2(inp["ln1_w"][0]), "ln1_b": r2(inp["ln1_b"][0]),
        "ln2_w": r2(inp["ln2_w"][0]), "ln2_b": r2(inp["ln2_b"][0]),
        "w_router": f(inp["w_router"][0]), "b_router": r2(inp["b_router"][0]),
        "iota32": np.ascontiguousarray(np.broadcast_to(np.arange(cfg.NE, dtype=np.float32), (128, cfg.NE))),
        "ustrict": (np.arange(128)[:, None] < np.arange(128)[None, :]).astype(np.float32),
        "proj_a": f(inp["proj_a"][0]), "proj_b": f(inp["proj_b"][0]), "w_out": f(inp["w_out"][0]),
        "w_gu": f(inp["w_gu"][0]), "b_gu": f(inp["b_gu"][0]), "w_down": f(inp["w_down"][0]), "b_down": f(inp["b_down"][0]),
    }
    return com


def host_core(x, cfg, b, half):
    NOWN = cfg.NOWN
    xe = np.zeros((cfg.NTOK, cfg.DM), np.float32)
    if half == 0:
        xe[cfg.NPRE:] = x[b, 0:NOWN]
        hm = np.full((128, 1), -30000.0, np.float32)
    else:
        xe[:] = x[b, 0:2 * NOWN]
        hm = np.zeros((128, 1), np.float32)
    return {"xe": xe, "halo_mask": hm}


_CACHE = {}


def kernel(**inputs):
    cfg = Cfg()
    if "k" not in _CACHE:
        _CACHE["k"] = build_full(cfg)
    k = _CACHE["k"]
    com = host_common(inputs, cfg)
    x = np.asarray(inputs["x"], dtype=np.float32)
    in_maps = []
    for core in range(8):
        m = dict(com)
        m.update(host_core(x, cfg, core // 2, core % 2))
        in_maps.append(m)
    res = run_bass_kernel_spmd(k.nc, in_maps, core_ids=list(range(8)))
    out = np.empty((4, 2 * cfg.NOWN, cfg.DM), np.float32)
    for core in range(8):
        h = core % 2
        out[core // 2, h * cfg.NOWN:(h + 1) * cfg.NOWN] = np.asarray(res.results[core]["out"])
    return out
```

```python
import contextlib
import os
import numpy as np
import concourse.bass as bass
import concourse.mybir as mybir
from concourse.bass_utils import run_bass_kernel_spmd


class Buf:
    __slots__ = ("name", "w", "r")

    def __init__(self, name=""):
        self.name = name
        self.w = None
        self.r = []


class Sched:
    ENGS = ("pe", "act", "dve", "pool", "sp")

    def __init__(self, nc, ndma_sems=10):
        self.nc = nc
        self.prog = {e: [] for e in self.ENGS}
        self.sems = {}
        self.cnt = {}
        self.known = {e: {} for e in self.ENGS}
        self._stack = []
        for e in ("pe", "act", "dve", "pool"):
            self._mksem("E_" + e)
        self.dpool = {}
        for q in ("sp", "pool", "act"):
            names = [f"D_{q}{i}" for i in range(ndma_sems)]
            for n in names:
                self._mksem(n)
            self.dpool[q] = [names, 0]
        self.ninstr = 0

    def _mksem(self, name):
        cm = self.nc.semaphore(name)
        s = cm.__enter__()
        self._stack.append(cm)
        self.sems[name] = s
        self.cnt[name] = 0

    def _wait(self, eng, ev):
        key, val = ev
        if eng == "pe" and key == "E_pe":
            return
        if self.known[eng].get(key, 0) >= val:
            return
        self.known[eng][key] = val
        sem = self.sems[key]
        self.prog[eng].append(lambda e, sem=sem, val=val: e.wait_ge(sem, val))

    def _deps(self, eng, reads, writes):
        for b in reads:
            if b.w is not None:
                self._wait(eng, b.w)
        for b in writes:
            if b.w is not None:
                self._wait(eng, b.w)
            for ev in b.r:
                self._wait(eng, ev)

    def _commit(self, ev, reads, writes):
        for b in writes:
            b.w = ev
            b.r = []
        for b in reads:
            if b.w is ev:
                continue
            b.r.append(ev)
            if len(b.r) > 24:
                last = {}
                for k, v in b.r:
                    if last.get(k, 0) < v:
                        last[k] = v
                b.r = list(last.items())

    def op(self, eng, fn, reads=(), writes=()):
        self._deps(eng, reads, writes)
        key = "E_" + eng
        self.cnt[key] += 1
        val = self.cnt[key]
        sem = self.sems[key]
        self.prog[eng].append(lambda e, fn=fn, sem=sem: fn(e).then_inc(sem, 1))
        self._commit((key, val), reads, writes)
        self.ninstr += 1

    def dma(self, q, fn, reads=(), writes=()):
        names, idx = self.dpool[q]
        name = names[idx % len(names)]
        self.dpool[q][1] = idx + 1
        if self.cnt[name] > 0:
            self._wait(q, (name, self.cnt[name]))
        self._deps(q, reads, writes)
        self.cnt[name] += 16
        val = self.cnt[name]
        sem = self.sems[name]
        self.prog[q].append(lambda e, fn=fn, sem=sem: fn(e).then_inc(sem, 16))
        self._commit((name, val), reads, writes)
        self.ninstr += 1

    def barrier(self):
        for eng in self.ENGS:
            for key, val in self.cnt.items():
                if val > 0:
                    if eng == "pe" and key == "E_pe":
                        continue
                    self._wait(eng, (key, val))

    def finish(self):
        self.barrier()
        nc = self.nc
        with nc.Block() as block:
            def mk(name):
                def f(e):
                    for t in self.prog[name]:
                        t(e)
                return f
            block.tensor(mk("pe"))
            block.scalar(mk("act"))
            block.vector(mk("dve"))
            block.gpsimd(mk("pool"))
            block.sync(mk("sp"))
        for cm in reversed(self._stack):
            cm.__exit__(None, None, None)


F32 = mybir.dt.float32
BF16 = mybir.dt.bfloat16
I32 = mybir.dt.int32
ALU = mybir.AluOpType
AF = mybir.ActivationFunctionType
AX = mybir.AxisListType

AW = 1024
NH = 16
HD = 64
SHIFTW = 3 * AW + 96 + 96 + 256
C0 = float(np.exp(-0.5))


class Cfg:
    def __init__(self, DM=2048, NPRE=4096, NOWN=4096, ST=2048, NE=32, CAP=640, depth_alpha=2 ** 0.25):
        self.DM = DM; self.NPRE = NPRE; self.NOWN = NOWN; self.ST = ST
        self.NE = NE; self.CAP = CAP; self.FF = DM
        self.NTOK = NPRE + NOWN
        self.KC = DM // 128
        self.DIN = 3 * AW + SHIFTW + 2 * DM
        self.alpha = depth_alpha


class K:
    def __init__(self, cfg, dbg=()):
        self.cfg = cfg
        self.nc = bass.Bass("TRN2", target_bir_lowering=False)
        self.S = Sched(self.nc)
        self.dbg = set(dbg)
        self.stack = contextlib.ExitStack()
        self.pstack = None
        self.ins = {}

    def inp(self, name, shape, dt=F32):
        t = self.nc.dram_tensor(name, list(shape), dt, kind="ExternalInput").ap()
        self.ins[name] = t
        return t

    def scratch(self, name, shape, dt):
        kind = "ExternalOutput" if name in self.dbg else "Internal"
        return self.nc.dram_tensor(name, list(shape), dt, kind=kind).ap()

    def phase(self):
        if self.pstack is not None:
            self.S.barrier()
            self.pstack.close()
        self.pstack = contextlib.ExitStack()

    def sb(self, name, shape, dt):
        return self.pstack.enter_context(self.nc.sbuf_tensor(name, list(shape), dt))

    def ps(self, name, shape, dt=F32):
        return self.pstack.enter_context(self.nc.psum_tensor(name, list(shape), dt))

    def gsb(self, name, shape, dt):
        return self.stack.enter_context(self.nc.sbuf_tensor(name, list(shape), dt))


_REGS = {}


def _bound_reg(e, val):
    key = (id(e), val)
    if key not in _REGS:
        _REGS[key] = e.to_reg(val)
    return _REGS[key]


class Ring:
    def __init__(self, tiles):
        self.t = tiles
        self.b = [Buf() for _ in tiles]
        self.i = 0

    def next(self):
        j = self.i % len(self.t)
        self.i += 1
        return self.t[j], self.b[j]


def load_consts(k):
    S = k.S
    c = {}
    ident = k.inp("c_ident", [128, 128])
    c["ident_f"] = k.gsb("ident_f", [128, 128], F32)
    c["ident_b"] = k.gsb("ident_b", [128, 128], BF16)
    c["b_ident"] = Buf()
    S.dma("sp", lambda e: e.dma_start(out=c["ident_f"][:], in_=ident[:, :]), writes=[c["b_ident"]])
    S.op("act", lambda e: e.activation(out=c["ident_b"][:], in_=c["ident_f"][:], func=AF.Copy),
         reads=[c["b_ident"]], writes=[c["b_ident"]])
    k.c = c
    NT = k.cfg.NOWN // 128
    k.dest_all = k.gsb("dest_all", [128, NT, 4], I32); k.b_dest = Buf()
    k.gate_all = k.gsb("gate_all", [128, NT, 4], F32); k.b_gate = Buf()
    k.bgT = k.gsb("bgT", [128, 2 * (k.cfg.FF // 128), k.cfg.NE], F32); k.b_bgT = Buf()


def phase1(k, xe, w_in, mu_cols):
    cfg, S, nc, c = k.cfg, k.S, k.nc, k.c
    DM, KC, ST, NTOK, NPRE, NOWN = cfg.DM, cfg.KC, cfg.ST, cfg.NTOK, cfg.NPRE, cfg.NOWN
    o1, o2 = 3 * AW, 3 * AW + SHIFTW
    HALO = 512
    sc = {}
    sc["QT"] = k.scratch("QT", [AW, NOWN], BF16)
    sc["KT"] = k.scratch("KT", [AW, HALO + NOWN], BF16)
    sc["V"] = k.scratch("V", [HALO + NOWN, NH, 65], BF16)
    sc["RB"] = k.scratch("RB", [AW, NTOK], BF16)
    sc["KB"] = k.scratch("KB", [AW, NTOK], BF16)
    sc["VB"] = k.scratch("VB", [AW, NTOK], BF16)
    sc["WD"] = k.scratch("WD", [96, NTOK], BF16)
    sc["AD"] = k.scratch("AD", [96, NTOK], BF16)
    sc["GD"] = k.scratch("GD", [256, NTOK], BF16)
    sc["GA"] = k.scratch("GA", [DM, NOWN], BF16)
    sc["GB"] = k.scratch("GB", [DM, NOWN], BF16)
    k.sc = sc

    k.phase()
    xT = k.sb("xT", [128, KC, ST], BF16); b_xT = Buf()
    xin = Ring([k.sb(f"xin{i}", [128, DM], F32) for i in range(2)])
    xbf = Ring([k.sb(f"xbf{i}", [128, DM], BF16) for i in range(2)])
    wst = Ring([k.sb(f"wst{i}", [128, KC, 256], F32) for i in range(2)])
    wbf = Ring([k.sb(f"wbf{i}", [128, KC, 256], BF16) for i in range(2)])
    lbuf = [k.sb(f"lbuf{i}", [128, 516], F32) for i in range(2)]
    b_lbuf = [Buf(), Buf()]
    ltmp = Ring([k.sb(f"ltmp{i}", [128, 512], F32) for i in range(2)])
    lseg = Ring([k.sb(f"lseg{i}", [128, 512], F32) for i in range(2)])
    ost = Ring([k.sb(f"ost{i}", [128, 512], BF16) for i in range(4)])
    vst = Ring([k.sb(f"vst{i}", [128, 4, 65], BF16) for i in range(3)])
    carry = k.sb("carry", [128, 32], F32); b_carry = Buf()
    mu = k.sb("mu", [128, 32], F32); b_mu = Buf()
    pst = Ring([k.ps(f"pT{i}", [128, 8, 128], BF16) for i in range(2)])
    psg = Ring([k.ps(f"pg{i}", [128, 512], F32) for i in range(4)])

    S.dma("sp", lambda e: e.dma_start(out=mu[:, 0:28], in_=mu_cols[:, :]), writes=[b_mu])
    S.op("pool", lambda e: e.memset(carry[:], 0.0), writes=[b_carry])
    for t, b in zip(vst.t, vst.b):
        S.op("pool", lambda e, t=t: e.memset(t[:], 1.0), writes=[b])

    blocks = []
    for c0 in range(0, AW, 256):
        blocks.append((c0, 256, "q", sc["QT"], c0, "own", None, None))
    for c0 in range(0, AW, 256):
        blocks.append((AW + c0, 256, "k", sc["KT"], c0, "halo", None, None))
    for c0 in range(0, AW, 256):
        blocks.append((2 * AW + c0, 256, "v", sc["V"], c0 // 64, "halo", None, None))
    for j, nm in enumerate(("RB", "KB", "VB")):
        for c0 in range(0, AW, 256):
            blocks.append((o1 + j * AW + c0, 256, "rw", sc[nm], c0, "all", None, (j * AW + c0) // 128))
    blocks.append((o1 + 3 * AW, 96, "rw", sc["WD"], 0, "all", AF.Tanh, 24))
    blocks.append((o1 + 3 * AW + 96, 96, "rw", sc["AD"], 0, "all", AF.Copy, 25))
    blocks.append((o1 + 3 * AW + 192, 256, "rw", sc["GD"], 0, "all", AF.Sigmoid, 26))
    for c0 in range(0, DM, 256):
        blocks.append((o2 + c0, 256, "gate", sc["GA"], c0, "own", None, None))
    for c0 in range(0, DM, 256):
        blocks.append((o2 + DM + c0, 256, "gate", sc["GB"], c0, "own", None, None))

    w_v = w_in.rearrange("(kc p) c -> p kc c", p=128)
    nST = NTOK // ST
    active = []
    for s in range(nST):
        for bi, blk in enumerate(blocks):
            tts = []
            for tt in range(ST // 512):
                t_ext = s * ST + tt * 512
                if blk[5] == "all" or (blk[5] == "own" and t_ext >= NPRE) or (blk[5] == "halo" and t_ext >= NPRE - HALO):
                    tts.append(tt)
            if tts:
                active.append((s, bi, tts))
    wloaded = {}

    def wload(ai):
        c0, ncols = blocks[active[ai][1]][0:2]
        ws, bws = wst.next()
        wb, bwb = wbf.next()
        S.dma("sp", lambda e: e.dma_start(out=ws[:, :, 0:ncols], in_=w_v[:, :, c0:c0 + ncols]), writes=[bws])
        S.op("pool", lambda e: e.tensor_copy(out=wb[:, :, 0:ncols], in_=ws[:, :, 0:ncols]), reads=[bws], writes=[bwb])
        wloaded[ai] = (wb, bwb)

    wload(0)
    for s in range(nST):
        tok0 = s * ST
        for i in range(ST // 128):
            xi, bxi = xin.next()
            xb, bxb = xbf.next()
            r0 = tok0 + i * 128
            S.dma("sp", lambda e, xi=xi, r0=r0: e.dma_start(out=xi[:], in_=xe[r0:r0 + 128, :]), writes=[bxi])
            S.op("act", lambda e, xi=xi, xb=xb: e.activation(out=xb[:], in_=xi[:], func=AF.Copy),
                 reads=[bxi], writes=[bxb])
            for g0 in range(0, KC, 8):
                ng = min(8, KC - g0)
                pt, bpt = pst.next()
                for j in range(ng):
                    S.op("pe", lambda e, pt=pt, xb=xb, j=j, g0=g0: e.transpose(
                        out=pt[:, j, :], in_=xb[:, (g0 + j) * 128:(g0 + j + 1) * 128], identity=c["ident_b"][:]),
                        reads=[bxb, c["b_ident"]], writes=[bpt])
                S.op("dve", lambda e, pt=pt, g0=g0, ng=ng, i=i: e.tensor_copy(
                    out=xT[:, g0:g0 + ng, i * 128:(i + 1) * 128], in_=pt[:, 0:ng, :]),
                    reads=[bpt], writes=[b_xT])
        own_s = tok0 >= NPRE
        for ai in [a for a in range(len(active)) if active[a][0] == s]:
            _, bi, tts = active[ai]
            c0, ncols, kind, dst, drow0, tokmode, post, rwc = blocks[bi]
            wb, bwb = wloaded.pop(ai)
            if ai + 1 < len(active):
                wload(ai + 1)
            if kind == "v":
                for tt in tts:
                    for sub in range(4):
                        tk = tt * 512 + sub * 128
                        pg, bpg = psg.next()
                        for kc in range(KC):
                            S.op("pe", lambda e, pg=pg, kc=kc, tk=tk, wb=wb: e.matmul(
                                pg[:, 0:256], lhsT=xT[:, kc, tk:tk + 128], rhs=wb[:, kc, 0:256],
                                start=(kc == 0), stop=(kc == KC - 1)), reads=[b_xT, bwb], writes=[bpg])
                        vs, bvs = vst.next()
                        S.op("act", lambda e, vs=vs, pg=pg: e.activation(
                            out=vs[:, :, 0:64], in_=pg[:, 0:256].rearrange("p (h d) -> p h d", d=64), func=AF.Copy),
                            reads=[bpg], writes=[bvs])
                        vrow = tok0 + tk - (NPRE - HALO)
                        S.dma("sp", lambda e, vs=vs, vrow=vrow, drow0=drow0, dst=dst: e.dma_start(
                            out=dst[vrow:vrow + 128, drow0:drow0 + 4, :], in_=vs[:]), reads=[bvs])
                continue
            nch = (ncols + 127) // 128
            for ch in range(nch):
                m = min(128, ncols - ch * 128)
                j0 = ch * 128
                if kind == "rw":
                    rci = rwc + ch
                    p = 0
                    S.op("act", lambda e, p=p, rci=rci, m=m: e.activation(
                        out=lbuf[p][0:m, 0:1], in_=carry[0:m, rci:rci + 1], func=AF.Copy),
                        reads=[b_carry], writes=[b_lbuf[p]])
                for tt in tts:
                    tk = tt * 512
                    pg, bpg = psg.next()
                    for kc in range(KC):
                        S.op("pe", lambda e, pg=pg, kc=kc, tk=tk, wb=wb, j0=j0, m=m: e.matmul(
                            pg[0:m, :], lhsT=wb[:, kc, j0:j0 + m], rhs=xT[:, kc, tk:tk + 512],
                            start=(kc == 0), stop=(kc == KC - 1)), reads=[b_xT, bwb], writes=[bpg])
                    o, bo = ost.next()
                    if kind == "q":
                        S.op("act", lambda e, o=o, pg=pg, m=m: e.activation(
                            out=o[0:m, :], in_=pg[0:m, :], func=AF.Copy, scale=0.125), reads=[bpg], writes=[bo])
                        dcol = tok0 + tk - NPRE
                    elif kind == "k":
                        S.op("act", lambda e, o=o, pg=pg, m=m: e.activation(
                            out=o[0:m, :], in_=pg[0:m, :], func=AF.Copy), reads=[bpg], writes=[bo])
                        dcol = tok0 + tk - (NPRE - HALO)
                    elif kind == "gate":
                        S.op("act", lambda e, o=o, pg=pg, m=m: e.activation(
                            out=o[0:m, :], in_=pg[0:m, :], func=AF.Sigmoid), reads=[bpg], writes=[bo])
                        dcol = tok0 + tk - NPRE
                    else:
                        lb, blb = lbuf[p], b_lbuf[p]
                        S.op("act", lambda e, lb=lb, pg=pg, m=m: e.activation(
                            out=lb[0:m, 1:513], in_=pg[0:m, :], func=AF.Copy), reads=[bpg], writes=[blb])
                        S.op("act", lambda e, lb=lb, p=p, m=m: e.activation(
                            out=lbuf[1 - p][0:m, 0:1], in_=lb[0:m, 512:513], func=AF.Copy),
                            reads=[blb], writes=[b_lbuf[1 - p]])
                        lt, blt = ltmp.next()
                        S.op("dve", lambda e, lt=lt, lb=lb, m=m: e.tensor_tensor(
                            out=lt[0:m, :], in0=lb[0:m, 0:512], in1=lb[0:m, 1:513], op=ALU.subtract),
                            reads=[blb], writes=[blt])
                        if post is None:
                            S.op("dve", lambda e, o=o, lt=lt, lb=lb, m=m, rci=rci: e.scalar_tensor_tensor(
                                out=o[0:m, :], in0=lt[0:m, :], scalar=mu[0:m, rci:rci + 1], in1=lb[0:m, 1:513],
                                op0=ALU.mult, op1=ALU.add), reads=[blt, blb, b_mu], writes=[bo])
                        else:
                            ls, bls = lseg.next()
                            S.op("dve", lambda e, ls=ls, lt=lt, lb=lb, m=m, rci=rci: e.scalar_tensor_tensor(
                                out=ls[0:m, :], in0=lt[0:m, :], scalar=mu[0:m, rci:rci + 1], in1=lb[0:m, 1:513],
                                op0=ALU.mult, op1=ALU.add), reads=[blt, blb, b_mu], writes=[bls])
                            S.op("act", lambda e, o=o, ls=ls, m=m, post=post: e.activation(
                                out=o[0:m, :], in_=ls[0:m, :], func=post), reads=[bls], writes=[bo])
                        p = 1 - p
                        dcol = tok0 + tk
                    r0 = drow0 + j0
                    S.dma("sp" if (tt % 2 == 0) else "pool", lambda e, o=o, r0=r0, m=m, dcol=dcol, dst=dst: e.dma_start(
                        out=dst[r0:r0 + m, dcol:dcol + 512], in_=o[0:m, :]), reads=[bo])
                if kind == "rw":
                    S.op("act", lambda e, p=p, rci=rci, m=m: e.activation(
                        out=carry[0:m, rci:rci + 1], in_=lbuf[p][0:m, 0:1], func=AF.Copy),
                        reads=[b_lbuf[p]], writes=[b_carry])


def phase2(k, att_bias, halo_mask):
    cfg, S, nc, c, sc = k.cfg, k.S, k.nc, k.c, k.sc
    NOWN = cfg.NOWN
    sc["YAT"] = k.scratch("YAT", [AW, NOWN], BF16)
    k.phase()
    QTv = sc["QT"].rearrange("(c p) t -> p c t", p=128)
    KTv = sc["KT"].rearrange("(c p) t -> p c t", p=128)
    YATv = sc["YAT"].rearrange("(c p) t -> p c t", p=128)
    qt = Ring([k.sb(f"qt{i}", [128, 8, 512], BF16) for i in range(2)])
    kt = Ring([k.sb(f"kt{i}", [128, 8, 1024], BF16) for i in range(2)])
    vv = Ring([k.sb(f"vv{i}", [128, 8, NH, 65], BF16) for i in range(2)])
    bias = k.sb("abias", [128, 5, NH, 128], F32); b_bias = Buf()
    halo = k.sb("halo", [128, 1], F32); b_halo = Buf()
    scs = Ring([k.sb(f"scs{i}", [128, 512], F32) for i in range(2)])
    pTs = Ring([k.sb(f"pTs{i}", [128, 512], BF16) for i in range(3)])
    ya = Ring([k.sb(f"ya{i}", [128, AW], BF16) for i in range(2)])
    yst = Ring([k.sb(f"yst{i}", [128, 8, 512], BF16) for i in range(2)])
    rec = Ring([k.sb(f"rec{i}", [128, 4], F32) for i in range(4)])
    ps_s = Ring([k.ps(f"ps_s{i}", [128, 512], F32) for i in range(2)])
    po = [k.ps(f"po{i}", [128, 512], F32) for i in range(4)]
    b_po = [Buf() for _ in range(4)]
    psT = Ring([k.ps("psT2", [128, 8, 128], BF16)])

    for kb in range(5):
        S.dma("sp", lambda e, kb=kb: e.dma_start(out=bias[:, kb, :, :], in_=att_bias[:, kb, :, :]), writes=[b_bias])
    S.dma("sp", lambda e: e.dma_start(out=halo[:], in_=halo_mask[:, :]), writes=[b_halo])

    HG = [[0, 2, 4, 6], [1, 3, 5, 7], [8, 10, 12, 14], [9, 11, 13, 15]]
    for g in range(NOWN // 512):
        q_, bq = qt.next(); k_, bk = kt.next(); v_, bv = vv.next()
        S.dma("sp", lambda e, q_=q_, g=g: e.dma_start(out=q_[:], in_=QTv[:, :, 512 * g:512 * g + 512]), writes=[bq])
        S.dma("act", lambda e, k_=k_, g=g: e.dma_start(out=k_[:], in_=KTv[:, :, 512 * g:512 * g + 1024]), writes=[bk])
        S.dma("pool", lambda e, v_=v_, g=g: e.dma_start(
            out=v_[:], in_=sc["V"][512 * g:512 * g + 1024, :, :].rearrange("(i p) h d -> p i h d", p=128)), writes=[bv])
        ys, bys = yst.next()
        for p in range(4):
            y_, by = ya.next()
            for hg in range(4):
                for kb in range(5):
                    ps, bps = ps_s.next()
                    for hh in range(4):
                        h = HG[hg][hh]
                        hp, h2 = h % 2, h // 2
                        S.op("pe", lambda e, ps=ps, k_=k_, q_=q_, hp=hp, h2=h2, p=p, kb=kb, hh=hh: e.matmul(
                            ps[:, hh * 128:(hh + 1) * 128],
                            lhsT=k_[hp * 64:(hp + 1) * 64, h2, 128 * (p + kb):128 * (p + kb) + 128],
                            rhs=q_[hp * 64:(hp + 1) * 64, h2, 128 * p:128 * p + 128],
                            start=True, stop=True), reads=[bk, bq], writes=[bps])
                    s_, bs = scs.next()
                    S.op("dve", lambda e, s_=s_, ps=ps, kb=kb, hg=hg: e.tensor_tensor(
                        out=s_[:].rearrange("p (h q) -> p h q", q=128), in0=ps[:].rearrange("p (h q) -> p h q", q=128),
                        in1=bias[:, kb, 4 * hg:4 * hg + 4, :], op=ALU.add), reads=[bps, b_bias], writes=[bs])
                    pt, bpt = pTs.next()
                    masked = (g == 0 and p + kb < 4)
                    if masked:
                        S.op("act", lambda e, pt=pt, s_=s_: e.activation(
                            out=pt[:], in_=s_[:], func=AF.Exp, bias=halo[:, 0:1]), reads=[bs, b_halo], writes=[bpt])
                    else:
                        S.op("act", lambda e, pt=pt, s_=s_: e.activation(
                            out=pt[:], in_=s_[:], func=AF.Exp), reads=[bs], writes=[bpt])
                    for hh in range(4):
                        h = HG[hg][hh]
                        S.op("pe", lambda e, pt=pt, v_=v_, hg=hg, hh=hh, h=h, p=p, kb=kb: e.matmul(
                            po[hg][:, hh * 65:(hh + 1) * 65], lhsT=pt[:, hh * 128:(hh + 1) * 128],
                            rhs=v_[:, p + kb, h, :], start=(kb == 0 and hh == 0), stop=(kb == 4 and hh == 3),
                            skip_group_check=True), reads=[bpt, bv], writes=[b_po[hg]])
                r_, br = rec.next()
                pov = po[hg][:, 0:260].rearrange("p (h d) -> p h d", d=65)
                S.op("dve", lambda e, r_=r_, pov=pov: e.reciprocal(out=r_[:, 0:4], in_=pov[:, :, 64]),
                     reads=[b_po[hg]], writes=[br])
                for hh in range(4):
                    h = HG[hg][hh]
                    eng = "act" if hh % 2 == 0 else "dve"
                    if eng == "act":
                        S.op("act", lambda e, y_=y_, pov=pov, r_=r_, hh=hh, h=h: e.activation(
                            out=y_[:, h * 64:(h + 1) * 64], in_=pov[:, hh, 0:64], func=AF.Copy, scale=r_[:, hh:hh + 1]),
                            reads=[b_po[hg], br], writes=[by])
                    else:
                        S.op("dve", lambda e, y_=y_, pov=pov, r_=r_, hh=hh, h=h: e.tensor_scalar(
                            out=y_[:, h * 64:(h + 1) * 64], in0=pov[:, hh, 0:64], scalar1=r_[:, hh:hh + 1], scalar2=None,
                            op0=ALU.mult), reads=[b_po[hg], br], writes=[by])
            pt_, bpt_ = psT.next()
            for j in range(8):
                S.op("pe", lambda e, pt_=pt_, y_=y_, j=j: e.transpose(
                    out=pt_[:, j, :], in_=y_[:, j * 128:(j + 1) * 128], identity=c["ident_b"][:]),
                    reads=[by, c["b_ident"]], writes=[bpt_])
            S.op("act", lambda e, ys=ys, pt_=pt_, p=p: e.activation(
                out=ys[:, :, p * 128:(p + 1) * 128], in_=pt_[:], func=AF.Copy), reads=[bpt_], writes=[bys])
        S.dma("sp", lambda e, ys=ys, g=g: e.dma_start(out=YATv[:, :, 512 * g:512 * g + 512], in_=ys[:]), reads=[bys])


def phase3(k, prm):
    cfg, S, nc, c, sc = k.cfg, k.S, k.nc, k.c, k.sc
    NTOK, NPRE, NOWN = cfg.NTOK, cfg.NPRE, cfg.NOWN
    sc["YB"] = k.scratch("YB", [NOWN, AW], F32)
    sc["BON"] = k.scratch("BON", [NH, NOWN], F32)
    k.phase()
    HB = 8
    def cload(name, shape, dt=F32, src=None, cast=None):
        t = k.sb("s3_" + name, shape, dt); b = Buf()
        S.dma("sp", lambda e: e.dma_start(out=t[:], in_=src), writes=[b])
        return t, b
    wupf, b_wupf = cload("wupf", [96, AW], F32, prm["w_up"][:, :])
    aupf, b_aupf = cload("aupf", [96, AW], F32, prm["a_up"][:, :])
    wup = k.sb("wup", [96, AW], BF16); aup = k.sb("aup", [96, AW], BF16)
    S.op("act", lambda e: e.activation(out=wup[:], in_=wupf[:], func=AF.Copy), reads=[b_wupf], writes=[b_wupf])
    S.op("act", lambda e: e.activation(out=aup[:], in_=aupf[:], func=AF.Copy), reads=[b_aupf], writes=[b_aupf])
    hc, b_hc = cload("hc", [64, 6, NH], F32, prm["hcols"][:, :, :])
    S.op("dve", lambda e: e.tensor_scalar(out=hc[:, 4, :], in0=hc[:, 3, :], scalar1=-1.0, scalar2=1.0, op0=ALU.mult, op1=ALU.add),
         reads=[b_hc], writes=[b_hc])
    rmask, b_rm = cload("rmask", [64, 512], F32, prm["rmask"][:, :])
    m_su, b_su = cload("m_su", [64, 8, 64], F32, prm["m_su"][:, :, :])
    m_ui, b_ui = cload("m_ui", [64, 8, 64], F32, prm["m_ui"][:, :, :])
    m_sl, b_sl = cload("m_sl", [64, 8, 64], F32, prm["m_sl"][:, :, :])
    i8, b_i8 = cload("i8", [64, 8, 64], F32, prm["i8"][:, :, :])
    ones64 = k.sb("ones64", [64, 64], F32); b_ones = Buf()
    S.op("pool", lambda e: e.memset(ones64[:], 1.0), writes=[b_ones])
    cb = [b_hc, b_rm]

    S32 = k.sb("S32", [64, NH, 64], F32); b_S32 = [Buf() for _ in range(NH)]
    Sb = k.sb("Sb", [64, NH, 64], BF16); b_Sb = [Buf() for _ in range(NH)]
    S.op("pool", lambda e: e.memset(S32[:], 0.0), writes=b_S32)
    S.op("pool", lambda e: e.memset(Sb[:], 0.0), writes=b_Sb)

    wdt = Ring([k.sb(f"wdt{i}", [96, 512], BF16) for i in range(2)])
    adt = Ring([k.sb(f"adt{i}", [96, 512], BF16) for i in range(2)])
    def htiles(name, shape, dt):
        return [k.sb(f"{name}{i}", shape, dt) for i in range(HB)], [Buf() for _ in range(HB)]
    AR, b_AR = htiles("AR", [64, 8, 2, 64], BF16)
    Tm, b_Tm = htiles("Tm", [64, 8, 64], BF16)
    Mka, b_Mka = htiles("Mka", [64, 8, 64], BF16)
    Mbr, b_Mbr = htiles("Mbr", [64, 8, 64], BF16)
    Mkr, b_Mkr = htiles("Mkr", [64, 8, 64], BF16)
    BhT, b_BhT = htiles("BhT", [64, 8, 64], BF16)
    KhT, b_KhT = htiles("KhT", [64, 8, 64], BF16)
    VT, b_VT = htiles("VT", [64, 8, 64], BF16)
    pC, b_pC = htiles("pC", [64, 8], F32)
    def tring(name, n, shape=[64, 512], dt=F32):
        return Ring([k.sb(f"{name}{i}", shape, dt) for i in range(n)])
    t_Ep = tring("t_Ep", 2, [64, 8, 64], F32)
    rin = tring("rin", 2, dt=BF16); kin = tring("kin", 2, dt=BF16); vin = tring("vin", 2, dt=BF16)
    t_s = tring("t_s", 2); t_cum = tring("t_cum", 2); t_a = tring("t_a", 2); t_kkr = tring("t_kkr", 2)
    t_sq = tring("t_sq", 2); t_nr = tring("t_nr", 2); t_kk = tring("t_kk", 2); t_t1 = t_sq
    t_kp = tring("t_kp", 2); t_bv = tring("t_bv", 2); t_d1 = t_sq; t_Epv = tring("t_Epv", 2)
    t_Em = tring("t_Em", 2); t_EC = tring("t_EC", 2)
    t_Bt = tring("t_Bt", 4, dt=BF16); t_Kt = tring("t_Kt", 4, dt=BF16)
    t_Bh = tring("t_Bh", 2, dt=BF16); t_Kh = tring("t_Kh", 2, dt=BF16)
    t_rkp = tring("t_rkp", 2); t_brow = tring("t_brow", 2, [1, 512], F32)
    t_N = tring("t_N", 4, [64, 8, 64], BF16); t_NT = tring("t_NT", 4, [64, 8, 64], BF16)
    WTs = tring("WTs", 2, [64, HB, 64], BF16); UTs = tring("UTs", 2, [64, HB, 64], BF16)
    ysb = tring("ysb", 1, [64, HB, 64], F32)
    pA = Ring([k.ps(f"p3a{i}", [64, 512], F32) for i in range(2)])
    pG = Ring([k.ps(f"p3g{i}", [64, 4, 128], F32) for i in range(2)])
    pTb = Ring([k.ps(f"p3t{i}", [64, 8, 128], BF16) for i in range(2)])
    pW = k.ps("p3W", [64, HB, 64], F32); b_pW = Buf()
    pU = k.ps("p3U", [64, HB, 64], F32); b_pU = Buf()
    pS = pW; b_pS = b_pW
    pX = Ring([pW[:].rearrange("p h v -> p (h v)"), pU[:].rearrange("p h v -> p (h v)")])
    pX.b = [b_pW, b_pU]

    RBv, KBv, VBv = sc["RB"], sc["KB"], sc["VB"]
    for tt in range(NTOK // 512):
        t0 = tt * 512
        own = t0 >= NPRE
        wd_, bwd = wdt.next(); ad_, bad = adt.next()
        S.dma("sp", lambda e, wd_=wd_, t0=t0: e.dma_start(out=wd_[:], in_=sc["WD"][:, t0:t0 + 512]), writes=[bwd])
        S.dma("sp", lambda e, ad_=ad_, t0=t0: e.dma_start(out=ad_[:], in_=sc["AD"][:, t0:t0 + 512]), writes=[bad])
        for hb0 in range(0, NH, HB):
            ctx = {}

            def prepA(hi, hb0=hb0, own=own, t0=t0, wd_=wd_, ad_=ad_, bwd=bwd, bad=bad):
                h = hb0 + hi
                hs = slice(h * 64, (h + 1) * 64)
                col = lambda j: hc[:, j, h:h + 1]
                r_, br = rin.next(); k_, bk = kin.next(); v_, bv = vin.next()
                yield S.dma("sp", lambda e, r_=r_, hs=hs, t0=t0: e.dma_start(out=r_[:], in_=RBv[hs, t0:t0 + 512]), writes=[br])
                yield S.dma("act", lambda e, k_=k_, hs=hs, t0=t0: e.dma_start(out=k_[:], in_=KBv[hs, t0:t0 + 512]), writes=[bk])
                yield S.dma("sp", lambda e, v_=v_, hs=hs, t0=t0: e.dma_start(out=v_[:], in_=VBv[hs, t0:t0 + 512]), writes=[bv])
                p1, bp1 = pX.next()
                yield S.op("pe", lambda e, p1=p1, hs=hs, wd_=wd_: e.matmul(p1[:, :], lhsT=wup[:, hs], rhs=wd_[:, :], start=True, stop=True),
                     reads=[b_wupf, bwd], writes=[bp1])
                s_, bs = t_s.next()
                yield S.op("act", lambda e, s_=s_, p1=p1, h=h: e.activation(out=s_[:], in_=p1[:, :], func=AF.Sigmoid, bias=hc[:, 0, h:h + 1]),
                     reads=[bp1, b_hc], writes=[bs])
                cum, bcum = t_cum.next()
                yield S.op("dve", lambda e, cum=cum, s_=s_: e.tensor_tensor_scan(out=cum[:], data0=rmask[:], data1=s_[:], initial=0.0,
                                                                          op0=ALU.mult, op1=ALU.add), reads=[bs, b_rm], writes=[bcum])
                p2, bp2 = pX.next()
                yield S.op("pe", lambda e, p2=p2, hs=hs, ad_=ad_: e.matmul(p2[:, :], lhsT=aup[:, hs], rhs=ad_[:, :], start=True, stop=True),
                     reads=[b_aupf, bad], writes=[bp2])
                a_, ba = t_a.next()
                yield S.op("act", lambda e, a_=a_, p2=p2, h=h: e.activation(out=a_[:], in_=p2[:, :], func=AF.Sigmoid, bias=hc[:, 1, h:h + 1]),
                     reads=[bp2, b_hc], writes=[ba])
                kkr, bkkr = t_kkr.next()
                yield S.op("dve", lambda e, kkr=kkr, k_=k_, h=h: e.tensor_scalar(out=kkr[:], in0=k_[:], scalar1=hc[:, 2, h:h + 1], scalar2=None,
                                                                          op0=ALU.mult), reads=[bk, b_hc], writes=[bkkr])
                sq, bsq = t_sq.next()
                yield S.op("pool", lambda e, sq=sq, kkr=kkr: e.tensor_tensor(out=sq[:], in0=kkr[:], in1=kkr[:], op=ALU.mult), reads=[bkkr], writes=[bsq])
                p3, bp3 = pX.next()
                yield S.op("pe", lambda e, p3=p3, sq=sq: e.matmul(p3[:, :], lhsT=ones64[:, :], rhs=sq[:, :], start=True, stop=True),
                     reads=[b_ones, bsq], writes=[bp3])
                nr, bnr = t_nr.next()
                yield S.op("act", lambda e, nr=nr, p3=p3: e.activation(out=nr[:], in_=p3[:, :], func=AF.Sqrt), reads=[bp3], writes=[bnr])
                yield S.op("dve", lambda e, nr=nr: e.tensor_scalar(out=nr[:], in0=nr[:], scalar1=1e-12, scalar2=None, op0=ALU.max), reads=[bnr], writes=[bnr])
                yield S.op("dve", lambda e, nr=nr: e.reciprocal(out=nr[:], in_=nr[:]), reads=[bnr], writes=[bnr])
                kk, bkk = t_kk.next()
                yield S.op("dve", lambda e, kk=kk, kkr=kkr, nr=nr: e.tensor_tensor(out=kk[:], in0=kkr[:], in1=nr[:], op=ALU.mult), reads=[bkkr, bnr], writes=[bkk])
                t1, bt1 = t_t1.next()
                yield S.op("dve", lambda e, t1=t1, a_=a_, h=h: e.tensor_scalar(out=t1[:], in0=a_[:], scalar1=hc[:, 3, h:h + 1], scalar2=hc[:, 4, h:h + 1],
                                                                        op0=ALU.mult, op1=ALU.add), reads=[ba, b_hc], writes=[bt1])
                kp, bkp = t_kp.next()
                yield S.op("pool", lambda e, kp=kp, k_=k_, t1=t1: e.tensor_tensor(out=kp[:], in0=k_[:], in1=t1[:], op=ALU.mult), reads=[bk, bt1], writes=[bkp])
                bv_, bbv = t_bv.next()
                yield S.op("pool", lambda e, bv_=bv_, kk=kk, a_=a_: e.tensor_tensor(out=bv_[:], in0=kk[:], in1=a_[:], op=ALU.mult), reads=[bkk, ba], writes=[bbv])
                if own:
                    rkp, brkp = t_rkp.next()
                    yield S.op("dve", lambda e, rkp=rkp, r_=r_, kp=kp, h=h: e.scalar_tensor_tensor(out=rkp[:], in0=r_[:], scalar=hc[:, 5, h:h + 1], in1=kp[:],
                                                                                           op0=ALU.mult, op1=ALU.mult), reads=[br, bkp, b_hc], writes=[brkp])
                    pb, bpb = pX.next()
                    yield S.op("pe", lambda e, pb=pb, rkp=rkp: e.matmul(pb[0:1, :], lhsT=ones64[:, 0:1], rhs=rkp[:, :], start=True, stop=True),
                         reads=[b_ones, brkp], writes=[bpb])
                    brow, bbrow = t_brow.next()
                    yield S.op("act", lambda e, brow=brow, pb=pb: e.activation(out=brow[0:1, :], in_=pb[0:1, :], func=AF.Copy), reads=[bpb], writes=[bbrow])
                    yield S.dma("sp", lambda e, brow=brow, h=h, t0=t0: e.dma_start(out=sc["BON"][h:h + 1, t0 - NPRE:t0 - NPRE + 512], in_=brow[0:1, :]), reads=[bbrow])
                ep, bep = t_Ep.next()
                epf = ep[:].rearrange("p c t -> p (c t)")
                yield S.op("act", lambda e, epf=epf, cum=cum: e.activation(out=epf, in_=cum[:], func=AF.Exp, scale=-C0), reads=[bcum], writes=[bep])
                d1, bd1 = t_d1.next()
                yield S.op("pool", lambda e, d1=d1, cum=cum, s_=s_: e.tensor_tensor(out=d1[:], in0=cum[:], in1=s_[:], op=ALU.subtract), reads=[bcum, bs], writes=[bd1])
                epv, bepv = t_Epv.next()
                yield S.op("act", lambda e, epv=epv, d1=d1: e.activation(out=epv[:], in_=d1[:], func=AF.Exp, scale=-C0), reads=[bd1], writes=[bepv])
                em, bem = t_Em.next()
                yield S.op("act", lambda e, em=em, cum=cum: e.activation(out=em[:], in_=cum[:], func=AF.Exp, scale=C0), reads=[bcum], writes=[bem])
                yield S.op("act", lambda e, ep=ep, hi=hi: e.activation(out=pC[hi][:, :], in_=ep[:, :, 63], func=AF.Copy), reads=[bep], writes=[b_pC[hi]])
                ec, bec = t_EC.next()
                for cc in range(8):
                    yield S.op("act", lambda e, ec=ec, em=em, ep=ep, cc=cc: e.activation(
                        out=ec[:, cc * 64:(cc + 1) * 64], in_=em[:, cc * 64:(cc + 1) * 64], func=AF.Copy, scale=ep[:, cc, 63:64]),
                        reads=[bem, bep], writes=[bec])
                ar = AR[hi]; bar = b_AR[hi]
                yield S.op("dve", lambda e, ar=ar, r_=r_, epf=epf: e.tensor_tensor(out=ar[:, :, 1, :], in0=r_[:].rearrange("p (c t) -> p c t", t=64),
                                                                           in1=epf.rearrange("p (c t) -> p c t", t=64), op=ALU.mult), reads=[br, bep], writes=[bar])
                yield S.op("dve", lambda e, ar=ar, kk=kk, epv=epv: e.scalar_tensor_tensor(out=ar[:, :, 0, :], in0=kk[:].rearrange("p (c t) -> p c t", t=64), scalar=-1.0,
                                                                                  in1=epv[:].rearrange("p (c t) -> p c t", t=64), op0=ALU.mult, op1=ALU.mult), reads=[bkk, bepv], writes=[bar])
                Bt, bBt = t_Bt.next(); Kt, bKt = t_Kt.next(); Bh, bBh = t_Bh.next(); Kh, bKh = t_Kh.next()
                yield S.op("pool", lambda e, Bt=Bt, bv_=bv_, em=em: e.tensor_tensor(out=Bt[:], in0=bv_[:], in1=em[:], op=ALU.mult), reads=[bbv, bem], writes=[bBt])
                yield S.op("dve", lambda e, Kt=Kt, kp=kp, em=em: e.tensor_tensor(out=Kt[:], in0=kp[:], in1=em[:], op=ALU.mult), reads=[bkp, bem], writes=[bKt])
                yield S.op("pool", lambda e, Bh=Bh, bv_=bv_, ec=ec: e.tensor_tensor(out=Bh[:], in0=bv_[:], in1=ec[:], op=ALU.mult), reads=[bbv, bec], writes=[bBh])
                yield S.op("dve", lambda e, Kh=Kh, kp=kp, ec=ec: e.tensor_tensor(out=Kh[:], in0=kp[:], in1=ec[:], op=ALU.mult), reads=[bkp, bec], writes=[bKh])
                for src, bsrc, dstl, bdstl in ((Bh, bBh, BhT, b_BhT), (Kh, bKh, KhT, b_KhT), (v_, bv, VT, b_VT)):
                    pt, bpt = pTb.next()
                    for cc in range(8):
                        yield S.op("pe", lambda e, pt=pt, src=src, cc=cc: e.transpose(out=pt[:, cc, 0:64], in_=src[:, cc * 64:(cc + 1) * 64],
                                                                               identity=c["ident_b"][0:64, 0:64]), reads=[bsrc, c["b_ident"]], writes=[bpt])
                    yield S.op("act", lambda e, pt=pt, d=dstl[hi]: e.activation(out=d[:], in_=pt[:, :, 0:64], func=AF.Copy), reads=[bpt], writes=[bdstl[hi]])
                ctx[hi] = (Bt, bBt, Kt, bKt, ar, bar)

            def prepB(hi):
                Bt, bBt, Kt, bKt, ar, bar = ctx[hi]
                N0, bN0 = t_N.next()
                for hv in range(2):
                    pg, bpg = pG.next()
                    for c4 in range(4):
                        cc = 4 * hv + c4
                        yield S.op("pe", lambda e, pg=pg, Bt=Bt, ar=ar, cc=cc, c4=c4: e.matmul(pg[:, c4, :], lhsT=Bt[:, cc * 64:(cc + 1) * 64],
                                                                                        rhs=ar[:, cc, :, :].rearrange("p a t -> p (a t)"), start=True, stop=True),
                             reads=[bBt, bar], writes=[bpg])
                    yield S.op("dve", lambda e, N0=N0, pg=pg, hv=hv: e.tensor_tensor(out=N0[:, 4 * hv:4 * hv + 4, :], in0=pg[:, :, 0:64], in1=m_su[:, 0:4, :], op=ALU.mult),
                         reads=[bpg, b_su], writes=[bN0])
                    yield S.op("dve", lambda e, pg=pg, d=Mbr[hi], hv=hv: e.tensor_tensor(out=d[:, 4 * hv:4 * hv + 4, :], in0=pg[:, :, 64:128], in1=m_ui[:, 0:4, :], op=ALU.mult),
                         reads=[bpg, b_ui], writes=[b_Mbr[hi]])
                for hv in range(2):
                    pg, bpg = pG.next()
                    for c4 in range(4):
                        cc = 4 * hv + c4
                        yield S.op("pe", lambda e, pg=pg, Kt=Kt, ar=ar, cc=cc, c4=c4: e.matmul(pg[:, c4, :], lhsT=Kt[:, cc * 64:(cc + 1) * 64],
                                                                                        rhs=ar[:, cc, :, :].rearrange("p a t -> p (a t)"), start=True, stop=True),
                             reads=[bKt, bar], writes=[bpg])
                    yield S.op("dve", lambda e, pg=pg, d=Mka[hi], hv=hv: e.tensor_tensor(out=d[:, 4 * hv:4 * hv + 4, :], in0=pg[:, :, 0:64], in1=m_su[:, 0:4, :], op=ALU.mult),
                         reads=[bpg, b_su], writes=[b_Mka[hi]])
                    yield S.op("dve", lambda e, pg=pg, d=Mkr[hi], hv=hv: e.tensor_tensor(out=d[:, 4 * hv:4 * hv + 4, :], in0=pg[:, :, 64:128], in1=m_ui[:, 0:4, :], op=ALU.mult),
                         reads=[bpg, b_ui], writes=[b_Mkr[hi]])
                p4, bp4 = pA.next()
                for cc in range(8):
                    yield S.op("pe", lambda e, p4=p4, ar=ar, Bt=Bt, cc=cc: e.matmul(p4[:, cc * 64:(cc + 1) * 64], lhsT=ar[:, cc, 0, :], rhs=Bt[:, cc * 64:(cc + 1) * 64],
                                                                             start=True, stop=True), reads=[bar, bBt], writes=[bp4])
                NT0, bNT0 = t_NT.next()
                yield S.op("dve", lambda e, NT0=NT0, p4=p4: e.tensor_tensor(out=NT0[:], in0=p4[:, :].rearrange("p (c t) -> p c t", t=64), in1=m_sl[:], op=ALU.mult),
                     reads=[bp4, b_sl], writes=[bNT0])
                T_ = Tm[hi]; bT = b_Tm[hi]
                yield S.op("pool", lambda e, T_=T_, N0=N0: e.tensor_tensor(out=T_[:], in0=N0[:], in1=i8[:], op=ALU.add), reads=[bN0, b_i8], writes=[bT])
                Nc, bNc, NTc, bNTc = N0, bN0, NT0, bNT0
                for lvl in range(1, 6):
                    pnt, bpnt = pA.next()
                    for cc in range(8):
                        yield S.op("pe", lambda e, pnt=pnt, Nc=Nc, NTc=NTc, cc=cc: e.matmul(pnt[:, cc * 64:(cc + 1) * 64], lhsT=Nc[:, cc, :], rhs=NTc[:, cc, :],
                                                                                     start=True, stop=True), reads=[bNc, bNTc], writes=[bpnt])
                    NTn, bNTn = t_NT.next()
                    yield S.op("act", lambda e, NTn=NTn, pnt=pnt: e.activation(out=NTn[:].rearrange("p c t -> p (c t)"), in_=pnt[:, :], func=AF.Copy),
                         reads=[bpnt], writes=[bNTn])
                    if lvl < 5:
                        pn, bpn = pA.next()
                        for cc in range(8):
                            yield S.op("pe", lambda e, pn=pn, Nc=Nc, NTc=NTc, cc=cc: e.matmul(pn[:, cc * 64:(cc + 1) * 64], lhsT=NTc[:, cc, :], rhs=Nc[:, cc, :],
                                                                                       start=True, stop=True), reads=[bNc, bNTc], writes=[bpn])
                        Nn, bNn = t_N.next()
                        yield S.op("act", lambda e, Nn=Nn, pn=pn: e.activation(out=Nn[:].rearrange("p c t -> p (c t)"), in_=pn[:, :], func=AF.Copy),
                             reads=[bpn], writes=[bNn])
                    ptt, bptt = pA.next()
                    for cc in range(8):
                        yield S.op("pe", lambda e, ptt=ptt, NTn=NTn, T_=T_, cc=cc: e.matmul(ptt[:, cc * 64:(cc + 1) * 64], lhsT=NTn[:, cc, :], rhs=T_[:, cc, :],
                                                                                     start=True, stop=True), reads=[bNTn, bT], writes=[bptt])
                    yield S.op("dve", lambda e, T_=T_, ptt=ptt: e.tensor_tensor(out=T_[:].rearrange("p c t -> p (c t)"), in0=ptt[:, :],
                                                                         in1=T_[:].rearrange("p c t -> p (c t)"), op=ALU.add), reads=[bptt, bT], writes=[bT])
                    NTc, bNTc = NTn, bNTn
                    if lvl < 5:
                        Nc, bNc = Nn, bNn
            GRP = 2
            pairs = [list(range(g, g + GRP)) for g in range(0, HB, GRP)]
            gB = []
            for pi in range(len(pairs) + 1):
                gA = [prepA(hi) for hi in pairs[pi]] if pi < len(pairs) else []
                tick = 0
                while gA or gB:
                    for g_ in list(gB):
                        try:
                            next(g_)
                        except StopIteration:
                            gB.remove(g_)
                    if tick % 2 == 0 or not gB:
                        for g_ in list(gA):
                            try:
                                next(g_)
                            except StopIteration:
                                gA.remove(g_)
                    tick += 1
                gB = [prepB(hi) for hi in pairs[pi]] if pi < len(pairs) else []
            for cc in range(8):
                hbufs = lambda lst: [lst[i] for i in range(HB)]
                for hi in range(HB):
                    h = hb0 + hi
                    S.op("pe", lambda e, hi=hi, h=h, cc=cc: e.matmul(pW[:, hi, :], lhsT=AR[hi][:, cc, 0, :], rhs=Sb[:, h, :], start=(hi == 0), stop=False,
                                                                    skip_group_check=True), reads=[b_AR[hi], b_Sb[h]], writes=[b_pW])
                    S.op("pe", lambda e, hi=hi, cc=cc: e.matmul(pW[:, hi, :], lhsT=Mka[hi][:, cc, :], rhs=VT[hi][:, cc, :], start=False, stop=True,
                                                               skip_group_check=True), reads=[b_Mka[hi], b_VT[hi]], writes=[b_pW])
                wt, bwt = WTs.next()
                S.op("act", lambda e, wt=wt: e.activation(out=wt[:], in_=pW[:], func=AF.Copy), reads=[b_pW], writes=[bwt])
                for hi in range(HB):
                    S.op("pe", lambda e, hi=hi, cc=cc, wt=wt: e.matmul(pU[:, hi, :], lhsT=Tm[hi][:, cc, :], rhs=wt[:, hi, :], start=(hi == 0), stop=True,
                                                                      skip_group_check=True), reads=[b_Tm[hi], bwt], writes=[b_pU])
                ut, but = UTs.next()
                S.op("dve", lambda e, ut=ut: e.tensor_copy(out=ut[:], in_=pU[:]), reads=[b_pU], writes=[but])
                if own:
                    py, bpy = pA.next()
                    for hi in range(HB):
                        h = hb0 + hi
                        S.op("pe", lambda e, py=py, hi=hi, h=h, cc=cc: e.matmul(py[:, hi * 64:(hi + 1) * 64], lhsT=AR[hi][:, cc, 1, :], rhs=Sb[:, h, :],
                                                                               start=(hi == 0), stop=False, skip_group_check=True),
                             reads=[b_AR[hi], b_Sb[h]], writes=[bpy])
                    for hi in range(HB):
                        S.op("pe", lambda e, py=py, hi=hi, cc=cc, ut=ut: e.matmul(py[:, hi * 64:(hi + 1) * 64], lhsT=Mbr[hi][:, cc, :], rhs=ut[:, hi, :],
                                                                                 start=False, stop=False, skip_group_check=True),
                             reads=[b_Mbr[hi], but], writes=[bpy])
                        S.op("pe", lambda e, py=py, hi=hi, cc=cc: e.matmul(py[:, hi * 64:(hi + 1) * 64], lhsT=Mkr[hi][:, cc, :], rhs=VT[hi][:, cc, :],
                                                                          start=False, stop=True, skip_group_check=True),
                             reads=[b_Mkr[hi], b_VT[hi]], writes=[bpy])
                    ys, bys = ysb.next()
                    S.op("act", lambda e, ys=ys, py=py: e.activation(out=ys[:].rearrange("p h v -> p (h v)"), in_=py[:, :], func=AF.Copy), reads=[bpy], writes=[bys])
                    trow = t0 - NPRE + cc * 64
                    S.dma("sp", lambda e, ys=ys, trow=trow, hb0=hb0: e.dma_start(
                        out=sc["YB"][trow:trow + 64, hb0 * 64:(hb0 + HB) * 64], in_=ys[:].rearrange("p h v -> p (h v)")), reads=[bys])
                for hi in range(HB):
                    S.op("pe", lambda e, hi=hi, cc=cc, ut=ut: e.matmul(pS[:, hi, :], lhsT=BhT[hi][:, cc, :], rhs=ut[:, hi, :], start=(hi == 0), stop=False,
                                                                      skip_group_check=True), reads=[b_BhT[hi], but], writes=[b_pS])
                    S.op("pe", lambda e, hi=hi, cc=cc: e.matmul(pS[:, hi, :], lhsT=KhT[hi][:, cc, :], rhs=VT[hi][:, cc, :], start=False, stop=True,
                                                               skip_group_check=True), reads=[b_KhT[hi], b_VT[hi]], writes=[b_pS])
                for hi in range(HB):
                    h = hb0 + hi
                    S.op("dve", lambda e, hi=hi, h=h, cc=cc: e.scalar_tensor_tensor(out=S32[:, h, :], in0=S32[:, h, :], scalar=pC[hi][:, cc:cc + 1],
                                                                                  in1=pS[:, hi, :], op0=ALU.mult, op1=ALU.add),
                         reads=[b_S32[h], b_pC[hi], b_pS], writes=[b_S32[h]])
                    S.op("act", lambda e, h=h: e.activation(out=Sb[:, h, :], in_=S32[:, h, :], func=AF.Copy), reads=[b_S32[h]], writes=[b_Sb[h]])


def phase3b(k, prm):
    cfg, S, nc, c, sc = k.cfg, k.S, k.nc, k.c, k.sc
    NTOK, NPRE, NOWN = cfg.NTOK, cfg.NPRE, cfg.NOWN
    sc["YBT"] = k.scratch("YBT", [AW, NOWN], BF16)
    k.phase()
    YBTv = sc["YBT"].rearrange("(c p) t -> p c t", p=128)
    VBv = sc["VB"].rearrange("(c p) t -> p c t", p=128)
    GDv = sc["GD"].rearrange("(c p) t -> p c t", p=128)
    gupf = k.sb("gupf", [128, 2, AW], F32); gup = k.sb("gup", [128, 2, AW], BF16); b_gup = Buf()
    S.dma("sp", lambda e: e.dma_start(out=gupf[:], in_=prm["g_up"].rearrange("(c p) n -> p c n", p=128)), writes=[b_gup])
    S.op("act", lambda e: e.activation(out=gup[:], in_=gupf[:], func=AF.Copy), reads=[b_gup], writes=[b_gup])
    lw = k.sb("lnxw", [128, AW], F32); lb = k.sb("lnxb", [128, AW], F32); b_l = Buf()
    S.dma("sp", lambda e: e.dma_start(out=lw[:], in_=prm["lnx_w"][0:1, :].partition_broadcast(128)), writes=[b_l])
    S.dma("sp", lambda e: e.dma_start(out=lb[:], in_=prm["lnx_b"][0:1, :].partition_broadcast(128)), writes=[b_l])
    yin = Ring([k.sb(f"yin{i}", [128, AW], F32) for i in range(2)])
    ysq = Ring([k.sb(f"ysq{i}", [128, AW], F32) for i in range(2)])
    st = Ring([k.sb(f"gst{i}", [128, 6, NH], F32) for i in range(2)])
    yn = Ring([k.sb(f"yn{i}", [128, AW], F32) for i in range(2)])
    vfm = Ring([k.sb(f"vfm{i}", [128, 8, 128], BF16) for i in range(2)])
    vtm = Ring([k.sb(f"vtm{i}", [128, AW], BF16) for i in range(2)])
    gdl = Ring([k.sb(f"gdl{i}", [128, 2, 128], BF16) for i in range(2)])
    bonf = Ring([k.sb(f"bonf{i}", [NH, 128], F32) for i in range(2)])
    bont = Ring([k.sb(f"bont{i}", [128, NH], F32) for i in range(2)])
    yo = Ring([k.sb(f"yo{i}", [128, AW], BF16) for i in range(2)])
    ost = Ring([k.sb(f"ybst{i}", [128, 8, 512], BF16) for i in range(2)])
    pT = Ring([k.ps(f"p3bT{i}", [128, 8, 128], BF16) for i in range(2)])
    pg = Ring([k.ps(f"p3bg{i}", [128, 512], F32) for i in range(2)])
    pbn = Ring([k.ps("p3bbn", [128, NH], F32)])
    bc = lambda ap: ap.unsqueeze(2).to_broadcast([128, NH, 64])
    v3 = lambda t: t[:].rearrange("p (h d) -> p h d", d=64)
    for ti in range(NOWN // 128):
        tok0 = ti * 128
        if ti % 4 == 0:
            os_, bos = ost.next()
        y_, by = yin.next()
        S.dma("sp", lambda e, y_=y_, tok0=tok0: e.dma_start(out=y_[:], in_=sc["YB"][tok0:tok0 + 128, :]), writes=[by])
        vf, bvf = vfm.next()
        S.dma("act", lambda e, vf=vf, tok0=tok0: e.dma_start(out=vf[:], in_=VBv[:, :, NPRE + tok0:NPRE + tok0 + 128]), writes=[bvf])
        gd, bgd = gdl.next()
        S.dma("act", lambda e, gd=gd, tok0=tok0: e.dma_start(out=gd[:], in_=GDv[:, :, NPRE + tok0:NPRE + tok0 + 128]), writes=[bgd])
        bf_, bbf = bonf.next()
        S.dma("sp", lambda e, bf_=bf_, tok0=tok0: e.dma_start(out=bf_[:], in_=sc["BON"][:, tok0:tok0 + 128]), writes=[bbf])
        s_, bs = st.next()
        S.op("dve", lambda e, s_=s_, y_=y_: e.tensor_reduce(out=s_[:, 0, :], in_=v3(y_), axis=AX.X, op=ALU.add), reads=[by], writes=[bs])
        q_, bq = ysq.next()
        S.op("act", lambda e, q_=q_, y_=y_: e.activation(out=q_[:], in_=y_[:], func=AF.Square), reads=[by], writes=[bq])
        S.op("dve", lambda e, s_=s_, q_=q_: e.tensor_reduce(out=s_[:, 1, :], in_=v3(q_), axis=AX.X, op=ALU.add), reads=[bq], writes=[bs])
        S.op("dve", lambda e, s_=s_: e.tensor_scalar(out=s_[:, 2, :], in0=s_[:, 0, :], scalar1=1.0 / 64, scalar2=None, op0=ALU.mult), reads=[bs], writes=[bs])
        S.op("dve", lambda e, s_=s_: e.tensor_tensor(out=s_[:, 3, :], in0=s_[:, 2, :], in1=s_[:, 2, :], op=ALU.mult), reads=[bs], writes=[bs])
        S.op("dve", lambda e, s_=s_: e.scalar_tensor_tensor(out=s_[:, 4, :], in0=s_[:, 1, :], scalar=1.0 / 64, in1=s_[:, 3, :], op0=ALU.mult, op1=ALU.subtract),
             reads=[bs], writes=[bs])
        S.op("dve", lambda e, s_=s_: e.tensor_scalar(out=s_[:, 4, :], in0=s_[:, 4, :], scalar1=64e-5, scalar2=None, op0=ALU.add), reads=[bs], writes=[bs])
        S.op("act", lambda e, s_=s_: e.activation(out=s_[:, 5, :], in_=s_[:, 4, :], func=AF.Sqrt), reads=[bs], writes=[bs])
        S.op("dve", lambda e, s_=s_: e.reciprocal(out=s_[:, 5, :], in_=s_[:, 5, :]), reads=[bs], writes=[bs])
        n_, bn = yn.next()
        S.op("dve", lambda e, n_=n_, y_=y_, s_=s_: e.tensor_tensor(out=v3(n_), in0=v3(y_), in1=bc(s_[:, 2, :]), op=ALU.subtract), reads=[by, bs], writes=[bn])
        S.op("pool", lambda e, n_=n_, s_=s_: e.tensor_tensor(out=v3(n_), in0=v3(n_), in1=bc(s_[:, 5, :]), op=ALU.mult), reads=[bn, bs], writes=[bn])
        S.op("dve", lambda e, n_=n_: e.tensor_tensor(out=n_[:], in0=n_[:], in1=lw[:], op=ALU.mult), reads=[bn, b_l], writes=[bn])
        S.op("pool", lambda e, n_=n_: e.tensor_tensor(out=n_[:], in0=n_[:], in1=lb[:], op=ALU.add), reads=[bn, b_l], writes=[bn])
        pb, bpb = pbn.next()
        S.op("pe", lambda e, pb=pb, bf_=bf_: e.transpose(out=pb[:, :], in_=bf_[:, :], identity=c["ident_f"][0:NH, 0:NH]), reads=[bbf, c["b_ident"]], writes=[bpb])
        bt, bbt = bont.next()
        S.op("act", lambda e, bt=bt, pb=pb: e.activation(out=bt[:], in_=pb[:, :], func=AF.Copy), reads=[bpb], writes=[bbt])
        pt, bpt = pT.next()
        for j in range(8):
            S.op("pe", lambda e, pt=pt, vf=vf, j=j: e.transpose(out=pt[:, j, :], in_=vf[:, j, :], identity=c["ident_b"][:]), reads=[bvf, c["b_ident"]], writes=[bpt])
        vt, bvt = vtm.next()
        S.op("act", lambda e, vt=vt, pt=pt: e.activation(out=vt[:].rearrange("p (j c) -> p j c", c=128), in_=pt[:], func=AF.Copy), reads=[bpt], writes=[bvt])
        q2, bq2 = ysq.next()
        S.op("pool", lambda e, q2=q2, vt=vt, bt=bt: e.tensor_tensor(out=v3(q2), in0=v3(vt), in1=bc(bt[:, :]), op=ALU.mult), reads=[bvt, bbt], writes=[bq2])
        S.op("dve", lambda e, n_=n_, q2=q2: e.tensor_tensor(out=n_[:], in0=n_[:], in1=q2[:], op=ALU.add), reads=[bn, bq2], writes=[bn])
        o_, bo = yo.next()
        for half in range(2):
            pg_, bpg = pg.next()
            for kc in range(2):
                S.op("pe", lambda e, pg_=pg_, gd=gd, kc=kc, half=half: e.matmul(pg_[:, :], lhsT=gd[:, kc, :], rhs=gup[:, kc, half * 512:(half + 1) * 512],
                                                                               start=(kc == 0), stop=(kc == 1)), reads=[bgd, b_gup], writes=[bpg])
            S.op("dve", lambda e, o_=o_, n_=n_, pg_=pg_, half=half: e.tensor_tensor(out=o_[:, half * 512:(half + 1) * 512], in0=pg_[:, :],
                                                                                   in1=n_[:, half * 512:(half + 1) * 512], op=ALU.mult), reads=[bpg, bn], writes=[bo])
        pt2, bpt2 = pT.next()
        for j in range(8):
            S.op("pe", lambda e, pt2=pt2, o_=o_, j=j: e.transpose(out=pt2[:, j, :], in_=o_[:, j * 128:(j + 1) * 128], identity=c["ident_b"][:]),
                 reads=[bo, c["b_ident"]], writes=[bpt2])
        S.op("act", lambda e, os_=os_, pt2=pt2, ti=ti: e.activation(out=os_[:, :, (ti % 4) * 128:(ti % 4 + 1) * 128], in_=pt2[:], func=AF.Copy), reads=[bpt2], writes=[bos])
        if ti % 4 == 3:
            g0 = (ti // 4) * 512
            S.dma("sp", lambda e, os_=os_, g0=g0: e.dma_start(out=YBTv[:, :, g0:g0 + 512], in_=os_[:]), reads=[bos])


def cast_load(k, S, dst_bf, bdst, src_ap_fn, nk, ncols, stg, step=256):
    for i, c0 in enumerate(range(0, ncols, step)):
        n = min(step, ncols - c0)
        st, bst = stg.next()
        S.dma("sp" if i % 2 == 0 else "act", lambda e, st=st, c0=c0, n=n: e.dma_start(out=st[:, 0:nk, 0:n], in_=src_ap_fn(c0, n)), writes=[bst])
        if i % 2 == 0:
            S.op("act", lambda e, st=st, c0=c0, n=n: e.activation(out=dst_bf[:, 0:nk, c0:c0 + n], in_=st[:, 0:nk, 0:n], func=AF.Copy), reads=[bst], writes=[bdst])
        else:
            S.op("pool", lambda e, st=st, c0=c0, n=n: e.tensor_copy(out=dst_bf[:, 0:nk, c0:c0 + n], in_=st[:, 0:nk, 0:n]), reads=[bst], writes=[bdst])


def phase4a(k, proj_a, proj_b):
    cfg, S, nc, c, sc = k.cfg, k.S, k.nc, k.c, k.sc
    DM, KC, NOWN = cfg.DM, cfg.KC, cfg.NOWN
    sc["MT"] = k.scratch("MT", [DM, NOWN], BF16)
    k.phase()
    PA = k.sb("PAb", [128, 8, DM], BF16); PB = k.sb("PBb", [128, 8, DM], BF16); bPA = Buf(); bPB = Buf()
    stg = Ring([k.sb(f"p4stg{i}", [128, 8, 256], F32) for i in range(2)])
    pav = proj_a.rearrange("(c p) n -> p c n", p=128); pbv = proj_b.rearrange("(c p) n -> p c n", p=128)
    cast_load(k, S, PA, bPA, lambda c0, n: pav[:, :, c0:c0 + n], 8, DM, stg)
    cast_load(k, S, PB, bPB, lambda c0, n: pbv[:, :, c0:c0 + n], 8, DM, stg)
    YATv = sc["YAT"].rearrange("(c p) t -> p c t", p=128); YBTv = sc["YBT"].rearrange("(c p) t -> p c t", p=128)
    ya = Ring([k.sb(f"p4ya{i}", [128, 8, 512], BF16) for i in range(2)])
    yb = Ring([k.sb(f"p4yb{i}", [128, 8, 512], BF16) for i in range(2)])
    ga = Ring([k.sb(f"p4ga{i}", [128, 512], BF16) for i in range(3)])
    gb = Ring([k.sb(f"p4gb{i}", [128, 512], BF16) for i in range(3)])
    t1 = Ring([k.sb(f"p4t1{i}", [128, 512], F32) for i in range(2)])
    t2 = Ring([k.sb(f"p4t2{i}", [128, 512], F32) for i in range(2)])
    mo = Ring([k.sb(f"p4mo{i}", [128, 512], BF16) for i in range(3)])
    psa = Ring([k.ps(f"p4pa{i}", [128, 512], F32) for i in range(2)])
    psb = Ring([k.ps(f"p4pb{i}", [128, 512], F32) for i in range(2)])
    for tt in range(NOWN // 512):
        t0 = tt * 512
        a_, ba = ya.next(); b_, bb = yb.next()
        S.dma("sp", lambda e, a_=a_, t0=t0: e.dma_start(out=a_[:], in_=YATv[:, :, t0:t0 + 512]), writes=[ba])
        S.dma("act", lambda e, b_=b_, t0=t0: e.dma_start(out=b_[:], in_=YBTv[:, :, t0:t0 + 512]), writes=[bb])
        for i in range(KC):
            g1, bg1 = ga.next(); g2, bg2 = gb.next()
            S.dma("sp", lambda e, g1=g1, i=i, t0=t0: e.dma_start(out=g1[:], in_=sc["GA"][i * 128:(i + 1) * 128, t0:t0 + 512]), writes=[bg1])
            S.dma("act", lambda e, g2=g2, i=i, t0=t0: e.dma_start(out=g2[:], in_=sc["GB"][i * 128:(i + 1) * 128, t0:t0 + 512]), writes=[bg2])
            p1, bp1 = psa.next(); p2, bp2 = psb.next()
            for kc in range(8):
                S.op("pe", lambda e, p1=p1, a_=a_, kc=kc, i=i: e.matmul(p1[:, :], lhsT=PA[:, kc, i * 128:(i + 1) * 128], rhs=a_[:, kc, :],
                                                                       start=(kc == 0), stop=(kc == 7)), reads=[bPA, ba], writes=[bp1])
            for kc in range(8):
                S.op("pe", lambda e, p2=p2, b_=b_, kc=kc, i=i: e.matmul(p2[:, :], lhsT=PB[:, kc, i * 128:(i + 1) * 128], rhs=b_[:, kc, :],
                                                                       start=(kc == 0), stop=(kc == 7)), reads=[bPB, bb], writes=[bp2])
            x1, bx1 = t1.next(); x2, bx2 = t2.next(); m_, bm = mo.next()
            S.op("dve", lambda e, x1=x1, p1=p1, g1=g1: e.tensor_tensor(out=x1[:], in0=p1[:, :], in1=g1[:], op=ALU.mult), reads=[bp1, bg1], writes=[bx1])
            S.op("dve", lambda e, x2=x2, p2=p2, g2=g2: e.tensor_tensor(out=x2[:], in0=p2[:, :], in1=g2[:], op=ALU.mult), reads=[bp2, bg2], writes=[bx2])
            S.op("pool", lambda e, m_=m_, x1=x1, x2=x2: e.tensor_tensor(out=m_[:], in0=x1[:], in1=x2[:], op=ALU.add), reads=[bx1, bx2], writes=[bm])
            S.dma("pool", lambda e, m_=m_, i=i, t0=t0: e.dma_start(out=sc["MT"][i * 128:(i + 1) * 128, t0:t0 + 512], in_=m_[:]), reads=[bm])


def phase4b(k, xe, w_out, prm):
    cfg, S, nc, c, sc = k.cfg, k.S, k.nc, k.c, k.sc
    DM, KC, NOWN, NPRE, NE, CAP = cfg.DM, cfg.KC, cfg.NOWN, cfg.NPRE, cfg.NE, cfg.CAP
    NT = NOWN // 128
    sc["H"] = k.scratch("H", [NOWN, DM], F32)
    sc["XE"] = k.scratch("XE", [NE * CAP, DM], BF16)
    k.phase()
    WO = k.sb("WOb", [128, KC, DM], BF16); bWO = Buf()
    stg = Ring([k.sb(f"p4bstg{i}", [128, KC, 128], F32) for i in range(2)])
    wov = w_out.rearrange("(c p) n -> p c n", p=128)
    cast_load(k, S, WO, bWO, lambda c0, n: wov[:, :, c0:c0 + n], KC, DM, stg, step=128)
    lw = k.sb("ln1w", [128, DM], F32); lb = k.sb("ln1b", [128, DM], F32); b_l = Buf()
    S.dma("sp", lambda e: e.dma_start(out=lw[:], in_=prm["ln1_w"][0:1, :].partition_broadcast(128)), writes=[b_l])
    S.dma("sp", lambda e: e.dma_start(out=lb[:], in_=prm["ln1_b"][0:1, :].partition_broadcast(128)), writes=[b_l])
    wr = k.sb("wr", [128, KC, NE], F32); b_wr = Buf()
    S.dma("sp", lambda e: e.dma_start(out=wr[:], in_=prm["w_router"].rearrange("(c p) n -> p c n", p=128)), writes=[b_wr])
    brt = k.sb("brt", [128, NE], F32)
    S.dma("sp", lambda e: e.dma_start(out=brt[:], in_=prm["b_router"][0:1, :].partition_broadcast(128)), writes=[b_wr])
    iot = k.sb("iot", [128, NE], F32); usf = k.sb("usf", [128, 128], F32); usb = k.sb("usb", [128, 128], BF16)
    onb = k.sb("onb", [128, 128], BF16); b_cst = Buf()
    S.dma("sp", lambda e: e.dma_start(out=iot[:], in_=prm["iota32"][:, :]), writes=[b_cst])
    S.dma("sp", lambda e: e.dma_start(out=usf[:], in_=prm["ustrict"][:, :]), writes=[b_cst])
    S.op("act", lambda e: e.activation(out=usb[:], in_=usf[:], func=AF.Copy), reads=[b_cst], writes=[b_cst])
    S.op("pool", lambda e: e.memset(onb[:], 1.0), writes=[b_cst])
    base = k.sb("rbase", [128, NE], F32); b_base = Buf()
    S.op("pool", lambda e: e.memset(base[:], 0.0), writes=[b_base])
    zt = k.sb("zt", [128, DM], BF16); b_zt = Buf()
    S.op("pool", lambda e: e.memset(zt[:], 0.0), writes=[b_zt])
    b_XE = Buf()
    for r0 in range(0, NE * CAP, 128):
        S.dma("sp" if (r0 // 128) % 2 == 0 else "act", lambda e, r0=r0: e.dma_start(out=sc["XE"][r0:r0 + 128, :], in_=zt[:]), reads=[b_zt], writes=[b_XE])
    MTv = sc["MT"].rearrange("(c p) t -> p c t", p=128)
    mt = Ring([k.sb(f"p4mt{i}", [128, KC, 128], BF16) for i in range(2)])
    xt = Ring([k.sb(f"p4xt{i}", [128, DM], F32) for i in range(1)])
    zz = Ring([k.sb(f"p4z{i}", [128, DM], F32) for i in range(2)])
    hb = Ring([k.sb(f"p4hb{i}", [128, DM], BF16) for i in range(2)])
    hT = Ring([k.sb(f"p4hT{i}", [128, KC, 128], F32) for i in range(1)])
    sm = Ring([k.sb(f"p4sm{i}", [128, 256], F32) for i in range(2)])
    oh = Ring([k.sb(f"p4oh{i}", [128, 4, NE], F32) for i in range(2)])
    pr = Ring([k.sb(f"p4pr{i}", [128, 4, NE], F32) for i in range(2)])
    selb = Ring([k.sb(f"p4selb{i}", [128, NE], BF16) for i in range(2)])
    bst = Ring([k.sb(f"p4bst{i}", [128, 4, 6], F32) for i in range(2)])
    pz = Ring([k.ps(f"p4pz{i}", [128, 512], F32) for i in range(3)])
    pT = Ring([k.ps(f"p4T{i}", [128, 4, 128], F32) for i in range(2)])
    pl = Ring([k.ps("p4l", [128, 128], F32)])
    for ti in range(NT):
        tok0 = ti * 128
        m_, bm = mt.next(); x_, bx = xt.next(); z_, bz = zz.next()
        S.dma("sp", lambda e, m_=m_, tok0=tok0: e.dma_start(out=m_[:], in_=MTv[:, :, tok0:tok0 + 128]), writes=[bm])
        S.dma("act", lambda e, x_=x_, tok0=tok0: e.dma_start(out=x_[:], in_=xe[NPRE + tok0:NPRE + tok0 + 128, :]), writes=[bx])
        s_, bs = bst.next()
        ncg = DM // 512 if DM >= 512 else 1
        cw = DM // ncg
        for cg in range(ncg):
            p_, bp = pz.next()
            for kc in range(KC):
                S.op("pe", lambda e, p_=p_, m_=m_, kc=kc, cg=cg: e.matmul(p_[:, 0:cw], lhsT=m_[:, kc, :], rhs=WO[:, kc, cg * cw:(cg + 1) * cw],
                                                                         start=(kc == 0), stop=(kc == KC - 1)), reads=[bm, bWO], writes=[bp])
            S.op("dve", lambda e, z_=z_, x_=x_, p_=p_, cg=cg: e.scalar_tensor_tensor(out=z_[:, cg * cw:(cg + 1) * cw], in0=x_[:, cg * cw:(cg + 1) * cw], scalar=float(cfg.alpha),
                                                                                   in1=p_[:, 0:cw], op0=ALU.mult, op1=ALU.add), reads=[bx, bp], writes=[bz])
            S.op("dve", lambda e, s_=s_, z_=z_, cg=cg: e.bn_stats(out=s_[:, cg, :], in_=z_[:, cg * cw:(cg + 1) * cw]), reads=[bz], writes=[bs])
        q_, bq = sm.next()
        S.op("dve", lambda e, q_=q_, s_=s_: e.bn_aggr(out=q_[:, 0:2], in_=s_[:, 0:ncg, :].rearrange("p a b -> p (a b)")), reads=[bs], writes=[bq])
        S.op("dve", lambda e, q_=q_: e.tensor_scalar(out=q_[:, 2:3], in0=q_[:, 1:2], scalar1=1e-5, scalar2=None, op0=ALU.add), reads=[bq], writes=[bq])
        S.op("act", lambda e, q_=q_: e.activation(out=q_[:, 3:4], in_=q_[:, 2:3], func=AF.Sqrt), reads=[bq], writes=[bq])
        S.op("dve", lambda e, q_=q_: e.reciprocal(out=q_[:, 4:5], in_=q_[:, 3:4]), reads=[bq], writes=[bq])
        S.op("dve", lambda e, z_=z_, q_=q_: e.tensor_scalar(out=z_[:], in0=z_[:], scalar1=q_[:, 0:1], scalar2=q_[:, 4:5], op0=ALU.subtract, op1=ALU.mult),
             reads=[bz, bq], writes=[bz])
        S.op("pool", lambda e, z_=z_: e.tensor_tensor(out=z_[:], in0=z_[:], in1=lw[:], op=ALU.mult), reads=[bz, b_l], writes=[bz])
        S.op("dve", lambda e, z_=z_: e.tensor_tensor(out=z_[:], in0=z_[:], in1=lb[:], op=ALU.add), reads=[bz, b_l], writes=[bz])
        S.dma("sp", lambda e, z_=z_, tok0=tok0: e.dma_start(out=sc["H"][tok0:tok0 + 128, :], in_=z_[:]), reads=[bz])
        h_, bh = hb.next()
        S.op("act", lambda e, h_=h_, z_=z_: e.activation(out=h_[:], in_=z_[:], func=AF.Copy), reads=[bz], writes=[bh])
        t_, bt = hT.next()
        for g0 in range(0, KC, 4):
            ng = min(4, KC - g0)
            pt, bpt = pT.next()
            for j in range(ng):
                S.op("pe", lambda e, pt=pt, z_=z_, j=j, g0=g0: e.transpose(out=pt[:, j, :], in_=z_[:, (g0 + j) * 128:(g0 + j + 1) * 128], identity=c["ident_f"][:]),
                     reads=[bz, c["b_ident"]], writes=[bpt])
            S.op("act", lambda e, t_=t_, pt=pt, g0=g0, ng=ng: e.activation(out=t_[:, g0:g0 + ng, :], in_=pt[:, 0:ng, :], func=AF.Copy), reads=[bpt], writes=[bt])
        pl_, bpl = pl.next()
        for kc in range(KC):
            S.op("pe", lambda e, pl_=pl_, t_=t_, kc=kc: e.matmul(pl_[:, 0:NE], lhsT=t_[:, kc, :], rhs=wr[:, kc, :], start=(kc == 0), stop=(kc == KC - 1)),
                 reads=[bt, b_wr], writes=[bpl])
        lg = q_[:, 8:8 + NE]
        S.op("dve", lambda e, lg=lg, pl_=pl_: e.tensor_tensor(out=lg, in0=pl_[:, 0:NE], in1=brt[:], op=ALU.add), reads=[bpl, b_wr], writes=[bq])
        top = q_[:, 48:56]
        S.op("dve", lambda e, top=top, lg=lg: e.max(out=top, in_=lg), reads=[bq], writes=[bq])
        S.op("dve", lambda e, q_=q_: e.tensor_scalar(out=q_[:, 56:57], in0=q_[:, 48:49], scalar1=-1.0, scalar2=None, op0=ALU.mult), reads=[bq], writes=[bq])
        S.op("act", lambda e, q_=q_: e.activation(out=q_[:, 60:64], in_=q_[:, 48:52], func=AF.Exp, bias=q_[:, 56:57]), reads=[bq], writes=[bq])
        S.op("dve", lambda e, q_=q_: e.tensor_reduce(out=q_[:, 57:58], in_=q_[:, 60:64], axis=AX.X, op=ALU.add), reads=[bq], writes=[bq])
        S.op("dve", lambda e, q_=q_: e.reciprocal(out=q_[:, 58:59], in_=q_[:, 57:58]), reads=[bq], writes=[bq])
        S.op("dve", lambda e, q_=q_, ti=ti: e.tensor_scalar(out=k.gate_all[:, ti, :], in0=q_[:, 60:64], scalar1=q_[:, 58:59], scalar2=None, op0=ALU.mult),
             reads=[bq], writes=[k.b_gate])
        o_, bo = oh.next()
        for kk_ in range(4):
            S.op("dve" if kk_ % 2 == 0 else "pool", lambda e, o_=o_, lg=lg, q_=q_, kk_=kk_: e.tensor_scalar(
                out=o_[:, kk_, :], in0=lg, scalar1=q_[:, 48 + kk_:49 + kk_], scalar2=None, op0=ALU.is_equal), reads=[bq], writes=[bo])
        sel = q_[:, 64:64 + NE]
        S.op("dve", lambda e, sel=sel, o_=o_: e.tensor_reduce(out=sel, in_=o_[:].rearrange("p k e -> p e k"), axis=AX.X, op=ALU.add), reads=[bo], writes=[bq])
        sb_, bsb = selb.next()
        S.op("act", lambda e, sb_=sb_, sel=sel: e.activation(out=sb_[:], in_=sel, func=AF.Copy), reads=[bq], writes=[bsb])
        pl2, bpl2 = pl.next()
        S.op("pe", lambda e, pl2=pl2, sb_=sb_: e.matmul(pl2[:, 0:NE], lhsT=usb[:, :], rhs=sb_[:, :], start=True, stop=True), reads=[b_cst, bsb], writes=[bpl2])
        S.op("pe", lambda e, pl2=pl2, sb_=sb_: e.matmul(pl2[:, 64:64 + NE], lhsT=onb[:, :], rhs=sb_[:, :], start=True, stop=True), reads=[b_cst, bsb], writes=[bpl2])
        pos = q_[:, 96:96 + NE]
        S.op("dve", lambda e, pos=pos, pl2=pl2: e.tensor_tensor(out=pos, in0=pl2[:, 0:NE], in1=base[:], op=ALU.add), reads=[bpl2, b_base], writes=[bq])
        S.op("dve", lambda e, pl2=pl2: e.tensor_tensor(out=base[:], in0=pl2[:, 64:64 + NE], in1=base[:], op=ALU.add), reads=[bpl2, b_base, bq], writes=[b_base])
        p_r, bpr = pr.next()
        bck = lambda ap: ap.unsqueeze(1).to_broadcast([128, 4, NE])
        S.op("pool", lambda e, p_r=p_r, o_=o_: e.tensor_tensor(out=p_r[:], in0=o_[:], in1=bck(iot[:, :]), op=ALU.mult), reads=[bo, b_cst], writes=[bpr])
        S.op("dve", lambda e, q_=q_, p_r=p_r: e.tensor_reduce(out=q_[:, 128:132], in_=p_r[:], axis=AX.X, op=ALU.add), reads=[bpr], writes=[bq])
        p_r2, bpr2 = pr.next()
        S.op("pool", lambda e, p_r2=p_r2, o_=o_, pos=pos: e.tensor_tensor(out=p_r2[:], in0=o_[:], in1=bck(pos), op=ALU.mult), reads=[bo, bq], writes=[bpr2])
        S.op("dve", lambda e, q_=q_, p_r2=p_r2: e.tensor_reduce(out=q_[:, 132:136], in_=p_r2[:], axis=AX.X, op=ALU.add), reads=[bpr2], writes=[bq])
        S.op("dve", lambda e, q_=q_: e.scalar_tensor_tensor(out=q_[:, 136:140], in0=q_[:, 128:132], scalar=float(CAP), in1=q_[:, 132:136], op0=ALU.mult, op1=ALU.add),
             reads=[bq], writes=[bq])
        S.op("dve", lambda e, q_=q_: e.tensor_scalar(out=q_[:, 140:144], in0=q_[:, 132:136], scalar1=float(CAP), scalar2=1.0e7, op0=ALU.is_ge, op1=ALU.mult),
             reads=[bq], writes=[bq])
        S.op("dve", lambda e, q_=q_: e.tensor_tensor(out=q_[:, 144:148], in0=q_[:, 136:140], in1=q_[:, 140:144], op=ALU.add), reads=[bq], writes=[bq])
        S.op("dve", lambda e, q_=q_, ti=ti: e.tensor_copy(out=k.dest_all[:, ti, :], in_=q_[:, 144:148]), reads=[bq], writes=[k.b_dest])
        for kk_ in range(4):
            S.dma("pool", lambda e, h_=h_, ti=ti, kk_=kk_: e.indirect_dma_start(
                out=sc["XE"][:, :], out_offset=bass.IndirectOffsetOnAxis(ap=k.dest_all[:, ti, kk_:kk_ + 1], axis=0),
                in_=h_[:, :], in_offset=None, bounds_check=_bound_reg(e, NE * CAP - 1), oob_is_err=False), reads=[bh, k.b_dest, b_XE])


def phase5(k, w_gu, b_gu, w_down, b_down):
    cfg, S, nc, c, sc = k.cfg, k.S, k.nc, k.c, k.sc
    DM, KC, NE, CAP, FF = cfg.DM, cfg.KC, cfg.NE, cfg.CAP, cfg.FF
    FC = FF // 128
    NS = CAP // 128
    nhalf = 2 if CAP > 512 else 1
    HALF = CAP // nhalf
    GW = min(512, FF)
    DW = min(512, DM)
    KH = max(1, KC // 2)
    sc["YE"] = k.scratch("YE", [NE * CAP, DM], F32)
    k.phase()
    bgT = k.bgT; b_bgT = k.b_bgT
    bgf = k.sb("bgf", [NE, 2 * FF], F32); b_bgf = Buf()
    S.dma("sp", lambda e: e.dma_start(out=bgf[:], in_=b_gu[:, :]), writes=[b_bgf])
    pTf = Ring([k.ps("p5Tf", [128, 4, NE], F32)])
    for g0 in range(0, 2 * FC, 4):
        pt, bpt = pTf.next()
        for j in range(4):
            S.op("pe", lambda e, pt=pt, j=j, g0=g0: e.transpose(out=pt[:, j, :], in_=bgf[:, (g0 + j) * 128:(g0 + j + 1) * 128], identity=c["ident_f"][0:NE, 0:NE]),
                 reads=[b_bgf, c["b_ident"]], writes=[bpt])
        S.op("act", lambda e, pt=pt, g0=g0: e.activation(out=bgT[:, g0:g0 + 4, :], in_=pt[:], func=AF.Copy), reads=[bpt], writes=[b_bgT])
    k.phase()
    xs = Ring([k.sb(f"p5xs{i}", [128, NS, DM], BF16) for i in range(1)])
    XT = Ring([k.sb(f"p5XT{i}", [128, KC, CAP], BF16) for i in range(1)])
    HT = Ring([k.sb(f"p5HT{i}", [128, FC, CAP], BF16) for i in range(1)])
    KP = max(1, KC // 4)
    NPK = KC // KP
    ws = Ring([k.sb(f"p5ws{i}", [128, KP, GW], F32) for i in range(5)])
    wb = [k.sb(f"p5wb{i}", [128, KC, 2 * GW], BF16) for i in range(2)]
    b_wb = [[Buf() for _ in range(2 * NPK)] for _ in range(2)]
    bd = Ring([k.sb(f"p5bd{i}", [128, DM], F32) for i in range(2)])
    tg = Ring([k.sb(f"p5tg{i}", [128, HALF], F32) for i in range(2)])
    tsg = Ring([k.sb(f"p5ts{i}", [128, HALF], F32) for i in range(2)])
    tu = Ring([k.sb(f"p5tu{i}", [128, HALF], F32) for i in range(2)])
    tgs = Ring([k.sb(f"p5tgs{i}", [128, HALF], F32) for i in range(2)])
    yst = Ring([k.sb(f"p5yst{i}", [128, DW], F32) for i in range(4)])
    pT = Ring([k.ps(f"p5T{i}", [128, 8, 128], BF16) for i in range(2)])
    pg = Ring([k.ps(f"p5g{i}", [128, 512], F32) for i in range(2)])
    pu = Ring([k.ps(f"p5u{i}", [128, 512], F32) for i in range(2)])
    py = Ring([k.ps(f"p5y{i}", [128, 512], F32) for i in range(2)])
    wgv = w_gu.rearrange("e (c p) n -> e p c n", p=128)
    wdv = w_down.rearrange("e (c p) n -> e p c n", p=128)
    cnt = [0]
    nblk = 2 * GW // DW
    groups = []
    for ex in range(NE):
        for gw in range(FF // GW):
            groups.append(("gu", ex, gw))
        for cb0 in range(0, DM // DW, nblk):
            groups.append(("dn", ex, cb0))

    def load_piece(wb_, bw_piece, src_fn, kc0, dcol, ncol):
        w_, bw = ws.next()
        cnt[0] += 1
        S.dma("sp", lambda e, w_=w_: e.dma_start(out=w_[:, 0:KP, 0:ncol], in_=src_fn()), writes=[bw])
        if cnt[0] % 2 == 0:
            S.op("act", lambda e, w_=w_, wb_=wb_: e.activation(out=wb_[:, kc0:kc0 + KP, dcol:dcol + ncol], in_=w_[:, 0:KP, 0:ncol], func=AF.Copy),
                 reads=[bw], writes=[bw_piece])
        else:
            S.op("dve", lambda e, w_=w_, wb_=wb_: e.tensor_copy(out=wb_[:, kc0:kc0 + KP, dcol:dcol + ncol], in_=w_[:, 0:KP, 0:ncol]),
                 reads=[bw], writes=[bw_piece])

    def piece_loads(gi):
        kind, ex, idx = groups[gi]
        wb_, bl = wb[gi % 2], b_wb[gi % 2]
        out = []
        if kind == "gu":
            for part in range(2):
                for kc0 in range(0, KC, KP):
                    c0 = part * FF + idx * GW
                    out.append((wb_, bl[part * NPK + kc0 // KP], (lambda ex=ex, kc0=kc0, c0=c0: wgv[ex, :, kc0:kc0 + KP, c0:c0 + GW]), kc0, part * GW, GW))
        else:
            nb = min(nblk, DM // DW - idx)
            for bi in range(nb):
                for kc0 in range(0, FC, KP):
                    c0 = (idx + bi) * DW
                    out.append((wb_, bl[bi * NPK + kc0 // KP], (lambda ex=ex, kc0=kc0, c0=c0: wdv[ex, :, kc0:kc0 + KP, c0:c0 + DW]), kc0, bi * DW, DW))
        return out

    x_, bx = xs.next(); xt, bxt = XT.next(); ht, bht = HT.next()
    cur = {}

    def xload(ex):
        S.dma("act", lambda e, ex=ex: e.dma_start(out=x_[:], in_=sc["XE"][ex * CAP:(ex + 1) * CAP, :].rearrange("(s p) d -> p s d", p=128)), writes=[bx])

    def preamble(ex):
        for s in range(NS):
            for g0 in range(0, KC, 8):
                ng = min(8, KC - g0)
                pt, bpt = pT.next()
                for j in range(ng):
                    S.op("pe", lambda e, pt=pt, s=s, j=j, g0=g0: e.transpose(out=pt[:, j, :], in_=x_[:, s, (g0 + j) * 128:(g0 + j + 1) * 128], identity=c["ident_b"][:]),
                         reads=[bx, c["b_ident"]], writes=[bpt])
                S.op("act", lambda e, pt=pt, s=s, g0=g0, ng=ng: e.activation(out=xt[:, g0:g0 + ng, s * 128:(s + 1) * 128], in_=pt[:, 0:ng, :], func=AF.Copy),
                     reads=[bpt], writes=[bxt])
        cur["b"] = bd.next()
        b_, bb = cur["b"]
        S.dma("act", lambda e, b_=b_, ex=ex: e.dma_start(out=b_[:], in_=b_down[ex:ex + 1, :].partition_broadcast(128)), writes=[bb])
        if ex + 1 < NE:
            xload(ex + 1)

    def gu_unit(ex, gw, fl, hf, wb_, bl):
        fb = gw * (GW // 128) + fl
        sl = slice(hf * HALF, (hf + 1) * HALF)
        pg_, bpg = pg.next(); pu_, bpu = pu.next()
        for kc in range(KC):
            S.op("pe", lambda e, pg_=pg_, kc=kc: e.matmul(pg_[:, 0:HALF], lhsT=wb_[:, kc, fl * 128:(fl + 1) * 128], rhs=xt[:, kc, sl],
                                                        start=(kc == 0), stop=(kc == KC - 1)), reads=[bl[kc // KP], bxt], writes=[bpg])
        for kc in range(KC):
            S.op("pe", lambda e, pu_=pu_, kc=kc: e.matmul(pu_[:, 0:HALF], lhsT=wb_[:, kc, GW + fl * 128:GW + (fl + 1) * 128], rhs=xt[:, kc, sl],
                                                        start=(kc == 0), stop=(kc == KC - 1)), reads=[bl[NPK + kc // KP], bxt], writes=[bpu])
        g_, bg = tg.next(); s_, bs = tsg.next(); u_, bu = tu.next(); gs_, bgs = tgs.next()
        S.op("dve", lambda e: e.tensor_scalar(out=g_[:], in0=pg_[:, 0:HALF], scalar1=bgT[:, fb, ex:ex + 1], scalar2=7.0,
                                              op0=ALU.add, op1=ALU.min), reads=[bpg, b_bgT], writes=[bg])
        S.op("act", lambda e: e.activation(out=s_[:], in_=g_[:], func=AF.Sigmoid, scale=1.702), reads=[bg], writes=[bs])
        S.op("dve", lambda e: e.tensor_scalar(out=u_[:], in0=pu_[:, 0:HALF], scalar1=bgT[:, FC + fb, ex:ex + 1], scalar2=7.0,
                                              op0=ALU.add, op1=ALU.min), reads=[bpu, b_bgT], writes=[bu])
        S.op("dve", lambda e: e.tensor_scalar(out=u_[:], in0=u_[:], scalar1=-7.0, scalar2=1.0, op0=ALU.max, op1=ALU.add), reads=[bu], writes=[bu])
        S.op("pool", lambda e: e.tensor_tensor(out=gs_[:], in0=g_[:], in1=s_[:], op=ALU.mult), reads=[bg, bs], writes=[bgs])
        S.op("pool", lambda e: e.tensor_tensor(out=ht[:, fb, sl], in0=gs_[:], in1=u_[:], op=ALU.mult), reads=[bgs, bu], writes=[bht])

    def dn_unit(ex, cb, bi, s, wb_, bl):
        b_, bb = cur["b"]
        py_, bpy = py.next()
        for fc in range(FC):
            S.op("pe", lambda e, fc=fc: e.matmul(py_[:, 0:DW], lhsT=ht[:, fc, s * 128:(s + 1) * 128], rhs=wb_[:, fc, bi * DW:(bi + 1) * DW],
                                               start=(fc == 0), stop=(fc == FC - 1)), reads=[bht, bl[bi * NPK + fc // KP]], writes=[bpy])
        y_, by = yst.next()
        S.op("dve", lambda e: e.tensor_tensor(out=y_[:], in0=py_[:, 0:DW], in1=b_[:, cb * DW:(cb + 1) * DW], op=ALU.add), reads=[bpy, bb], writes=[by])
        r0 = ex * CAP + s * 128
        S.dma("pool", lambda e: e.dma_start(out=sc["YE"][r0:r0 + 128, cb * DW:(cb + 1) * DW], in_=y_[:]), reads=[by])

    def make_units(gi):
        kind, ex, idx = groups[gi]
        wb_, bl = wb[gi % 2], b_wb[gi % 2]
        if kind == "gu":
            return [(lambda fl=fl, hf=hf: gu_unit(ex, idx, fl, hf, wb_, bl)) for fl in range(GW // 128) for hf in range(nhalf)]
        nb = min(nblk, DM // DW - idx)
        return [(lambda bi=bi, s=s: dn_unit(ex, idx + bi, bi, s, wb_, bl)) for bi in range(nb) for s in range(NS)]

    xload(0)
    for ld in piece_loads(0):
        load_piece(*ld)
    for gi in range(len(groups)):
        kind, ex, idx = groups[gi]
        nxt = piece_loads(gi + 1) if gi + 1 < len(groups) else []
        units = make_units(gi)
        if kind == "gu" and idx == 0:
            preamble(ex)
        nu = len(units)
        for ui, u in enumerate(units):
            for ld in nxt[ui * len(nxt) // nu:(ui + 1) * len(nxt) // nu]:
                load_piece(*ld)
            u()


def phase6(k, out, prm):
    cfg, S, nc, c, sc = k.cfg, k.S, k.nc, k.c, k.sc
    DM, NOWN, NE, CAP = cfg.DM, cfg.NOWN, cfg.NE, cfg.CAP
    NT = NOWN // 128
    k.phase()
    lw = k.sb("ln2w", [128, DM], F32); lb = k.sb("ln2b", [128, DM], F32); b_l = Buf()
    S.dma("sp", lambda e: e.dma_start(out=lw[:], in_=prm["ln2_w"][0:1, :].partition_broadcast(128)), writes=[b_l])
    S.dma("sp", lambda e: e.dma_start(out=lb[:], in_=prm["ln2_b"][0:1, :].partition_broadcast(128)), writes=[b_l])
    hh = Ring([k.sb(f"p6h{i}", [128, DM], F32) for i in range(2)])
    yk = Ring([k.sb(f"p6y{i}", [128, DM], F32) for i in range(4)])
    sm = Ring([k.sb(f"p6sm{i}", [128, 16], F32) for i in range(2)])
    bst = Ring([k.sb(f"p6bst{i}", [128, 4, 6], F32) for i in range(2)])
    ncg = DM // 512 if DM >= 512 else 1
    cw = DM // ncg
    for ti in range(NT):
        tok0 = ti * 128
        h_, bh = hh.next()
        S.dma("sp", lambda e, h_=h_, tok0=tok0: e.dma_start(out=h_[:], in_=sc["H"][tok0:tok0 + 128, :]), writes=[bh])
        S.op("dve", lambda e, h_=h_: e.tensor_scalar(out=h_[:], in0=h_[:], scalar1=float(cfg.alpha), scalar2=None, op0=ALU.mult), reads=[bh], writes=[bh])
        for kk_ in range(4):
            y_, by = yk.next()
            S.op("pool", lambda e, y_=y_: e.memset(y_[:], 0.0), writes=[by])
            S.dma("pool", lambda e, y_=y_, ti=ti, kk_=kk_: e.indirect_dma_start(
                out=y_[:, :], out_offset=None, in_=sc["YE"][:, :], in_offset=bass.IndirectOffsetOnAxis(ap=k.dest_all[:, ti, kk_:kk_ + 1], axis=0),
                bounds_check=_bound_reg(e, NE * CAP - 1), oob_is_err=False), reads=[k.b_dest], writes=[by])
            S.op("dve", lambda e, h_=h_, y_=y_, ti=ti, kk_=kk_: e.scalar_tensor_tensor(out=h_[:], in0=y_[:], scalar=k.gate_all[:, ti, kk_:kk_ + 1], in1=h_[:],
                                                                                     op0=ALU.mult, op1=ALU.add), reads=[by, bh, k.b_gate], writes=[bh])
        s_, bs = bst.next(); q_, bq = sm.next()
        for cg in range(ncg):
            S.op("dve", lambda e, s_=s_, h_=h_, cg=cg: e.bn_stats(out=s_[:, cg, :], in_=h_[:, cg * cw:(cg + 1) * cw]), reads=[bh], writes=[bs])
        S.op("dve", lambda e, q_=q_, s_=s_: e.bn_aggr(out=q_[:, 0:2], in_=s_[:, 0:ncg, :].rearrange("p a b -> p (a b)")), reads=[bs], writes=[bq])
        S.op("dve", lambda e, q_=q_: e.tensor_scalar(out=q_[:, 2:3], in0=q_[:, 1:2], scalar1=1e-5, scalar2=None, op0=ALU.add), reads=[bq], writes=[bq])
        S.op("act", lambda e, q_=q_: e.activation(out=q_[:, 3:4], in_=q_[:, 2:3], func=AF.Sqrt), reads=[bq], writes=[bq])
        S.op("dve", lambda e, q_=q_: e.reciprocal(out=q_[:, 4:5], in_=q_[:, 3:4]), reads=[bq], writes=[bq])
        S.op("dve", lambda e, h_=h_, q_=q_: e.tensor_scalar(out=h_[:], in0=h_[:], scalar1=q_[:, 0:1], scalar2=q_[:, 4:5], op0=ALU.subtract, op1=ALU.mult),
             reads=[bh, bq], writes=[bh])
        S.op("pool", lambda e, h_=h_: e.tensor_tensor(out=h_[:], in0=h_[:], in1=lw[:], op=ALU.mult), reads=[bh, b_l], writes=[bh])
        S.op("dve", lambda e, h_=h_: e.tensor_tensor(out=h_[:], in0=h_[:], in1=lb[:], op=ALU.add), reads=[bh, b_l], writes=[bh])
        S.dma("sp", lambda e, h_=h_, tok0=tok0: e.dma_start(out=out[tok0:tok0 + 128, :], in_=h_[:]), reads=[bh])


HORD = [0, 2, 4, 6, 1, 3, 5, 7, 8, 10, 12, 14, 9, 11, 13, 15]


def build_full(cfg, dbg=()):
    k = K(cfg, dbg=dbg)
    DM, NE = cfg.DM, cfg.NE
    xe = k.inp("xe", [cfg.NTOK, DM])
    w_in = k.inp("w_in", [DM, cfg.DIN])
    mu_cols = k.inp("mu_cols", [128, 28])
    att_bias = k.inp("att_bias", [128, 5, 16, 128]); halo_mask = k.inp("halo_mask", [128, 1])
    prm = {n: k.inp(n, s) for n, s in [
        ("w_up", [96, 1024]), ("a_up", [96, 1024]), ("g_up", [256, 1024]), ("hcols", [64, 6, 16]), ("rmask", [64, 512]),
        ("m_su", [64, 8, 64]), ("m_ui", [64, 8, 64]), ("m_sl", [64, 8, 64]), ("i8", [64, 8, 64]),
        ("lnx_w", [1, 1024]), ("lnx_b", [1, 1024]), ("ln1_w", [1, DM]), ("ln1_b", [1, DM]), ("ln2_w", [1, DM]), ("ln2_b", [1, DM]),
        ("w_router", [DM, NE]), ("b_router", [1, NE]), ("iota32", [128, NE]), ("ustrict", [128, 128])]}
    proj_a = k.inp("proj_a", [1024, DM]); proj_b = k.inp("proj_b", [1024, DM]); w_out = k.inp("w_out", [DM, DM])
    w_gu = k.inp("w_gu", [NE, DM, 2 * cfg.FF]); b_gu = k.inp("b_gu", [NE, 2 * cfg.FF])
    w_down = k.inp("w_down", [NE, cfg.FF, DM]); b_down = k.inp("b_down", [NE, DM])
    out = k.nc.dram_tensor("out", [cfg.NOWN, DM], F32, kind="ExternalOutput").ap()
    load_consts(k)
    phase1(k, xe, w_in, mu_cols)
    phase2(k, att_bias, halo_mask)
    phase3(k, prm)
    phase3b(k, prm)
    phase4a(k, proj_a, proj_b)
    phase4b(k, xe, w_out, prm)
    phase5(k, w_gu, b_gu, w_down, b_down)
    phase6(k, out, prm)
    k.S.finish()
    return k


def host_common(inp, cfg):
    f = lambda a: np.ascontiguousarray(np.asarray(a, dtype=np.float32))
    smu = f(inp["shift_mu"][0])
    m = np.zeros((128, 28), np.float32)
    for ci in range(24):
        m[:, ci] = smu[ci * 128:(ci + 1) * 128]
    m[:96, 24] = smu[3072:3168]; m[:96, 25] = smu[3168:3264]
    m[:, 26] = smu[3264:3392]; m[:, 27] = smu[3392:3520]
    relb = f(inp["rel_bias"][0])
    kr = np.arange(640)[:, None]; q = np.arange(128)[None, :]
    dist = 512 + q - kr
    qc = (512 + q) // 64; kc = kr // 64
    valid = (qc - kc >= 0) & (qc - kc <= 8)
    idx = np.clip(np.minimum(dist, 256) + 63, 0, 319)
    b = relb[:, idx]
    b = np.where(valid[None], b, np.float32(-30000.0)).astype(np.float32)[HORD]
    att_bias = np.ascontiguousarray(b.reshape(16, 5, 128, 128).transpose(2, 1, 0, 3))
    hv = lambda v: f(v).reshape(16, 64).T
    k_a = f(inp["k_a"][0])
    one_minus_ka = np.zeros_like(k_a)
    hcols = np.ascontiguousarray(np.stack([hv(inp["w0"][0]), hv(inp["a0"][0]), hv(inp["k_k"][0]), hv(k_a), hv(one_minus_ka), hv(inp["r_k"][0])], 1))
    rmask = np.ones((64, 512), np.float32); rmask[:, ::64] = 0
    j = np.arange(64)[:, None]; t = np.arange(64)[None, :]
    rep = lambda mm: np.ascontiguousarray(np.repeat(mm.astype(np.float32)[:, None, :], 8, 1))
    r2 = lambda v: f(v).reshape(1, -1)
    com = {
        "w_in": f(inp["w_in"][0]), "mu_cols": m, "att_bias": att_bias, "c_ident": np.eye(128, dtype=np.float32),
        "w_up": f(inp["w_up"][0]), "a_up": f(inp["a_up"][0]), "g_up": f(inp["g_up"][0]), "hcols": hcols, "rmask": rmask,
        "m_su": rep(j < t), "m_ui": rep(j <= t), "m_sl": rep(j > t), "i8": rep(j == t),
        "lnx_w": r2(inp["lnx_w"][0]), "lnx_b": r2(inp["lnx_b"][0]), "ln1_w": r2(inp["ln1_w"][0]), "ln1_b": r2(inp["ln1_b"][0]),
        "ln2_w": r2(inp["ln2_w"][0]), "ln2_b": r2(inp["ln2_b"][0]),
        "w_router": f(inp["w_router"][0]), "b_router": r2(inp["b_router"][0]),
        "iota32": np.ascontiguousarray(np.broadcast_to(np.arange(cfg.NE, dtype=np.float32), (128, cfg.NE))),
        "ustrict": (np.arange(128)[:, None] < np.arange(128)[None, :]).astype(np.float32),
        "proj_a": f(inp["proj_a"][0]), "proj_b": f(inp["proj_b"][0]), "w_out": f(inp["w_out"][0]),
        "w_gu": f(inp["w_gu"][0]), "b_gu": f(inp["b_gu"][0]), "w_down": f(inp["w_down"][0]), "b_down": f(inp["b_down"][0]),
    }
    return com


def host_core(x, cfg, b, half):
    NOWN = cfg.NOWN
    xe = np.zeros((cfg.NTOK, cfg.DM), np.float32)
    if half == 0:
        xe[cfg.NPRE:] = x[b, 0:NOWN]
        hm = np.full((128, 1), -30000.0, np.float32)
    else:
        xe[:] = x[b, 0:2 * NOWN]
        hm = np.zeros((128, 1), np.float32)
    return {"xe": xe, "halo_mask": hm}


_CACHE = {}


def kernel(**inputs):
    cfg = Cfg()
    if "k" not in _CACHE:
        _CACHE["k"] = build_full(cfg)
    k = _CACHE["k"]
    com = host_common(inputs, cfg)
    x = np.asarray(inputs["x"], dtype=np.float32)
    in_maps = []
    for core in range(8):
        m = dict(com)
        m.update(host_core(x, cfg, core // 2, core % 2))
        in_maps.append(m)
    res = run_bass_kernel_spmd(k.nc, in_maps, core_ids=list(range(8)))
    out = np.empty((4, 2 * cfg.NOWN, cfg.DM), np.float32)
    for core in range(8):
        h = core % 2
        out[core // 2, h * cfg.NOWN:(h + 1) * cfg.NOWN] = np.asarray(res.results[core]["out"])
    return out
```

```python
import contextlib
import os
import numpy as np
import concourse.bass as bass
import concourse.mybir as mybir
from concourse.bass_utils import run_bass_kernel_spmd


class Buf:
    __slots__ = ("name", "w", "r")

    def __init__(self, name=""):
        self.name = name
        self.w = None
        self.r = []


class Sched:
    ENGS = ("pe", "act", "dve", "pool", "sp")

    def __init__(self, nc, ndma_sems=10):
        self.nc = nc
        self.prog = {e: [] for e in self.ENGS}
        self.sems = {}
        self.cnt = {}
        self.known = {e: {} for e in self.ENGS}
        self._stack = []
        for e in ("pe", "act", "dve", "pool"):
            self._mksem("E_" + e)
        self.dpool = {}
        for q in ("sp", "pool", "act"):
            names = [f"D_{q}{i}" for i in range(ndma_sems)]
            for n in names:
                self._mksem(n)
            self.dpool[q] = [names, 0]
        self.ninstr = 0

    def _mksem(self, name):
        cm = self.nc.semaphore(name)
        s = cm.__enter__()
        self._stack.append(cm)
        self.sems[name] = s
        self.cnt[name] = 0

    def _wait(self, eng, ev):
        key, val = ev
        if eng == "pe" and key == "E_pe":
            return
        if self.known[eng].get(key, 0) >= val:
            return
        self.known[eng][key] = val
        sem = self.sems[key]
        self.prog[eng].append(lambda e, sem=sem, val=val: e.wait_ge(sem, val))

    def _deps(self, eng, reads, writes):
        for b in reads:
            if b.w is not None:
                self._wait(eng, b.w)
        for b in writes:
            if b.w is not None:
                self._wait(eng, b.w)
            for ev in b.r:
                self._wait(eng, ev)

    def _commit(self, ev, reads, writes):
        for b in writes:
            b.w = ev
            b.r = []
        for b in reads:
            if b.w is ev:
                continue
            b.r.append(ev)
            if len(b.r) > 24:
                last = {}
                for k, v in b.r:
                    if last.get(k, 0) < v:
                        last[k] = v
                b.r = list(last.items())

    def op(self, eng, fn, reads=(), writes=()):
        self._deps(eng, reads, writes)
        key = "E_" + eng
        self.cnt[key] += 1
        val = self.cnt[key]
        sem = self.sems[key]
        self.prog[eng].append(lambda e, fn=fn, sem=sem: fn(e).then_inc(sem, 1))
        self._commit((key, val), reads, writes)
        self.ninstr += 1

    def dma(self, q, fn, reads=(), writes=()):
        names, idx = self.dpool[q]
        name = names[idx % len(names)]
        self.dpool[q][1] = idx + 1
        if self.cnt[name] > 0:
            self._wait(q, (name, self.cnt[name]))
        self._deps(q, reads, writes)
        self.cnt[name] += 16
        val = self.cnt[name]
        sem = self.sems[name]
        self.prog[q].append(lambda e, fn=fn, sem=sem: fn(e).then_inc(sem, 16))
        self._commit((name, val), reads, writes)
        self.ninstr += 1

    def barrier(self):
        for eng in self.ENGS:
            for key, val in self.cnt.items():
                if val > 0:
                    if eng == "pe" and key == "E_pe":
                        continue
                    self._wait(eng, (key, val))

    def finish(self):
        self.barrier()
        nc = self.nc
        with nc.Block() as block:
            def mk(name):
                def f(e):
                    for t in self.prog[name]:
                        t(e)
                return f
            block.tensor(mk("pe"))
            block.scalar(mk("act"))
            block.vector(mk("dve"))
            block.gpsimd(mk("pool"))
            block.sync(mk("sp"))
        for cm in reversed(self._stack):
            cm.__exit__(None, None, None)


F32 = mybir.dt.float32
BF16 = mybir.dt.bfloat16
I32 = mybir.dt.int32
ALU = mybir.AluOpType
AF = mybir.ActivationFunctionType
AX = mybir.AxisListType

AW = 1024
NH = 16
HD = 64
SHIFTW = 3 * AW + 96 + 96 + 256
C0 = float(np.exp(-0.5))


class Cfg:
    def __init__(self, DM=2048, NPRE=4096, NOWN=4096, ST=2048, NE=32, CAP=640, depth_alpha=2 ** 0.25):
        self.DM = DM; self.NPRE = NPRE; self.NOWN = NOWN; self.ST = ST
        self.NE = NE; self.CAP = CAP; self.FF = DM
        self.NTOK = NPRE + NOWN
        self.KC = DM // 128
        self.DIN = 3 * AW + SHIFTW + 2 * DM
        self.alpha = depth_alpha


class K:
    def __init__(self, cfg, dbg=()):
        self.cfg = cfg
        self.nc = bass.Bass("TRN2", target_bir_lowering=False)
        self.S = Sched(self.nc)
        self.dbg = set(dbg)
        self.stack = contextlib.ExitStack()
        self.pstack = None
        self.ins = {}

    def inp(self, name, shape, dt=F32):
        t = self.nc.dram_tensor(name, list(shape), dt, kind="ExternalInput").ap()
        self.ins[name] = t
        return t

    def scratch(self, name, shape, dt):
        kind = "ExternalOutput" if name in self.dbg else "Internal"
        return self.nc.dram_tensor(name, list(shape), dt, kind=kind).ap()

    def phase(self):
        if self.pstack is not None:
            self.S.barrier()
            self.pstack.close()
        self.pstack = contextlib.ExitStack()

    def sb(self, name, shape, dt):
        return self.pstack.enter_context(self.nc.sbuf_tensor(name, list(shape), dt))

    def ps(self, name, shape, dt=F32):
        return self.pstack.enter_context(self.nc.psum_tensor(name, list(shape), dt))

    def gsb(self, name, shape, dt):
        return self.stack.enter_context(self.nc.sbuf_tensor(name, list(shape), dt))


_REGS = {}


def _bound_reg(e, val):
    key = (id(e), val)
    if key not in _REGS:
        _REGS[key] = e.to_reg(val)
    return _REGS[key]


class Ring:
    def __init__(self, tiles):
        self.t = tiles
        self.b = [Buf() for _ in tiles]
        self.i = 0

    def next(self):
        j = self.i % len(self.t)
        self.i += 1
        return self.t[j], self.b[j]


def load_consts(k):
    S = k.S
    c = {}
    ident = k.inp("c_ident", [128, 128])
    c["ident_f"] = k.gsb("ident_f", [128, 128], F32)
    c["ident_b"] = k.gsb("ident_b", [128, 128], BF16)
    c["b_ident"] = Buf()
    S.dma("sp", lambda e: e.dma_start(out=c["ident_f"][:], in_=ident[:, :]), writes=[c["b_ident"]])
    S.op("act", lambda e: e.activation(out=c["ident_b"][:], in_=c["ident_f"][:], func=AF.Copy),
         reads=[c["b_ident"]], writes=[c["b_ident"]])
    k.c = c
    NT = k.cfg.NOWN // 128
    k.dest_all = k.gsb("dest_all", [128, NT, 4], I32); k.b_dest = Buf()
    k.gate_all = k.gsb("gate_all", [128, NT, 4], F32); k.b_gate = Buf()
    k.bgT = k.gsb("bgT", [128, 2 * (k.cfg.FF // 128), k.cfg.NE], F32); k.b_bgT = Buf()


def phase1(k, xe, w_in, mu_cols):
    cfg, S, nc, c = k.cfg, k.S, k.nc, k.c
    DM, KC, ST, NTOK, NPRE, NOWN = cfg.DM, cfg.KC, cfg.ST, cfg.NTOK, cfg.NPRE, cfg.NOWN
    o1, o2 = 3 * AW, 3 * AW + SHIFTW
    HALO = 512
    sc = {}
    sc["QT"] = k.scratch("QT", [AW, NOWN], BF16)
    sc["KT"] = k.scratch("KT", [AW, HALO + NOWN], BF16)
    sc["V"] = k.scratch("V", [HALO + NOWN, NH, 65], BF16)
    sc["RB"] = k.scratch("RB", [AW, NTOK], BF16)
    sc["KB"] = k.scratch("KB", [AW, NTOK], BF16)
    sc["VB"] = k.scratch("VB", [AW, NTOK], BF16)
    sc["WD"] = k.scratch("WD", [96, NTOK], BF16)
    sc["AD"] = k.scratch("AD", [96, NTOK], BF16)
    sc["GD"] = k.scratch("GD", [256, NTOK], BF16)
    sc["GA"] = k.scratch("GA", [DM, NOWN], BF16)
    sc["GB"] = k.scratch("GB", [DM, NOWN], BF16)
    k.sc = sc

    k.phase()
    xT = k.sb("xT", [128, KC, ST], BF16); b_xT = Buf()
    xin = Ring([k.sb(f"xin{i}", [128, DM], F32) for i in range(2)])
    xbf = Ring([k.sb(f"xbf{i}", [128, DM], BF16) for i in range(2)])
    wst = Ring([k.sb(f"wst{i}", [128, KC, 256], F32) for i in range(2)])
    wbf = Ring([k.sb(f"wbf{i}", [128, KC, 256], BF16) for i in range(2)])
    lbuf = [k.sb(f"lbuf{i}", [128, 516], F32) for i in range(2)]
    b_lbuf = [Buf(), Buf()]
    ltmp = Ring([k.sb(f"ltmp{i}", [128, 512], F32) for i in range(2)])
    lseg = Ring([k.sb(f"lseg{i}", [128, 512], F32) for i in range(2)])
    ost = Ring([k.sb(f"ost{i}", [128, 512], BF16) for i in range(4)])
    vst = Ring([k.sb(f"vst{i}", [128, 4, 65], BF16) for i in range(3)])
    carry = k.sb("carry", [128, 32], F32); b_carry = Buf()
    mu = k.sb("mu", [128, 32], F32); b_mu = Buf()
    pst = Ring([k.ps(f"pT{i}", [128, 8, 128], BF16) for i in range(2)])
    psg = Ring([k.ps(f"pg{i}", [128, 512], F32) for i in range(4)])

    S.dma("sp", lambda e: e.dma_start(out=mu[:, 0:28], in_=mu_cols[:, :]), writes=[b_mu])
    S.op("pool", lambda e: e.memset(carry[:], 0.0), writes=[b_carry])
    for t, b in zip(vst.t, vst.b):
        S.op("pool", lambda e, t=t: e.memset(t[:], 1.0), writes=[b])

    blocks = []
    for c0 in range(0, AW, 256):
        blocks.append((c0, 256, "q", sc["QT"], c0, "own", None, None))
    for c0 in range(0, AW, 256):
        blocks.append((AW + c0, 256, "k", sc["KT"], c0, "halo", None, None))
    for c0 in range(0, AW, 256):
        blocks.append((2 * AW + c0, 256, "v", sc["V"], c0 // 64, "halo", None, None))
    for j, nm in enumerate(("RB", "KB", "VB")):
        for c0 in range(0, AW, 256):
            blocks.append((o1 + j * AW + c0, 256, "rw", sc[nm], c0, "all", None, (j * AW + c0) // 128))
    blocks.append((o1 + 3 * AW, 96, "rw", sc["WD"], 0, "all", AF.Tanh, 24))
    blocks.append((o1 + 3 * AW + 96, 96, "rw", sc["AD"], 0, "all", AF.Copy, 25))
    blocks.append((o1 + 3 * AW + 192, 256, "rw", sc["GD"], 0, "all", AF.Sigmoid, 26))
    for c0 in range(0, DM, 256):
        blocks.append((o2 + c0, 256, "gate", sc["GA"], c0, "own", None, None))
    for c0 in range(0, DM, 256):
        blocks.append((o2 + DM + c0, 256, "gate", sc["GB"], c0, "own", None, None))

    w_v = w_in.rearrange("(kc p) c -> p kc c", p=128)
    nST = NTOK // ST
    active = []
    for s in range(nST):
        for bi, blk in enumerate(blocks):
            tts = []
            for tt in range(ST // 512):
                t_ext = s * ST + tt * 512
                if blk[5] == "all" or (blk[5] == "own" and t_ext >= NPRE) or (blk[5] == "halo" and t_ext >= NPRE - HALO):
                    tts.append(tt)
            if tts:
                active.append((s, bi, tts))
    wloaded = {}

    def wload(ai):
        c0, ncols = blocks[active[ai][1]][0:2]
        ws, bws = wst.next()
        wb, bwb = wbf.next()
        S.dma("sp", lambda e: e.dma_start(out=ws[:, :, 0:ncols], in_=w_v[:, :, c0:c0 + ncols]), writes=[bws])
        S.op("pool", lambda e: e.tensor_copy(out=wb[:, :, 0:ncols], in_=ws[:, :, 0:ncols]), reads=[bws], writes=[bwb])
        wloaded[ai] = (wb, bwb)

    wload(0)
    for s in range(nST):
        tok0 = s * ST
        for i in range(ST // 128):
            xi, bxi = xin.next()
            xb, bxb = xbf.next()
            r0 = tok0 + i * 128
            S.dma("sp", lambda e, xi=xi, r0=r0: e.dma_start(out=xi[:], in_=xe[r0:r0 + 128, :]), writes=[bxi])
            S.op("act", lambda e, xi=xi, xb=xb: e.activation(out=xb[:], in_=xi[:], func=AF.Copy),
                 reads=[bxi], writes=[bxb])
            for g0 in range(0, KC, 8):
                ng = min(8, KC - g0)
                pt, bpt = pst.next()
                for j in range(ng):
                    S.op("pe", lambda e, pt=pt, xb=xb, j=j, g0=g0: e.transpose(
                        out=pt[:, j, :], in_=xb[:, (g0 + j) * 128:(g0 + j + 1) * 128], identity=c["ident_b"][:]),
                        reads=[bxb, c["b_ident"]], writes=[bpt])
                S.op("dve", lambda e, pt=pt, g0=g0, ng=ng, i=i: e.tensor_copy(
                    out=xT[:, g0:g0 + ng, i * 128:(i + 1) * 128], in_=pt[:, 0:ng, :]),
                    reads=[bpt], writes=[b_xT])
        own_s = tok0 >= NPRE
        for ai in [a for a in range(len(active)) if active[a][0] == s]:
            _, bi, tts = active[ai]
            c0, ncols, kind, dst, drow0, tokmode, post, rwc = blocks[bi]
            wb, bwb = wloaded.pop(ai)
            if ai + 1 < len(active):
                wload(ai + 1)
            if kind == "v":
                for tt in tts:
                    for sub in range(4):
                        tk = tt * 512 + sub * 128
                        pg, bpg = psg.next()
                        for kc in range(KC):
                            S.op("pe", lambda e, pg=pg, kc=kc, tk=tk, wb=wb: e.matmul(
                                pg[:, 0:256], lhsT=xT[:, kc, tk:tk + 128], rhs=wb[:, kc, 0:256],
                                start=(kc == 0), stop=(kc == KC - 1)), reads=[b_xT, bwb], writes=[bpg])
                        vs, bvs = vst.next()
                        S.op("act", lambda e, vs=vs, pg=pg: e.activation(
                            out=vs[:, :, 0:64], in_=pg[:, 0:256].rearrange("p (h d) -> p h d", d=64), func=AF.Copy),
                            reads=[bpg], writes=[bvs])
                        vrow = tok0 + tk - (NPRE - HALO)
                        S.dma("sp", lambda e, vs=vs, vrow=vrow, drow0=drow0, dst=dst: e.dma_start(
                            out=dst[vrow:vrow + 128, drow0:drow0 + 4, :], in_=vs[:]), reads=[bvs])
                continue
            nch = (ncols + 127) // 128
            for ch in range(nch):
                m = min(128, ncols - ch * 128)
                j0 = ch * 128
                if kind == "rw":
                    rci = rwc + ch
                    p = 0
                    S.op("act", lambda e, p=p, rci=rci, m=m: e.activation(
                        out=lbuf[p][0:m, 0:1], in_=carry[0:m, rci:rci + 1], func=AF.Copy),
                        reads=[b_carry], writes=[b_lbuf[p]])
                for tt in tts:
                    tk = tt * 512
                    pg, bpg = psg.next()
                    for kc in range(KC):
                        S.op("pe", lambda e, pg=pg, kc=kc, tk=tk, wb=wb, j0=j0, m=m: e.matmul(
                            pg[0:m, :], lhsT=wb[:, kc, j0:j0 + m], rhs=xT[:, kc, tk:tk + 512],
                            start=(kc == 0), stop=(kc == KC - 1)), reads=[b_xT, bwb], writes=[bpg])
                    o, bo = ost.next()
                    if kind == "q":
                        S.op("act", lambda e, o=o, pg=pg, m=m: e.activation(
                            out=o[0:m, :], in_=pg[0:m, :], func=AF.Copy, scale=0.125), reads=[bpg], writes=[bo])
                        dcol = tok0 + tk - NPRE
                    elif kind == "k":
                        S.op("act", lambda e, o=o, pg=pg, m=m: e.activation(
                            out=o[0:m, :], in_=pg[0:m, :], func=AF.Copy), reads=[bpg], writes=[bo])
                        dcol = tok0 + tk - (NPRE - HALO)
                    elif kind == "gate":
                        S.op("act", lambda e, o=o, pg=pg, m=m: e.activation(
                            out=o[0:m, :], in_=pg[0:m, :], func=AF.Sigmoid), reads=[bpg], writes=[bo])
                        dcol = tok0 + tk - NPRE
                    else:
                        lb, blb = lbuf[p], b_lbuf[p]
                        S.op("act", lambda e, lb=lb, pg=pg, m=m: e.activation(
                            out=lb[0:m, 1:513], in_=pg[0:m, :], func=AF.Copy), reads=[bpg], writes=[blb])
                        S.op("act", lambda e, lb=lb, p=p, m=m: e.activation(
                            out=lbuf[1 - p][0:m, 0:1], in_=lb[0:m, 512:513], func=AF.Copy),
                            reads=[blb], writes=[b_lbuf[1 - p]])
                        lt, blt = ltmp.next()
                        S.op("dve", lambda e, lt=lt, lb=lb, m=m: e.tensor_tensor(
                            out=lt[0:m, :], in0=lb[0:m, 0:512], in1=lb[0:m, 1:513], op=ALU.subtract),
                            reads=[blb], writes=[blt])
                        if post is None:
                            S.op("dve", lambda e, o=o, lt=lt, lb=lb, m=m, rci=rci: e.scalar_tensor_tensor(
                                out=o[0:m, :], in0=lt[0:m, :], scalar=mu[0:m, rci:rci + 1], in1=lb[0:m, 1:513],
                                op0=ALU.mult, op1=ALU.add), reads=[blt, blb, b_mu], writes=[bo])
                        else:
                            ls, bls = lseg.next()
                            S.op("dve", lambda e, ls=ls, lt=lt, lb=lb, m=m, rci=rci: e.scalar_tensor_tensor(
                                out=ls[0:m, :], in0=lt[0:m, :], scalar=mu[0:m, rci:rci + 1], in1=lb[0:m, 1:513],
                                op0=ALU.mult, op1=ALU.add), reads=[blt, blb, b_mu], writes=[bls])
                            S.op("act", lambda e, o=o, ls=ls, m=m, post=post: e.activation(
                                out=o[0:m, :], in_=ls[0:m, :], func=post), reads=[bls], writes=[bo])
                        p = 1 - p
                        dcol = tok0 + tk
                    r0 = drow0 + j0
                    S.dma("sp" if (tt % 2 == 0) else "pool", lambda e, o=o, r0=r0, m=m, dcol=dcol, dst=dst: e.dma_start(
                        out=dst[r0:r0 + m, dcol:dcol + 512], in_=o[0:m, :]), reads=[bo])
                if kind == "rw":
                    S.op("act", lambda e, p=p, rci=rci, m=m: e.activation(
                        out=carry[0:m, rci:rci + 1], in_=lbuf[p][0:m, 0:1], func=AF.Copy),
                        reads=[b_lbuf[p]], writes=[b_carry])


def phase2(k, att_bias, halo_mask):
    cfg, S, nc, c, sc = k.cfg, k.S, k.nc, k.c, k.sc
    NOWN = cfg.NOWN
    sc["YAT"] = k.scratch("YAT", [AW, NOWN], BF16)
    k.phase()
    QTv = sc["QT"].rearrange("(c p) t -> p c t", p=128)
    KTv = sc["KT"].rearrange("(c p) t -> p c t", p=128)
    YATv = sc["YAT"].rearrange("(c p) t -> p c t", p=128)
    qt = Ring([k.sb(f"qt{i}", [128, 8, 512], BF16) for i in range(2)])
    kt = Ring([k.sb(f"kt{i}", [128, 8, 1024], BF16) for i in range(2)])
    vv = Ring([k.sb(f"vv{i}", [128, 8, NH, 65], BF16) for i in range(2)])
    bias = k.sb("abias", [128, 5, NH, 128], F32); b_bias = Buf()
    halo = k.sb("halo", [128, 1], F32); b_halo = Buf()
    scs = Ring([k.sb(f"scs{i}", [128, 512], F32) for i in range(2)])
    pTs = Ring([k.sb(f"pTs{i}", [128, 512], BF16) for i in range(3)])
    ya = Ring([k.sb(f"ya{i}", [128, AW], BF16) for i in range(2)])
    yst = Ring([k.sb(f"yst{i}", [128, 8, 512], BF16) for i in range(2)])
    rec = Ring([k.sb(f"rec{i}", [128, 4], F32) for i in range(4)])
    ps_s = Ring([k.ps(f"ps_s{i}", [128, 512], F32) for i in range(2)])
    po = [k.ps(f"po{i}", [128, 512], F32) for i in range(4)]
    b_po = [Buf() for _ in range(4)]
    psT = Ring([k.ps("psT2", [128, 8, 128], BF16)])

    for kb in range(5):
        S.dma("sp", lambda e, kb=kb: e.dma_start(out=bias[:, kb, :, :], in_=att_bias[:, kb, :, :]), writes=[b_bias])
    S.dma("sp", lambda e: e.dma_start(out=halo[:], in_=halo_mask[:, :]), writes=[b_halo])

    HG = [[0, 2, 4, 6], [1, 3, 5, 7], [8, 10, 12, 14], [9, 11, 13, 15]]
    for g in range(NOWN // 512):
        q_, bq = qt.next(); k_, bk = kt.next(); v_, bv = vv.next()
        S.dma("sp", lambda e, q_=q_, g=g: e.dma_start(out=q_[:], in_=QTv[:, :, 512 * g:512 * g + 512]), writes=[bq])
        S.dma("act", lambda e, k_=k_, g=g: e.dma_start(out=k_[:], in_=KTv[:, :, 512 * g:512 * g + 1024]), writes=[bk])
        S.dma("pool", lambda e, v_=v_, g=g: e.dma_start(
            out=v_[:], in_=sc["V"][512 * g:512 * g + 1024, :, :].rearrange("(i p) h d -> p i h d", p=128)), writes=[bv])
        ys, bys = yst.next()
        for p in range(4):
            y_, by = ya.next()
            for hg in range(4):
                for kb in range(5):
                    ps, bps = ps_s.next()
                    for hh in range(4):
                        h = HG[hg][hh]
                        hp, h2 = h % 2, h // 2
                        S.op("pe", lambda e, ps=ps, k_=k_, q_=q_, hp=hp, h2=h2, p=p, kb=kb, hh=hh: e.matmul(
                            ps[:, hh * 128:(hh + 1) * 128],
                            lhsT=k_[hp * 64:(hp + 1) * 64, h2, 128 * (p + kb):128 * (p + kb) + 128],
                            rhs=q_[hp * 64:(hp + 1) * 64, h2, 128 * p:128 * p + 128],
                            start=True, stop=True), reads=[bk, bq], writes=[bps])
                    s_, bs = scs.next()
                    S.op("dve", lambda e, s_=s_, ps=ps, kb=kb, hg=hg: e.tensor_tensor(
                        out=s_[:].rearrange("p (h q) -> p h q", q=128), in0=ps[:].rearrange("p (h q) -> p h q", q=128),
                        in1=bias[:, kb, 4 * hg:4 * hg + 4, :], op=ALU.add), reads=[bps, b_bias], writes=[bs])
                    pt, bpt = pTs.next()
                    masked = (g == 0 and p + kb < 4)
                    if masked:
                        S.op("act", lambda e, pt=pt, s_=s_: e.activation(
                            out=pt[:], in_=s_[:], func=AF.Exp, bias=halo[:, 0:1]), reads=[bs, b_halo], writes=[bpt])
                    else:
                        S.op("act", lambda e, pt=pt, s_=s_: e.activation(
                            out=pt[:], in_=s_[:], func=AF.Exp), reads=[bs], writes=[bpt])
                    for hh in range(4):
                        h = HG[hg][hh]
                        S.op("pe", lambda e, pt=pt, v_=v_, hg=hg, hh=hh, h=h, p=p, kb=kb: e.matmul(
                            po[hg][:, hh * 65:(hh + 1) * 65], lhsT=pt[:, hh * 128:(hh + 1) * 128],
                            rhs=v_[:, p + kb, h, :], start=(kb == 0 and hh == 0), stop=(kb == 4 and hh == 3),
                            skip_group_check=True), reads=[bpt, bv], writes=[b_po[hg]])
                r_, br = rec.next()
                pov = po[hg][:, 0:260].rearrange("p (h d) -> p h d", d=65)
                S.op("dve", lambda e, r_=r_, pov=pov: e.reciprocal(out=r_[:, 0:4], in_=pov[:, :, 64]),
                     reads=[b_po[hg]], writes=[br])
                for hh in range(4):
                    h = HG[hg][hh]
                    eng = "act" if hh % 2 == 0 else "dve"
                    if eng == "act":
                        S.op("act", lambda e, y_=y_, pov=pov, r_=r_, hh=hh, h=h: e.activation(
                            out=y_[:, h * 64:(h + 1) * 64], in_=pov[:, hh, 0:64], func=AF.Copy, scale=r_[:, hh:hh + 1]),
                            reads=[b_po[hg], br], writes=[by])
                    else:
                        S.op("dve", lambda e, y_=y_, pov=pov, r_=r_, hh=hh, h=h: e.tensor_scalar(
                            out=y_[:, h * 64:(h + 1) * 64], in0=pov[:, hh, 0:64], scalar1=r_[:, hh:hh + 1], scalar2=None,
                            op0=ALU.mult), reads=[b_po[hg], br], writes=[by])
            pt_, bpt_ = psT.next()
            for j in range(8):
                S.op("pe", lambda e, pt_=pt_, y_=y_, j=j: e.transpose(
                    out=pt_[:, j, :], in_=y_[:, j * 128:(j + 1) * 128], identity=c["ident_b"][:]),
                    reads=[by, c["b_ident"]], writes=[bpt_])
            S.op("act", lambda e, ys=ys, pt_=pt_, p=p: e.activation(
                out=ys[:, :, p * 128:(p + 1) * 128], in_=pt_[:], func=AF.Copy), reads=[bpt_], writes=[bys])
        S.dma("sp", lambda e, ys=ys, g=g: e.dma_start(out=YATv[:, :, 512 * g:512 * g + 512], in_=ys[:]), reads=[bys])


def phase3(k, prm):
    cfg, S, nc, c, sc = k.cfg, k.S, k.nc, k.c, k.sc
    NTOK, NPRE, NOWN = cfg.NTOK, cfg.NPRE, cfg.NOWN
    sc["YB"] = k.scratch("YB", [NOWN, AW], F32)
    sc["BON"] = k.scratch("BON", [NH, NOWN], F32)
    k.phase()
    HB = 8
    def cload(name, shape, dt=F32, src=None, cast=None):
        t = k.sb("s3_" + name, shape, dt); b = Buf()
        S.dma("sp", lambda e: e.dma_start(out=t[:], in_=src), writes=[b])
        return t, b
    wupf, b_wupf = cload("wupf", [96, AW], F32, prm["w_up"][:, :])
    aupf, b_aupf = cload("aupf", [96, AW], F32, prm["a_up"][:, :])
    wup = k.sb("wup", [96, AW], BF16); aup = k.sb("aup", [96, AW], BF16)
    S.op("act", lambda e: e.activation(out=wup[:], in_=wupf[:], func=AF.Copy), reads=[b_wupf], writes=[b_wupf])
    S.op("act", lambda e: e.activation(out=aup[:], in_=aupf[:], func=AF.Copy), reads=[b_aupf], writes=[b_aupf])
    hc, b_hc = cload("hc", [64, 6, NH], F32, prm["hcols"][:, :, :])
    S.op("dve", lambda e: e.tensor_scalar(out=hc[:, 4, :], in0=hc[:, 3, :], scalar1=-1.0, scalar2=1.0, op0=ALU.mult, op1=ALU.add),
         reads=[b_hc], writes=[b_hc])
    rmask, b_rm = cload("rmask", [64, 512], F32, prm["rmask"][:, :])
    m_su, b_su = cload("m_su", [64, 8, 64], F32, prm["m_su"][:, :, :])
    m_ui, b_ui = cload("m_ui", [64, 8, 64], F32, prm["m_ui"][:, :, :])
    m_sl, b_sl = cload("m_sl", [64, 8, 64], F32, prm["m_sl"][:, :, :])
    i8, b_i8 = cload("i8", [64, 8, 64], F32, prm["i8"][:, :, :])
    ones64 = k.sb("ones64", [64, 64], F32); b_ones = Buf()
    S.op("pool", lambda e: e.memset(ones64[:], 1.0), writes=[b_ones])
    cb = [b_hc, b_rm]

    S32 = k.sb("S32", [64, NH, 64], F32); b_S32 = [Buf() for _ in range(NH)]
    Sb = k.sb("Sb", [64, NH, 64], BF16); b_Sb = [Buf() for _ in range(NH)]
    S.op("pool", lambda e: e.memset(S32[:], 0.0), writes=b_S32)
    S.op("pool", lambda e: e.memset(Sb[:], 0.0), writes=b_Sb)

    wdt = Ring([k.sb(f"wdt{i}", [96, 512], BF16) for i in range(2)])
    adt = Ring([k.sb(f"adt{i}", [96, 512], BF16) for i in range(2)])
    def htiles(name, shape, dt):
        return [k.sb(f"{name}{i}", shape, dt) for i in range(HB)], [Buf() for _ in range(HB)]
    AR, b_AR = htiles("AR", [64, 8, 2, 64], BF16)
    Tm, b_Tm = htiles("Tm", [64, 8, 64], BF16)
    Mka, b_Mka = htiles("Mka", [64, 8, 64], BF16)
    Mbr, b_Mbr = htiles("Mbr", [64, 8, 64], BF16)
    Mkr, b_Mkr = htiles("Mkr", [64, 8, 64], BF16)
    BhT, b_BhT = htiles("BhT", [64, 8, 64], BF16)
    KhT, b_KhT = htiles("KhT", [64, 8, 64], BF16)
    VT, b_VT = htiles("VT", [64, 8, 64], BF16)
    pC, b_pC = htiles("pC", [64, 8], F32)
    def tring(name, n, shape=[64, 512], dt=F32):
        return Ring([k.sb(f"{name}{i}", shape, dt) for i in range(n)])
    t_Ep = tring("t_Ep", 2, [64, 8, 64], F32)
    rin = tring("rin", 2, dt=BF16); kin = tring("kin", 2, dt=BF16); vin = tring("vin", 2, dt=BF16)
    t_s = tring("t_s", 2); t_cum = tring("t_cum", 2); t_a = tring("t_a", 2); t_kkr = tring("t_kkr", 2)
    t_sq = tring("t_sq", 2); t_nr = tring("t_nr", 2); t_kk = tring("t_kk", 2); t_t1 = t_sq
    t_kp = tring("t_kp", 2); t_bv = tring("t_bv", 2); t_d1 = tring("t_d1", 2); t_Epv = tring("t_Epv", 2)
    t_Em = tring("t_Em", 2); t_EC = tring("t_EC", 2)
    t_Bt = tring("t_Bt", 4, dt=BF16); t_Kt = tring("t_Kt", 4, dt=BF16)
    t_Bh = tring("t_Bh", 2, dt=BF16); t_Kh = tring("t_Kh", 2, dt=BF16)
    t_rkp = tring("t_rkp", 2); t_brow = tring("t_brow", 2, [1, 512], F32)
    t_N = tring("t_N", 4, [64, 8, 64], BF16); t_NT = tring("t_NT", 4, [64, 8, 64], BF16)
    WTs = tring("WTs", 2, [64, HB, 64], BF16); UTs = tring("UTs", 2, [64, HB, 64], BF16)
    ysb = tring("ysb", 1, [64, HB, 64], F32)
    pA = Ring([k.ps(f"p3a{i}", [64, 512], F32) for i in range(2)])
    pG = Ring([k.ps(f"p3g{i}", [64, 4, 128], F32) for i in range(2)])
    pTb = Ring([k.ps(f"p3t{i}", [64, 8, 128], BF16) for i in range(2)])
    pW = k.ps("p3W", [64, HB, 64], F32); b_pW = Buf()
    pU = k.ps("p3U", [64, HB, 64], F32); b_pU = Buf()
    pS = pW; b_pS = b_pW
    pX = Ring([pW[:].rearrange("p h v -> p (h v)"), pU[:].rearrange("p h v -> p (h v)")])
    pX.b = [b_pW, b_pU]

    RBv, KBv, VBv = sc["RB"], sc["KB"], sc["VB"]
    for tt in range(NTOK // 512):
        t0 = tt * 512
        own = t0 >= NPRE
        wd_, bwd = wdt.next(); ad_, bad = adt.next()
        S.dma("sp", lambda e, wd_=wd_, t0=t0: e.dma_start(out=wd_[:], in_=sc["WD"][:, t0:t0 + 512]), writes=[bwd])
        S.dma("sp", lambda e, ad_=ad_, t0=t0: e.dma_start(out=ad_[:], in_=sc["AD"][:, t0:t0 + 512]), writes=[bad])
        for hb0 in range(0, NH, HB):
            ctx = {}

            def prepA(hi, hb0=hb0, own=own, t0=t0, wd_=wd_, ad_=ad_, bwd=bwd, bad=bad):
                h = hb0 + hi
                hs = slice(h * 64, (h + 1) * 64)
                col = lambda j: hc[:, j, h:h + 1]
                r_, br = rin.next(); k_, bk = kin.next(); v_, bv = vin.next()
                yield S.dma("sp", lambda e, r_=r_, hs=hs, t0=t0: e.dma_start(out=r_[:], in_=RBv[hs, t0:t0 + 512]), writes=[br])
                yield S.dma("act", lambda e, k_=k_, hs=hs, t0=t0: e.dma_start(out=k_[:], in_=KBv[hs, t0:t0 + 512]), writes=[bk])
                yield S.dma("sp", lambda e, v_=v_, hs=hs, t0=t0: e.dma_start(out=v_[:], in_=VBv[hs, t0:t0 + 512]), writes=[bv])
                p1, bp1 = pX.next()
                yield S.op("pe", lambda e, p1=p1, hs=hs, wd_=wd_: e.matmul(p1[:, :], lhsT=wup[:, hs], rhs=wd_[:, :], start=True, stop=True),
                     reads=[b_wupf, bwd], writes=[bp1])
                kkr, bkkr = t_kkr.next()
                yield S.op("dve", lambda e, kkr=kkr, k_=k_, h=h: e.tensor_scalar(out=kkr[:], in0=k_[:], scalar1=hc[:, 2, h:h + 1], scalar2=None,
                                                                          op0=ALU.mult), reads=[bk, b_hc], writes=[bkkr])
                sq, bsq = t_sq.next()
                yield S.op("pool", lambda e, sq=sq, kkr=kkr: e.tensor_tensor(out=sq[:], in0=kkr[:], in1=kkr[:], op=ALU.mult), reads=[bkkr], writes=[bsq])
                s_, bs = t_s.next()
                yield S.op("act", lambda e, s_=s_, p1=p1, h=h: e.activation(out=s_[:], in_=p1[:, :], func=AF.Sigmoid, bias=hc[:, 0, h:h + 1]),
                     reads=[bp1, b_hc], writes=[bs])
                p2, bp2 = pX.next()
                yield S.op("pe", lambda e, p2=p2, hs=hs, ad_=ad_: e.matmul(p2[:, :], lhsT=aup[:, hs], rhs=ad_[:, :], start=True, stop=True),
                     reads=[b_aupf, bad], writes=[bp2])
                cum, bcum = t_cum.next()
                yield S.op("dve", lambda e, cum=cum, s_=s_: e.tensor_tensor_scan(out=cum[:], data0=rmask[:], data1=s_[:], initial=0.0,
                                                                          op0=ALU.mult, op1=ALU.add), reads=[bs, b_rm], writes=[bcum])
                a_, ba = t_a.next()
                yield S.op("act", lambda e, a_=a_, p2=p2, h=h: e.activation(out=a_[:], in_=p2[:, :], func=AF.Sigmoid, bias=hc[:, 1, h:h + 1]),
                     reads=[bp2, b_hc], writes=[ba])
                p3, bp3 = pX.next()
                yield S.op("pe", lambda e, p3=p3, sq=sq: e.matmul(p3[:, :], lhsT=ones64[:, :], rhs=sq[:, :], start=True, stop=True),
                     reads=[b_ones, bsq], writes=[bp3])
                nr, bnr = t_nr.next()
                yield S.op("act", lambda e, nr=nr, p3=p3: e.activation(out=nr[:], in_=p3[:, :], func=AF.Sqrt), reads=[bp3], writes=[bnr])
                d1, bd1 = t_d1.next()
                yield S.op("pool", lambda e, d1=d1, cum=cum, s_=s_: e.tensor_tensor(out=d1[:], in0=cum[:], in1=s_[:], op=ALU.subtract), reads=[bcum, bs], writes=[bd1])
                t1, bt1 = t_t1.next()
                yield S.op("dve", lambda e, t1=t1, a_=a_, h=h: e.tensor_scalar(out=t1[:], in0=a_[:], scalar1=hc[:, 3, h:h + 1], scalar2=hc[:, 4, h:h + 1],
                                                                        op0=ALU.mult, op1=ALU.add), reads=[ba, b_hc], writes=[bt1])
                kp, bkp = t_kp.next()
                yield S.op("pool", lambda e, kp=kp, k_=k_, t1=t1: e.tensor_tensor(out=kp[:], in0=k_[:], in1=t1[:], op=ALU.mult), reads=[bk, bt1], writes=[bkp])
                yield S.op("dve", lambda e, nr=nr: e.tensor_scalar(out=nr[:], in0=nr[:], scalar1=1e-12, scalar2=None, op0=ALU.max), reads=[bnr], writes=[bnr])
                yield S.op("dve", lambda e, nr=nr: e.reciprocal(out=nr[:], in_=nr[:]), reads=[bnr], writes=[bnr])
                kk, bkk = t_kk.next()
                yield S.op("dve", lambda e, kk=kk, kkr=kkr, nr=nr: e.tensor_tensor(out=kk[:], in0=kkr[:], in1=nr[:], op=ALU.mult), reads=[bkkr, bnr], writes=[bkk])
                ep, bep = t_Ep.next()
                epf = ep[:].rearrange("p c t -> p (c t)")
                yield S.op("act", lambda e, epf=epf, cum=cum: e.activation(out=epf, in_=cum[:], func=AF.Exp, scale=-C0), reads=[bcum], writes=[bep])
                em, bem = t_Em.next()
                yield S.op("act", lambda e, em=em, cum=cum: e.activation(out=em[:], in_=cum[:], func=AF.Exp, scale=C0), reads=[bcum], writes=[bem])
                epv, bepv = t_Epv.next()
                yield S.op("act", lambda e, epv=epv, d1=d1: e.activation(out=epv[:], in_=d1[:], func=AF.Exp, scale=-C0), reads=[bd1], writes=[bepv])
                bv_, bbv = t_bv.next()
                yield S.op("pool", lambda e, bv_=bv_, kk=kk, a_=a_: e.tensor_tensor(out=bv_[:], in0=kk[:], in1=a_[:], op=ALU.mult), reads=[bkk, ba], writes=[bbv])
                if own:
                    rkp, brkp = t_rkp.next()
                    yield S.op("dve", lambda e, rkp=rkp, r_=r_, kp=kp, h=h: e.scalar_tensor_tensor(out=rkp[:], in0=r_[:], scalar=hc[:, 5, h:h + 1], in1=kp[:],
                                                                                           op0=ALU.mult, op1=ALU.mult), reads=[br, bkp, b_hc], writes=[brkp])
                    pb, bpb = pX.next()
                    yield S.op("pe", lambda e, pb=pb, rkp=rkp: e.matmul(pb[0:1, :], lhsT=ones64[:, 0:1], rhs=rkp[:, :], start=True, stop=True),
                         reads=[b_ones, brkp], writes=[bpb])
                    brow, bbrow = t_brow.next()
                    yield S.op("act", lambda e, brow=brow, pb=pb: e.activation(out=brow[0:1, :], in_=pb[0:1, :], func=AF.Copy), reads=[bpb], writes=[bbrow])
                    yield S.dma("sp", lambda e, brow=brow, h=h, t0=t0: e.dma_start(out=sc["BON"][h:h + 1, t0 - NPRE:t0 - NPRE + 512], in_=brow[0:1, :]), reads=[bbrow])
                yield S.op("act", lambda e, ep=ep, hi=hi: e.activation(out=pC[hi][:, :], in_=ep[:, :, 63], func=AF.Copy), reads=[bep], writes=[b_pC[hi]])
                ec, bec = t_EC.next()
                for cc in range(8):
                    yield S.op("act", lambda e, ec=ec, em=em, ep=ep, cc=cc: e.activation(
                        out=ec[:, cc * 64:(cc + 1) * 64], in_=em[:, cc * 64:(cc + 1) * 64], func=AF.Copy, scale=ep[:, cc, 63:64]),
                        reads=[bem, bep], writes=[bec])
                ar = AR[hi]; bar = b_AR[hi]
                yield S.op("dve", lambda e, ar=ar, r_=r_, epf=epf: e.tensor_tensor(out=ar[:, :, 1, :], in0=r_[:].rearrange("p (c t) -> p c t", t=64),
                                                                           in1=epf.rearrange("p (c t) -> p c t", t=64), op=ALU.mult), reads=[br, bep], writes=[bar])
                yield S.op("dve", lambda e, ar=ar, kk=kk, epv=epv: e.scalar_tensor_tensor(out=ar[:, :, 0, :], in0=kk[:].rearrange("p (c t) -> p c t", t=64), scalar=-1.0,
                                                                                  in1=epv[:].rearrange("p (c t) -> p c t", t=64), op0=ALU.mult, op1=ALU.mult), reads=[bkk, bepv], writes=[bar])
                Bt, bBt = t_Bt.next(); Kt, bKt = t_Kt.next(); Bh, bBh = t_Bh.next(); Kh, bKh = t_Kh.next()
                yield S.op("pool", lambda e, Bt=Bt, bv_=bv_, em=em: e.tensor_tensor(out=Bt[:], in0=bv_[:], in1=em[:], op=ALU.mult), reads=[bbv, bem], writes=[bBt])
                yield S.op("dve", lambda e, Kt=Kt, kp=kp, em=em: e.tensor_tensor(out=Kt[:], in0=kp[:], in1=em[:], op=ALU.mult), reads=[bkp, bem], writes=[bKt])
                yield S.op("pool", lambda e, Bh=Bh, bv_=bv_, ec=ec: e.tensor_tensor(out=Bh[:], in0=bv_[:], in1=ec[:], op=ALU.mult), reads=[bbv, bec], writes=[bBh])
                yield S.op("dve", lambda e, Kh=Kh, kp=kp, ec=ec: e.tensor_tensor(out=Kh[:], in0=kp[:], in1=ec[:], op=ALU.mult), reads=[bkp, bec], writes=[bKh])
                for src, bsrc, dstl, bdstl in ((Bh, bBh, BhT, b_BhT), (Kh, bKh, KhT, b_KhT), (v_, bv, VT, b_VT)):
                    pt, bpt = pTb.next()
                    for cc in range(8):
                        yield S.op("pe", lambda e, pt=pt, src=src, cc=cc: e.transpose(out=pt[:, cc, 0:64], in_=src[:, cc * 64:(cc + 1) * 64],
                                                                               identity=c["ident_b"][0:64, 0:64]), reads=[bsrc, c["b_ident"]], writes=[bpt])
                    yield S.op("act", lambda e, pt=pt, d=dstl[hi]: e.activation(out=d[:], in_=pt[:, :, 0:64], func=AF.Copy), reads=[bpt], writes=[bdstl[hi]])
                ctx[hi] = (Bt, bBt, Kt, bKt, ar, bar)

            def prepB(hi):
                Bt, bBt, Kt, bKt, ar, bar = ctx[hi]
                N0, bN0 = t_N.next()
                for hv in range(2):
                    pg, bpg = pG.next()
                    for c4 in range(4):
                        cc = 4 * hv + c4
                        yield S.op("pe", lambda e, pg=pg, Bt=Bt, ar=ar, cc=cc, c4=c4: e.matmul(pg[:, c4, :], lhsT=Bt[:, cc * 64:(cc + 1) * 64],
                                                                                        rhs=ar[:, cc, :, :].rearrange("p a t -> p (a t)"), start=True, stop=True),
                             reads=[bBt, bar], writes=[bpg])
                    yield S.op("dve", lambda e, N0=N0, pg=pg, hv=hv: e.tensor_tensor(out=N0[:, 4 * hv:4 * hv + 4, :], in0=pg[:, :, 0:64], in1=m_su[:, 0:4, :], op=ALU.mult),
                         reads=[bpg, b_su], writes=[bN0])
                    yield S.op("dve", lambda e, pg=pg, d=Mbr[hi], hv=hv: e.tensor_tensor(out=d[:, 4 * hv:4 * hv + 4, :], in0=pg[:, :, 64:128], in1=m_ui[:, 0:4, :], op=ALU.mult),
                         reads=[bpg, b_ui], writes=[b_Mbr[hi]])
                for hv in range(2):
                    pg, bpg = pG.next()
                    for c4 in range(4):
                        cc = 4 * hv + c4
                        yield S.op("pe", lambda e, pg=pg, Kt=Kt, ar=ar, cc=cc, c4=c4: e.matmul(pg[:, c4, :], lhsT=Kt[:, cc * 64:(cc + 1) * 64],
                                                                                        rhs=ar[:, cc, :, :].rearrange("p a t -> p (a t)"), start=True, stop=True),
                             reads=[bKt, bar], writes=[bpg])
                    yield S.op("dve", lambda e, pg=pg, d=Mka[hi], hv=hv: e.tensor_tensor(out=d[:, 4 * hv:4 * hv + 4, :], in0=pg[:, :, 0:64], in1=m_su[:, 0:4, :], op=ALU.mult),
                         reads=[bpg, b_su], writes=[b_Mka[hi]])
                    yield S.op("dve", lambda e, pg=pg, d=Mkr[hi], hv=hv: e.tensor_tensor(out=d[:, 4 * hv:4 * hv + 4, :], in0=pg[:, :, 64:128], in1=m_ui[:, 0:4, :], op=ALU.mult),
                         reads=[bpg, b_ui], writes=[b_Mkr[hi]])
                p4, bp4 = pA.next()
                for cc in range(8):
                    yield S.op("pe", lambda e, p4=p4, ar=ar, Bt=Bt, cc=cc: e.matmul(p4[:, cc * 64:(cc + 1) * 64], lhsT=ar[:, cc, 0, :], rhs=Bt[:, cc * 64:(cc + 1) * 64],
                                                                             start=True, stop=True), reads=[bar, bBt], writes=[bp4])
                NT0, bNT0 = t_NT.next()
                yield S.op("dve", lambda e, NT0=NT0, p4=p4: e.tensor_tensor(out=NT0[:], in0=p4[:, :].rearrange("p (c t) -> p c t", t=64), in1=m_sl[:], op=ALU.mult),
                     reads=[bp4, b_sl], writes=[bNT0])
                T_ = Tm[hi]; bT = b_Tm[hi]
                yield S.op("pool", lambda e, T_=T_, N0=N0: e.tensor_tensor(out=T_[:], in0=N0[:], in1=i8[:], op=ALU.add), reads=[bN0, b_i8], writes=[bT])
                Nc, bNc, NTc, bNTc = N0, bN0, NT0, bNT0
                for lvl in range(1, 6):
                    pnt, bpnt = pA.next()
                    for cc in range(8):
                        yield S.op("pe", lambda e, pnt=pnt, Nc=Nc, NTc=NTc, cc=cc: e.matmul(pnt[:, cc * 64:(cc + 1) * 64], lhsT=Nc[:, cc, :], rhs=NTc[:, cc, :],
                                                                                     start=True, stop=True), reads=[bNc, bNTc], writes=[bpnt])
                    NTn, bNTn = t_NT.next()
                    yield S.op("act", lambda e, NTn=NTn, pnt=pnt: e.activation(out=NTn[:].rearrange("p c t -> p (c t)"), in_=pnt[:, :], func=AF.Copy),
                         reads=[bpnt], writes=[bNTn])
                    if lvl < 5:
                        pn, bpn = pA.next()
                        for cc in range(8):
                            yield S.op("pe", lambda e, pn=pn, Nc=Nc, NTc=NTc, cc=cc: e.matmul(pn[:, cc * 64:(cc + 1) * 64], lhsT=NTc[:, cc, :], rhs=Nc[:, cc, :],
                                                                                       start=True, stop=True), reads=[bNc, bNTc], writes=[bpn])
                        Nn, bNn = t_N.next()
                        yield S.op("dve", lambda e, Nn=Nn, pn=pn: e.tensor_copy(out=Nn[:].rearrange("p c t -> p (c t)"), in_=pn[:, :]),
                             reads=[bpn], writes=[bNn])
                    ptt, bptt = pA.next()
                    for cc in range(8):
                        yield S.op("pe", lambda e, ptt=ptt, NTn=NTn, T_=T_, cc=cc: e.matmul(ptt[:, cc * 64:(cc + 1) * 64], lhsT=NTn[:, cc, :], rhs=T_[:, cc, :],
                                                                                     start=True, stop=True), reads=[bNTn, bT], writes=[bptt])
                    yield S.op("dve", lambda e, T_=T_, ptt=ptt: e.tensor_tensor(out=T_[:].rearrange("p c t -> p (c t)"), in0=ptt[:, :],
                                                                         in1=T_[:].rearrange("p c t -> p (c t)"), op=ALU.add), reads=[bptt, bT], writes=[bT])
                    NTc, bNTc = NTn, bNTn
                    if lvl < 5:
                        Nc, bNc = Nn, bNn
            GRP = 2
            pairs = [list(range(g, g + GRP)) for g in range(0, HB, GRP)]
            gB = []
            for pi in range(len(pairs) + 1):
                gA = [prepA(hi) for hi in pairs[pi]] if pi < len(pairs) else []
                tick = 0
                while gA or gB:
                    for g_ in list(gB):
                        try:
                            next(g_)
                        except StopIteration:
                            gB.remove(g_)
                    if tick % 2 == 0 or not gB:
                        for g_ in list(gA):
                            try:
                                next(g_)
                            except StopIteration:
                                gA.remove(g_)
                    tick += 1
                gB = [prepB(hi) for hi in pairs[pi]] if pi < len(pairs) else []
            for cc in range(8):
                hbufs = lambda lst: [lst[i] for i in range(HB)]
                for hi in range(HB):
                    h = hb0 + hi
                    S.op("pe", lambda e, hi=hi, h=h, cc=cc: e.matmul(pW[:, hi, :], lhsT=AR[hi][:, cc, 0, :], rhs=Sb[:, h, :], start=(hi == 0), stop=False,
                                                                    skip_group_check=True), reads=[b_AR[hi], b_Sb[h]], writes=[b_pW])
                    S.op("pe", lambda e, hi=hi, cc=cc: e.matmul(pW[:, hi, :], lhsT=Mka[hi][:, cc, :], rhs=VT[hi][:, cc, :], start=False, stop=True,
                                                               skip_group_check=True), reads=[b_Mka[hi], b_VT[hi]], writes=[b_pW])
                wt, bwt = WTs.next()
                S.op("act", lambda e, wt=wt: e.activation(out=wt[:], in_=pW[:], func=AF.Copy), reads=[b_pW], writes=[bwt])
                for hi in range(HB):
                    S.op("pe", lambda e, hi=hi, cc=cc, wt=wt: e.matmul(pU[:, hi, :], lhsT=Tm[hi][:, cc, :], rhs=wt[:, hi, :], start=(hi == 0), stop=True,
                                                                      skip_group_check=True), reads=[b_Tm[hi], bwt], writes=[b_pU])
                ut, but = UTs.next()
                S.op("dve", lambda e, ut=ut: e.tensor_copy(out=ut[:], in_=pU[:]), reads=[b_pU], writes=[but])
                if own:
                    py, bpy = pA.next()
                    for hi in range(HB):
                        h = hb0 + hi
                        S.op("pe", lambda e, py=py, hi=hi, h=h, cc=cc: e.matmul(py[:, hi * 64:(hi + 1) * 64], lhsT=AR[hi][:, cc, 1, :], rhs=Sb[:, h, :],
                                                                               start=(hi == 0), stop=False, skip_group_check=True),
                             reads=[b_AR[hi], b_Sb[h]], writes=[bpy])
                    for hi in range(HB):
                        S.op("pe", lambda e, py=py, hi=hi, cc=cc, ut=ut: e.matmul(py[:, hi * 64:(hi + 1) * 64], lhsT=Mbr[hi][:, cc, :], rhs=ut[:, hi, :],
                                                                                 start=False, stop=False, skip_group_check=True),
                             reads=[b_Mbr[hi], but], writes=[bpy])
                        S.op("pe", lambda e, py=py, hi=hi, cc=cc: e.matmul(py[:, hi * 64:(hi + 1) * 64], lhsT=Mkr[hi][:, cc, :], rhs=VT[hi][:, cc, :],
                                                                          start=False, stop=True, skip_group_check=True),
                             reads=[b_Mkr[hi], b_VT[hi]], writes=[bpy])
                    ys, bys = ysb.next()
                    S.op("act", lambda e, ys=ys, py=py: e.activation(out=ys[:].rearrange("p h v -> p (h v)"), in_=py[:, :], func=AF.Copy), reads=[bpy], writes=[bys])
                    trow = t0 - NPRE + cc * 64
                    S.dma("sp", lambda e, ys=ys, trow=trow, hb0=hb0: e.dma_start(
                        out=sc["YB"][trow:trow + 64, hb0 * 64:(hb0 + HB) * 64], in_=ys[:].rearrange("p h v -> p (h v)")), reads=[bys])
                for hi in range(HB):
                    S.op("pe", lambda e, hi=hi, cc=cc, ut=ut: e.matmul(pS[:, hi, :], lhsT=BhT[hi][:, cc, :], rhs=ut[:, hi, :], start=(hi == 0), stop=False,
                                                                      skip_group_check=True), reads=[b_BhT[hi], but], writes=[b_pS])
                    S.op("pe", lambda e, hi=hi, cc=cc: e.matmul(pS[:, hi, :], lhsT=KhT[hi][:, cc, :], rhs=VT[hi][:, cc, :], start=False, stop=True,
                                                               skip_group_check=True), reads=[b_KhT[hi], b_VT[hi]], writes=[b_pS])
                for hi in range(HB):
                    h = hb0 + hi
                    S.op("dve", lambda e, hi=hi, h=h, cc=cc: e.scalar_tensor_tensor(out=S32[:, h, :], in0=S32[:, h, :], scalar=pC[hi][:, cc:cc + 1],
                                                                                  in1=pS[:, hi, :], op0=ALU.mult, op1=ALU.add),
                         reads=[b_S32[h], b_pC[hi], b_pS], writes=[b_S32[h]])
                    S.op("act", lambda e, h=h: e.activation(out=Sb[:, h, :], in_=S32[:, h, :], func=AF.Copy), reads=[b_S32[h]], writes=[b_Sb[h]])


def phase3b(k, prm):
    cfg, S, nc, c, sc = k.cfg, k.S, k.nc, k.c, k.sc
    NTOK, NPRE, NOWN = cfg.NTOK, cfg.NPRE, cfg.NOWN
    sc["YBT"] = k.scratch("YBT", [AW, NOWN], BF16)
    k.phase()
    YBTv = sc["YBT"].rearrange("(c p) t -> p c t", p=128)
    VBv = sc["VB"].rearrange("(c p) t -> p c t", p=128)
    GDv = sc["GD"].rearrange("(c p) t -> p c t", p=128)
    gupf = k.sb("gupf", [128, 2, AW], F32); gup = k.sb("gup", [128, 2, AW], BF16); b_gup = Buf()
    S.dma("sp", lambda e: e.dma_start(out=gupf[:], in_=prm["g_up"].rearrange("(c p) n -> p c n", p=128)), writes=[b_gup])
    S.op("act", lambda e: e.activation(out=gup[:], in_=gupf[:], func=AF.Copy), reads=[b_gup], writes=[b_gup])
    lw = k.sb("lnxw", [128, AW], F32); lb = k.sb("lnxb", [128, AW], F32); b_l = Buf()
    S.dma("sp", lambda e: e.dma_start(out=lw[:], in_=prm["lnx_w"][0:1, :].partition_broadcast(128)), writes=[b_l])
    S.dma("sp", lambda e: e.dma_start(out=lb[:], in_=prm["lnx_b"][0:1, :].partition_broadcast(128)), writes=[b_l])
    yin = Ring([k.sb(f"yin{i}", [128, AW], F32) for i in range(2)])
    ysq = Ring([k.sb(f"ysq{i}", [128, AW], F32) for i in range(2)])
    st = Ring([k.sb(f"gst{i}", [128, 6, NH], F32) for i in range(2)])
    yn = Ring([k.sb(f"yn{i}", [128, AW], F32) for i in range(2)])
    vfm = Ring([k.sb(f"vfm{i}", [128, 8, 128], BF16) for i in range(2)])
    vtm = Ring([k.sb(f"vtm{i}", [128, AW], BF16) for i in range(2)])
    gdl = Ring([k.sb(f"gdl{i}", [128, 2, 128], BF16) for i in range(2)])
    bonf = Ring([k.sb(f"bonf{i}", [NH, 128], F32) for i in range(2)])
    bont = Ring([k.sb(f"bont{i}", [128, NH], F32) for i in range(2)])
    yo = Ring([k.sb(f"yo{i}", [128, AW], BF16) for i in range(2)])
    ost = Ring([k.sb(f"ybst{i}", [128, 8, 512], BF16) for i in range(2)])
    pT = Ring([k.ps(f"p3bT{i}", [128, 8, 128], BF16) for i in range(2)])
    pg = Ring([k.ps(f"p3bg{i}", [128, 512], F32) for i in range(2)])
    pbn = Ring([k.ps("p3bbn", [128, NH], F32)])
    bc = lambda ap: ap.unsqueeze(2).to_broadcast([128, NH, 64])
    v3 = lambda t: t[:].rearrange("p (h d) -> p h d", d=64)
    for ti in range(NOWN // 128):
        tok0 = ti * 128
        if ti % 4 == 0:
            os_, bos = ost.next()
        y_, by = yin.next()
        S.dma("sp", lambda e, y_=y_, tok0=tok0: e.dma_start(out=y_[:], in_=sc["YB"][tok0:tok0 + 128, :]), writes=[by])
        vf, bvf = vfm.next()
        S.dma("act", lambda e, vf=vf, tok0=tok0: e.dma_start(out=vf[:], in_=VBv[:, :, NPRE + tok0:NPRE + tok0 + 128]), writes=[bvf])
        gd, bgd = gdl.next()
        S.dma("act", lambda e, gd=gd, tok0=tok0: e.dma_start(out=gd[:], in_=GDv[:, :, NPRE + tok0:NPRE + tok0 + 128]), writes=[bgd])
        bf_, bbf = bonf.next()
        S.dma("sp", lambda e, bf_=bf_, tok0=tok0: e.dma_start(out=bf_[:], in_=sc["BON"][:, tok0:tok0 + 128]), writes=[bbf])
        s_, bs = st.next()
        S.op("dve", lambda e, s_=s_, y_=y_: e.tensor_reduce(out=s_[:, 0, :], in_=v3(y_), axis=AX.X, op=ALU.add), reads=[by], writes=[bs])
        q_, bq = ysq.next()
        S.op("act", lambda e, q_=q_, y_=y_: e.activation(out=q_[:], in_=y_[:], func=AF.Square), reads=[by], writes=[bq])
        S.op("dve", lambda e, s_=s_, q_=q_: e.tensor_reduce(out=s_[:, 1, :], in_=v3(q_), axis=AX.X, op=ALU.add), reads=[bq], writes=[bs])
        S.op("dve", lambda e, s_=s_: e.tensor_scalar(out=s_[:, 2, :], in0=s_[:, 0, :], scalar1=1.0 / 64, scalar2=None, op0=ALU.mult), reads=[bs], writes=[bs])
        S.op("dve", lambda e, s_=s_: e.tensor_tensor(out=s_[:, 3, :], in0=s_[:, 2, :], in1=s_[:, 2, :], op=ALU.mult), reads=[bs], writes=[bs])
        S.op("dve", lambda e, s_=s_: e.scalar_tensor_tensor(out=s_[:, 4, :], in0=s_[:, 1, :], scalar=1.0 / 64, in1=s_[:, 3, :], op0=ALU.mult, op1=ALU.subtract),
             reads=[bs], writes=[bs])
        S.op("dve", lambda e, s_=s_: e.tensor_scalar(out=s_[:, 4, :], in0=s_[:, 4, :], scalar1=64e-5, scalar2=None, op0=ALU.add), reads=[bs], writes=[bs])
        S.op("act", lambda e, s_=s_: e.activation(out=s_[:, 5, :], in_=s_[:, 4, :], func=AF.Sqrt), reads=[bs], writes=[bs])
        S.op("dve", lambda e, s_=s_: e.reciprocal(out=s_[:, 5, :], in_=s_[:, 5, :]), reads=[bs], writes=[bs])
        n_, bn = yn.next()
        S.op("dve", lambda e, n_=n_, y_=y_, s_=s_: e.tensor_tensor(out=v3(n_), in0=v3(y_), in1=bc(s_[:, 2, :]), op=ALU.subtract), reads=[by, bs], writes=[bn])
        S.op("pool", lambda e, n_=n_, s_=s_: e.tensor_tensor(out=v3(n_), in0=v3(n_), in1=bc(s_[:, 5, :]), op=ALU.mult), reads=[bn, bs], writes=[bn])
        S.op("dve", lambda e, n_=n_: e.tensor_tensor(out=n_[:], in0=n_[:], in1=lw[:], op=ALU.mult), reads=[bn, b_l], writes=[bn])
        S.op("pool", lambda e, n_=n_: e.tensor_tensor(out=n_[:], in0=n_[:], in1=lb[:], op=ALU.add), reads=[bn, b_l], writes=[bn])
        pb, bpb = pbn.next()
        S.op("pe", lambda e, pb=pb, bf_=bf_: e.transpose(out=pb[:, :], in_=bf_[:, :], identity=c["ident_f"][0:NH, 0:NH]), reads=[bbf, c["b_ident"]], writes=[bpb])
        bt, bbt = bont.next()
        S.op("act", lambda e, bt=bt, pb=pb: e.activation(out=bt[:], in_=pb[:, :], func=AF.Copy), reads=[bpb], writes=[bbt])
        pt, bpt = pT.next()
        for j in range(8):
            S.op("pe", lambda e, pt=pt, vf=vf, j=j: e.transpose(out=pt[:, j, :], in_=vf[:, j, :], identity=c["ident_b"][:]), reads=[bvf, c["b_ident"]], writes=[bpt])
        vt, bvt = vtm.next()
        S.op("act", lambda e, vt=vt, pt=pt: e.activation(out=vt[:].rearrange("p (j c) -> p j c", c=128), in_=pt[:], func=AF.Copy), reads=[bpt], writes=[bvt])
        q2, bq2 = ysq.next()
        S.op("pool", lambda e, q2=q2, vt=vt, bt=bt: e.tensor_tensor(out=v3(q2), in0=v3(vt), in1=bc(bt[:, :]), op=ALU.mult), reads=[bvt, bbt], writes=[bq2])
        S.op("dve", lambda e, n_=n_, q2=q2: e.tensor_tensor(out=n_[:], in0=n_[:], in1=q2[:], op=ALU.add), reads=[bn, bq2], writes=[bn])
        o_, bo = yo.next()
        for half in range(2):
            pg_, bpg = pg.next()
            for kc in range(2):
                S.op("pe", lambda e, pg_=pg_, gd=gd, kc=kc, half=half: e.matmul(pg_[:, :], lhsT=gd[:, kc, :], rhs=gup[:, kc, half * 512:(half + 1) * 512],
                                                                               start=(kc == 0), stop=(kc == 1)), reads=[bgd, b_gup], writes=[bpg])
            S.op("dve", lambda e, o_=o_, n_=n_, pg_=pg_, half=half: e.tensor_tensor(out=o_[:, half * 512:(half + 1) * 512], in0=pg_[:, :],
                                                                                   in1=n_[:, half * 512:(half + 1) * 512], op=ALU.mult), reads=[bpg, bn], writes=[bo])
        pt2, bpt2 = pT.next()
        for j in range(8):
            S.op("pe", lambda e, pt2=pt2, o_=o_, j=j: e.transpose(out=pt2[:, j, :], in_=o_[:, j * 128:(j + 1) * 128], identity=c["ident_b"][:]),
                 reads=[bo, c["b_ident"]], writes=[bpt2])
        S.op("act", lambda e, os_=os_, pt2=pt2, ti=ti: e.activation(out=os_[:, :, (ti % 4) * 128:(ti % 4 + 1) * 128], in_=pt2[:], func=AF.Copy), reads=[bpt2], writes=[bos])
        if ti % 4 == 3:
            g0 = (ti // 4) * 512
            S.dma("sp", lambda e, os_=os_, g0=g0: e.dma_start(out=YBTv[:, :, g0:g0 + 512], in_=os_[:]), reads=[bos])


def cast_load(k, S, dst_bf, bdst, src_ap_fn, nk, ncols, stg, step=256):
    for i, c0 in enumerate(range(0, ncols, step)):
        n = min(step, ncols - c0)
        st, bst = stg.next()
        S.dma("sp" if i % 2 == 0 else "act", lambda e, st=st, c0=c0, n=n: e.dma_start(out=st[:, 0:nk, 0:n], in_=src_ap_fn(c0, n)), writes=[bst])
        if i % 2 == 0:
            S.op("act", lambda e, st=st, c0=c0, n=n: e.activation(out=dst_bf[:, 0:nk, c0:c0 + n], in_=st[:, 0:nk, 0:n], func=AF.Copy), reads=[bst], writes=[bdst])
        else:
            S.op("pool", lambda e, st=st, c0=c0, n=n: e.tensor_copy(out=dst_bf[:, 0:nk, c0:c0 + n], in_=st[:, 0:nk, 0:n]), reads=[bst], writes=[bdst])


def phase4a(k, proj_a, proj_b):
    cfg, S, nc, c, sc = k.cfg, k.S, k.nc, k.c, k.sc
    DM, KC, NOWN = cfg.DM, cfg.KC, cfg.NOWN
    sc["MT"] = k.scratch("MT", [DM, NOWN], BF16)
    k.phase()
    PA = k.sb("PAb", [128, 8, DM], BF16); PB = k.sb("PBb", [128, 8, DM], BF16); bPA = Buf(); bPB = Buf()
    stg = Ring([k.sb(f"p4stg{i}", [128, 8, 256], F32) for i in range(2)])
    pav = proj_a.rearrange("(c p) n -> p c n", p=128); pbv = proj_b.rearrange("(c p) n -> p c n", p=128)
    cast_load(k, S, PA, bPA, lambda c0, n: pav[:, :, c0:c0 + n], 8, DM, stg)
    cast_load(k, S, PB, bPB, lambda c0, n: pbv[:, :, c0:c0 + n], 8, DM, stg)
    YATv = sc["YAT"].rearrange("(c p) t -> p c t", p=128); YBTv = sc["YBT"].rearrange("(c p) t -> p c t", p=128)
    ya = Ring([k.sb(f"p4ya{i}", [128, 8, 512], BF16) for i in range(2)])
    yb = Ring([k.sb(f"p4yb{i}", [128, 8, 512], BF16) for i in range(2)])
    ga = Ring([k.sb(f"p4ga{i}", [128, 512], BF16) for i in range(3)])
    gb = Ring([k.sb(f"p4gb{i}", [128, 512], BF16) for i in range(3)])
    t1 = Ring([k.sb(f"p4t1{i}", [128, 512], F32) for i in range(2)])
    t2 = Ring([k.sb(f"p4t2{i}", [128, 512], F32) for i in range(2)])
    mo = Ring([k.sb(f"p4mo{i}", [128, 512], BF16) for i in range(3)])
    psa = Ring([k.ps(f"p4pa{i}", [128, 512], F32) for i in range(2)])
    psb = Ring([k.ps(f"p4pb{i}", [128, 512], F32) for i in range(2)])
    for tt in range(NOWN // 512):
        t0 = tt * 512
        a_, ba = ya.next(); b_, bb = yb.next()
        S.dma("sp", lambda e, a_=a_, t0=t0: e.dma_start(out=a_[:], in_=YATv[:, :, t0:t0 + 512]), writes=[ba])
        S.dma("act", lambda e, b_=b_, t0=t0: e.dma_start(out=b_[:], in_=YBTv[:, :, t0:t0 + 512]), writes=[bb])
        for i in range(KC):
            g1, bg1 = ga.next(); g2, bg2 = gb.next()
            S.dma("sp", lambda e, g1=g1, i=i, t0=t0: e.dma_start(out=g1[:], in_=sc["GA"][i * 128:(i + 1) * 128, t0:t0 + 512]), writes=[bg1])
            S.dma("act", lambda e, g2=g2, i=i, t0=t0: e.dma_start(out=g2[:], in_=sc["GB"][i * 128:(i + 1) * 128, t0:t0 + 512]), writes=[bg2])
            p1, bp1 = psa.next(); p2, bp2 = psb.next()
            for kc in range(8):
                S.op("pe", lambda e, p1=p1, a_=a_, kc=kc, i=i: e.matmul(p1[:, :], lhsT=PA[:, kc, i * 128:(i + 1) * 128], rhs=a_[:, kc, :],
                                                                       start=(kc == 0), stop=(kc == 7)), reads=[bPA, ba], writes=[bp1])
            for kc in range(8):
                S.op("pe", lambda e, p2=p2, b_=b_, kc=kc, i=i: e.matmul(p2[:, :], lhsT=PB[:, kc, i * 128:(i + 1) * 128], rhs=b_[:, kc, :],
                                                                       start=(kc == 0), stop=(kc == 7)), reads=[bPB, bb], writes=[bp2])
            x1, bx1 = t1.next(); x2, bx2 = t2.next(); m_, bm = mo.next()
            S.op("dve", lambda e, x1=x1, p1=p1, g1=g1: e.tensor_tensor(out=x1[:], in0=p1[:, :], in1=g1[:], op=ALU.mult), reads=[bp1, bg1], writes=[bx1])
            S.op("dve", lambda e, x2=x2, p2=p2, g2=g2: e.tensor_tensor(out=x2[:], in0=p2[:, :], in1=g2[:], op=ALU.mult), reads=[bp2, bg2], writes=[bx2])
            S.op("pool", lambda e, m_=m_, x1=x1, x2=x2: e.tensor_tensor(out=m_[:], in0=x1[:], in1=x2[:], op=ALU.add), reads=[bx1, bx2], writes=[bm])
            S.dma("pool", lambda e, m_=m_, i=i, t0=t0: e.dma_start(out=sc["MT"][i * 128:(i + 1) * 128, t0:t0 + 512], in_=m_[:]), reads=[bm])


def phase4b(k, xe, w_out, prm):
    cfg, S, nc, c, sc = k.cfg, k.S, k.nc, k.c, k.sc
    DM, KC, NOWN, NPRE, NE, CAP = cfg.DM, cfg.KC, cfg.NOWN, cfg.NPRE, cfg.NE, cfg.CAP
    NT = NOWN // 128
    sc["H"] = k.scratch("H", [NOWN, DM], F32)
    sc["XE"] = k.scratch("XE", [NE * CAP, DM], BF16)
    k.phase()
    WO = k.sb("WOb", [128, KC, DM], BF16); bWO = Buf()
    stg = Ring([k.sb(f"p4bstg{i}", [128, KC, 128], F32) for i in range(2)])
    wov = w_out.rearrange("(c p) n -> p c n", p=128)
    cast_load(k, S, WO, bWO, lambda c0, n: wov[:, :, c0:c0 + n], KC, DM, stg, step=128)
    lw = k.sb("ln1w", [128, DM], F32); lb = k.sb("ln1b", [128, DM], F32); b_l = Buf()
    S.dma("sp", lambda e: e.dma_start(out=lw[:], in_=prm["ln1_w"][0:1, :].partition_broadcast(128)), writes=[b_l])
    S.dma("sp", lambda e: e.dma_start(out=lb[:], in_=prm["ln1_b"][0:1, :].partition_broadcast(128)), writes=[b_l])
    wr = k.sb("wr", [128, KC, NE], F32); b_wr = Buf()
    S.dma("sp", lambda e: e.dma_start(out=wr[:], in_=prm["w_router"].rearrange("(c p) n -> p c n", p=128)), writes=[b_wr])
    brt = k.sb("brt", [128, NE], F32)
    S.dma("sp", lambda e: e.dma_start(out=brt[:], in_=prm["b_router"][0:1, :].partition_broadcast(128)), writes=[b_wr])
    iot = k.sb("iot", [128, NE], F32); usf = k.sb("usf", [128, 128], F32); usb = k.sb("usb", [128, 128], BF16)
    onb = k.sb("onb", [128, 128], BF16); b_cst = Buf()
    S.dma("sp", lambda e: e.dma_start(out=iot[:], in_=prm["iota32"][:, :]), writes=[b_cst])
    S.dma("sp", lambda e: e.dma_start(out=usf[:], in_=prm["ustrict"][:, :]), writes=[b_cst])
    S.op("act", lambda e: e.activation(out=usb[:], in_=usf[:], func=AF.Copy), reads=[b_cst], writes=[b_cst])
    S.op("pool", lambda e: e.memset(onb[:], 1.0), writes=[b_cst])
    base = k.sb("rbase", [128, NE], F32); b_base = Buf()
    S.op("pool", lambda e: e.memset(base[:], 0.0), writes=[b_base])
    zt = k.sb("zt", [128, DM], BF16); b_zt = Buf()
    S.op("pool", lambda e: e.memset(zt[:], 0.0), writes=[b_zt])
    b_XE = Buf()
    for r0 in range(0, NE * CAP, 128):
        S.dma("sp" if (r0 // 128) % 2 == 0 else "act", lambda e, r0=r0: e.dma_start(out=sc["XE"][r0:r0 + 128, :], in_=zt[:]), reads=[b_zt], writes=[b_XE])
    MTv = sc["MT"].rearrange("(c p) t -> p c t", p=128)
    mt = Ring([k.sb(f"p4mt{i}", [128, KC, 128], BF16) for i in range(2)])
    xt = Ring([k.sb(f"p4xt{i}", [128, DM], F32) for i in range(1)])
    zz = Ring([k.sb(f"p4z{i}", [128, DM], F32) for i in range(2)])
    hb = Ring([k.sb(f"p4hb{i}", [128, DM], BF16) for i in range(2)])
    hT = Ring([k.sb(f"p4hT{i}", [128, KC, 128], F32) for i in range(1)])
    sm = Ring([k.sb(f"p4sm{i}", [128, 256], F32) for i in range(2)])
    oh = Ring([k.sb(f"p4oh{i}", [128, 4, NE], F32) for i in range(2)])
    pr = Ring([k.sb(f"p4pr{i}", [128, 4, NE], F32) for i in range(2)])
    selb = Ring([k.sb(f"p4selb{i}", [128, NE], BF16) for i in range(2)])
    bst = Ring([k.sb(f"p4bst{i}", [128, 4, 6], F32) for i in range(2)])
    pz = Ring([k.ps(f"p4pz{i}", [128, 512], F32) for i in range(3)])
    pT = Ring([k.ps(f"p4T{i}", [128, 4, 128], F32) for i in range(2)])
    pl = Ring([k.ps("p4l", [128, 128], F32)])
    for ti in range(NT):
        tok0 = ti * 128
        m_, bm = mt.next(); x_, bx = xt.next(); z_, bz = zz.next()
        S.dma("sp", lambda e, m_=m_, tok0=tok0: e.dma_start(out=m_[:], in_=MTv[:, :, tok0:tok0 + 128]), writes=[bm])
        S.dma("act", lambda e, x_=x_, tok0=tok0: e.dma_start(out=x_[:], in_=xe[NPRE + tok0:NPRE + tok0 + 128, :]), writes=[bx])
        s_, bs = bst.next()
        ncg = DM // 512 if DM >= 512 else 1
        cw = DM // ncg
        for cg in range(ncg):
            p_, bp = pz.next()
            for kc in range(KC):
                S.op("pe", lambda e, p_=p_, m_=m_, kc=kc, cg=cg: e.matmul(p_[:, 0:cw], lhsT=m_[:, kc, :], rhs=WO[:, kc, cg * cw:(cg + 1) * cw],
                                                                         start=(kc == 0), stop=(kc == KC - 1)), reads=[bm, bWO], writes=[bp])
            S.op("dve", lambda e, z_=z_, x_=x_, p_=p_, cg=cg: e.scalar_tensor_tensor(out=z_[:, cg * cw:(cg + 1) * cw], in0=x_[:, cg * cw:(cg + 1) * cw], scalar=float(cfg.alpha),
                                                                                   in1=p_[:, 0:cw], op0=ALU.mult, op1=ALU.add), reads=[bx, bp], writes=[bz])
            S.op("dve", lambda e, s_=s_, z_=z_, cg=cg: e.bn_stats(out=s_[:, cg, :], in_=z_[:, cg * cw:(cg + 1) * cw]), reads=[bz], writes=[bs])
        q_, bq = sm.next()
        S.op("dve", lambda e, q_=q_, s_=s_: e.bn_aggr(out=q_[:, 0:2], in_=s_[:, 0:ncg, :].rearrange("p a b -> p (a b)")), reads=[bs], writes=[bq])
        S.op("dve", lambda e, q_=q_: e.tensor_scalar(out=q_[:, 2:3], in0=q_[:, 1:2], scalar1=1e-5, scalar2=None, op0=ALU.add), reads=[bq], writes=[bq])
        S.op("act", lambda e, q_=q_: e.activation(out=q_[:, 3:4], in_=q_[:, 2:3], func=AF.Sqrt), reads=[bq], writes=[bq])
        S.op("dve", lambda e, q_=q_: e.reciprocal(out=q_[:, 4:5], in_=q_[:, 3:4]), reads=[bq], writes=[bq])
        S.op("dve", lambda e, z_=z_, q_=q_: e.tensor_scalar(out=z_[:], in0=z_[:], scalar1=q_[:, 0:1], scalar2=q_[:, 4:5], op0=ALU.subtract, op1=ALU.mult),
             reads=[bz, bq], writes=[bz])
        S.op("pool", lambda e, z_=z_: e.tensor_tensor(out=z_[:], in0=z_[:], in1=lw[:], op=ALU.mult), reads=[bz, b_l], writes=[bz])
        S.op("dve", lambda e, z_=z_: e.tensor_tensor(out=z_[:], in0=z_[:], in1=lb[:], op=ALU.add), reads=[bz, b_l], writes=[bz])
        S.dma("sp", lambda e, z_=z_, tok0=tok0: e.dma_start(out=sc["H"][tok0:tok0 + 128, :], in_=z_[:]), reads=[bz])
        h_, bh = hb.next()
        S.op("act", lambda e, h_=h_, z_=z_: e.activation(out=h_[:], in_=z_[:], func=AF.Copy), reads=[bz], writes=[bh])
        t_, bt = hT.next()
        for g0 in range(0, KC, 4):
            ng = min(4, KC - g0)
            pt, bpt = pT.next()
            for j in range(ng):
                S.op("pe", lambda e, pt=pt, z_=z_, j=j, g0=g0: e.transpose(out=pt[:, j, :], in_=z_[:, (g0 + j) * 128:(g0 + j + 1) * 128], identity=c["ident_f"][:]),
                     reads=[bz, c["b_ident"]], writes=[bpt])
            S.op("act", lambda e, t_=t_, pt=pt, g0=g0, ng=ng: e.activation(out=t_[:, g0:g0 + ng, :], in_=pt[:, 0:ng, :], func=AF.Copy), reads=[bpt], writes=[bt])
        pl_, bpl = pl.next()
        for kc in range(KC):
            S.op("pe", lambda e, pl_=pl_, t_=t_, kc=kc: e.matmul(pl_[:, 0:NE], lhsT=t_[:, kc, :], rhs=wr[:, kc, :], start=(kc == 0), stop=(kc == KC - 1)),
                 reads=[bt, b_wr], writes=[bpl])
        lg = q_[:, 8:8 + NE]
        S.op("dve", lambda e, lg=lg, pl_=pl_: e.tensor_tensor(out=lg, in0=pl_[:, 0:NE], in1=brt[:], op=ALU.add), reads=[bpl, b_wr], writes=[bq])
        top = q_[:, 48:56]
        S.op("dve", lambda e, top=top, lg=lg: e.max(out=top, in_=lg), reads=[bq], writes=[bq])
        S.op("dve", lambda e, q_=q_: e.tensor_scalar(out=q_[:, 56:57], in0=q_[:, 48:49], scalar1=-1.0, scalar2=None, op0=ALU.mult), reads=[bq], writes=[bq])
        S.op("act", lambda e, q_=q_: e.activation(out=q_[:, 60:64], in_=q_[:, 48:52], func=AF.Exp, bias=q_[:, 56:57]), reads=[bq], writes=[bq])
        S.op("dve", lambda e, q_=q_: e.tensor_reduce(out=q_[:, 57:58], in_=q_[:, 60:64], axis=AX.X, op=ALU.add), reads=[bq], writes=[bq])
        S.op("dve", lambda e, q_=q_: e.reciprocal(out=q_[:, 58:59], in_=q_[:, 57:58]), reads=[bq], writes=[bq])
        S.op("dve", lambda e, q_=q_, ti=ti: e.tensor_scalar(out=k.gate_all[:, ti, :], in0=q_[:, 60:64], scalar1=q_[:, 58:59], scalar2=None, op0=ALU.mult),
             reads=[bq], writes=[k.b_gate])
        o_, bo = oh.next()
        for kk_ in range(4):
            S.op("dve" if kk_ % 2 == 0 else "pool", lambda e, o_=o_, lg=lg, q_=q_, kk_=kk_: e.tensor_scalar(
                out=o_[:, kk_, :], in0=lg, scalar1=q_[:, 48 + kk_:49 + kk_], scalar2=None, op0=ALU.is_equal), reads=[bq], writes=[bo])
        sel = q_[:, 64:64 + NE]
        S.op("dve", lambda e, sel=sel, o_=o_: e.tensor_reduce(out=sel, in_=o_[:].rearrange("p k e -> p e k"), axis=AX.X, op=ALU.add), reads=[bo], writes=[bq])
        sb_, bsb = selb.next()
        S.op("act", lambda e, sb_=sb_, sel=sel: e.activation(out=sb_[:], in_=sel, func=AF.Copy), reads=[bq], writes=[bsb])
        pl2, bpl2 = pl.next()
        S.op("pe", lambda e, pl2=pl2, sb_=sb_: e.matmul(pl2[:, 0:NE], lhsT=usb[:, :], rhs=sb_[:, :], start=True, stop=True), reads=[b_cst, bsb], writes=[bpl2])
        S.op("pe", lambda e, pl2=pl2, sb_=sb_: e.matmul(pl2[:, 64:64 + NE], lhsT=onb[:, :], rhs=sb_[:, :], start=True, stop=True), reads=[b_cst, bsb], writes=[bpl2])
        pos = q_[:, 96:96 + NE]
        S.op("dve", lambda e, pos=pos, pl2=pl2: e.tensor_tensor(out=pos, in0=pl2[:, 0:NE], in1=base[:], op=ALU.add), reads=[bpl2, b_base], writes=[bq])
        S.op("dve", lambda e, pl2=pl2: e.tensor_tensor(out=base[:], in0=pl2[:, 64:64 + NE], in1=base[:], op=ALU.add), reads=[bpl2, b_base, bq], writes=[b_base])
        p_r, bpr = pr.next()
        bck = lambda ap: ap.unsqueeze(1).to_broadcast([128, 4, NE])
        S.op("pool", lambda e, p_r=p_r, o_=o_: e.tensor_tensor(out=p_r[:], in0=o_[:], in1=bck(iot[:, :]), op=ALU.mult), reads=[bo, b_cst], writes=[bpr])
        S.op("dve", lambda e, q_=q_, p_r=p_r: e.tensor_reduce(out=q_[:, 128:132], in_=p_r[:], axis=AX.X, op=ALU.add), reads=[bpr], writes=[bq])
        p_r2, bpr2 = pr.next()
        S.op("pool", lambda e, p_r2=p_r2, o_=o_, pos=pos: e.tensor_tensor(out=p_r2[:], in0=o_[:], in1=bck(pos), op=ALU.mult), reads=[bo, bq], writes=[bpr2])
        S.op("dve", lambda e, q_=q_, p_r2=p_r2: e.tensor_reduce(out=q_[:, 132:136], in_=p_r2[:], axis=AX.X, op=ALU.add), reads=[bpr2], writes=[bq])
        S.op("dve", lambda e, q_=q_: e.scalar_tensor_tensor(out=q_[:, 136:140], in0=q_[:, 128:132], scalar=float(CAP), in1=q_[:, 132:136], op0=ALU.mult, op1=ALU.add),
             reads=[bq], writes=[bq])
        S.op("dve", lambda e, q_=q_: e.tensor_scalar(out=q_[:, 140:144], in0=q_[:, 132:136], scalar1=float(CAP), scalar2=1.0e7, op0=ALU.is_ge, op1=ALU.mult),
             reads=[bq], writes=[bq])
        S.op("dve", lambda e, q_=q_: e.tensor_tensor(out=q_[:, 144:148], in0=q_[:, 136:140], in1=q_[:, 140:144], op=ALU.add), reads=[bq], writes=[bq])
        S.op("dve", lambda e, q_=q_, ti=ti: e.tensor_copy(out=k.dest_all[:, ti, :], in_=q_[:, 144:148]), reads=[bq], writes=[k.b_dest])
        for kk_ in range(4):
            S.dma("pool", lambda e, h_=h_, ti=ti, kk_=kk_: e.indirect_dma_start(
                out=sc["XE"][:, :], out_offset=bass.IndirectOffsetOnAxis(ap=k.dest_all[:, ti, kk_:kk_ + 1], axis=0),
                in_=h_[:, :], in_offset=None, bounds_check=_bound_reg(e, NE * CAP - 1), oob_is_err=False), reads=[bh, k.b_dest, b_XE])


def phase5(k, w_gu, b_gu, w_down, b_down):
    cfg, S, nc, c, sc = k.cfg, k.S, k.nc, k.c, k.sc
    DM, KC, NE, CAP, FF = cfg.DM, cfg.KC, cfg.NE, cfg.CAP, cfg.FF
    FC = FF // 128
    NS = CAP // 128
    nhalf = 2 if CAP > 512 else 1
    HALF = CAP // nhalf
    GW = min(512, FF)
    DW = min(512, DM)
    KH = max(1, KC // 2)
    sc["YE"] = k.scratch("YE", [NE * CAP, DM], F32)
    k.phase()
    bgT = k.bgT; b_bgT = k.b_bgT
    bgf = k.sb("bgf", [NE, 2 * FF], F32); b_bgf = Buf()
    S.dma("sp", lambda e: e.dma_start(out=bgf[:], in_=b_gu[:, :]), writes=[b_bgf])
    pTf = Ring([k.ps("p5Tf", [128, 4, NE], F32)])
    for g0 in range(0, 2 * FC, 4):
        pt, bpt = pTf.next()
        for j in range(4):
            S.op("pe", lambda e, pt=pt, j=j, g0=g0: e.transpose(out=pt[:, j, :], in_=bgf[:, (g0 + j) * 128:(g0 + j + 1) * 128], identity=c["ident_f"][0:NE, 0:NE]),
                 reads=[b_bgf, c["b_ident"]], writes=[bpt])
        S.op("act", lambda e, pt=pt, g0=g0: e.activation(out=bgT[:, g0:g0 + 4, :], in_=pt[:], func=AF.Copy), reads=[bpt], writes=[b_bgT])
    k.phase()
    xs = Ring([k.sb(f"p5xs{i}", [128, NS, DM], BF16) for i in range(1)])
    XT = Ring([k.sb(f"p5XT{i}", [128, KC, CAP], BF16) for i in range(1)])
    HT = Ring([k.sb(f"p5HT{i}", [128, FC, CAP], BF16) for i in range(1)])
    KP = max(1, KC // 4)
    NPK = KC // KP
    ws = Ring([k.sb(f"p5ws{i}", [128, KP, GW], F32) for i in range(5)])
    wb = [k.sb(f"p5wb{i}", [128, KC, 2 * GW], BF16) for i in range(2)]
    b_wb = [[Buf() for _ in range(2 * NPK)] for _ in range(2)]
    bd = Ring([k.sb(f"p5bd{i}", [128, DM], F32) for i in range(2)])
    tg = Ring([k.sb(f"p5tg{i}", [128, HALF], F32) for i in range(2)])
    tsg = Ring([k.sb(f"p5ts{i}", [128, HALF], F32) for i in range(2)])
    tu = Ring([k.sb(f"p5tu{i}", [128, HALF], F32) for i in range(2)])
    tgs = Ring([k.sb(f"p5tgs{i}", [128, HALF], F32) for i in range(2)])
    yst = Ring([k.sb(f"p5yst{i}", [128, DW], F32) for i in range(4)])
    pT = Ring([k.ps(f"p5T{i}", [128, 8, 128], BF16) for i in range(2)])
    pg = Ring([k.ps(f"p5g{i}", [128, 512], F32) for i in range(2)])
    pu = Ring([k.ps(f"p5u{i}", [128, 512], F32) for i in range(2)])
    py = Ring([k.ps(f"p5y{i}", [128, 512], F32) for i in range(2)])
    wgv = w_gu.rearrange("e (c p) n -> e p c n", p=128)
    wdv = w_down.rearrange("e (c p) n -> e p c n", p=128)
    cnt = [0]
    nblk = 2 * GW // DW
    groups = []
    for ex in range(NE):
        for gw in range(FF // GW):
            groups.append(("gu", ex, gw))
        for cb0 in range(0, DM // DW, nblk):
            groups.append(("dn", ex, cb0))

    def load_piece(wb_, bw_piece, src_fn, kc0, dcol, ncol):
        w_, bw = ws.next()
        cnt[0] += 1
        S.dma("sp", lambda e, w_=w_: e.dma_start(out=w_[:, 0:KP, 0:ncol], in_=src_fn()), writes=[bw])
        if cnt[0] % 2 == 0:
            S.op("act", lambda e, w_=w_, wb_=wb_: e.activation(out=wb_[:, kc0:kc0 + KP, dcol:dcol + ncol], in_=w_[:, 0:KP, 0:ncol], func=AF.Copy),
                 reads=[bw], writes=[bw_piece])
        else:
            S.op("dve", lambda e, w_=w_, wb_=wb_: e.tensor_copy(out=wb_[:, kc0:kc0 + KP, dcol:dcol + ncol], in_=w_[:, 0:KP, 0:ncol]),
                 reads=[bw], writes=[bw_piece])

    def piece_loads(gi):
        kind, ex, idx = groups[gi]
        wb_, bl = wb[gi % 2], b_wb[gi % 2]
        out = []
        if kind == "gu":
            for part in range(2):
                for kc0 in range(0, KC, KP):
                    c0 = part * FF + idx * GW
                    out.append((wb_, bl[part * NPK + kc0 // KP], (lambda ex=ex, kc0=kc0, c0=c0: wgv[ex, :, kc0:kc0 + KP, c0:c0 + GW]), kc0, part * GW, GW))
        else:
            nb = min(nblk, DM // DW - idx)
            for bi in range(nb):
                for kc0 in range(0, FC, KP):
                    c0 = (idx + bi) * DW
                    out.append((wb_, bl[bi * NPK + kc0 // KP], (lambda ex=ex, kc0=kc0, c0=c0: wdv[ex, :, kc0:kc0 + KP, c0:c0 + DW]), kc0, bi * DW, DW))
        return out

    x_, bx = xs.next(); xt, bxt = XT.next(); ht, bht = HT.next()
    cur = {}

    def xload(ex):
        S.dma("act", lambda e, ex=ex: e.dma_start(out=x_[:], in_=sc["XE"][ex * CAP:(ex + 1) * CAP, :].rearrange("(s p) d -> p s d", p=128)), writes=[bx])

    def preamble(ex):
        for s in range(NS):
            for g0 in range(0, KC, 8):
                ng = min(8, KC - g0)
                pt, bpt = pT.next()
                for j in range(ng):
                    S.op("pe", lambda e, pt=pt, s=s, j=j, g0=g0: e.transpose(out=pt[:, j, :], in_=x_[:, s, (g0 + j) * 128:(g0 + j + 1) * 128], identity=c["ident_b"][:]),
                         reads=[bx, c["b_ident"]], writes=[bpt])
                S.op("act", lambda e, pt=pt, s=s, g0=g0, ng=ng: e.activation(out=xt[:, g0:g0 + ng, s * 128:(s + 1) * 128], in_=pt[:, 0:ng, :], func=AF.Copy),
                     reads=[bpt], writes=[bxt])
        cur["b"] = bd.next()
        b_, bb = cur["b"]
        S.dma("act", lambda e, b_=b_, ex=ex: e.dma_start(out=b_[:], in_=b_down[ex:ex + 1, :].partition_broadcast(128)), writes=[bb])
        if ex + 1 < NE:
            xload(ex + 1)

    def gu_unit(ex, gw, fl, hf, wb_, bl):
        fb = gw * (GW // 128) + fl
        sl = slice(hf * HALF, (hf + 1) * HALF)
        pg_, bpg = pg.next(); pu_, bpu = pu.next()
        for kc in range(KC):
            S.op("pe", lambda e, pg_=pg_, kc=kc: e.matmul(pg_[:, 0:HALF], lhsT=wb_[:, kc, fl * 128:(fl + 1) * 128], rhs=xt[:, kc, sl],
                                                        start=(kc == 0), stop=(kc == KC - 1)), reads=[bl[kc // KP], bxt], writes=[bpg])
        for kc in range(KC):
            S.op("pe", lambda e, pu_=pu_, kc=kc: e.matmul(pu_[:, 0:HALF], lhsT=wb_[:, kc, GW + fl * 128:GW + (fl + 1) * 128], rhs=xt[:, kc, sl],
                                                        start=(kc == 0), stop=(kc == KC - 1)), reads=[bl[NPK + kc // KP], bxt], writes=[bpu])
        g_, bg = tg.next(); s_, bs = tsg.next(); u_, bu = tu.next(); gs_, bgs = tgs.next()
        S.op("dve", lambda e: e.tensor_scalar(out=g_[:], in0=pg_[:, 0:HALF], scalar1=bgT[:, fb, ex:ex + 1], scalar2=7.0,
                                              op0=ALU.add, op1=ALU.min), reads=[bpg, b_bgT], writes=[bg])
        S.op("act", lambda e: e.activation(out=s_[:], in_=g_[:], func=AF.Sigmoid, scale=1.702), reads=[bg], writes=[bs])
        S.op("dve", lambda e: e.tensor_scalar(out=u_[:], in0=pu_[:, 0:HALF], scalar1=bgT[:, FC + fb, ex:ex + 1], scalar2=7.0,
                                              op0=ALU.add, op1=ALU.min), reads=[bpu, b_bgT], writes=[bu])
        S.op("dve", lambda e: e.tensor_scalar(out=u_[:], in0=u_[:], scalar1=-7.0, scalar2=1.0, op0=ALU.max, op1=ALU.add), reads=[bu], writes=[bu])
        S.op("pool", lambda e: e.tensor_tensor(out=gs_[:], in0=g_[:], in1=s_[:], op=ALU.mult), reads=[bg, bs], writes=[bgs])
        S.op("pool", lambda e: e.tensor_tensor(out=ht[:, fb, sl], in0=gs_[:], in1=u_[:], op=ALU.mult), reads=[bgs, bu], writes=[bht])

    def dn_unit(ex, cb, bi, s, wb_, bl):
        b_, bb = cur["b"]
        py_, bpy = py.next()
        for fc in range(FC):
            S.op("pe", lambda e, fc=fc: e.matmul(py_[:, 0:DW], lhsT=ht[:, fc, s * 128:(s + 1) * 128], rhs=wb_[:, fc, bi * DW:(bi + 1) * DW],
                                               start=(fc == 0), stop=(fc == FC - 1)), reads=[bht, bl[bi * NPK + fc // KP]], writes=[bpy])
        y_, by = yst.next()
        S.op("dve", lambda e: e.tensor_tensor(out=y_[:], in0=py_[:, 0:DW], in1=b_[:, cb * DW:(cb + 1) * DW], op=ALU.add), reads=[bpy, bb], writes=[by])
        r0 = ex * CAP + s * 128
        S.dma("pool", lambda e: e.dma_start(out=sc["YE"][r0:r0 + 128, cb * DW:(cb + 1) * DW], in_=y_[:]), reads=[by])

    def make_units(gi):
        kind, ex, idx = groups[gi]
        wb_, bl = wb[gi % 2], b_wb[gi % 2]
        if kind == "gu":
            return [(lambda fl=fl, hf=hf: gu_unit(ex, idx, fl, hf, wb_, bl)) for fl in range(GW // 128) for hf in range(nhalf)]
        nb = min(nblk, DM // DW - idx)
        return [(lambda bi=bi, s=s: dn_unit(ex, idx + bi, bi, s, wb_, bl)) for bi in range(nb) for s in range(NS)]

    xload(0)
    for ld in piece_loads(0):
        load_piece(*ld)
    for gi in range(len(groups)):
        kind, ex, idx = groups[gi]
        nxt = piece_loads(gi + 1) if gi + 1 < len(groups) else []
        units = make_units(gi)
        if kind == "gu" and idx == 0:
            preamble(ex)
        nu = len(units)
        for ui, u in enumerate(units):
            for ld in nxt[ui * len(nxt) // nu:(ui + 1) * len(nxt) // nu]:
                load_piece(*ld)
            u()


def phase6(k, out, prm):
    cfg, S, nc, c, sc = k.cfg, k.S, k.nc, k.c, k.sc
    DM, NOWN, NE, CAP = cfg.DM, cfg.NOWN, cfg.NE, cfg.CAP
    NT = NOWN // 128
    k.phase()
    lw = k.sb("ln2w", [128, DM], F32); lb = k.sb("ln2b", [128, DM], F32); b_l = Buf()
    S.dma("sp", lambda e: e.dma_start(out=lw[:], in_=prm["ln2_w"][0:1, :].partition_broadcast(128)), writes=[b_l])
    S.dma("sp", lambda e: e.dma_start(out=lb[:], in_=prm["ln2_b"][0:1, :].partition_broadcast(128)), writes=[b_l])
    hh = Ring([k.sb(f"p6h{i}", [128, DM], F32) for i in range(3)])
    yk = Ring([k.sb(f"p6y{i}", [128, DM], F32) for i in range(8)])
    zt = k.sb("p6z", [128, DM], F32); b_zt = Buf()
    S.op("pool", lambda e: e.memset(zt[:], 0.0), writes=[b_zt])
    sm = Ring([k.sb(f"p6sm{i}", [128, 16], F32) for i in range(2)])
    bst = Ring([k.sb(f"p6bst{i}", [128, 4, 6], F32) for i in range(2)])
    ncg = DM // 512 if DM >= 512 else 1
    cw = DM // ncg
    for ti in range(NT):
        tok0 = ti * 128
        h_, bh = hh.next()
        S.dma("sp", lambda e, h_=h_, tok0=tok0: e.dma_start(out=h_[:], in_=sc["H"][tok0:tok0 + 128, :]), writes=[bh])
        S.op("dve", lambda e, h_=h_: e.tensor_scalar(out=h_[:], in0=h_[:], scalar1=float(cfg.alpha), scalar2=None, op0=ALU.mult), reads=[bh], writes=[bh])
        for kk_ in range(4):
            y_, by = yk.next()
            S.op("act", lambda e, y_=y_: e.activation(out=y_[:], in_=zt[:], func=AF.Copy), reads=[b_zt], writes=[by])
            S.dma("pool", lambda e, y_=y_, ti=ti, kk_=kk_: e.indirect_dma_start(
                out=y_[:, :], out_offset=None, in_=sc["YE"][:, :], in_offset=bass.IndirectOffsetOnAxis(ap=k.dest_all[:, ti, kk_:kk_ + 1], axis=0),
                bounds_check=_bound_reg(e, NE * CAP - 1), oob_is_err=False), reads=[k.b_dest], writes=[by])
            S.op("dve", lambda e, h_=h_, y_=y_, ti=ti, kk_=kk_: e.scalar_tensor_tensor(out=h_[:], in0=y_[:], scalar=k.gate_all[:, ti, kk_:kk_ + 1], in1=h_[:],
                                                                                     op0=ALU.mult, op1=ALU.add), reads=[by, bh, k.b_gate], writes=[bh])
        s_, bs = bst.next(); q_, bq = sm.next()
        for cg in range(ncg):
            S.op("dve", lambda e, s_=s_, h_=h_, cg=cg: e.bn_stats(out=s_[:, cg, :], in_=h_[:, cg * cw:(cg + 1) * cw]), reads=[bh], writes=[bs])
        S.op("dve", lambda e, q_=q_, s_=s_: e.bn_aggr(out=q_[:, 0:2], in_=s_[:, 0:ncg, :].rearrange("p a b -> p (a b)")), reads=[bs], writes=[bq])
        S.op("dve", lambda e, q_=q_: e.tensor_scalar(out=q_[:, 2:3], in0=q_[:, 1:2], scalar1=1e-5, scalar2=None, op0=ALU.add), reads=[bq], writes=[bq])
        S.op("act", lambda e, q_=q_: e.activation(out=q_[:, 3:4], in_=q_[:, 2:3], func=AF.Sqrt), reads=[bq], writes=[bq])
        S.op("dve", lambda e, q_=q_: e.reciprocal(out=q_[:, 4:5], in_=q_[:, 3:4]), reads=[bq], writes=[bq])
        S.op("dve", lambda e, h_=h_, q_=q_: e.tensor_scalar(out=h_[:], in0=h_[:], scalar1=q_[:, 0:1], scalar2=q_[:, 4:5], op0=ALU.subtract, op1=ALU.mult),
             reads=[bh, bq], writes=[bh])
        S.op("pool", lambda e, h_=h_: e.tensor_tensor(out=h_[:], in0=h_[:], in1=lw[:], op=ALU.mult), reads=[bh, b_l], writes=[bh])
        S.op("dve", lambda e, h_=h_: e.tensor_tensor(out=h_[:], in0=h_[:], in1=lb[:], op=ALU.add), reads=[bh, b_l], writes=[bh])
        S.dma("sp", lambda e, h_=h_, tok0=tok0: e.dma_start(out=out[tok0:tok0 + 128, :], in_=h_[:]), reads=[bh])


HORD = [0, 2, 4, 6, 1, 3, 5, 7, 8, 10, 12, 14, 9, 11, 13, 15]


def build_full(cfg, dbg=()):
    k = K(cfg, dbg=dbg)
    DM, NE = cfg.DM, cfg.NE
    xe = k.inp("xe", [cfg.NTOK, DM])
    w_in = k.inp("w_in", [DM, cfg.DIN])
    mu_cols = k.inp("mu_cols", [128, 28])
    att_bias = k.inp("att_bias", [128, 5, 16, 128]); halo_mask = k.inp("halo_mask", [128, 1])
    prm = {n: k.inp(n, s) for n, s in [
        ("w_up", [96, 1024]), ("a_up", [96, 1024]), ("g_up", [256, 1024]), ("hcols", [64, 6, 16]), ("rmask", [64, 512]),
        ("m_su", [64, 8, 64]), ("m_ui", [64, 8, 64]), ("m_sl", [64, 8, 64]), ("i8", [64, 8, 64]),
        ("lnx_w", [1, 1024]), ("lnx_b", [1, 1024]), ("ln1_w", [1, DM]), ("ln1_b", [1, DM]), ("ln2_w", [1, DM]), ("ln2_b", [1, DM]),
        ("w_router", [DM, NE]), ("b_router", [1, NE]), ("iota32", [128, NE]), ("ustrict", [128, 128])]}
    proj_a = k.inp("proj_a", [1024, DM]); proj_b = k.inp("proj_b", [1024, DM]); w_out = k.inp("w_out", [DM, DM])
    w_gu = k.inp("w_gu", [NE, DM, 2 * cfg.FF]); b_gu = k.inp("b_gu", [NE, 2 * cfg.FF])
    w_down = k.inp("w_down", [NE, cfg.FF, DM]); b_down = k.inp("b_down", [NE, DM])
    out = k.nc.dram_tensor("out", [cfg.NOWN, DM], F32, kind="ExternalOutput").ap()
    load_consts(k)
    phase1(k, xe, w_in, mu_cols)
    phase2(k, att_bias, halo_mask)
    phase3(k, prm)
    phase3b(k, prm)
    phase4a(k, proj_a, proj_b)
    phase4b(k, xe, w_out, prm)
    phase5(k, w_gu, b_gu, w_down, b_down)
    phase6(k, out, prm)
    k.S.finish()
    return k


def host_common(inp, cfg):
    f = lambda a: np.ascontiguousarray(np.asarray(a, dtype=np.float32))
    smu = f(inp["shift_mu"][0])
    m = np.zeros((128, 28), np.float32)
    for ci in range(24):
        m[:, ci] = smu[ci * 128:(ci + 1) * 128]
    m[:96, 24] = smu[3072:3168]; m[:96, 25] = smu[3168:3264]
    m[:, 26] = smu[3264:3392]; m[:, 27] = smu[3392:3520]
    relb = f(inp["rel_bias"][0])
    kr = np.arange(640)[:, None]; q = np.arange(128)[None, :]
    dist = 512 + q - kr
    qc = (512 + q) // 64; kc = kr // 64
    valid = (qc - kc >= 0) & (qc - kc <= 8)
    idx = np.clip(np.minimum(dist, 256) + 63, 0, 319)
    b = relb[:, idx]
    b = np.where(valid[None], b, np.float32(-30000.0)).astype(np.float32)[HORD]
    att_bias = np.ascontiguousarray(b.reshape(16, 5, 128, 128).transpose(2, 1, 0, 3))
    hv = lambda v: f(v).reshape(16, 64).T
    k_a = f(inp["k_a"][0])
    one_minus_ka = np.zeros_like(k_a)
    hcols = np.ascontiguousarray(np.stack([hv(inp["w0"][0]), hv(inp["a0"][0]), hv(inp["k_k"][0]), hv(k_a), hv(one_minus_ka), hv(inp["r_k"][0])], 1))
    rmask = np.ones((64, 512), np.float32); rmask[:, ::64] = 0
    j = np.arange(64)[:, None]; t = np.arange(64)[None, :]
    rep = lambda mm: np.ascontiguousarray(np.repeat(mm.astype(np.float32)[:, None, :], 8, 1))
    r2 = lambda v: f(v).reshape(1, -1)
    com = {
        "w_in": f(inp["w_in"][0]), "mu_cols": m, "att_bias": att_bias, "c_ident": np.eye(128, dtype=np.float32),
        "w_up": f(inp["w_up"][0]), "a_up": f(inp["a_up"][0]), "g_up": f(inp["g_up"][0]), "hcols": hcols, "rmask": rmask,
        "m_su": rep(j < t), "m_ui": rep(j <= t), "m_sl": rep(j > t), "i8": rep(j == t),
        "lnx_w": r2(inp["lnx_w"][0]), "lnx_b": r2(inp["lnx_b"][0]), "ln1_w": r2(inp["ln1_w"][0]), "ln1_b": r2(inp["ln1_b"][0]),
        "ln2_w": r2(inp["ln2_w"][0]), "ln2_b": r2(inp["ln2_b"][0]),
        "w_router": f(inp["w_router"][0]), "b_router": r2(inp["b_router"][0]),
        "iota32": np.ascontiguousarray(np.broadcast_to(np.arange(cfg.NE, dtype=np.float32), (128, cfg.NE))),
        "ustrict": (np.arange(128)[:, None] < np.arange(128)[None, :]).astype(np.float32),
        "proj_a": f(inp["proj_a"][0]), "proj_b": f(inp["proj_b"][0]), "w_out": f(inp["w_out"][0]),
        "w_gu": f(inp["w_gu"][0]), "b_gu": f(inp["b_gu"][0]), "w_down": f(inp["w_down"][0]), "b_down": f(inp["b_down"][0]),
    }
    return com


def host_core(x, cfg, b, half):
    NOWN = cfg.NOWN
    xe = np.zeros((cfg.NTOK, cfg.DM), np.float32)
    if half == 0:
        xe[cfg.NPRE:] = x[b, 0:NOWN]
        hm = np.full((128, 1), -30000.0, np.float32)
    else:
        xe[:] = x[b, 0:2 * NOWN]
        hm = np.zeros((128, 1), np.float32)
    return {"xe": xe, "halo_mask": hm}


_CACHE = {}


def kernel(**inputs):
    cfg = Cfg()
    if "k" not in _CACHE:
        _CACHE["k"] = build_full(cfg)
    k = _CACHE["k"]
    com = host_common(inputs, cfg)
    x = np.asarray(inputs["x"], dtype=np.float32)
    in_maps = []
    for core in range(8):
        m = dict(com)
        m.update(host_core(x, cfg, core // 2, core % 2))
        in_maps.append(m)
    res = run_bass_kernel_spmd(k.nc, in_maps, core_ids=list(range(8)))
    out = np.empty((4, 2 * cfg.NOWN, cfg.DM), np.float32)
    for core in range(8):
        h = core % 2
        out[core // 2, h * cfg.NOWN:(h + 1) * cfg.NOWN] = np.asarray(res.results[core]["out"])
    return out
```
